# Optimizing a Trainium2 kernel written in Bass

```python
import math
import jax
import jax.numpy as jnp
from jax import lax
import numpy as np


D_MODEL = 2048
BATCH = 1
SEQ = 8192
DEPTH = 2

GRID_W = 64
CTX_LEN = 256
EPS = 1e-6
N_MOD = 6
S5_WIDTH = 1024
S5_GROUP_CH = 16
S5_GROUPS = S5_WIDTH // S5_GROUP_CH
S5_STATE = 64
S5_DT_MIN = 1e-3
S5_DT_MAX = 1e-1
GLA_HEADS = 4
GLA_DK = 128
GLA_DV = 256
GLA_KW = GLA_HEADS * GLA_DK
GLA_VW = GLA_HEADS * GLA_DV
GLA_RANK = 16
GLA_TAU = 16.0
GLA_CHUNK = 64
ATT_HEADS = 16
ATT_KV_HEADS = 4
ATT_HEAD_DIM = 64
ATT_QW = ATT_HEADS * ATT_HEAD_DIM
ATT_KVW = ATT_KV_HEADS * ATT_HEAD_DIM
WINDOW = 128
ATT_BLOCK = 128
ROPE_BASE = 10000.0
N_BRANCH = 3
IN_SPLITS = (S5_WIDTH, GLA_KW, GLA_KW, GLA_VW, GLA_VW, ATT_QW, ATT_KVW, ATT_KVW, N_BRANCH * D_MODEL)
IN_WIDTH = S5_WIDTH + 2 * GLA_KW + 2 * GLA_VW + ATT_QW + 2 * ATT_KVW + N_BRANCH * D_MODEL
N_EXPERTS = 16
EXPERT_FF = 2048
EC_CAPACITY = 2

kernel_name = 'hybrid_s5_gla_swa_ec_diffusion_block'


def rms_norm(x, g):
    x32 = x.astype(jnp.float32)
    r = lax.rsqrt(jnp.mean(x32 * x32, axis=-1, keepdims=True) + EPS)
    return (x32 * r).astype(x.dtype) * g


def modulate(h, shift, scale):
    return h * (1 + scale) + shift


def split_heads(t, n_heads):
    return t.reshape(t.shape[:-1] + (n_heads, t.shape[-1] // n_heads))


def flip_seq(t):
    return jnp.flip(t, axis=1)


def keep_seq(t):
    return t


def split_projection(z):
    offsets = []
    acc = 0
    for w in IN_SPLITS[:-1]:
        acc += w
        offsets.append(acc)
    return jnp.split(z, offsets, axis=-1)


def axial_rope_tables(n_rows, dtype):
    row = jnp.repeat(jnp.arange(n_rows), GRID_W).astype(jnp.float32)
    col = jnp.tile(jnp.arange(GRID_W), n_rows).astype(jnp.float32)
    n_freq = ATT_HEAD_DIM // 4
    inv_freq = ROPE_BASE ** (-jnp.arange(n_freq, dtype=jnp.float32) / n_freq)
    ang = jnp.concatenate([row[:, None] * inv_freq, col[:, None] * inv_freq], axis=-1)
    return jnp.cos(ang).astype(dtype), jnp.sin(ang).astype(dtype)


def apply_rope(t, cos, sin):
    half = t.shape[-1] // 2
    t1, t2 = t[..., :half], t[..., half:]
    cs, sn = cos[None, :, None, :], sin[None, :, None, :]
    return jnp.concatenate([t1 * cs - t2 * sn, t1 * sn + t2 * cs], axis=-1)


def s5_discretize(lam_re, lam_im, log_dt, b_re, b_im):
    dt = jnp.exp(log_dt)[:, None]
    mag = jnp.exp(lam_re * dt)
    ab_re = mag * jnp.cos(lam_im * dt)
    ab_im = mag * jnp.sin(lam_im * dt)
    den = lam_re * lam_re + lam_im * lam_im
    nr = ab_re - 1
    f_re = ((nr * lam_re + ab_im * lam_im) / den)[..., None]
    f_im = ((ab_im * lam_re - nr * lam_im) / den)[..., None]
    bb_re = f_re * b_re - f_im * b_im
    bb_im = f_re * b_im + f_im * b_re
    return ab_re, ab_im, bb_re, bb_im


def complex_affine_combine(e1, e2):
    a1r, a1i, b1r, b1i = e1
    a2r, a2i, b2r, b2i = e2
    return (a2r * a1r - a2i * a1i, a2r * a1i + a2i * a1r,
            a2r * b1r - a2i * b1i + b2r, a2r * b1i + a2i * b1r + b2i)


def s5_states(u, h0_re, h0_im, disc):
    ab_re, ab_im, bb_re, bb_im = disc
    bu_re = jnp.einsum('blgc,gnc->blgn', u, bb_re)
    bu_im = jnp.einsum('blgc,gnc->blgn', u, bb_im)
    bu_re = bu_re.at[:, 0].add(ab_re * h0_re - ab_im * h0_im)
    bu_im = bu_im.at[:, 0].add(ab_re * h0_im + ab_im * h0_re)
    a_re = jnp.broadcast_to(ab_re, bu_re.shape)
    a_im = jnp.broadcast_to(ab_im, bu_im.shape)
    _, _, h_re, h_im = lax.associative_scan(complex_affine_combine, (a_re, a_im, bu_re, bu_im), axis=1)
    return h_re, h_im


def s5_readout(h_re, h_im, c_re, c_im):
    return jnp.einsum('blgn,gcn->blgc', h_re, c_re) - jnp.einsum('blgn,gcn->blgc', h_im, c_im)


def s5_glu(y, w_glu):
    g = jax.nn.gelu(y)
    return g * jax.nn.sigmoid(g @ w_glu)


def s5_branch(u, uc, lp, with_ctx_out):
    B, L, _ = u.shape
    Lc = uc.shape[1]
    ug = u.reshape(B, L, S5_GROUPS, S5_GROUP_CH)
    ucg = uc.reshape(B, Lc, S5_GROUPS, S5_GROUP_CH)
    zero = jnp.zeros((B, S5_GROUPS, S5_STATE), u.dtype)
    y = u * lp['s5_d']
    yc = uc * lp['s5_d'] if with_ctx_out else None
    for d in range(2):
        rev = flip_seq if d == 1 else keep_seq
        disc = s5_discretize(lp['s5_lam_re'][d], lp['s5_lam_im'][d], lp['s5_log_dt'][d],
                             lp['s5_b_re'][d], lp['s5_b_im'][d])
        c_re, c_im = lp['s5_c_re'][d], lp['s5_c_im'][d]
        hc_re, hc_im = s5_states(rev(ucg), zero, zero, disc)
        if with_ctx_out:
            yc = yc + rev(s5_readout(hc_re, hc_im, c_re, c_im)).reshape(B, Lc, S5_WIDTH)
        h_re, h_im = s5_states(rev(ug), hc_re[:, -1], hc_im[:, -1], disc)
        y = y + rev(s5_readout(h_re, h_im, c_re, c_im)).reshape(B, L, S5_WIDTH)
    y = s5_glu(y, lp['s5_w_glu'])
    yc = s5_glu(yc, lp['s5_w_glu']) if with_ctx_out else None
    return y, yc


def gla_log_decay(h, w1, w2, b):
    return jax.nn.log_sigmoid((h @ w1) @ w2 + b) / GLA_TAU


def gla_direction(q, k, v, log_a, s0):
    B, L, H, dk = q.shape
    n = L // GLA_CHUNK

    def chunked(t):
        return jnp.moveaxis(t.reshape(B, n, GLA_CHUNK, H, t.shape[-1]), 1, 0)

    cum = jnp.cumsum(chunked(log_a), axis=2)
    lower = jnp.tril(jnp.ones((GLA_CHUNK, GLA_CHUNK), dtype=bool))[None, :, :, None, None]

    def step(s, inp):
        qc, kc, vc, bc = inp
        o_inter = jnp.einsum('bihd,bhdv->bihv', qc * jnp.exp(bc), s)
        rel = jnp.exp(jnp.where(lower, bc[:, :, None] - bc[:, None, :], -jnp.inf))
        scores = jnp.einsum('bihd,bjhd,bijhd->bhij', qc, kc, rel)
        o_intra = jnp.einsum('bhij,bjhv->bihv', scores, vc)
        b_last = bc[:, -1]
        s_new = (jnp.exp(b_last)[..., None] * s
                 + jnp.einsum('bjhd,bjhv->bhdv', kc * jnp.exp(b_last[:, None] - bc), vc))
        return s_new, o_inter + o_intra

    s_fin, o = lax.scan(step, s0, (chunked(q), chunked(k), chunked(v), cum))
    return jnp.moveaxis(o, 0, 1).reshape(B, L, H, v.shape[-1]), s_fin


def gla_final_state(k, v, log_a):
    cum = jnp.cumsum(log_a, axis=1)
    b_last = cum[:, -1]
    return jnp.einsum('blhd,blhv->bhdv', k * jnp.exp(b_last[:, None] - cum), v)


def gla_branch(zl, zc, h, hc, lp, with_ctx_out):
    q, k, v, r = [split_heads(t, GLA_HEADS) for t in zl]
    qc, kc, vc, rc = [split_heads(t, GLA_HEADS) for t in zc]
    B, L = q.shape[:2]
    Lc = qc.shape[1]
    scale = GLA_DK ** -0.5
    s0 = jnp.zeros((B, GLA_HEADS, GLA_DK, GLA_DV), v.dtype)
    o = jnp.zeros_like(v)
    oc = jnp.zeros_like(vc) if with_ctx_out else None
    for d in range(2):
        rev = flip_seq if d == 1 else keep_seq
        la = split_heads(gla_log_decay(h, lp['gla_w1'][d], lp['gla_w2'][d], lp['gla_b'][d]), GLA_HEADS)
        lac = split_heads(gla_log_decay(hc, lp['gla_w1'][d], lp['gla_w2'][d], lp['gla_b'][d]), GLA_HEADS)
        if with_ctx_out:
            oc_d, sc = gla_direction(rev(qc) * scale, rev(kc), rev(vc), rev(lac), s0)
            oc = oc + rev(oc_d)
        else:
            sc = gla_final_state(rev(kc), rev(vc), rev(lac))
        o_d, _ = gla_direction(rev(q) * scale, rev(k), rev(v), rev(la), sc)
        o = o + rev(o_d)
    y = (rms_norm(o, lp['gla_norm_g']) * jax.nn.silu(r)).reshape(B, L, GLA_VW)
    yc = (rms_norm(oc, lp['gla_norm_g']) * jax.nn.silu(rc)).reshape(B, Lc, GLA_VW) if with_ctx_out else None
    return y, yc


def window_attention(q, k, v, kc, vc, sink):
    B, L = q.shape[:2]
    Lc = kc.shape[1]
    grp = ATT_HEADS // ATT_KV_HEADS
    nb = L // ATT_BLOCK
    nj = 3 * ATT_BLOCK
    qb = q.reshape(B, nb, ATT_BLOCK, ATT_KV_HEADS, grp, ATT_HEAD_DIM) * ATT_HEAD_DIM ** -0.5
    pad = ((0, 0), (ATT_BLOCK, ATT_BLOCK), (0, 0), (0, 0))

    def band(t):
        tb = jnp.pad(t, pad).reshape(B, nb + 2, ATT_BLOCK, ATT_KV_HEADS, ATT_HEAD_DIM)
        return jnp.concatenate([tb[:, :-2], tb[:, 1:-1], tb[:, 2:]], axis=2)

    kb, vb = band(k), band(v)
    qpos = jnp.arange(L).reshape(nb, ATT_BLOCK, 1)
    kpos = ((jnp.arange(nb) - 1) * ATT_BLOCK)[:, None, None] + jnp.arange(nj)[None, None, :]
    valid = (jnp.abs(kpos - qpos) <= WINDOW) & (kpos >= 0) & (kpos < L)
    s_lat = jnp.einsum('bnqkgd,bnjkd->bnkgqj', qb, kb).astype(jnp.float32)
    s_lat = jnp.where(valid[None, :, None, None], s_lat, -jnp.inf)
    s_ctx = jnp.einsum('bnqkgd,bckd->bnkgqc', qb, kc).astype(jnp.float32)
    s_sink = jnp.broadcast_to(sink.astype(jnp.float32).reshape(1, 1, ATT_KV_HEADS, grp, 1, 1),
                              s_lat.shape[:-1] + (1,))
    p = jax.nn.softmax(jnp.concatenate([s_lat, s_ctx, s_sink], axis=-1), axis=-1).astype(v.dtype)
    o = (jnp.einsum('bnkgqj,bnjkd->bnqkgd', p[..., :nj], vb)
         + jnp.einsum('bnkgqc,bckd->bnqkgd', p[..., nj:nj + Lc], vc))
    return o.reshape(B, L, ATT_QW)


def context_attention(qc, kc, vc, sink):
    B, Lc = qc.shape[:2]
    grp = ATT_HEADS // ATT_KV_HEADS
    qg = qc.reshape(B, Lc, ATT_KV_HEADS, grp, ATT_HEAD_DIM) * ATT_HEAD_DIM ** -0.5
    s = jnp.einsum('bqkgd,bckd->bkgqc', qg, kc).astype(jnp.float32)
    s_sink = jnp.broadcast_to(sink.astype(jnp.float32).reshape(1, ATT_KV_HEADS, grp, 1, 1), s.shape[:-1] + (1,))
    p = jax.nn.softmax(jnp.concatenate([s, s_sink], axis=-1), axis=-1).astype(vc.dtype)
    o = jnp.einsum('bkgqc,bckd->bqkgd', p[..., :Lc], vc)
    return o.reshape(B, Lc, ATT_QW)


def merge_branches(gates, y_s5, y_gla, y_att, lp):
    g = jax.nn.sigmoid(split_heads(gates, N_BRANCH))
    m = (g[..., 0, :] * (y_s5 @ lp['w_branch_s5'])
         + g[..., 1, :] * (y_gla @ lp['w_branch_gla'])
         + g[..., 2, :] * (y_att @ lp['w_branch_attn']))
    return m @ lp['w_out']


def token_mixer(h, hc, lp, cos, sin, with_ctx_out):
    u, qg, kg, vg, rg, qa, ka, va, gates = split_projection(h @ lp['w_in'])
    uc, qgc, kgc, vgc, rgc, qac, kac, vac, gatesc = split_projection(hc @ lp['w_in'])
    y_s5, yc_s5 = s5_branch(u, uc, lp, with_ctx_out)
    y_gla, yc_gla = gla_branch((qg, kg, vg, rg), (qgc, kgc, vgc, rgc), h, hc, lp, with_ctx_out)
    kc_h = split_heads(kac, ATT_KV_HEADS)
    vc_h = split_heads(vac, ATT_KV_HEADS)
    q = apply_rope(split_heads(qa, ATT_HEADS), cos, sin)
    k = apply_rope(split_heads(ka, ATT_KV_HEADS), cos, sin)
    y_att = window_attention(q, k, split_heads(va, ATT_KV_HEADS), kc_h, vc_h, lp['attn_sink'])
    y = merge_branches(gates, y_s5, y_gla, y_att, lp)
    if not with_ctx_out:
        return y, None
    yc_att = context_attention(split_heads(qac, ATT_HEADS), kc_h, vc_h, lp['attn_sink'])
    return y, merge_branches(gatesc, yc_s5, yc_gla, yc_att, lp)


def expert_choice_ffn(h, router, w_gate, w_up, w_down):
    B, N, _ = h.shape
    cap = EC_CAPACITY * N // N_EXPERTS
    aff = jax.nn.softmax((h @ router).astype(jnp.float32), axis=-1)
    gate, idx = lax.top_k(jnp.swapaxes(aff, 1, 2), cap)
    bidx = jnp.arange(B)[:, None, None]
    xs = h[bidx, idx]
    a = jnp.einsum('becd,edf->becf', xs, w_gate)
    up = jnp.einsum('becd,edf->becf', xs, w_up)
    ye = jnp.einsum('becf,efd->becd', jax.nn.silu(a) * up, w_down) * gate[..., None].astype(h.dtype)
    return jnp.zeros_like(h).at[bidx, idx].add(ye)


def setup_inputs(seed: int = 0) -> dict:
    key = jax.random.key(seed)
    ks = iter(jax.random.split(key, 40))
    f32 = jnp.float32

    def nrm(shape, scale):
        return jax.random.normal(next(ks), shape, f32) * scale

    s5_lam_shape = (DEPTH, 2, S5_GROUPS, S5_STATE)
    n_idx = jnp.arange(S5_STATE, dtype=f32)
    return {
        'x': nrm((BATCH, SEQ, D_MODEL), 1.0),
        'c': nrm((BATCH, D_MODEL), 1.0),
        'ctx': nrm((BATCH, CTX_LEN, D_MODEL), 1.0),
        'c_ctx': nrm((D_MODEL,), 1.0),
        'ada_w': nrm((DEPTH, D_MODEL, N_MOD * D_MODEL), 0.5 * D_MODEL ** -0.5),
        'ada_b': nrm((DEPTH, N_MOD * D_MODEL), 0.02),
        'norm1_g': 1.0 + nrm((DEPTH, D_MODEL), 0.02),
        'norm2_g': 1.0 + nrm((DEPTH, D_MODEL), 0.02),
        'w_in': nrm((DEPTH, D_MODEL, IN_WIDTH), D_MODEL ** -0.5),
        's5_lam_re': -0.5 + nrm(s5_lam_shape, 0.01),
        's5_lam_im': math.pi * n_idx + nrm(s5_lam_shape, 0.01),
        's5_log_dt': jax.random.uniform(next(ks), (DEPTH, 2, S5_GROUPS), f32,
                                        math.log(S5_DT_MIN), math.log(S5_DT_MAX)),
        's5_b_re': nrm((DEPTH, 2, S5_GROUPS, S5_STATE, S5_GROUP_CH), (2 * S5_GROUP_CH) ** -0.5),
        's5_b_im': nrm((DEPTH, 2, S5_GROUPS, S5_STATE, S5_GROUP_CH), (2 * S5_GROUP_CH) ** -0.5),
        's5_c_re': nrm((DEPTH, 2, S5_GROUPS, S5_GROUP_CH, S5_STATE), S5_STATE ** -0.5),
        's5_c_im': nrm((DEPTH, 2, S5_GROUPS, S5_GROUP_CH, S5_STATE), S5_STATE ** -0.5),
        's5_d': nrm((DEPTH, S5_WIDTH), 1.0),
        's5_w_glu': nrm((DEPTH, S5_WIDTH, S5_WIDTH), S5_WIDTH ** -0.5),
        'gla_w1': nrm((DEPTH, 2, D_MODEL, GLA_RANK), D_MODEL ** -0.5),
        'gla_w2': nrm((DEPTH, 2, GLA_RANK, GLA_KW), GLA_RANK ** -0.5),
        'gla_b': nrm((DEPTH, 2, GLA_KW), 0.1),
        'gla_norm_g': 1.0 + nrm((DEPTH, GLA_HEADS, GLA_DV), 0.02),
        'attn_sink': nrm((DEPTH, ATT_HEADS), 0.5),
        'w_branch_s5': nrm((DEPTH, S5_WIDTH, D_MODEL), S5_WIDTH ** -0.5),
        'w_branch_gla': nrm((DEPTH, GLA_VW, D_MODEL), GLA_VW ** -0.5),
        'w_branch_attn': nrm((DEPTH, ATT_QW, D_MODEL), ATT_QW ** -0.5),
        'w_out': nrm((DEPTH, D_MODEL, D_MODEL), D_MODEL ** -0.5),
        'moe_router': nrm((DEPTH, D_MODEL, N_EXPERTS), D_MODEL ** -0.5),
        'moe_w_gate': nrm((DEPTH, N_EXPERTS, D_MODEL, EXPERT_FF), D_MODEL ** -0.5),
        'moe_w_up': nrm((DEPTH, N_EXPERTS, D_MODEL, EXPERT_FF), D_MODEL ** -0.5),
        'moe_w_down': nrm((DEPTH, N_EXPERTS, EXPERT_FF, D_MODEL), EXPERT_FF ** -0.5),
        'final_g': 1.0 + nrm((D_MODEL,), 0.02),
    }


def reference(x, c, ctx, c_ctx, ada_w, ada_b, norm1_g, norm2_g, w_in, s5_lam_re, s5_lam_im, s5_log_dt,
              s5_b_re, s5_b_im, s5_c_re, s5_c_im, s5_d, s5_w_glu, gla_w1, gla_w2, gla_b, gla_norm_g,
              attn_sink, w_branch_s5, w_branch_gla, w_branch_attn, w_out, moe_router, moe_w_gate,
              moe_w_up, moe_w_down, final_g):
    B, L = x.shape[:2]
    n_rows = L // GRID_W
    cos, sin = axial_rope_tables(n_rows, x.dtype)
    xc = ctx
    for l in range(DEPTH):
        last = l == DEPTH - 1
        lp = {
            'w_in': w_in[l], 's5_lam_re': s5_lam_re[l], 's5_lam_im': s5_lam_im[l], 's5_log_dt': s5_log_dt[l],
            's5_b_re': s5_b_re[l], 's5_b_im': s5_b_im[l], 's5_c_re': s5_c_re[l], 's5_c_im': s5_c_im[l],
            's5_d': s5_d[l], 's5_w_glu': s5_w_glu[l], 'gla_w1': gla_w1[l], 'gla_w2': gla_w2[l],
            'gla_b': gla_b[l], 'gla_norm_g': gla_norm_g[l], 'attn_sink': attn_sink[l],
            'w_branch_s5': w_branch_s5[l], 'w_branch_gla': w_branch_gla[l],
            'w_branch_attn': w_branch_attn[l], 'w_out': w_out[l],
        }
        mod = (jax.nn.silu(c) @ ada_w[l] + ada_b[l]).reshape(B, N_MOD, 1, D_MODEL)
        modc = (jax.nn.silu(c_ctx) @ ada_w[l] + ada_b[l]).reshape(N_MOD, D_MODEL)
        h = modulate(rms_norm(x, norm1_g[l]), mod[:, 0], mod[:, 1])
        hc = modulate(rms_norm(xc, norm1_g[l]), modc[0], modc[1])
        y, yc = token_mixer(h, hc, lp, cos, sin, not last)
        x = x + mod[:, 2] * y
        h = modulate(rms_norm(x, norm2_g[l]), mod[:, 3], mod[:, 4])
        x = x + mod[:, 5] * expert_choice_ffn(h, moe_router[l], moe_w_gate[l], moe_w_up[l], moe_w_down[l])
        if not last:
            xc = xc + modc[2] * yc
            hc = modulate(rms_norm(xc, norm2_g[l]), modc[3], modc[4])
            xc = xc + modc[5] * expert_choice_ffn(hc, moe_router[l], moe_w_gate[l], moe_w_up[l], moe_w_down[l])
    return rms_norm(x, final_g)
```

```python
import numpy as np
import ml_dtypes
from contextlib import ExitStack
import concourse.bass as bass
import concourse.mybir as mybir
from concourse.bass_utils import run_bass_kernel_spmd

F32 = mybir.dt.float32
BF16 = mybir.dt.bfloat16
I32 = mybir.dt.int32
U32 = mybir.dt.uint32
AF = mybir.ActivationFunctionType
ALU = mybir.AluOpType
AX = mybir.AxisListType
NPBF16 = ml_dtypes.bfloat16
NCORES = 8


class T:
    def __init__(self, name, h):
        self.name = name
        self.h = h
        self.w = {}
        self.r = {}
        self.gen_need = {}
        self.dsem = None
        self.dcount = 0

    def __getitem__(self, idx):
        return self.h[idx]

    def ap(self):
        return self.h.ap() if hasattr(self.h, "ap") else self.h[:]


class KB:
    def __init__(self, same_engine_sync=True):
        self.nc = bass.Bass("TRN2", target_bir_lowering=False)
        nc = self.nc
        self.st = ExitStack()
        self.E = {"pe": nc.tensor, "act": nc.scalar, "dve": nc.vector, "pool": nc.gpsimd, "sp": nc.sync}
        self.sem = {}
        self.cnt = {}
        self.known = {}
        for e in self.E:
            self.sem[e] = self.st.enter_context(nc.semaphore("s_" + e))
            self.cnt[e] = 0
            self.known[e] = {}
        self.qsem = {}
        self.qcnt = {}
        for q in ("sp", "act", "pool"):
            self.qsem[q] = self.st.enter_context(nc.semaphore("q_" + q))
            self.qcnt[q] = 0
        self.ses = same_engine_sync
        self.outs = []
        self.nid = 0

    def sb(self, shape, dt=F32, name=None):
        self.nid += 1
        name = name or f"t{self.nid}"
        h = self.st.enter_context(self.nc.sbuf_tensor(name, list(shape), dt))
        return T(name, h)

    def ps(self, shape, dt=F32, name=None):
        self.nid += 1
        name = name or f"p{self.nid}"
        h = self.st.enter_context(self.nc.psum_tensor(name, list(shape), dt))
        return T(name, h)

    def dram(self, name, shape, dt=F32, kind="Internal"):
        h = self.nc.dram_tensor(name, list(shape), dt, kind=kind)
        t = T(name, h)
        if kind == "ExternalOutput":
            self.outs.append(t)
        return t

    def _need(self, r, w, dkey=None):
        need = {}

        def add(key, sem, val):
            if key not in need or need[key][1] < val:
                need[key] = (sem, val)

        for t in r:
            for k, (s, v) in t.w.items():
                add(k, s, v)
        for t in w:
            for k, (s, v) in t.w.items():
                if dkey is not None and k == dkey:
                    continue
                add(k, s, v)
            for k, (s, v) in t.r.items():
                add(k, s, v)
        return need

    def _wait(self, e, need):
        eng = self.E[e]
        for key, (sem, val) in need.items():
            if self.known[e].get(key, 0) >= val:
                continue
            if key == e and (not self.ses or e == "pe"):
                continue
            eng.wait_ge(sem, val)
            self.known[e][key] = val

    def op(self, e, fn, r=(), w=()):
        need = self._need(r, w)
        self._wait(e, need)
        ins = fn(self.E[e])
        self.cnt[e] += 1
        ins.then_inc(self.sem[e], 1)
        ev = (self.sem[e], self.cnt[e])
        for t in r:
            t.r[e] = ev
        for t in w:
            t.w[e] = ev
        return ins

    def dma(self, q, out_ap, in_ap, r=(), w=(), **kw):
        wt = w[0] if w else None
        dkey = None
        if wt is not None and wt.dsem is None:
            wt.dsem = self.st.enter_context(self.nc.semaphore("d_" + wt.name))
        if wt is not None:
            dkey = "d_" + wt.name
        need = self._need(r, w, dkey)
        self._wait(q, need)
        ins = self.E[q].dma_start(out=out_ap, in_=in_ap, **kw)
        if wt is not None:
            wt.dcount += 16
            sem, val, key = wt.dsem, wt.dcount, dkey
        else:
            self.qcnt[q] += 16
            sem, val, key = self.qsem[q], self.qcnt[q], "q_" + q
        ins.then_inc(sem, 16)
        for t in r:
            t.r[key] = (sem, val)
        for t in w:
            t.w[key] = (sem, val)
        return ins

    def finish(self):
        need = {}
        for t in self.outs:
            for k, (s, v) in t.w.items():
                if k not in need or need[k][1] < v:
                    need[k] = (s, v)
        self._wait("sp", need)
        for q in self.qsem:
            if self.qcnt[q]:
                self.E["sp"].wait_ge(self.qsem[q], self.qcnt[q])
        return self.nc


def run(kb, in_maps, trace=False):
    res = run_bass_kernel_spmd(kb.nc, in_maps, core_ids=list(range(len(in_maps))), trace=trace)
    return res


D = 2048
KT = 16
NW = 13184
TOK = 1056
BLKS = [(0, 512), (512, 512), (1024, 32)]
EPS = 1e-6


def build_k0():
    kb = KB()
    cT = kb.dram("cT", [128, KT, 2], F32, kind="ExternalInput")
    adaw = kb.dram("adaw", [2, D, 1536], F32, kind="ExternalInput")
    adab = kb.dram("adab", [2, 128, 12], F32, kind="ExternalInput")
    modT = kb.dram("modT", [2, 128, 12, 2], F32, kind="ExternalOutput")
    c_sb = kb.sb([128, KT, 2], F32, "c_sb")
    sc = kb.sb([128, KT, 2], F32, "sc")
    kb.dma("sp", c_sb[:], cT.ap(), w=[c_sb])
    kb.op("act", lambda e: e.activation(out=sc[:], in_=c_sb[:], func=AF.Silu), r=[c_sb], w=[sc])
    wts = [kb.sb([128, KT, 1536], F32, f"w{l}") for l in range(2)]
    for l in range(2):
        src = adaw.ap()[l].rearrange("(kt p) n -> p kt n", p=128)
        for kt in range(KT):
            kb.dma("sp" if kt % 2 == 0 else "act", wts[l][:, kt, :], src[:, kt, :], w=[wts[l]])
    for l in range(2):
        b_sb = kb.sb([128, 12], F32, f"b{l}")
        kb.dma("sp", b_sb[:], adab.ap()[l], w=[b_sb])
        pt = kb.ps([128, 12, 2], F32, f"pm{l}")
        for j in range(12):
            for kt in range(KT):
                kb.op("pe", lambda e, j=j, kt=kt: e.matmul(pt[:, j, :], lhsT=wts[l][:, kt, j * 128:(j + 1) * 128],
                                                            rhs=sc[:, kt, :], start=(kt == 0), stop=(kt == KT - 1)),
                      r=[wts[l], sc], w=[pt])
        o = kb.sb([128, 12, 2], F32, f"o{l}")
        for col in range(2):
            kb.op("dve", lambda e, col=col: e.tensor_tensor(out=o[:, :, col], in0=pt[:, :, col], in1=b_sb[:], op=ALU.add),
                  r=[pt, b_sb], w=[o])
        kb.dma("sp", modT.ap()[l], o[:], r=[o], w=[modT])
    kb.finish()
    return kb


def emit_norm_mod(kb, xs, hT, g_sb, sc_l, sh_l, sc_c, sh_c, ones, tagp=""):
    A_l = kb.sb([128, KT], F32, tagp + "A_l")
    A_c = kb.sb([128, KT], F32, tagp + "A_c")
    kb.op("dve", lambda e: e.scalar_tensor_tensor(out=A_l[:], in0=sc_l[:], scalar=1.0, in1=g_sb[:], op0=ALU.add, op1=ALU.mult),
          r=[sc_l, g_sb], w=[A_l])
    kb.op("dve", lambda e: e.scalar_tensor_tensor(out=A_c[:], in0=sc_c[:], scalar=1.0, in1=g_sb[:], op0=ALU.add, op1=ALU.mult),
          r=[sc_c, g_sb], w=[A_c])
    rstd = kb.sb([128, TOK], F32, tagp + "rstd")
    sqs = [kb.sb([128, 512], F32, tagp + f"sq{i}") for i in range(2)]
    pss = [kb.ps([128, 512], F32, tagp + f"pss{i}") for i in range(2)]
    n = 0
    for bi, (c0, cn) in enumerate(BLKS):
        ps = pss[bi % 2]
        for kt in range(KT):
            sq = sqs[n % 2]
            n += 1
            kb.op("act", lambda e, kt=kt, sq=sq: e.activation(out=sq[:, :cn], in_=xs[:, kt, c0:c0 + cn], func=AF.Square),
                  r=[xs], w=[sq])
            kb.op("pe", lambda e, kt=kt, sq=sq: e.matmul(ps[:, :cn], lhsT=ones[:], rhs=sq[:, :cn], start=(kt == 0), stop=(kt == KT - 1)),
                  r=[ones, sq], w=[ps])
        sd = sqs[n % 2]
        n += 1
        kb.op("act", lambda e: e.activation(out=sd[:, :cn], in_=ps[:, :cn], func=AF.Sqrt, bias=EPS, scale=1.0 / D),
              r=[ps], w=[sd])
        kb.op("dve", lambda e: e.reciprocal(out=rstd[:, c0:c0 + cn], in_=sd[:, :cn]), r=[sd], w=[rstd])
    tmps = [kb.sb([128, 512], F32, tagp + f"tmp{i}") for i in range(2)]
    n = 0
    for bi, (c0, cn) in enumerate(BLKS):
        A = A_c if bi == 2 else A_l
        Bt = sh_c if bi == 2 else sh_l
        for kt in range(KT):
            tmp = tmps[n % 2]
            n += 1
            kb.op("dve", lambda e, kt=kt, tmp=tmp: e.tensor_tensor(out=tmp[:, :cn], in0=xs[:, kt, c0:c0 + cn], in1=rstd[:, c0:c0 + cn], op=ALU.mult),
                  r=[xs, rstd], w=[tmp])
            kb.op("pool", lambda e, kt=kt, tmp=tmp, A=A, Bt=Bt: e.tensor_scalar(out=hT[:, kt, c0:c0 + cn], in0=tmp[:, :cn], scalar1=A[:, kt:kt + 1],
                                                                     scalar2=Bt[:, kt:kt + 1], op0=ALU.mult, op1=ALU.add),
                  r=[tmp, A, Bt], w=[hT])


def emit_linear(kb, hT, W, ntiles, out_dram, kt_n=KT, tagp="", evac=None):
    wbs = [kb.sb([128, kt_n, 128], BF16, tagp + f"wb{i}") for i in range(3)]
    pps = [kb.ps([128, 512], F32, tagp + f"pp{i}") for i in range(4)]
    obs = [kb.sb([128, TOK], F32, tagp + f"ob{i}") for i in range(2)]
    Wv = W.ap().rearrange("(kt p) n -> p kt n", p=128)
    pi = 0
    for nt in range(ntiles):
        wb = wbs[nt % 3]
        kb.dma("pool", wb[:], Wv[:, :, nt * 128:(nt + 1) * 128], w=[wb])
        ob = obs[nt % 2]
        for bi, (c0, cn) in enumerate(BLKS):
            pp = pps[pi % 4]
            pi += 1
            for kt in range(kt_n):
                kb.op("pe", lambda e, kt=kt, pp=pp, wb=wb: e.matmul(pp[:, :cn], lhsT=wb[:, kt, :], rhs=hT[:, kt, c0:c0 + cn],
                                                             start=(kt == 0), stop=(kt == kt_n - 1)),
                      r=[wb, hT], w=[pp])
            eng = "act" if pi % 2 == 0 else "dve"
            if eng == "act":
                kb.op("act", lambda e, pp=pp, ob=ob: e.copy(out=ob[:, c0:c0 + cn], in_=pp[:, :cn]), r=[pp], w=[ob])
            else:
                kb.op("dve", lambda e, pp=pp, ob=ob: e.tensor_copy(out=ob[:, c0:c0 + cn], in_=pp[:, :cn]), r=[pp], w=[ob])
        kb.dma("sp", out_dram.ap()[nt * 128:(nt + 1) * 128, :], ob[:], r=[ob], w=[out_dram])


def build_k1(ntiles=NW // 128):
    kb = KB()
    xT = kb.dram("xT", [D, TOK], F32, kind="ExternalInput")
    W = kb.dram("W", [D, ntiles * 128], F32, kind="ExternalInput")
    vecs = kb.dram("vecs", [5, 128, KT], F32, kind="ExternalInput")
    zT = kb.dram("zT", [ntiles * 128, TOK], F32, kind="ExternalOutput")
    xs = kb.sb([128, KT, TOK], F32, "xs")
    xv = xT.ap().rearrange("(kt p) t -> p kt t", p=128)
    for kt in range(KT):
        kb.dma("sp" if kt % 2 == 0 else "act", xs[:, kt, :], xv[:, kt, :], w=[xs])
    vt = []
    for i in range(5):
        t = kb.sb([128, KT], F32, f"vec{i}")
        kb.dma("sp", t[:], vecs.ap()[i], w=[t])
        vt.append(t)
    ones = kb.sb([128, 128], F32, "ones")
    kb.op("dve", lambda e: e.memset(ones[:], 1.0), w=[ones])
    hT = kb.sb([128, KT, TOK], BF16, "hT")
    emit_norm_mod(kb, xs, hT, vt[0], vt[1], vt[2], vt[3], vt[4], ones)
    emit_linear(kb, hT, W, ntiles, zT)
    kb.finish()
    return kb

import math

LTOT = 8448
TC = 256
NCH = LTOT // TC
NTD = 8


def build_k2a():
    kb = KB()
    uT = kb.dram("uT", [2, 128, LTOT], F32, kind="ExternalInput")
    Bblk = kb.dram("Bblk", [2, 2, 4, 128, 128], F32, kind="ExternalInput")
    Cblk = kb.dram("Cblk", [2, 2, 4, 128, 128], F32, kind="ExternalInput")
    lam = kb.dram("lam", [3, 128, NTD], F32, kind="ExternalInput")
    dvec = kb.dram("dvec", [128, 1], F32, kind="ExternalInput")
    yT = kb.dram("yT", [2, 128, LTOT], F32, kind="ExternalOutput")

    ubf = [kb.sb([128, LTOT], BF16, f"ubf{d}") for d in range(2)]
    for d in range(2):
        for c0 in range(0, LTOT, 2048):
            cn = min(2048, LTOT - c0)
            kb.dma("pool", ubf[d][:, c0:c0 + cn], uT.ap()[d, :, c0:c0 + cn], w=[ubf[d]])
    uf = kb.sb([128, LTOT], F32, "uf")
    kb.dma("sp", uf[:], uT.ap()[0], w=[uf])
    dv = kb.sb([128, 1], F32, "dv")
    kb.dma("sp", dv[:], dvec.ap(), w=[dv])
    Bb = kb.sb([128, 2, 2, 4, 128], BF16, "Bb")
    for d in range(2):
        for p in range(2):
            kb.dma("pool", Bb[:, d, p], Bblk.ap()[d, p].rearrange("q k n -> k q n"), w=[Bb])
    Cf = kb.sb([128, 2, 2, 4, 128], F32, "Cf")
    for d in range(2):
        for p in range(2):
            kb.dma("act", Cf[:, d, p], Cblk.ap()[d, p].rearrange("q k n -> k q n"), w=[Cf])
    Cb = kb.sb([128, 2, 2, 4, 128], BF16, "Cb")
    for d in range(2):
        kb.op("dve", lambda e, d=d: e.tensor_copy(out=Cb[:, d, 0], in_=Cf[:, d, 0]), r=[Cf], w=[Cb])
        kb.op("dve", lambda e, d=d: e.tensor_scalar(out=Cb[:, d, 1], in0=Cf[:, d, 1], scalar1=-1.0, scalar2=None, op0=ALU.mult),
              r=[Cf], w=[Cb])
    lr = kb.sb([128, NTD], F32, "lr")
    li = kb.sb([128, NTD], F32, "li")
    ldt = kb.sb([128, NTD], F32, "ldt")
    kb.dma("sp", lr[:], lam.ap()[0], w=[lr])
    kb.dma("sp", li[:], lam.ap()[1], w=[li])
    kb.dma("sp", ldt[:], lam.ap()[2], w=[ldt])

    nid = [0]

    def sm(name=None):
        nid[0] += 1
        return kb.sb([128, NTD], F32, name or f"sm{nid[0]}")

    def tt(out, a, b, op):
        kb.op("dve", lambda e: e.tensor_tensor(out=out[:], in0=a[:], in1=b[:], op=op), r=[a, b], w=[out])

    def ts(out, a, s1, op0, s2=None, op1=None):
        if op1 is None:
            kb.op("dve", lambda e: e.tensor_scalar(out=out[:], in0=a[:], scalar1=s1, scalar2=None, op0=op0), r=[a], w=[out])
        else:
            kb.op("dve", lambda e: e.tensor_scalar(out=out[:], in0=a[:], scalar1=s1, scalar2=s2, op0=op0, op1=op1), r=[a], w=[out])

    dt = sm("dt")
    kb.op("act", lambda e: e.activation(out=dt[:], in_=ldt[:], func=AF.Exp), r=[ldt], w=[dt])
    lrd = sm()
    tt(lrd, lr, dt, ALU.mult)
    mag = sm("mag")
    kb.op("act", lambda e: e.activation(out=mag[:], in_=lrd[:], func=AF.Exp), r=[lrd], w=[mag])
    ang = sm("ang")
    tt(ang, li, dt, ALU.mult)
    kf = sm()
    ts(kf, ang, 1.0 / (2 * math.pi), ALU.mult)
    ki = kb.sb([128, NTD], I32, "ki")
    kb.op("dve", lambda e: e.tensor_copy(out=ki[:], in_=kf[:]), r=[kf], w=[ki])
    kf2 = sm()
    kb.op("dve", lambda e: e.tensor_copy(out=kf2[:], in_=ki[:]), r=[ki], w=[kf2])
    C1 = 6.28125
    C2 = 2 * math.pi - C1
    r1 = sm()
    kb.op("dve", lambda e: e.scalar_tensor_tensor(out=r1[:], in0=kf2[:], scalar=-C1, in1=ang[:], op0=ALU.mult, op1=ALU.add),
          r=[kf2, ang], w=[r1])
    r2 = sm()
    kb.op("dve", lambda e: e.scalar_tensor_tensor(out=r2[:], in0=kf2[:], scalar=-C2, in1=r1[:], op0=ALU.mult, op1=ALU.add),
          r=[kf2, r1], w=[r2])
    xx = sm("xx")
    ts(xx, r2, 0.125, ALU.mult)
    x2 = sm("x2")
    tt(x2, xx, xx, ALU.mult)

    def horner(coefs):
        p = sm()
        ts(p, x2, -1.0 / coefs[-1], ALU.mult, 1.0, ALU.add)
        for cf in reversed(coefs[:-1]):
            q_ = sm()
            tt(q_, p, x2, ALU.mult)
            p = sm()
            ts(p, q_, -1.0 / cf, ALU.mult, 1.0, ALU.add)
        return p

    ps_ = horner([6.0, 20.0, 42.0, 72.0, 110.0, 156.0])
    sn = sm("sn")
    tt(sn, ps_, xx, ALU.mult)
    cs = horner([2.0, 12.0, 30.0, 56.0, 90.0, 132.0])
    for _ in range(3):
        c2 = sm(); s2 = sm(); sc_ = sm()
        tt(c2, cs, cs, ALU.mult)
        tt(s2, sn, sn, ALU.mult)
        tt(sc_, sn, cs, ALU.mult)
        cs = sm(); sn = sm()
        tt(cs, c2, s2, ALU.subtract)
        ts(sn, sc_, 2.0, ALU.mult)
    ab_re = sm("ab_re"); ab_im = sm("ab_im")
    tt(ab_re, mag, cs, ALU.mult)
    tt(ab_im, mag, sn, ALU.mult)
    den = sm(); t_a = sm(); t_b = sm()
    tt(t_a, lr, lr, ALU.mult)
    tt(t_b, li, li, ALU.mult)
    tt(den, t_a, t_b, ALU.add)
    rden = sm()
    kb.op("dve", lambda e: e.reciprocal(out=rden[:], in_=den[:]), r=[den], w=[rden])
    nr = sm()
    ts(nr, ab_re, -1.0, ALU.add)
    f_re = sm("f_re"); f_im = sm("f_im")
    u1 = sm(); u2 = sm(); u3 = sm()
    tt(u1, nr, lr, ALU.mult)
    tt(u2, ab_im, li, ALU.mult)
    tt(u3, u1, u2, ALU.add)
    tt(f_re, u3, rden, ALU.mult)
    v1 = sm(); v2 = sm(); v3 = sm()
    tt(v1, ab_im, lr, ALU.mult)
    tt(v2, nr, li, ALU.mult)
    tt(v3, v1, v2, ALU.subtract)
    tt(f_im, v3, rden, ALU.mult)
    nsn_unused = None

    Er = kb.sb([128, NTD, TC + 1], F32, "Er")
    Ei = kb.sb([128, NTD, TC + 1], F32, "Ei")
    kb.op("dve", lambda e: e.memset(Er[:, :, 0:1], 1.0), w=[Er])
    kb.op("dve", lambda e: e.memset(Ei[:, :, 0:1], 0.0), w=[Ei])
    pr, pi_ = cs, sn
    n = 1
    while n <= TC:
        m = min(n, TC + 1 - n)
        npi = sm()
        ts(npi, pi_, -1.0, ALU.mult)
        ta = kb.sb([128, NTD, m], F32, f"eta{n}")
        tb = kb.sb([128, NTD, m], F32, f"etb{n}")
        for td in range(NTD):
            kb.op("dve", lambda e, td=td: e.tensor_scalar(out=ta[:, td, :], in0=Er[:, td, 0:m], scalar1=pr[:, td:td + 1], scalar2=None, op0=ALU.mult),
                  r=[Er, pr], w=[ta])
            kb.op("dve", lambda e, td=td: e.scalar_tensor_tensor(out=Er[:, td, n:n + m], in0=Ei[:, td, 0:m], scalar=npi[:, td:td + 1], in1=ta[:, td, :],
                                                                 op0=ALU.mult, op1=ALU.add), r=[Ei, npi, ta], w=[Er])
            kb.op("dve", lambda e, td=td: e.tensor_scalar(out=tb[:, td, :], in0=Er[:, td, 0:m], scalar1=pi_[:, td:td + 1], scalar2=None, op0=ALU.mult),
                  r=[Er, pi_], w=[tb])
            kb.op("dve", lambda e, td=td: e.scalar_tensor_tensor(out=Ei[:, td, n:n + m], in0=Ei[:, td, 0:m], scalar=pr[:, td:td + 1], in1=tb[:, td, :],
                                                                 op0=ALU.mult, op1=ALU.add), r=[Ei, pr, tb], w=[Ei])
        c2 = sm(); s2 = sm(); sc_ = sm()
        tt(c2, pr, pr, ALU.mult)
        tt(s2, pi_, pi_, ALU.mult)
        tt(sc_, pr, pi_, ALU.mult)
        pr = sm(); pi_ = sm()
        tt(pr, c2, s2, ALU.subtract)
        ts(pi_, sc_, 2.0, ALU.mult)
        n *= 2
    Rr = kb.sb([128, NTD, TC], F32, "Rr")
    Ri = kb.sb([128, NTD, TC], F32, "Ri")
    rmul = kb.sb([128, NTD, TC], F32, "rmul")
    onesT = kb.sb([128, TC], F32, "onesT")
    kb.op("dve", lambda e: e.memset(onesT[:], 1.0), w=[onesT])
    nf_re = sm()
    ts(nf_re, f_re, -1.0, ALU.mult)
    tr = kb.sb([128, NTD, TC], F32, "trtmp")
    for td in range(NTD):
        kb.op("dve", lambda e, td=td: e.tensor_scalar(out=tr[:, td, :], in0=Er[:, td, 0:TC], scalar1=f_re[:, td:td + 1], scalar2=None, op0=ALU.mult),
              r=[Er, f_re], w=[tr])
        kb.op("dve", lambda e, td=td: e.scalar_tensor_tensor(out=Rr[:, td, :], in0=Ei[:, td, 0:TC], scalar=f_im[:, td:td + 1], in1=tr[:, td, :],
                                                             op0=ALU.mult, op1=ALU.add), r=[Ei, f_im, tr], w=[Rr])
        kb.op("dve", lambda e, td=td: e.tensor_scalar(out=tr[:, td, :], in0=Er[:, td, 0:TC], scalar1=f_im[:, td:td + 1], scalar2=None, op0=ALU.mult),
              r=[Er, f_im], w=[tr])
        kb.op("dve", lambda e, td=td: e.scalar_tensor_tensor(out=Ri[:, td, :], in0=Ei[:, td, 0:TC], scalar=nf_re[:, td:td + 1], in1=tr[:, td, :],
                                                             op0=ALU.mult, op1=ALU.add), r=[Ei, nf_re, tr], w=[Ri])
        kb.op("dve", lambda e, td=td: e.tensor_scalar(out=rmul[:, td, :], in0=onesT[:], scalar1=mag[:, td:td + 1], scalar2=None, op0=ALU.mult),
              r=[onesT, mag], w=[rmul])

    NB = 2
    pX = [[kb.ps([128, 512], F32, f"pX{i}_{p}") for p in range(2)] for i in range(NB)]
    pY = [kb.ps([128, 512], F32, f"pY{i}") for i in range(2)]
    W = {}
    for nm in ("xre", "xim", "t1", "t2", "t3", "t4", "xr", "xi", "gr", "gi", "p1", "p2", "p3", "p4"):
        W[nm] = [kb.sb([128, TC], F32, f"w_{nm}{i}") for i in range(NB)]
    for nm in ("hr", "hi"):
        W[nm] = [kb.sb([128, TC], BF16, f"w_{nm}{i}") for i in range(NB)]
    yo = [kb.sb([128, TC], F32, f"yo{i}") for i in range(2)]
    carry = {}
    for td in range(NTD):
        carry[td] = [kb.sb([128, 2], F32, f"carry{td}_{i}") for i in range(2)]
        kb.op("dve", lambda e, td=td: e.memset(carry[td][0][:], 0.0), w=[carry[td][0]])
    ctmp = [kb.sb([128, 2], F32, f"ctmp{i}") for i in range(2)]
    it = 0
    for d in range(2):
        for c in range(NCH):
            c0 = c * TC
            py = pY[c % 2]
            for q in range(4):
                td = d * 4 + q
                b = it % NB
                it += 1
                px = pX[b]
                w = {k: v[b] for k, v in W.items()}
                for p in range(2):
                    kb.op("pe", lambda e, p=p: e.matmul(px[p][:, :TC], lhsT=Bb[:, d, p, q, :], rhs=ubf[d][:, c0:c0 + TC], start=True, stop=True),
                          r=[Bb, ubf[d]], w=[px[p]])
                kb.op("act", lambda e: e.copy(out=w["xre"][:], in_=px[0][:, :TC]), r=[px[0]], w=[w["xre"]])
                kb.op("act", lambda e: e.copy(out=w["xim"][:], in_=px[1][:, :TC]), r=[px[1]], w=[w["xim"]])
                kb.op("pool", lambda e: e.tensor_tensor(out=w["t1"][:], in0=w["xre"][:], in1=Rr[:, td, :], op=ALU.mult), r=[w["xre"], Rr], w=[w["t1"]])
                kb.op("pool", lambda e: e.tensor_tensor(out=w["t2"][:], in0=w["xim"][:], in1=Ri[:, td, :], op=ALU.mult), r=[w["xim"], Ri], w=[w["t2"]])
                kb.op("pool", lambda e: e.tensor_tensor(out=w["t3"][:], in0=w["xre"][:], in1=Ri[:, td, :], op=ALU.mult), r=[w["xre"], Ri], w=[w["t3"]])
                kb.op("pool", lambda e: e.tensor_tensor(out=w["t4"][:], in0=w["xim"][:], in1=Rr[:, td, :], op=ALU.mult), r=[w["xim"], Rr], w=[w["t4"]])
                kb.op("dve", lambda e: e.tensor_tensor(out=w["xr"][:], in0=w["t1"][:], in1=w["t2"][:], op=ALU.subtract), r=[w["t1"], w["t2"]], w=[w["xr"]])
                kb.op("dve", lambda e: e.tensor_tensor(out=w["xi"][:], in0=w["t3"][:], in1=w["t4"][:], op=ALU.add), r=[w["t3"], w["t4"]], w=[w["xi"]])
                cin = carry[td][c % 2]
                cout = carry[td][(c + 1) % 2]
                kb.op("dve", lambda e: e.tensor_tensor_scan(out=w["gr"][:], data0=rmul[:, td, :], data1=w["xr"][:], initial=cin[:, 0:1],
                                                            op0=ALU.mult, op1=ALU.add), r=[rmul, w["xr"], cin], w=[w["gr"]])
                kb.op("dve", lambda e: e.tensor_tensor_scan(out=w["gi"][:], data0=rmul[:, td, :], data1=w["xi"][:], initial=cin[:, 1:2],
                                                            op0=ALU.mult, op1=ALU.add), r=[rmul, w["xi"], cin], w=[w["gi"]])
                ct = ctmp[it % 2]
                kb.op("dve", lambda e: e.tensor_tensor(out=ct[:, 0:1], in0=w["gi"][:, TC - 1:TC], in1=Ei[:, td, TC:TC + 1], op=ALU.mult),
                      r=[w["gi"], Ei], w=[ct])
                kb.op("dve", lambda e: e.tensor_tensor(out=ct[:, 1:2], in0=w["gr"][:, TC - 1:TC], in1=Ei[:, td, TC:TC + 1], op=ALU.mult),
                      r=[w["gr"], Ei], w=[ct])
                kb.op("dve", lambda e: e.scalar_tensor_tensor(out=cout[:, 0:1], in0=w["gr"][:, TC - 1:TC], scalar=Er[:, td, TC:TC + 1], in1=ct[:, 0:1],
                                                              op0=ALU.mult, op1=ALU.subtract), r=[w["gr"], Er, ct], w=[cout])
                kb.op("dve", lambda e: e.scalar_tensor_tensor(out=cout[:, 1:2], in0=w["gi"][:, TC - 1:TC], scalar=Er[:, td, TC:TC + 1], in1=ct[:, 1:2],
                                                              op0=ALU.mult, op1=ALU.add), r=[w["gi"], Er, ct], w=[cout])
                kb.op("pool", lambda e: e.tensor_tensor(out=w["p1"][:], in0=w["gr"][:], in1=Er[:, td, 0:TC], op=ALU.mult), r=[w["gr"], Er], w=[w["p1"]])
                kb.op("pool", lambda e: e.tensor_tensor(out=w["p2"][:], in0=w["gi"][:], in1=Ei[:, td, 0:TC], op=ALU.mult), r=[w["gi"], Ei], w=[w["p2"]])
                kb.op("pool", lambda e: e.tensor_tensor(out=w["p3"][:], in0=w["gr"][:], in1=Ei[:, td, 0:TC], op=ALU.mult), r=[w["gr"], Ei], w=[w["p3"]])
                kb.op("pool", lambda e: e.tensor_tensor(out=w["p4"][:], in0=w["gi"][:], in1=Er[:, td, 0:TC], op=ALU.mult), r=[w["gi"], Er], w=[w["p4"]])
                kb.op("dve", lambda e: e.tensor_tensor(out=w["hr"][:], in0=w["p1"][:], in1=w["p2"][:], op=ALU.subtract), r=[w["p1"], w["p2"]], w=[w["hr"]])
                kb.op("dve", lambda e: e.tensor_tensor(out=w["hi"][:], in0=w["p3"][:], in1=w["p4"][:], op=ALU.add), r=[w["p3"], w["p4"]], w=[w["hi"]])
                kb.op("pe", lambda e: e.matmul(py[:, :TC], lhsT=Cb[:, d, 0, q, :], rhs=w["hr"][:], start=(q == 0), stop=False), r=[Cb, w["hr"]], w=[py])
                kb.op("pe", lambda e: e.matmul(py[:, :TC], lhsT=Cb[:, d, 1, q, :], rhs=w["hi"][:], start=False, stop=(q == 3)), r=[Cb, w["hi"]], w=[py])
            o = yo[c % 2]
            if d == 0:
                kb.op("dve", lambda e: e.scalar_tensor_tensor(out=o[:], in0=uf[:, c0:c0 + TC], scalar=dv[:, 0:1], in1=py[:, :TC], op0=ALU.mult, op1=ALU.add),
                      r=[uf, dv, py], w=[o])
            else:
                kb.op("act", lambda e: e.copy(out=o[:], in_=py[:, :TC]), r=[py], w=[o])
            kb.dma("sp", yT.ap()[d, :, c0:c0 + TC], o[:], r=[o], w=[yT])
    kb.finish()
    return kb


LT = 8448
NC64 = LT // 64


def build_k2b():
    kb = KB()
    qk = kb.dram("qk", [2, 128, LT], F32, kind="ExternalInput")
    hw1 = kb.dram("hw1", [16, LT], F32, kind="ExternalInput")
    w2b = kb.dram("w2b", [16, 128], F32, kind="ExternalInput")
    nb = kb.dram("nb", [128, 1], F32, kind="ExternalInput")
    vtok = kb.dram("vtok", [64, NC64, 256], F32, kind="ExternalInput")
    cst = kb.dram("cst", [128, 512 + 64 + 128], F32, kind="ExternalInput")
    oT = kb.dram("oT", [2, 128, LT], F32, kind="ExternalOutput")

    vb = kb.sb([64, NC64, 256], BF16, "vb")
    for c0 in range(0, NC64, 8):
        c1 = min(NC64, c0 + 8)
        kb.dma("pool", vb[:, c0:c1, :], vtok.ap()[:, c0:c1, :], w=[vb])
    cs_ = kb.sb([128, 704], F32, "cs_")
    kb.dma("sp", cs_[:], cst.ap(), w=[cs_])
    identb = kb.sb([128, 128], BF16, "identb")
    kb.op("dve", lambda e: e.tensor_copy(out=identb[:], in_=cs_[:, 576:704]), r=[cs_], w=[identb])
    w2s = kb.sb([16, 128], F32, "w2s")
    kb.dma("sp", w2s[:], w2b.ap(), w=[w2s])
    bs_ = kb.sb([128, 1], F32, "bs_")
    kb.dma("sp", bs_[:], nb.ap(), w=[bs_])
    nbs = kb.sb([128, 1], F32, "nbs")
    kb.op("dve", lambda e: e.tensor_scalar(out=nbs[:], in0=bs_[:], scalar1=-1.0, scalar2=None, op0=ALU.mult), r=[bs_], w=[nbs])
    qe = kb.sb([128, LT], BF16, "qe")
    ke = kb.sb([128, LT], BF16, "ke")
    kl = kb.sb([128, LT], BF16, "kl")
    ebl = kb.sb([128, NC64], F32, "ebl")

    BL = 512
    nblk = (LT + BL - 1) // BL
    A = {}
    for nm in ("q", "k", "e1", "la", "bc", "eb", "enb", "kef"):
        A[nm] = [kb.sb([128, BL], F32, f"a_{nm}{i}") for i in range(2)]
    A["h"] = [kb.sb([16, BL], F32, f"a_h{i}") for i in range(2)]
    pso = [kb.ps([128, 512], F32, f"pso{i}") for i in range(2)]
    pg = pso
    scale = 128 ** -0.5
    for bi in range(nblk):
        c0 = bi * BL
        cn = min(BL, LT - c0)
        b = bi % 2
        a = {k: v[b] for k, v in A.items()}
        kb.dma("sp", a["q"][:, :cn], qk.ap()[0, :, c0:c0 + cn], w=[a["q"]])
        kb.dma("act", a["k"][:, :cn], qk.ap()[1, :, c0:c0 + cn], w=[a["k"]])
        kb.dma("sp", a["h"][:, :cn], hw1.ap()[:, c0:c0 + cn], w=[a["h"]])
        kb.op("pe", lambda e: e.matmul(pg[b][:, :cn], lhsT=w2s[:], rhs=a["h"][:, :cn], start=True, stop=True), r=[w2s, a["h"]], w=[pg[b]])
        kb.op("act", lambda e: e.activation(out=a["e1"][:, :cn], in_=pg[b][:, :cn], func=AF.Exp, bias=nbs[:, 0:1], scale=-1.0),
              r=[pg[b], nbs], w=[a["e1"]])
        kb.op("act", lambda e: e.activation(out=a["la"][:, :cn], in_=a["e1"][:, :cn], func=AF.Ln, bias=1.0), r=[a["e1"]], w=[a["la"]])
        kb.op("pool", lambda e: e.tensor_scalar(out=a["la"][:, :cn], in0=a["la"][:, :cn], scalar1=-1.0 / 16.0, scalar2=None, op0=ALU.mult),
              r=[a["la"]], w=[a["la"]])
        kb.op("dve", lambda e: e.tensor_tensor_scan(out=a["bc"][:, :cn], data0=cs_[:, 0:cn], data1=a["la"][:, :cn], initial=0.0,
                                                    op0=ALU.mult, op1=ALU.add), r=[cs_, a["la"]], w=[a["bc"]])
        kb.op("act", lambda e: e.activation(out=a["eb"][:, :cn], in_=a["bc"][:, :cn], func=AF.Exp), r=[a["bc"]], w=[a["eb"]])
        kb.op("act", lambda e: e.activation(out=a["enb"][:, :cn], in_=a["bc"][:, :cn], func=AF.Exp, scale=-1.0), r=[a["bc"]], w=[a["enb"]])
        kb.op("dve", lambda e: e.scalar_tensor_tensor(out=qe[:, c0:c0 + cn], in0=a["q"][:, :cn], scalar=scale, in1=a["eb"][:, :cn],
                                                      op0=ALU.mult, op1=ALU.mult), r=[a["q"], a["eb"]], w=[qe])
        kb.op("pool", lambda e: e.tensor_tensor(out=a["kef"][:, :cn], in0=a["k"][:, :cn], in1=a["enb"][:, :cn], op=ALU.mult),
              r=[a["k"], a["enb"]], w=[a["kef"]])
        kb.op("act", lambda e: e.copy(out=ke[:, c0:c0 + cn], in_=a["kef"][:, :cn]), r=[a["kef"]], w=[ke])
        nch = cn // 64
        ch0 = c0 // 64
        kb.op("dve", lambda e: e.tensor_copy(out=ebl[:, ch0:ch0 + nch],
                                             in_=a["eb"][:, :cn].rearrange("p (c j) -> p c j", j=64)[:, :, 63]), r=[a["eb"]], w=[ebl])
        for cc in range(nch):
            kb.op("dve", lambda e, cc=cc: e.tensor_scalar(out=kl[:, c0 + cc * 64:c0 + (cc + 1) * 64], in0=a["kef"][:, cc * 64:(cc + 1) * 64],
                                                          scalar1=ebl[:, ch0 + cc:ch0 + cc + 1], scalar2=None, op0=ALU.mult),
                  r=[a["kef"], ebl], w=[kl])

    S = [kb.sb([128, 256], F32, f"S{i}") for i in range(2)]
    Sb = [kb.sb([128, 256], BF16, f"Sb{i}") for i in range(2)]
    kb.op("dve", lambda e: e.memset(S[0][:], 0.0), w=[S[0]])
    kb.op("dve", lambda e: e.memset(Sb[0][:], 0.0), w=[Sb[0]])
    psc = [kb.ps([64, 512], F32, f"psc{i}") for i in range(2)]
    ptr = [kb.ps([64, 128], BF16, f"ptr{i}") for i in range(2)]
    pst = [kb.ps([128, 512], F32, f"pst{i}") for i in range(1)]
    pm = [kb.sb([64, 64], BF16, f"pm{i}") for i in range(2)]
    klT = [kb.sb([64, 128], BF16, f"klT{i}") for i in range(2)]
    GB = 8
    ost = [kb.sb([128, 2, GB * 64], F32, f"ost{i}") for i in range(2)]
    for c in range(NC64):
        b = c % 2
        cols = slice(c * 64, (c + 1) * 64)
        So, Sn = S[c % 2], S[(c + 1) % 2]
        Sbo, Sbn = Sb[c % 2], Sb[(c + 1) % 2]
        kb.op("pe", lambda e: e.matmul(psc[b][:, 0:64], lhsT=ke[:, cols], rhs=qe[:, cols], start=True, stop=True), r=[ke, qe], w=[psc[b]])
        kb.op("dve", lambda e: e.tensor_tensor(out=pm[b][:], in0=psc[b][:, 0:64], in1=cs_[0:64, 512:576], op=ALU.mult), r=[psc[b], cs_], w=[pm[b]])
        kb.op("pe", lambda e: e.transpose(ptr[b][:], kl[:, cols], identb[:]), r=[kl, identb], w=[ptr[b]])
        kb.op("act", lambda e: e.copy(out=klT[b][:], in_=ptr[b][:]), r=[ptr[b]], w=[klT[b]])
        for vt in range(2):
            kb.op("pe", lambda e, vt=vt: e.matmul(pso[b][:, vt * 64:(vt + 1) * 64], lhsT=vb[:, c, vt * 128:(vt + 1) * 128], rhs=pm[b][:], start=True, stop=False),
                  r=[vb, pm[b]], w=[pso[b]])
            kb.op("pe", lambda e, vt=vt: e.matmul(pso[b][:, vt * 64:(vt + 1) * 64], lhsT=Sbo[:, vt * 128:(vt + 1) * 128], rhs=qe[:, cols], start=False, stop=True),
                  r=[Sbo, qe], w=[pso[b]])
        o = ost[(c // GB) % 2]
        oc = (c % GB) * 64
        kb.op("act", lambda e: e.copy(out=o[:, :, oc:oc + 64], in_=pso[b][:, 0:128].rearrange("p (v i) -> p v i", v=2)), r=[pso[b]], w=[o])
        kb.op("pe", lambda e: e.matmul(pst[0][:, 0:256], lhsT=klT[b][:], rhs=vb[:, c, :], start=True, stop=True), r=[klT[b], vb], w=[pst[0]])
        kb.op("dve", lambda e: e.scalar_tensor_tensor(out=Sn[:], in0=So[:], scalar=ebl[:, c:c + 1], in1=pst[0][:, 0:256], op0=ALU.mult, op1=ALU.add),
              r=[So, ebl, pst[0]], w=[Sn])
        kb.op("pool", lambda e: e.tensor_copy(out=Sbn[:], in_=Sn[:]), r=[Sn], w=[Sbn])
        if c % GB == GB - 1 or c == NC64 - 1:
            g0 = (c // GB) * GB * 64
            gn = (c % GB + 1) * 64
            for vt in range(2):
                kb.dma("sp", oT.ap()[vt, :, g0:g0 + gn], o[:, vt, 0:gn], r=[o], w=[oT])
    kb.finish()
    return kb


L = 8192
NBQ = 64


def build_k2c():
    kb = KB()
    qT = kb.dram("qT", [2, 128, L], F32, kind="ExternalInput")
    kT = kb.dram("kT", [2, 128, L], F32, kind="ExternalInput")
    cs = kb.dram("cs", [2, 128, L], F32, kind="ExternalInput")
    cx = kb.dram("cx", [2, 128, 256], F32, kind="ExternalInput")
    vtok = kb.dram("vtok", [128, 66, 64], F32, kind="ExternalInput")
    masks = kb.dram("masks", [2, 128, 128], F32, kind="ExternalInput")
    sink = kb.dram("sink", [64, 2], F32, kind="ExternalInput")
    yT = kb.dram("yT", [128, L], F32, kind="ExternalOutput")
    ycT = kb.dram("ycT", [128, 256], F32, kind="ExternalOutput")

    qr = kb.sb([128, L + 256], BF16, "qr")
    kr = kb.sb([128, L + 256], BF16, "kr")
    vb = kb.sb([128, 66, 64], BF16, "vb")
    kb.dma("pool", vb[:], vtok.ap(), w=[vb])
    mk = kb.sb([128, 2, 128], BF16, "mk")
    kb.dma("pool", mk[:], masks.ap().rearrange("m j i -> j m i"), w=[mk])
    onesb = kb.sb([128, 64], BF16, "onesb")
    kb.op("dve", lambda e: e.memset(onesb[:], 1.0), w=[onesb])
    sk = kb.sb([64, 2], F32, "sk")
    kb.dma("sp", sk[:], sink.ap(), w=[sk])
    es = kb.sb([64, 2], F32, "es")
    kb.op("act", lambda e: e.activation(out=es[:], in_=sk[:], func=AF.Exp), r=[sk], w=[es])
    kb.dma("pool", qr[:, L:L + 256], cx.ap()[0], w=[qr])
    kb.dma("pool", kr[:, L:L + 256], cx.ap()[1], w=[kr])
    RB = 1024
    bufs = {}
    for nm in ("a", "ap", "c", "s", "t1", "t2"):
        bufs[nm] = [kb.sb([128, RB], F32, f"r_{nm}{i}") for i in range(2)]
    it = 0
    for src, dst in ((qT, qr), (kT, kr)):
        for c0 in range(0, L, RB):
            b = it % 2
            it += 1
            a, ap_, c_, s_, t1, t2 = (bufs[nm][b] for nm in ("a", "ap", "c", "s", "t1", "t2"))
            kb.dma("sp", a[:], src.ap()[0, :, c0:c0 + RB], w=[a])
            kb.dma("act", ap_[:], src.ap()[1, :, c0:c0 + RB], w=[ap_])
            kb.dma("sp", c_[:], cs.ap()[0, :, c0:c0 + RB], w=[c_])
            kb.dma("act", s_[:], cs.ap()[1, :, c0:c0 + RB], w=[s_])
            kb.op("dve", lambda e: e.tensor_tensor(out=t1[:], in0=a[:], in1=c_[:], op=ALU.mult), r=[a, c_], w=[t1])
            kb.op("pool", lambda e: e.tensor_tensor(out=t2[:], in0=ap_[:], in1=s_[:], op=ALU.mult), r=[ap_, s_], w=[t2])
            kb.op("dve", lambda e: e.tensor_tensor(out=dst[:, c0:c0 + RB], in0=t1[:], in1=t2[:], op=ALU.add), r=[t1, t2], w=[dst])

    ps1 = [kb.ps([128, 512], F32, f"ps1_{i}") for i in range(2)]
    ps2 = [kb.ps([128, 512], F32, f"ps2_{i}") for i in range(2)]
    po = [kb.ps([64, 512], F32, f"po{i}") for i in range(2)]
    pb1 = [kb.sb([128, 512], BF16, f"pb1_{i}") for i in range(2)]
    pb2 = [kb.sb([128, 128], BF16, f"pb2_{i}") for i in range(2)]
    den = [kb.sb([64, 128], F32, f"den{i}") for i in range(2)]
    rden = [kb.sb([64, 128], F32, f"rden{i}") for i in range(2)]
    GB = 8
    ob = [kb.sb([64, 2, GB * 128], F32, f"ob{i}") for i in range(2)]
    it = 0
    nblocks = NBQ + 2
    for n in range(nblocks):
        isctx = n >= NBQ
        o = ob[(n // GB) % 2]
        for hh in range(2):
            b = it % 2
            it += 1
            hs = slice(hh * 64, hh * 64 + 64)
            if not isctx:
                qcols = slice(n * 128, (n + 1) * 128)
                tiles = []
                if n > 0:
                    tiles.append((0, n - 1, 0))
                tiles.append((1, n, None))
                if n < NBQ - 1:
                    tiles.append((2, n + 1, 1))
                tiles.append((3, 64, None))
                tiles.append((4, 65, None))
            else:
                qcols = slice(L + (n - NBQ) * 128, L + (n - NBQ + 1) * 128)
                tiles = [(3, 64, None), (4, 65, None)]
            p1, p2 = ps1[b], ps2[b]
            for slot, kt, m in tiles:
                dstp = p1[:, slot * 128:(slot + 1) * 128] if slot < 4 else p2[:, 0:128]
                pt = p1 if slot < 4 else p2
                kb.op("pe", lambda e: e.matmul(dstp, lhsT=kr[hs, kt * 128:(kt + 1) * 128], rhs=qr[hs, qcols], start=True, stop=True),
                      r=[kr, qr], w=[pt])
            slots = [t[0] for t in tiles if t[0] < 4]
            lo, hi = min(slots) * 128, (max(slots) + 1) * 128
            kb.op("act", lambda e: e.activation(out=pb1[b][:, lo:hi], in_=p1[:, lo:hi], func=AF.Exp, scale=0.125), r=[p1], w=[pb1[b]])
            kb.op("act", lambda e: e.activation(out=pb2[b][:], in_=p2[:, 0:128], func=AF.Exp, scale=0.125), r=[p2], w=[pb2[b]])
            for slot, kt, m in tiles:
                if m is not None:
                    kb.op("dve", lambda e: e.tensor_tensor(out=pb1[b][:, slot * 128:(slot + 1) * 128], in0=pb1[b][:, slot * 128:(slot + 1) * 128],
                                                           in1=mk[:, m, :], op=ALU.mult), r=[pb1[b], mk], w=[pb1[b]])
            pp = po[b]
            for ti, (slot, kt, m) in enumerate(tiles):
                src = pb1[b][:, slot * 128:(slot + 1) * 128] if slot < 4 else pb2[b][:]
                st = pb1[b] if slot < 4 else pb2[b]
                kb.op("pe", lambda e: e.matmul(pp[:, 0:128], lhsT=vb[:, kt, :], rhs=src, start=(ti == 0), stop=(ti == len(tiles) - 1)),
                      r=[vb, st], w=[pp])
            for ti, (slot, kt, m) in enumerate(tiles):
                src = pb1[b][:, slot * 128:(slot + 1) * 128] if slot < 4 else pb2[b][:]
                st = pb1[b] if slot < 4 else pb2[b]
                kb.op("pe", lambda e: e.matmul(pp[:, 128:256], lhsT=onesb[:], rhs=src, start=(ti == 0), stop=(ti == len(tiles) - 1)),
                      r=[onesb, st], w=[pp])
            kb.op("dve", lambda e: e.tensor_scalar(out=den[b][:], in0=pp[:, 128:256], scalar1=es[:, hh:hh + 1], scalar2=None, op0=ALU.add),
                  r=[pp, es], w=[den[b]])
            kb.op("dve", lambda e: e.reciprocal(out=rden[b][:], in_=den[b][:]), r=[den[b]], w=[rden[b]])
            oc = (n % GB) * 128
            kb.op("dve", lambda e: e.tensor_tensor(out=o[:, hh, oc:oc + 128], in0=pp[:, 0:128], in1=rden[b][:], op=ALU.mult),
                  r=[pp, rden[b]], w=[o])
        if n < NBQ and n % GB == GB - 1:
            g0 = (n // GB) * GB * 128
            for hh in range(2):
                kb.dma("sp", yT.ap()[hh * 64:(hh + 1) * 64, g0:g0 + GB * 128], o[:, hh, :], r=[o], w=[yT])
        if n == nblocks - 1:
            for hh in range(2):
                kb.dma("sp", ycT.ap()[hh * 64:(hh + 1) * 64, :], o[:, hh, 0:256], r=[o], w=[ycT])
    kb.finish()
    return kb


def build_k3a():
    kb = KB()
    ys = kb.dram("ys", [2, 1024, TOK], F32, kind="ExternalInput")
    og = kb.dram("og", [2, 1024, TOK], F32, kind="ExternalInput")
    ya = kb.dram("ya", [1024, TOK], F32, kind="ExternalInput")
    zr = kb.dram("zr", [1024, TOK], F32, kind="ExternalInput")
    zg = kb.dram("zg", [6144, TOK], F32, kind="ExternalInput")
    xT = kb.dram("xT", [D, TOK], F32, kind="ExternalInput")
    wglu = kb.dram("wglu", [1024, 1024], F32, kind="ExternalInput")
    wbr = kb.dram("wbr", [3, 1024, D], F32, kind="ExternalInput")
    wout = kb.dram("wout", [D, D], F32, kind="ExternalInput")
    vecs = kb.dram("vecs", [3, 128, KT], F32, kind="ExternalInput")
    xo = kb.dram("xo", [D, TOK], F32, kind="ExternalOutput")

    vt_ = []
    for i in range(3):
        t = kb.sb([128, KT], F32, f"vec{i}")
        kb.dma("sp", t[:], vecs.ap()[i], w=[t])
        vt_.append(t)
    gn, m2l, m2c = vt_
    ones = kb.sb([128, 128], F32, "ones")
    kb.op("dve", lambda e: e.memset(ones[:], 1.0), w=[ones])
    PS = [kb.ps([128, 512], F32, f"ps{i}") for i in range(6)]
    psi = [0]

    def nps():
        psi[0] += 1
        return PS[psi[0] % 6]

    W = {}

    def wk(nm, n=2):
        if nm not in W:
            W[nm] = [[kb.sb([128, 512], F32, f"w_{nm}{i}") for i in range(n)], 0]
        W[nm][1] += 1
        return W[nm][0][W[nm][1] % len(W[nm][0])]

    ys5 = kb.sb([128, 8, TOK], BF16, "ys5")
    ygla = kb.sb([128, 8, TOK], BF16, "ygla")
    yatt = kb.sb([128, 8, TOK], BF16, "yatt")
    for kt in range(8):
        kb.dma("pool", yatt[:, kt, :], ya.ap()[kt * 128:(kt + 1) * 128, :], w=[yatt])

    gf = kb.sb([128, 8, TOK], F32, "gf")
    gb = kb.sb([128, 8, TOK], BF16, "gb")
    for kt in range(8):
        for (c0, cn) in BLKS:
            a = wk("a"); b = wk("b"); y = wk("y"); t = wk("t"); s = wk("s")
            kb.dma("sp", a[:, :cn], ys.ap()[0, kt * 128:(kt + 1) * 128, c0:c0 + cn], w=[a])
            kb.dma("act", b[:, :cn], ys.ap()[1, kt * 128:(kt + 1) * 128, c0:c0 + cn], w=[b])
            kb.op("dve", lambda e: e.tensor_tensor(out=y[:, :cn], in0=a[:, :cn], in1=b[:, :cn], op=ALU.add), r=[a, b], w=[y])
            kb.op("pool", lambda e: e.tensor_tensor(out=t[:, :cn], in0=y[:, :cn], in1=y[:, :cn], op=ALU.mult), r=[y], w=[t])
            kb.op("pool", lambda e: e.tensor_scalar(out=t[:, :cn], in0=t[:, :cn], scalar1=0.044715, scalar2=1.0, op0=ALU.mult, op1=ALU.add), r=[t], w=[t])
            kb.op("dve", lambda e: e.tensor_tensor(out=t[:, :cn], in0=t[:, :cn], in1=y[:, :cn], op=ALU.mult), r=[t, y], w=[t])
            kb.op("act", lambda e: e.activation(out=s[:, :cn], in_=t[:, :cn], func=AF.Sigmoid, scale=1.5957691216057308), r=[t], w=[s])
            kb.op("dve", lambda e: e.tensor_tensor(out=gf[:, kt, c0:c0 + cn], in0=y[:, :cn], in1=s[:, :cn], op=ALU.mult), r=[y, s], w=[gf])
            kb.op("pool", lambda e: e.tensor_copy(out=gb[:, kt, c0:c0 + cn], in_=gf[:, kt, c0:c0 + cn]), r=[gf], w=[gb])
    wgs = [kb.sb([128, 8, 128], BF16, f"wg{i}") for i in range(2)]
    wgv = wglu.ap().rearrange("(kt p) n -> p kt n", p=128)
    for nt in range(8):
        wg = wgs[nt % 2]
        kb.dma("pool", wg[:], wgv[:, :, nt * 128:(nt + 1) * 128], w=[wg])
        for (c0, cn) in BLKS:
            ps = nps()
            for kt in range(8):
                kb.op("pe", lambda e, kt=kt: e.matmul(ps[:, :cn], lhsT=wg[:, kt, :], rhs=gb[:, kt, c0:c0 + cn], start=(kt == 0), stop=(kt == 7)),
                      r=[wg, gb], w=[ps])
            s = wk("s")
            kb.op("act", lambda e: e.activation(out=s[:, :cn], in_=ps[:, :cn], func=AF.Sigmoid), r=[ps], w=[s])
            kb.op("dve", lambda e: e.tensor_tensor(out=ys5[:, nt, c0:c0 + cn], in0=gf[:, nt, c0:c0 + cn], in1=s[:, :cn], op=ALU.mult), r=[gf, s], w=[ys5])

    of = gf
    for hd in range(4):
        for (c0, cn) in BLKS:
            ps = nps()
            for v in range(2):
                kt = hd * 2 + v
                a = wk("a"); b = wk("b"); sq = wk("y")
                kb.dma("sp", a[:, :cn], og.ap()[0, kt * 128:(kt + 1) * 128, c0:c0 + cn], w=[a])
                kb.dma("act", b[:, :cn], og.ap()[1, kt * 128:(kt + 1) * 128, c0:c0 + cn], w=[b])
                kb.op("dve", lambda e: e.tensor_tensor(out=of[:, kt, c0:c0 + cn], in0=a[:, :cn], in1=b[:, :cn], op=ALU.add), r=[a, b], w=[of])
                kb.op("act", lambda e: e.activation(out=sq[:, :cn], in_=of[:, kt, c0:c0 + cn], func=AF.Square), r=[of], w=[sq])
                kb.op("pe", lambda e: e.matmul(ps[:, :cn], lhsT=ones[:], rhs=sq[:, :cn], start=(v == 0), stop=(v == 1)), r=[ones, sq], w=[ps])
            sd = wk("t"); rs = wk("s", 3)
            kb.op("act", lambda e: e.activation(out=sd[:, :cn], in_=ps[:, :cn], func=AF.Sqrt, bias=EPS, scale=1.0 / 256), r=[ps], w=[sd])
            kb.op("dve", lambda e: e.reciprocal(out=rs[:, :cn], in_=sd[:, :cn]), r=[sd], w=[rs])
            for v in range(2):
                kt = hd * 2 + v
                r_ = wk("a"); sg = wk("b"); sr = wk("y"); yy = wk("t")
                kb.dma("sp", r_[:, :cn], zr.ap()[kt * 128:(kt + 1) * 128, c0:c0 + cn], w=[r_])
                kb.op("act", lambda e: e.activation(out=sg[:, :cn], in_=r_[:, :cn], func=AF.Sigmoid), r=[r_], w=[sg])
                kb.op("pool", lambda e: e.tensor_tensor(out=sr[:, :cn], in0=r_[:, :cn], in1=sg[:, :cn], op=ALU.mult), r=[r_, sg], w=[sr])
                kb.op("dve", lambda e: e.tensor_tensor(out=yy[:, :cn], in0=of[:, kt, c0:c0 + cn], in1=rs[:, :cn], op=ALU.mult), r=[of, rs], w=[yy])
                kb.op("dve", lambda e: e.scalar_tensor_tensor(out=ygla[:, kt, c0:c0 + cn], in0=yy[:, :cn], scalar=gn[:, kt:kt + 1], in1=sr[:, :cn],
                                                              op0=ALU.mult, op1=ALU.mult), r=[yy, gn, sr], w=[ygla])

    mb = kb.sb([128, KT, TOK], BF16, "mb")
    wbs = [[kb.sb([128, 8, 128], BF16, f"wb{b}_{i}") for i in range(2)] for b in range(3)]
    ysrc = [ys5, ygla, yatt]
    for nt in range(KT):
        wts = []
        for b in range(3):
            wt = wbs[b][nt % 2]
            kb.dma("pool", wt[:], wbr.ap()[b].rearrange("(kt p) n -> p kt n", p=128)[:, :, nt * 128:(nt + 1) * 128], w=[wt])
            wts.append(wt)
        for (c0, cn) in BLKS:
            pss = []
            for b in range(3):
                ps = nps()
                for kt in range(8):
                    kb.op("pe", lambda e, kt=kt, b=b: e.matmul(ps[:, :cn], lhsT=wts[b][:, kt, :], rhs=ysrc[b][:, kt, c0:c0 + cn], start=(kt == 0), stop=(kt == 7)),
                          r=[wts[b], ysrc[b]], w=[ps])
                pss.append(ps)
            acc = wk("acc")
            for b in range(3):
                gt = wk("a"); sg = wk("b"); tm = wk("y")
                kb.dma("sp" if b != 1 else "act", gt[:, :cn], zg.ap()[b * 2048 + nt * 128:b * 2048 + (nt + 1) * 128, c0:c0 + cn], w=[gt])
                kb.op("act", lambda e: e.activation(out=sg[:, :cn], in_=gt[:, :cn], func=AF.Sigmoid), r=[gt], w=[sg])
                if b == 0:
                    kb.op("dve", lambda e: e.tensor_tensor(out=acc[:, :cn], in0=pss[b][:, :cn], in1=sg[:, :cn], op=ALU.mult), r=[pss[b], sg], w=[acc])
                else:
                    kb.op("dve", lambda e: e.tensor_tensor(out=tm[:, :cn], in0=pss[b][:, :cn], in1=sg[:, :cn], op=ALU.mult), r=[pss[b], sg], w=[tm])
                    if b == 1:
                        kb.op("pool", lambda e: e.tensor_tensor(out=acc[:, :cn], in0=acc[:, :cn], in1=tm[:, :cn], op=ALU.add), r=[acc, tm], w=[acc])
                    else:
                        kb.op("pool", lambda e: e.tensor_tensor(out=mb[:, nt, c0:c0 + cn], in0=acc[:, :cn], in1=tm[:, :cn], op=ALU.add), r=[acc, tm], w=[mb])

    wos = [kb.sb([128, KT, 128], BF16, f"wo{i}") for i in range(2)]
    xbs = [kb.sb([128, TOK], F32, f"xb{i}") for i in range(2)]
    wov = wout.ap().rearrange("(kt p) n -> p kt n", p=128)
    for nt in range(KT):
        wo = wos[nt % 2]
        xb = xbs[nt % 2]
        kb.dma("pool", wo[:], wov[:, :, nt * 128:(nt + 1) * 128], w=[wo])
        kb.dma("sp", xb[:], xT.ap()[nt * 128:(nt + 1) * 128, :], w=[xb])
        for bi, (c0, cn) in enumerate(BLKS):
            ps = nps()
            for kt in range(KT):
                kb.op("pe", lambda e, kt=kt: e.matmul(ps[:, :cn], lhsT=wo[:, kt, :], rhs=mb[:, kt, c0:c0 + cn], start=(kt == 0), stop=(kt == KT - 1)),
                      r=[wo, mb], w=[ps])
            mm = m2c if bi == 2 else m2l
            kb.op("dve", lambda e: e.scalar_tensor_tensor(out=xb[:, c0:c0 + cn], in0=ps[:, :cn], scalar=mm[:, nt:nt + 1], in1=xb[:, c0:c0 + cn],
                                                          op0=ALU.mult, op1=ALU.add), r=[ps, mm, xb], w=[xb])
        kb.dma("act", xo.ap()[nt * 128:(nt + 1) * 128, :], xb[:], r=[xb], w=[xo])
    kb.finish()
    return kb


def build_k3b():
    kb = KB()
    xT = kb.dram("xT", [D, TOK], F32, kind="ExternalInput")
    vecs = kb.dram("vecs", [5, 128, KT], F32, kind="ExternalInput")
    rt = kb.dram("rt", [D, 16], F32, kind="ExternalInput")
    h2 = kb.dram("h2", [D, TOK], F32, kind="ExternalOutput")
    aff = kb.dram("aff", [16, TOK], F32, kind="ExternalOutput")
    xs = kb.sb([128, KT, TOK], F32, "xs")
    xv = xT.ap().rearrange("(kt p) t -> p kt t", p=128)
    for kt in range(KT):
        kb.dma("sp" if kt % 2 == 0 else "act", xs[:, kt, :], xv[:, kt, :], w=[xs])
    vt = []
    for i in range(5):
        t = kb.sb([128, KT], F32, f"vec{i}")
        kb.dma("sp", t[:], vecs.ap()[i], w=[t])
        vt.append(t)
    ones = kb.sb([128, 128], F32, "ones")
    kb.op("dve", lambda e: e.memset(ones[:], 1.0), w=[ones])
    hT = kb.sb([128, KT, TOK], F32, "hT")
    emit_norm_mod(kb, xs, hT, vt[0], vt[1], vt[2], vt[3], vt[4], ones)
    h2v = h2.ap().rearrange("(kt p) t -> p kt t", p=128)
    for kt in range(KT):
        kb.dma("sp" if kt % 2 == 0 else "act", h2v[:, kt, :], hT[:, kt, :], r=[hT], w=[h2])
    rs = kb.sb([128, KT, 16], F32, "rs")
    kb.dma("sp", rs[:], rt.ap().rearrange("(kt p) e -> p kt e", p=128), w=[rs])
    pl = [kb.ps([16, 512], F32, f"pl{i}") for i in range(2)]
    pz = [kb.ps([16, 512], F32, f"pz{i}") for i in range(2)]
    ex = kb.sb([16, TOK], F32, "ex")
    rz = kb.sb([16, TOK], F32, "rz")
    af = kb.sb([16, TOK], F32, "af")
    for bi, (c0, cn) in enumerate(BLKS):
        p = pl[bi % 2]
        for kt in range(KT):
            kb.op("pe", lambda e, kt=kt: e.matmul(p[:, :cn], lhsT=rs[:, kt, :], rhs=hT[:, kt, c0:c0 + cn], start=(kt == 0), stop=(kt == KT - 1)),
                  r=[rs, hT], w=[p])
        kb.op("act", lambda e: e.activation(out=ex[:, c0:c0 + cn], in_=p[:, :cn], func=AF.Exp), r=[p], w=[ex])
        z = pz[bi % 2]
        kb.op("pe", lambda e: e.matmul(z[:, :cn], lhsT=ones[0:16, 0:16], rhs=ex[:, c0:c0 + cn], start=True, stop=True), r=[ones, ex], w=[z])
        kb.op("dve", lambda e: e.reciprocal(out=rz[:, c0:c0 + cn], in_=z[:, :cn]), r=[z], w=[rz])
        kb.op("dve", lambda e: e.tensor_tensor(out=af[:, c0:c0 + cn], in0=ex[:, c0:c0 + cn], in1=rz[:, c0:c0 + cn], op=ALU.mult), r=[ex, rz], w=[af])
    kb.dma("sp", aff.ap(), af[:], r=[af], w=[aff])
    kb.finish()
    return kb


D = 2048
NIT = 34
BIG = 1.0e6
SLOTS = 1056
SBLK = [(0, 512), (512, 512), (1024, 32)]


def emit_route(kb, A, ntt, cap, nst, tri, ones, iota_s, tokid, tagp, P):
    def sm(shape, dt=F32, nm=""):
        return kb.sb(shape, dt, tagp + nm)
    lo = sm([128, 1], nm="lo"); hi = sm([128, 1], nm="hi"); mid = sm([128, 1], nm="mid")
    kb.op("dve", lambda e: e.memset(lo[:], 0.0), w=[lo])
    kb.op("dve", lambda e: e.memset(hi[:], 1.0), w=[hi])
    kb.op("dve", lambda e: e.memset(mid[:], 0.5), w=[mid])
    junk = sm([128, ntt], nm="junk"); cnt = sm([128, 1], nm="cnt"); ge = sm([128, 1], nm="ge")
    d1 = sm([128, 1], nm="d1"); d2 = sm([128, 1], nm="d2"); sm_ = sm([128, 1], nm="sm")
    pc = P[0]
    for it in range(NIT):
        kb.op("dve", lambda e: e.tensor_scalar(out=junk[:], in0=A[:], scalar1=mid[:, 0:1], scalar2=0.0, op0=ALU.is_gt, op1=ALU.add, accum_out=cnt[:, 0:1]),
              r=[A, mid], w=[junk, cnt])
        kb.op("pe", lambda e: e.matmul(pc[:, 0:1], lhsT=ones[:], rhs=cnt[:, 0:1], start=True, stop=True), r=[ones, cnt], w=[pc])
        kb.op("dve", lambda e: e.tensor_scalar(out=ge[:], in0=pc[:, 0:1], scalar1=float(cap) - 0.5, scalar2=None, op0=ALU.is_gt), r=[pc], w=[ge])
        kb.op("dve", lambda e: e.tensor_tensor(out=d1[:], in0=mid[:], in1=lo[:], op=ALU.subtract), r=[mid, lo], w=[d1])
        kb.op("dve", lambda e: e.tensor_tensor(out=d2[:], in0=hi[:], in1=mid[:], op=ALU.subtract), r=[hi, mid], w=[d2])
        kb.op("dve", lambda e: e.scalar_tensor_tensor(out=lo[:], in0=d1[:], scalar=ge[:, 0:1], in1=lo[:], op0=ALU.mult, op1=ALU.add), r=[d1, ge, lo], w=[lo])
        kb.op("dve", lambda e: e.scalar_tensor_tensor(out=hi[:], in0=d2[:], scalar=ge[:, 0:1], in1=mid[:], op0=ALU.mult, op1=ALU.add), r=[d2, ge, mid], w=[hi])
        kb.op("dve", lambda e: e.tensor_tensor(out=sm_[:], in0=lo[:], in1=hi[:], op=ALU.add), r=[lo, hi], w=[sm_])
        kb.op("dve", lambda e: e.tensor_scalar(out=mid[:], in0=sm_[:], scalar1=0.5, scalar2=None, op0=ALU.mult), r=[sm_], w=[mid])
    mask = sm([128, ntt], nm="mask")
    kb.op("dve", lambda e: e.tensor_scalar(out=mask[:], in0=A[:], scalar1=lo[:, 0:1], scalar2=None, op0=ALU.is_gt), r=[A, lo], w=[mask])
    pp = P[1]
    kb.op("pe", lambda e: e.matmul(pp[:, 0:ntt], lhsT=tri[:], rhs=mask[:], start=True, stop=True), r=[tri, mask], w=[pp])
    kb.op("pe", lambda e: e.matmul(pp[:, 128:128 + ntt], lhsT=ones[:], rhs=mask[:], start=True, stop=True), r=[ones, mask], w=[pp])
    tot = sm([128, ntt], nm="tot"); cum = sm([128, ntt], nm="cum"); pos = sm([128, ntt], nm="pos")
    onesr = sm([128, ntt], nm="onesr")
    kb.op("dve", lambda e: e.memset(onesr[:], 1.0), w=[onesr])
    kb.op("dve", lambda e: e.tensor_copy(out=tot[:], in_=pp[:, 128:128 + ntt]), r=[pp], w=[tot])
    kb.op("dve", lambda e: e.tensor_tensor_scan(out=cum[:], data0=onesr[:], data1=tot[:], initial=0.0, op0=ALU.mult, op1=ALU.add), r=[onesr, tot], w=[cum])
    kb.op("dve", lambda e: e.tensor_tensor(out=cum[:], in0=cum[:], in1=tot[:], op=ALU.subtract), r=[cum, tot], w=[cum])
    kb.op("dve", lambda e: e.tensor_tensor(out=pos[:], in0=pp[:, 0:ntt], in1=cum[:], op=ALU.add), r=[pp, cum], w=[pos])
    pen = sm([128, ntt], nm="pen"); posm = sm([128, ntt], nm="posm")
    kb.op("dve", lambda e: e.tensor_scalar(out=pen[:], in0=mask[:], scalar1=-BIG, scalar2=BIG, op0=ALU.mult, op1=ALU.add), r=[mask], w=[pen])
    kb.op("dve", lambda e: e.tensor_tensor(out=posm[:], in0=pos[:], in1=pen[:], op=ALU.add), r=[pos, pen], w=[posm])
    posc_ = sm([128, ntt], nm="poscl")
    kb.op("dve", lambda e: e.tensor_scalar(out=posc_[:], in0=posm[:], scalar1=float(cap), scalar2=None, op0=ALU.min), r=[posm], w=[posc_])
    posi = sm([128, ntt], I32, nm="posi")
    kb.op("dve", lambda e: e.tensor_copy(out=posi[:], in_=posc_[:]), r=[posc_], w=[posi])
    ns = nst * 128
    pidx = P[2]
    pgr = [P[3 + i] for i in range((ns + 511) // 512)]
    ohs = [sm([128, ns], nm=f"oh{i}") for i in range(2)]
    arep = [sm([128, 128], nm=f"arep{i}") for i in range(2)]
    for tt in range(ntt):
        oh = ohs[tt % 2]; ar = arep[tt % 2]
        kb.op("dve", lambda e: e.tensor_scalar(out=oh[:], in0=iota_s[:, 0:ns], scalar1=posm[:, tt:tt + 1], scalar2=None, op0=ALU.is_equal),
              r=[iota_s, posm], w=[oh])
        kb.op("pool", lambda e: e.tensor_scalar(out=ar[:], in0=ones[:], scalar1=A[:, tt:tt + 1], scalar2=None, op0=ALU.mult), r=[ones, A], w=[ar])
        for st in range(nst):
            kb.op("pe", lambda e, st=st: e.matmul(pidx[:, st:st + 1], lhsT=oh[:, st * 128:(st + 1) * 128], rhs=tokid[:, tt:tt + 1],
                                                  start=(tt == 0 and st == 0), stop=(tt == ntt - 1 and st == nst - 1), skip_group_check=True), r=[oh, tokid], w=[pidx])
        for gi, pg in enumerate(pgr):
            n0 = gi * 512
            nn = min(512, ns - n0)
            kb.op("pe", lambda e, pg=pg: e.matmul(pg[:, 0:nn], lhsT=ar[:], rhs=oh[:, n0:n0 + nn], start=(tt == 0), stop=(tt == ntt - 1)),
                  r=[ar, oh], w=[pg])
    idxf = sm([128, nst], nm="idxf")
    idxi = sm([128, nst], I32, nm="idxi")
    kb.op("dve", lambda e: e.tensor_copy(out=idxf[:], in_=pidx[:, 0:nst]), r=[pidx], w=[idxf])
    kb.op("dve", lambda e: e.tensor_copy(out=idxi[:], in_=idxf[:]), r=[idxf], w=[idxi])
    grow = sm([128, ns], nm="grow")
    for gi, pg in enumerate(pgr):
        n0 = gi * 512
        nn = min(512, ns - n0)
        kb.op("act", lambda e, pg=pg: e.copy(out=grow[:, n0:n0 + nn], in_=pg[:, 0:nn]), r=[pg], w=[grow])
    return posi, idxi, grow


def build_k4(with_ffn=True):
    kb = KB()
    nc = kb.nc
    affl = kb.dram("affl", [2, 128, 64], F32, kind="ExternalInput")
    affc = kb.dram("affc", [2, 128, 2], F32, kind="ExternalInput")
    h2 = kb.dram("h2", [8192, D], F32, kind="ExternalInput")
    h2c = kb.dram("h2c", [256, D], F32, kind="ExternalInput")
    wg = kb.dram("wg", [2, D, D], F32, kind="ExternalInput")
    wu = kb.dram("wu", [2, D, D], F32, kind="ExternalInput")
    wd = kb.dram("wd", [2, D, D], F32, kind="ExternalInput")
    cst = kb.dram("cst", [128, 128 + 1024 + 64 + 128], F32, kind="ExternalInput")
    yeT = kb.dram("yeT", [2, D, SLOTS], F32, kind="ExternalOutput")
    posl = kb.dram("posl", [2, 128, 64], I32, kind="ExternalOutput")
    posc = kb.dram("posc", [2, 128, 2], I32, kind="ExternalOutput")
    dbg = kb.dram("dbg", [2, 128, 9], I32, kind="ExternalOutput")

    cs_ = kb.sb([128, 1344], F32, "cs_")
    kb.dma("sp", cs_[:], cst.ap(), w=[cs_])
    tri = kb.sb([128, 128], F32, "tri"); iota_s = kb.sb([128, 1024], F32, "iota_s"); tokid = kb.sb([128, 64], F32, "tokid")
    identb = kb.sb([128, 128], BF16, "identb"); ones = kb.sb([128, 128], F32, "ones")
    kb.op("dve", lambda e: e.tensor_copy(out=tri[:], in_=cs_[:, 0:128]), r=[cs_], w=[tri])
    kb.op("dve", lambda e: e.tensor_copy(out=iota_s[:], in_=cs_[:, 128:1152]), r=[cs_], w=[iota_s])
    kb.op("dve", lambda e: e.tensor_copy(out=tokid[:], in_=cs_[:, 1152:1216]), r=[cs_], w=[tokid])
    kb.op("dve", lambda e: e.tensor_copy(out=identb[:], in_=cs_[:, 1216:1344]), r=[cs_], w=[identb])
    kb.op("dve", lambda e: e.memset(ones[:], 1.0), w=[ones])

    P = [kb.ps([128, 512], F32, f"P{i}") for i in range(6)]
    xsT = kb.sb([128, 16, SLOTS], BF16, "xsT")
    actT = kb.sb([128, 16, SLOTS], BF16, "actT")
    xg = [kb.sb([128, D], F32, f"xg{i}") for i in range(2)]
    xgb = [kb.sb([128, D], BF16, f"xgb{i}") for i in range(2)]
    ptr = [kb.ps([128, 1024], BF16, f"ptr{i}") for i in range(2)]
    wgs = [kb.sb([128, 16, 128], BF16, f"wgs{i}") for i in range(2)]
    wus = [kb.sb([128, 16, 128], BF16, f"wus{i}") for i in range(2)]
    sg = [kb.sb([128, 512], F32, f"sg{i}") for i in range(2)]
    tq = [kb.sb([128, 512], F32, f"tq{i}") for i in range(2)]
    ob = [kb.sb([128, SLOTS], F32, f"ob{i}") for i in range(2)]
    for e2 in range(2):
        Al = kb.sb([128, 64], F32, f"Al{e2}")
        Ac = kb.sb([128, 2], F32, f"Ac{e2}")
        kb.dma("sp", Al[:], affl.ap()[e2], w=[Al])
        kb.dma("sp", Ac[:], affc.ap()[e2], w=[Ac])
        posi_l, idx_l, grow_l = emit_route(kb, Al, 64, 1024, 8, tri, ones, iota_s, tokid, f"rl{e2}_", P)
        posi_c, idx_c, grow_c = emit_route(kb, Ac, 2, 32, 1, tri, ones, iota_s, tokid, f"rc{e2}_", P)
        kb.dma("sp", posl.ap()[e2], posi_l[:], r=[posi_l], w=[posl])
        kb.dma("sp", posc.ap()[e2], posi_c[:], r=[posi_c], w=[posc])
        kb.dma("sp", dbg.ap()[e2, :, 0:8], idx_l[:], r=[idx_l], w=[dbg], allow_slow_non_contiguous=True)
        kb.dma("sp", dbg.ap()[e2, :, 8:9], idx_c[:], r=[idx_c], w=[dbg], allow_slow_non_contiguous=True)
        if not with_ffn:
            continue
        for st in range(9):
            b = st % 2
            rows = 128 if st < 8 else 32
            idx_ap = idx_l[:, st:st + 1] if st < 8 else idx_c[0:32, 0:1]
            src = h2.ap() if st < 8 else h2c.ap()
            need = kb._need([idx_l if st < 8 else idx_c], [xg[b]])
            kb._wait("pool", need)
            ins = nc.gpsimd.indirect_dma_start(out=xg[b][0:rows, :], out_offset=None, in_=src,
                                               in_offset=bass.IndirectOffsetOnAxis(ap=idx_ap, axis=0))
            t = xg[b]
            if t.dsem is None:
                t.dsem = kb.st.enter_context(nc.semaphore("d_" + t.name))
            t.dcount += 16
            ins.then_inc(t.dsem, 16)
            key = "d_" + t.name
            (idx_l if st < 8 else idx_c).r[key] = (t.dsem, t.dcount)
            t.w[key] = (t.dsem, t.dcount)
            kb.op("act", lambda e: e.copy(out=xgb[b][0:rows, :], in_=xg[b][0:rows, :]), r=[xg[b]], w=[xgb[b]])
            for half in range(2):
                pt = ptr[half]
                for k8 in range(8):
                    kt = half * 8 + k8
                    kb.op("pe", lambda e, kt=kt, k8=k8: e.transpose(pt[:, k8 * 128:k8 * 128 + rows], xgb[b][0:rows, kt * 128:(kt + 1) * 128], identb[0:rows, 0:rows]),
                          r=[xgb[b], identb], w=[pt])
                kb.op("dve", lambda e: e.tensor_copy(out=xsT[:, half * 8:(half + 1) * 8, st * 128:st * 128 + rows],
                                                     in_=pt[:].rearrange("p (k c) -> p k c", k=8)[:, :, 0:rows]), r=[pt], w=[xsT])
        PA = [P[0], P[1]]
        PU = [P[2], P[3]]
        wgv = wg.ap()[e2].rearrange("(kt p) n -> p kt n", p=128)
        wuv = wu.ap()[e2].rearrange("(kt p) n -> p kt n", p=128)
        wdv = wd.ap()[e2].rearrange("(kt p) n -> p kt n", p=128)
        n = 0
        for ft in range(16):
            wgt = wgs[ft % 2]; wut = wus[ft % 2]
            kb.dma("pool", wgt[:], wgv[:, :, ft * 128:(ft + 1) * 128], w=[wgt])
            kb.dma("pool", wut[:], wuv[:, :, ft * 128:(ft + 1) * 128], w=[wut])
            for (c0, cn) in SBLK:
                pA = PA[n % 2]; pU = PU[n % 2]; s_ = sg[n % 2]; t_ = tq[n % 2]
                n += 1
                for kt in range(16):
                    kb.op("pe", lambda e, kt=kt: e.matmul(pA[:, :cn], lhsT=wgt[:, kt, :], rhs=xsT[:, kt, c0:c0 + cn], start=(kt == 0), stop=(kt == 15)),
                          r=[wgt, xsT], w=[pA])
                for kt in range(16):
                    kb.op("pe", lambda e, kt=kt: e.matmul(pU[:, :cn], lhsT=wut[:, kt, :], rhs=xsT[:, kt, c0:c0 + cn], start=(kt == 0), stop=(kt == 15)),
                          r=[wut, xsT], w=[pU])
                kb.op("act", lambda e: e.activation(out=s_[:, :cn], in_=pA[:, :cn], func=AF.Sigmoid), r=[pA], w=[s_])
                kb.op("dve", lambda e: e.tensor_tensor(out=t_[:, :cn], in0=pA[:, :cn], in1=s_[:, :cn], op=ALU.mult), r=[pA, s_], w=[t_])
                kb.op("dve", lambda e: e.tensor_tensor(out=actT[:, ft, c0:c0 + cn], in0=pU[:, :cn], in1=t_[:, :cn], op=ALU.mult), r=[pU, t_], w=[actT])
        for dt_ in range(16):
            wdt = wgs[dt_ % 2]
            kb.dma("pool", wdt[:], wdv[:, :, dt_ * 128:(dt_ + 1) * 128], w=[wdt])
            o = ob[dt_ % 2]
            for (c0, cn) in SBLK:
                pA = PA[n % 2]
                n += 1
                for kt in range(16):
                    kb.op("pe", lambda e, kt=kt: e.matmul(pA[:, :cn], lhsT=wdt[:, kt, :], rhs=actT[:, kt, c0:c0 + cn], start=(kt == 0), stop=(kt == 15)),
                          r=[wdt, actT], w=[pA])
                if c0 < 1024:
                    kb.op("dve", lambda e: e.tensor_tensor(out=o[:, c0:c0 + cn], in0=pA[:, :cn], in1=grow_l[:, c0:c0 + cn], op=ALU.mult), r=[pA, grow_l], w=[o])
                else:
                    kb.op("dve", lambda e: e.tensor_tensor(out=o[:, c0:c0 + cn], in0=pA[:, :cn], in1=grow_c[:, 0:cn], op=ALU.mult), r=[pA, grow_c], w=[o])
            kb.dma("sp", yeT.ap()[e2, dt_ * 128:(dt_ + 1) * 128, :], o[:], r=[o], w=[yeT])
    kb.finish()
    return kb


D = 2048


def build_k5(final):
    kb = KB()
    nc = kb.nc
    x = kb.dram("x", [1056, D], F32, kind="ExternalInput")
    yl = [kb.dram(f"yl{e}", [1025, D], F32, kind="ExternalInput") for e in range(16)]
    yc = [kb.dram(f"yc{e}", [33, D], F32, kind="ExternalInput") for e in range(16)]
    pos = kb.dram("pos", [128, 9, 16], I32, kind="ExternalInput")
    rep = kb.dram("rep", [3, 128, D], F32, kind="ExternalInput")
    xo = kb.dram("xo", [1056, D], F32, kind="ExternalOutput")
    ps_ = kb.sb([128, 9, 16], I32, "pos_sb")
    kb.dma("sp", ps_[:], pos.ap(), w=[ps_])
    reps = []
    for i in range(3):
        t = kb.sb([128, D], F32, f"rep{i}")
        kb.dma("sp" if i % 2 == 0 else "act", t[:], rep.ap()[i], w=[t])
        reps.append(t)
    G = [kb.sb([128, D], F32, f"g{i}") for i in range(4)]
    ACC = [kb.sb([128, D], F32, f"acc{i}") for i in range(2)]
    XT = [kb.sb([128, D], F32, f"xt{i}") for i in range(2)]
    ss = [kb.sb([128, 1], F32, f"ss{i}") for i in range(2)]
    sd = [kb.sb([128, 1], F32, f"sd{i}") for i in range(2)]
    junk = kb.sb([128, D], F32, "junk")
    gi = 0
    for tl in range(9):
        rows = 128 if tl < 8 else 32
        r0 = tl * 128
        acc = ACC[tl % 2]; xt = XT[tl % 2]
        kb.dma("sp", xt[0:rows, :], x.ap()[r0:r0 + rows, :], w=[xt])
        for e in range(16):
            g = G[gi % 4] if e > 0 else acc
            gi += 1
            src = yl[e] if tl < 8 else yc[e]
            need = kb._need([ps_], [g])
            kb._wait("pool", need)
            ins = nc.gpsimd.indirect_dma_start(out=g[0:rows, :], out_offset=None, in_=src.ap(),
                                               in_offset=bass.IndirectOffsetOnAxis(ap=ps_[0:rows, tl, e:e + 1], axis=0))
            if g.dsem is None:
                g.dsem = kb.st.enter_context(nc.semaphore("d_" + g.name))
            g.dcount += 16
            ins.then_inc(g.dsem, 16)
            key = "d_" + g.name
            ps_.r[key] = (g.dsem, g.dcount)
            g.w[key] = (g.dsem, g.dcount)
            if e > 0:
                eng = "dve" if e % 2 == 0 else "pool"
                kb.op(eng, lambda en: en.tensor_tensor(out=acc[0:rows, :], in0=acc[0:rows, :], in1=g[0:rows, :], op=ALU.add), r=[acc, g], w=[acc])
        m5 = reps[0] if tl < 8 else reps[1]
        kb.op("dve", lambda en: en.tensor_tensor(out=acc[0:rows, :], in0=acc[0:rows, :], in1=m5[0:rows, :], op=ALU.mult), r=[acc, m5], w=[acc])
        kb.op("dve", lambda en: en.tensor_tensor(out=xt[0:rows, :], in0=xt[0:rows, :], in1=acc[0:rows, :], op=ALU.add), r=[xt, acc], w=[xt])
        if final and tl < 8:
            s_ = ss[tl % 2]; d_ = sd[tl % 2]
            kb.op("act", lambda en: en.activation(out=junk[0:rows, :], in_=xt[0:rows, :], func=AF.Square), r=[xt], w=[junk])
            kb.op("dve", lambda en: en.tensor_scalar(out=junk[0:rows, :], in0=junk[0:rows, :], scalar1=1.0, scalar2=0.0, op0=ALU.mult, op1=ALU.add,
                                                     accum_out=s_[0:rows, 0:1]), r=[junk], w=[junk, s_])
            kb.op("act", lambda en: en.activation(out=d_[0:rows, :], in_=s_[0:rows, :], func=AF.Sqrt, bias=1e-6, scale=1.0 / D), r=[s_], w=[d_])
            kb.op("dve", lambda en: en.reciprocal(out=s_[0:rows, :], in_=d_[0:rows, :]), r=[d_], w=[s_])
            kb.op("dve", lambda en: en.scalar_tensor_tensor(out=xt[0:rows, :], in0=xt[0:rows, :], scalar=s_[0:rows, 0:1], in1=reps[2][0:rows, :],
                                                            op0=ALU.mult, op1=ALU.mult), r=[xt, s_, reps[2]], w=[xt])
        kb.dma("act", xo.ap()[r0:r0 + rows, :], xt[0:rows, :], r=[xt], w=[xo])
    kb.finish()
    return kb

import numpy as np
QA0 = 4096
def rope_tables():
    L = 8192
    row = np.repeat(np.arange(L // 64), 64).astype(np.float32)
    col = np.tile(np.arange(64), L // 64).astype(np.float32)
    inv = (np.float32(10000.0) ** (-np.arange(16, dtype=np.float32) / np.float32(16))).astype(np.float32)
    ang = np.concatenate([row[:, None] * inv, col[:, None] * inv], -1).astype(np.float32)
    c = np.cos(ang).astype(np.float32); s = np.sin(ang).astype(np.float32)
    cosT = np.concatenate([c, c], 1).T
    sinT = np.concatenate([-s, s], 1).T
    cs = np.stack([np.concatenate([cosT, cosT], 0), np.concatenate([sinT, sinT], 0)])
    return np.ascontiguousarray(cs.astype(np.float32))
def attn_masks():
    j = np.arange(128)[:, None]; i = np.arange(128)[None, :]
    return np.ascontiguousarray(np.stack([(j >= i), (j <= i)]).astype(np.float32))
def perm64(w, nh):
    sh = w.shape[:-1]
    w = w.reshape(sh + (nh, 2, 32))
    return w[..., ::-1, :].reshape(sh + (nh * 64,))
def attn_inputs(ci, q_all, k_all, v_all, qc_all, kc_all, vc_all, sink_l, cs, masks):
    kvh = ci // 2
    q = q_all[:, ci * 128:(ci + 1) * 128]
    qp = perm64(q, 2)
    k = k_all[:, kvh * 64:(kvh + 1) * 64]; kp = perm64(k, 1)
    kd = np.concatenate([k, k], 1); kpd = np.concatenate([kp, kp], 1)
    qT = np.ascontiguousarray(np.stack([q.T, qp.T])); kT = np.ascontiguousarray(np.stack([kd.T, kpd.T]))
    kc = kc_all[:, kvh * 64:(kvh + 1) * 64]
    cx = np.ascontiguousarray(np.stack([qc_all[:, ci * 128:(ci + 1) * 128].T, np.concatenate([kc, kc], 1).T]))
    v = np.concatenate([v_all[:, kvh * 64:(kvh + 1) * 64], vc_all[:, kvh * 64:(kvh + 1) * 64]], 0)
    vtok = np.ascontiguousarray(v.reshape(66, 128, 64).transpose(1, 0, 2))
    sk = np.ascontiguousarray(np.broadcast_to(sink_l[2 * ci:2 * ci + 2][None, :], (64, 2)).astype(np.float32))
    return {"qT": qT, "kT": kT, "cs": cs, "cx": cx, "vtok": vtok, "masks": masks, "sink": sk}
GQ0 = 1024; GK0 = 1536; GV0 = 2048; GR0 = 3072
def dir_order(a, dd):
    if dd == 0: return a
    return np.concatenate([a[:256][::-1], a[256:][::-1]], 0)
def gla_consts():
    c = np.zeros((128, 704), np.float32)
    m = np.ones(512, np.float32); m[::64] = 0
    c[:, :512] = m
    j = np.arange(64)[:, None]; i = np.arange(64)[None, :]
    c[:64, 512:576] = (j <= i)
    c[:, 576:704] = np.eye(128)
    return c
def gla_inputs(ci, l, qg, kg, vg, hw1_all, d, consts):
    hd = ci // 2; dd = ci % 2
    q = dir_order(qg[:, hd * 128:(hd + 1) * 128], dd); k = dir_order(kg[:, hd * 128:(hd + 1) * 128], dd)
    v = dir_order(vg[:, hd * 256:(hd + 1) * 256], dd)
    hw = dir_order(hw1_all[:, dd * 16:(dd + 1) * 16], dd)
    return {"qk": np.ascontiguousarray(np.stack([q.T, k.T]).astype(np.float32)), "hw1": np.ascontiguousarray(hw.T.astype(np.float32)),
            "w2b": np.ascontiguousarray(d["gla_w2"][l, dd][:, hd * 128:(hd + 1) * 128]),
            "nb": np.ascontiguousarray(d["gla_b"][l, dd, hd * 128:(hd + 1) * 128].reshape(128, 1)),
            "vtok": np.ascontiguousarray(v.reshape(132, 64, 256).transpose(1, 0, 2).astype(np.float32)), "cst": consts}
def moe_consts():
    c = np.zeros((128, 1344), np.float32)
    pj = np.arange(128)
    c[:, 0:128] = (pj[:, None] < pj[None, :])
    c[:, 128:1152] = np.arange(1024)[None, :]
    c[:, 1152:1216] = (np.arange(64)[None, :] * 128 + pj[:, None])
    c[:, 1216:1344] = np.eye(128)
    return c
def moe_inputs(ci, l, aff_lat, aff_ctx, h2_tok, d, consts):
    es = [2 * ci, 2 * ci + 1]
    affl = np.stack([aff_lat[:, e].reshape(64, 128).T for e in es]).astype(np.float32)
    affc = np.stack([aff_ctx[:, e].reshape(2, 128).T for e in es]).astype(np.float32)
    return {"affl": np.ascontiguousarray(affl), "affc": np.ascontiguousarray(affc), "h2": np.ascontiguousarray(h2_tok[:8192]), "h2c": np.ascontiguousarray(h2_tok[8192:]),
            "wg": np.ascontiguousarray(d["moe_w_gate"][l, es[0]:es[0] + 2]), "wu": np.ascontiguousarray(d["moe_w_up"][l, es[0]:es[0] + 2]),
            "wd": np.ascontiguousarray(d["moe_w_down"][l, es[0]:es[0] + 2]), "cst": consts}

QA0_ = 4096
GQ0_, GK0_, GV0_, GR0_ = 1024, 1536, 2048, 3072


def _pvec(v):
    return np.ascontiguousarray(np.asarray(v, np.float32).reshape(-1, 128).T)


def _build_wext(w_in, w1):
    qa = w_in[:, QA0_:QA0_ + 1024]
    ka = w_in[:, QA0_ + 1024:QA0_ + 1280]
    W = np.zeros((2048, NW), np.float32)
    W[:, :11776] = w_in
    W[:, 11776:12800] = perm64(qa, 16)
    W[:, 12800:13056] = perm64(ka, 4)
    W[:, 13056:13072] = w1[0]
    W[:, 13072:13088] = w1[1]
    return W


def _rev_cols(a):
    return np.concatenate([a[:, :256][:, ::-1], a[:, 256:][:, ::-1]], 1)


def _s5_inputs(l, gi, uall_T, p):
    uT = np.ascontiguousarray(np.stack([uall_T, _rev_cols(uall_T)]).astype(np.float32))
    Bblk = np.zeros((2, 2, 4, 128, 128), np.float32)
    Cblk = np.zeros((2, 2, 4, 128, 128), np.float32)
    lam = np.zeros((3, 128, 8), np.float32)
    for dd in range(2):
        for q in range(4):
            for g2 in range(2):
                gl = 2 * q + g2
                g = 8 * gi + gl
                for pi, (bn, cn) in enumerate((("s5_b_re", "s5_c_re"), ("s5_b_im", "s5_c_im"))):
                    Bblk[dd, pi, q, gl * 16:(gl + 1) * 16, g2 * 64:(g2 + 1) * 64] = p[bn][l, dd, g].T
                    Cblk[dd, pi, q, g2 * 64:(g2 + 1) * 64, gl * 16:(gl + 1) * 16] = p[cn][l, dd, g].T
                lam[0, g2 * 64:(g2 + 1) * 64, dd * 4 + q] = p["s5_lam_re"][l, dd, g]
                lam[1, g2 * 64:(g2 + 1) * 64, dd * 4 + q] = p["s5_lam_im"][l, dd, g]
                lam[2, g2 * 64:(g2 + 1) * 64, dd * 4 + q] = p["s5_log_dt"][l, dd, g]
    dvec = np.ascontiguousarray(p["s5_d"][l, gi * 128:(gi + 1) * 128].reshape(128, 1))
    return {"uT": uT, "Bblk": Bblk, "Cblk": Cblk, "lam": lam, "dvec": dvec}


def _run(kb, ims):
    res = run_bass_kernel_spmd(kb.nc, ims, core_ids=list(range(NCORES)))
    return res.results


def kernel(x, c, ctx, c_ctx, ada_w, ada_b, norm1_g, norm2_g, w_in, s5_lam_re, s5_lam_im, s5_log_dt,
           s5_b_re, s5_b_im, s5_c_re, s5_c_im, s5_d, s5_w_glu, gla_w1, gla_w2, gla_b, gla_norm_g,
           attn_sink, w_branch_s5, w_branch_gla, w_branch_attn, w_out, moe_router, moe_w_gate,
           moe_w_up, moe_w_down, final_g):
    p = {k: np.asarray(v, np.float32) for k, v in dict(
        s5_lam_re=s5_lam_re, s5_lam_im=s5_lam_im, s5_log_dt=s5_log_dt, s5_b_re=s5_b_re, s5_b_im=s5_b_im,
        s5_c_re=s5_c_re, s5_c_im=s5_c_im, s5_d=s5_d, gla_w2=gla_w2, gla_b=gla_b, moe_w_gate=moe_w_gate,
        moe_w_up=moe_w_up, moe_w_down=moe_w_down).items()}
    f32 = np.float32
    x_lat = np.asarray(x, f32)[0].copy()
    xc = np.asarray(ctx, f32)[0].copy()
    ada_w = np.asarray(ada_w, f32); ada_b = np.asarray(ada_b, f32)
    cT = np.stack([np.asarray(c, f32)[0], np.asarray(c_ctx, f32)], axis=1)
    cT = np.ascontiguousarray(cT.reshape(16, 128, 2).transpose(1, 0, 2))
    ims = []
    for i in range(NCORES):
        aw = np.ascontiguousarray(ada_w[:, :, i * 1536:(i + 1) * 1536])
        ab = np.ascontiguousarray(ada_b[:, i * 1536:(i + 1) * 1536].reshape(2, 12, 128).transpose(0, 2, 1))
        ims.append({"cT": cT, "adaw": aw, "adab": ab})
    r = _run(build_k0(), ims)
    mod = np.zeros((2, 12288, 2), f32)
    for i in range(NCORES):
        mod[:, i * 1536:(i + 1) * 1536, :] = r[i]["modT"].transpose(0, 2, 1, 3).reshape(2, 1536, 2)
    cs_tab = rope_tables(); amask = attn_masks(); gconst = gla_consts(); mconst = moe_consts()

    def shard_cols(A, ci):
        return np.ascontiguousarray(np.concatenate([A[:, 256 + ci * 1024:256 + (ci + 1) * 1024], A[:, ci * 32:(ci + 1) * 32]], 1))

    for l in range(2):
        m = mod[l].reshape(6, 2048, 2)
        W = _build_wext(np.asarray(w_in[l], f32), np.asarray(gla_w1[l], f32))
        vecs = np.stack([_pvec(norm1_g[l]), _pvec(m[1, :, 0]), _pvec(m[0, :, 0]), _pvec(m[1, :, 1]), _pvec(m[0, :, 1])])
        xTs = [np.ascontiguousarray(np.concatenate([x_lat[i * 1024:(i + 1) * 1024], xc[i * 32:(i + 1) * 32]], 0).T) for i in range(NCORES)]
        r = _run(build_k1(), [{"xT": xTs[i], "W": W, "vecs": vecs} for i in range(NCORES)])
        ZT = np.empty((NW, 8448), f32)
        for i in range(NCORES):
            z = r[i]["zT"]
            ZT[:, 256 + i * 1024:256 + (i + 1) * 1024] = z[:, :1024]
            ZT[:, i * 32:(i + 1) * 32] = z[:, 1024:]
        del r, W
        r = _run(build_k2a(), [_s5_inputs(l, gi, ZT[gi * 128:(gi + 1) * 128], p) for gi in range(NCORES)])
        YS = np.empty((2, 1024, 8448), f32)
        for gi in range(NCORES):
            YS[0, gi * 128:(gi + 1) * 128] = r[gi]["yT"][0]
            YS[1, gi * 128:(gi + 1) * 128] = _rev_cols(r[gi]["yT"][1])
        ims = []
        for ci in range(NCORES):
            hd, dd = ci // 2, ci % 2
            od = (lambda a: a) if dd == 0 else _rev_cols
            q = od(ZT[GQ0_ + hd * 128:GQ0_ + (hd + 1) * 128]); k = od(ZT[GK0_ + hd * 128:GK0_ + (hd + 1) * 128])
            v = od(ZT[GV0_ + hd * 256:GV0_ + (hd + 1) * 256])
            hw = od(ZT[13056 + dd * 16:13056 + (dd + 1) * 16])
            ims.append({"qk": np.ascontiguousarray(np.stack([q, k])), "hw1": np.ascontiguousarray(hw),
                        "w2b": np.ascontiguousarray(p["gla_w2"][l, dd][:, hd * 128:(hd + 1) * 128]),
                        "nb": np.ascontiguousarray(p["gla_b"][l, dd, hd * 128:(hd + 1) * 128].reshape(128, 1)),
                        "vtok": np.ascontiguousarray(v.T.reshape(132, 64, 256).transpose(1, 0, 2)), "cst": gconst})
        r = _run(build_k2b(), ims)
        OG = np.empty((2, 1024, 8448), f32)
        for ci in range(NCORES):
            hd, dd = ci // 2, ci % 2
            o = r[ci]["oT"].reshape(256, 8448)
            OG[dd, hd * 256:(hd + 1) * 256] = o if dd == 0 else _rev_cols(o)
        ims = []
        for ci in range(NCORES):
            kvh = ci // 2
            lat = slice(256, 8448); cx_ = slice(0, 256)
            q = ZT[QA0_ + ci * 128:QA0_ + (ci + 1) * 128]; qp = ZT[11776 + ci * 128:11776 + (ci + 1) * 128]
            k = ZT[QA0_ + 1024 + kvh * 64:QA0_ + 1024 + (kvh + 1) * 64]; kp = ZT[12800 + kvh * 64:12800 + (kvh + 1) * 64]
            v = ZT[QA0_ + 1280 + kvh * 64:QA0_ + 1280 + (kvh + 1) * 64]
            qT = np.ascontiguousarray(np.stack([q[:, lat], qp[:, lat]]))
            kT = np.ascontiguousarray(np.stack([np.concatenate([k[:, lat], k[:, lat]], 0), np.concatenate([kp[:, lat], kp[:, lat]], 0)]))
            cxx = np.ascontiguousarray(np.stack([q[:, cx_], np.concatenate([k[:, cx_], k[:, cx_]], 0)]))
            vt = np.concatenate([v[:, lat], v[:, cx_]], 1).T
            vtok = np.ascontiguousarray(vt.reshape(66, 128, 64).transpose(1, 0, 2))
            sk = np.ascontiguousarray(np.broadcast_to(np.asarray(attn_sink[l], f32)[2 * ci:2 * ci + 2][None, :], (64, 2)))
            ims.append({"qT": qT, "kT": kT, "cs": cs_tab, "cx": cxx, "vtok": vtok, "masks": amask, "sink": sk})
        r = _run(build_k2c(), ims)
        YA = np.empty((1024, 8448), f32)
        for ci in range(NCORES):
            YA[ci * 128:(ci + 1) * 128, 256:] = r[ci]["yT"]
            YA[ci * 128:(ci + 1) * 128, :256] = r[ci]["ycT"]
        gng = np.zeros(2048, f32); gng[:1024] = np.asarray(gla_norm_g[l], f32).reshape(-1)
        vecs = np.stack([_pvec(gng), _pvec(m[2, :, 0]), _pvec(m[2, :, 1])])
        wbr = np.ascontiguousarray(np.stack([np.asarray(w_branch_s5[l], f32), np.asarray(w_branch_gla[l], f32), np.asarray(w_branch_attn[l], f32)]))
        wgl = np.ascontiguousarray(np.asarray(s5_w_glu[l], f32)); wo_ = np.ascontiguousarray(np.asarray(w_out[l], f32))
        ims = []
        for ci in range(NCORES):
            ims.append({"ys": np.stack([shard_cols(YS[0], ci), shard_cols(YS[1], ci)]), "og": np.stack([shard_cols(OG[0], ci), shard_cols(OG[1], ci)]),
                        "ya": shard_cols(YA, ci), "zr": shard_cols(ZT[GR0_:GR0_ + 1024], ci), "zg": shard_cols(ZT[5632:11776], ci),
                        "xT": xTs[ci], "wglu": wgl, "wbr": wbr, "wout": wo_, "vecs": vecs})
        r = _run(build_k3a(), ims)
        xmidT = [r[ci]["xo"] for ci in range(NCORES)]
        del ims, YS, OG, YA, ZT
        vecs = np.stack([_pvec(norm2_g[l]), _pvec(m[4, :, 0]), _pvec(m[3, :, 0]), _pvec(m[4, :, 1]), _pvec(m[3, :, 1])])
        rt = np.ascontiguousarray(np.asarray(moe_router[l], f32))
        r = _run(build_k3b(), [{"xT": xmidT[ci], "vecs": vecs, "rt": rt} for ci in range(NCORES)])
        h2l = np.empty((8192, 2048), f32); h2c = np.empty((256, 2048), f32)
        affl = np.empty((8192, 16), f32); affc = np.empty((256, 16), f32)
        for ci in range(NCORES):
            h = r[ci]["h2"]; a = r[ci]["aff"]
            h2l[ci * 1024:(ci + 1) * 1024] = h[:, :1024].T; h2c[ci * 32:(ci + 1) * 32] = h[:, 1024:].T
            affl[ci * 1024:(ci + 1) * 1024] = a[:, :1024].T; affc[ci * 32:(ci + 1) * 32] = a[:, 1024:].T
        ims = []
        for ci in range(NCORES):
            es = [2 * ci, 2 * ci + 1]
            ims.append({"affl": np.ascontiguousarray(np.stack([affl[:, e].reshape(64, 128).T for e in es])),
                        "affc": np.ascontiguousarray(np.stack([affc[:, e].reshape(2, 128).T for e in es])),
                        "h2": h2l, "h2c": h2c,
                        "wg": np.ascontiguousarray(p["moe_w_gate"][l, es[0]:es[0] + 2]), "wu": np.ascontiguousarray(p["moe_w_up"][l, es[0]:es[0] + 2]),
                        "wd": np.ascontiguousarray(p["moe_w_down"][l, es[0]:es[0] + 2]), "cst": mconst})
        r = _run(build_k4(), ims)
        del ims
        yl = []; yc = []
        posl = np.empty((16, 128, 64), np.int32); posc = np.empty((16, 128, 2), np.int32)
        zrow = np.zeros((1, 2048), f32)
        for ci in range(NCORES):
            for e2 in range(2):
                e = 2 * ci + e2
                yt = r[ci]["yeT"][e2]
                yl.append(np.ascontiguousarray(np.concatenate([yt[:, :1024].T, zrow], 0)))
                yc.append(np.ascontiguousarray(np.concatenate([yt[:, 1024:1056].T, zrow], 0)))
                posl[e] = r[ci]["posl"][e2]; posc[e] = r[ci]["posc"][e2]
        rep = np.ascontiguousarray(np.stack([np.broadcast_to(m[5, :, 0], (128, 2048)), np.broadcast_to(m[5, :, 1], (128, 2048)),
                                             np.broadcast_to(np.asarray(final_g, f32), (128, 2048))]).astype(f32))
        ims = []
        for cj in range(NCORES):
            pos = np.full((128, 9, 16), 32, np.int32)
            pos[:, 0:8, :] = posl[:, :, 8 * cj:8 * cj + 8].transpose(1, 2, 0)
            pos[0:32, 8, :] = posc[:, (cj % 4) * 32:(cj % 4) * 32 + 32, cj // 4].T
            im = {"x": np.ascontiguousarray(xmidT[cj].T), "pos": np.ascontiguousarray(pos), "rep": rep}
            for e in range(16):
                im[f"yl{e}"] = yl[e]; im[f"yc{e}"] = yc[e]
            ims.append(im)
        r = _run(build_k5(l == 1), ims)
        del ims
        for cj in range(NCORES):
            xo = r[cj]["xo"]
            x_lat[cj * 1024:(cj + 1) * 1024] = xo[:1024]
            xc[cj * 32:(cj + 1) * 32] = xo[1024:]
    return x_lat.reshape(1, 8192, 2048).astype(np.float32)
```

```python
import numpy as np
import ml_dtypes
from contextlib import ExitStack
import concourse.bass as bass
import concourse.mybir as mybir
from concourse.bass_utils import run_bass_kernel_spmd

F32 = mybir.dt.float32
BF16 = mybir.dt.bfloat16
I32 = mybir.dt.int32
U32 = mybir.dt.uint32
AF = mybir.ActivationFunctionType
ALU = mybir.AluOpType
AX = mybir.AxisListType
NPBF16 = ml_dtypes.bfloat16
NCORES = 8


class T:
    def __init__(self, name, h):
        self.name = name
        self.h = h
        self.w = {}
        self.r = {}
        self.gen_need = {}
        self.dsem = None
        self.dcount = 0

    def __getitem__(self, idx):
        return self.h[idx]

    def ap(self):
        return self.h.ap() if hasattr(self.h, "ap") else self.h[:]


class KB:
    def __init__(self, same_engine_sync=True):
        self.nc = bass.Bass("TRN2", target_bir_lowering=False)
        nc = self.nc
        self.st = ExitStack()
        self.E = {"pe": nc.tensor, "act": nc.scalar, "dve": nc.vector, "pool": nc.gpsimd, "sp": nc.sync}
        self.sem = {}
        self.cnt = {}
        self.known = {}
        for e in self.E:
            self.sem[e] = self.st.enter_context(nc.semaphore("s_" + e))
            self.cnt[e] = 0
            self.known[e] = {}
        self.qsem = {}
        self.qcnt = {}
        for q in ("sp", "act", "pool"):
            self.qsem[q] = self.st.enter_context(nc.semaphore("q_" + q))
            self.qcnt[q] = 0
        self.ses = same_engine_sync
        self.outs = []
        self.nid = 0

    def sb(self, shape, dt=F32, name=None):
        self.nid += 1
        name = name or f"t{self.nid}"
        h = self.st.enter_context(self.nc.sbuf_tensor(name, list(shape), dt))
        return T(name, h)

    def ps(self, shape, dt=F32, name=None):
        self.nid += 1
        name = name or f"p{self.nid}"
        h = self.st.enter_context(self.nc.psum_tensor(name, list(shape), dt))
        return T(name, h)

    def dram(self, name, shape, dt=F32, kind="Internal"):
        h = self.nc.dram_tensor(name, list(shape), dt, kind=kind)
        t = T(name, h)
        if kind == "ExternalOutput":
            self.outs.append(t)
        return t

    def _need(self, r, w, dkey=None):
        need = {}

        def add(key, sem, val):
            if key not in need or need[key][1] < val:
                need[key] = (sem, val)

        for t in r:
            for k, (s, v) in t.w.items():
                add(k, s, v)
        for t in w:
            for k, (s, v) in t.w.items():
                if dkey is not None and k == dkey:
                    continue
                add(k, s, v)
            for k, (s, v) in t.r.items():
                add(k, s, v)
        return need

    def _wait(self, e, need):
        eng = self.E[e]
        for key, (sem, val) in need.items():
            if self.known[e].get(key, 0) >= val:
                continue
            if key == e and (not self.ses or e == "pe"):
                continue
            eng.wait_ge(sem, val)
            self.known[e][key] = val

    def op(self, e, fn, r=(), w=()):
        need = self._need(r, w)
        self._wait(e, need)
        ins = fn(self.E[e])
        self.cnt[e] += 1
        ins.then_inc(self.sem[e], 1)
        ev = (self.sem[e], self.cnt[e])
        for t in r:
            t.r[e] = ev
        for t in w:
            t.w[e] = ev
        return ins

    def dma(self, q, out_ap, in_ap, r=(), w=(), **kw):
        wt = w[0] if w else None
        dkey = None
        if wt is not None and wt.dsem is None:
            wt.dsem = self.st.enter_context(self.nc.semaphore("d_" + wt.name))
        if wt is not None:
            dkey = "d_" + wt.name
        need = self._need(r, w, dkey)
        self._wait(q, need)
        ins = self.E[q].dma_start(out=out_ap, in_=in_ap, **kw)
        if wt is not None:
            wt.dcount += 16
            sem, val, key = wt.dsem, wt.dcount, dkey
        else:
            self.qcnt[q] += 16
            sem, val, key = self.qsem[q], self.qcnt[q], "q_" + q
        ins.then_inc(sem, 16)
        for t in r:
            t.r[key] = (sem, val)
        for t in w:
            t.w[key] = (sem, val)
        return ins

    def finish(self):
        need = {}
        for t in self.outs:
            for k, (s, v) in t.w.items():
                if k not in need or need[k][1] < v:
                    need[k] = (s, v)
        self._wait("sp", need)
        for q in self.qsem:
            if self.qcnt[q]:
                self.E["sp"].wait_ge(self.qsem[q], self.qcnt[q])
        return self.nc


def run(kb, in_maps, trace=False):
    res = run_bass_kernel_spmd(kb.nc, in_maps, core_ids=list(range(len(in_maps))), trace=trace)
    return res


D = 2048
KT = 16
NW = 13184
TOK = 1056
BLKS = [(0, 512), (512, 512), (1024, 32)]
EPS = 1e-6


def build_k0():
    kb = KB()
    cT = kb.dram("cT", [128, KT, 2], F32, kind="ExternalInput")
    adaw = kb.dram("adaw", [2, D, 1536], F32, kind="ExternalInput")
    adab = kb.dram("adab", [2, 128, 12], F32, kind="ExternalInput")
    modT = kb.dram("modT", [2, 128, 12, 2], F32, kind="ExternalOutput")
    c_sb = kb.sb([128, KT, 2], F32, "c_sb")
    sc = kb.sb([128, KT, 2], F32, "sc")
    kb.dma("sp", c_sb[:], cT.ap(), w=[c_sb])
    kb.op("act", lambda e: e.activation(out=sc[:], in_=c_sb[:], func=AF.Silu), r=[c_sb], w=[sc])
    wts = [kb.sb([128, KT, 1536], F32, f"w{l}") for l in range(2)]
    for l in range(2):
        src = adaw.ap()[l].rearrange("(kt p) n -> p kt n", p=128)
        for kt in range(KT):
            kb.dma("sp" if kt % 2 == 0 else "act", wts[l][:, kt, :], src[:, kt, :], w=[wts[l]])
    for l in range(2):
        b_sb = kb.sb([128, 12], F32, f"b{l}")
        kb.dma("sp", b_sb[:], adab.ap()[l], w=[b_sb])
        pt = kb.ps([128, 12, 2], F32, f"pm{l}")
        for j in range(12):
            for kt in range(KT):
                kb.op("pe", lambda e, j=j, kt=kt: e.matmul(pt[:, j, :], lhsT=wts[l][:, kt, j * 128:(j + 1) * 128],
                                                            rhs=sc[:, kt, :], start=(kt == 0), stop=(kt == KT - 1)),
                      r=[wts[l], sc], w=[pt])
        o = kb.sb([128, 12, 2], F32, f"o{l}")
        for col in range(2):
            kb.op("dve", lambda e, col=col: e.tensor_tensor(out=o[:, :, col], in0=pt[:, :, col], in1=b_sb[:], op=ALU.add),
                  r=[pt, b_sb], w=[o])
        kb.dma("sp", modT.ap()[l], o[:], r=[o], w=[modT])
    kb.finish()
    return kb


def emit_norm_mod(kb, xs, hT, g_sb, sc_l, sh_l, sc_c, sh_c, ones, tagp=""):
    A_l = kb.sb([128, KT], F32, tagp + "A_l")
    A_c = kb.sb([128, KT], F32, tagp + "A_c")
    kb.op("dve", lambda e: e.scalar_tensor_tensor(out=A_l[:], in0=sc_l[:], scalar=1.0, in1=g_sb[:], op0=ALU.add, op1=ALU.mult),
          r=[sc_l, g_sb], w=[A_l])
    kb.op("dve", lambda e: e.scalar_tensor_tensor(out=A_c[:], in0=sc_c[:], scalar=1.0, in1=g_sb[:], op0=ALU.add, op1=ALU.mult),
          r=[sc_c, g_sb], w=[A_c])
    rstd = kb.sb([128, TOK], F32, tagp + "rstd")
    sqs = [kb.sb([128, 512], F32, tagp + f"sq{i}") for i in range(2)]
    pss = [kb.ps([128, 512], F32, tagp + f"pss{i}") for i in range(2)]
    n = 0
    for bi, (c0, cn) in enumerate(BLKS):
        ps = pss[bi % 2]
        for kt in range(KT):
            sq = sqs[n % 2]
            n += 1
            kb.op("act", lambda e, kt=kt, sq=sq: e.activation(out=sq[:, :cn], in_=xs[:, kt, c0:c0 + cn], func=AF.Square),
                  r=[xs], w=[sq])
            kb.op("pe", lambda e, kt=kt, sq=sq: e.matmul(ps[:, :cn], lhsT=ones[:], rhs=sq[:, :cn], start=(kt == 0), stop=(kt == KT - 1)),
                  r=[ones, sq], w=[ps])
        sd = sqs[n % 2]
        n += 1
        kb.op("act", lambda e: e.activation(out=sd[:, :cn], in_=ps[:, :cn], func=AF.Sqrt, bias=EPS, scale=1.0 / D),
              r=[ps], w=[sd])
        kb.op("dve", lambda e: e.reciprocal(out=rstd[:, c0:c0 + cn], in_=sd[:, :cn]), r=[sd], w=[rstd])
    tmps = [kb.sb([128, 512], F32, tagp + f"tmp{i}") for i in range(2)]
    n = 0
    for bi, (c0, cn) in enumerate(BLKS):
        A = A_c if bi == 2 else A_l
        Bt = sh_c if bi == 2 else sh_l
        for kt in range(KT):
            tmp = tmps[n % 2]
            n += 1
            kb.op("dve", lambda e, kt=kt, tmp=tmp: e.tensor_tensor(out=tmp[:, :cn], in0=xs[:, kt, c0:c0 + cn], in1=rstd[:, c0:c0 + cn], op=ALU.mult),
                  r=[xs, rstd], w=[tmp])
            kb.op("act", lambda e, kt=kt, tmp=tmp, A=A, Bt=Bt: e.activation(out=hT[:, kt, c0:c0 + cn], in_=tmp[:, :cn], func=AF.Identity,
                                                                    bias=Bt[:, kt:kt + 1], scale=A[:, kt:kt + 1]),
                  r=[tmp, A, Bt], w=[hT])


def emit_linear(kb, hT, W, ntiles, out_dram, kt_n=KT, tagp="", evac=None):
    wbs = [kb.sb([128, kt_n, 128], BF16, tagp + f"wb{i}") for i in range(3)]
    pps = [kb.ps([128, 512], F32, tagp + f"pp{i}") for i in range(4)]
    obs = [kb.sb([128, TOK], F32, tagp + f"ob{i}") for i in range(2)]
    Wv = W.ap().rearrange("(kt p) n -> p kt n", p=128)
    pi = 0
    for nt in range(ntiles):
        wb = wbs[nt % 3]
        kb.dma("pool", wb[:], Wv[:, :, nt * 128:(nt + 1) * 128], w=[wb])
        ob = obs[nt % 2]
        for bi, (c0, cn) in enumerate(BLKS):
            pp = pps[pi % 4]
            pi += 1
            for kt in range(kt_n):
                kb.op("pe", lambda e, kt=kt, pp=pp, wb=wb: e.matmul(pp[:, :cn], lhsT=wb[:, kt, :], rhs=hT[:, kt, c0:c0 + cn],
                                                             start=(kt == 0), stop=(kt == kt_n - 1)),
                      r=[wb, hT], w=[pp])
            eng = "act" if pi % 2 == 0 else "dve"
            if eng == "act":
                kb.op("act", lambda e, pp=pp, ob=ob: e.copy(out=ob[:, c0:c0 + cn], in_=pp[:, :cn]), r=[pp], w=[ob])
            else:
                kb.op("dve", lambda e, pp=pp, ob=ob: e.tensor_copy(out=ob[:, c0:c0 + cn], in_=pp[:, :cn]), r=[pp], w=[ob])
        kb.dma("sp", out_dram.ap()[nt * 128:(nt + 1) * 128, :], ob[:], r=[ob], w=[out_dram])


def build_k1(ntiles=NW // 128):
    kb = KB()
    xT = kb.dram("xT", [D, TOK], F32, kind="ExternalInput")
    W = kb.dram("W", [D, ntiles * 128], F32, kind="ExternalInput")
    vecs = kb.dram("vecs", [5, 128, KT], F32, kind="ExternalInput")
    zT = kb.dram("zT", [ntiles * 128, TOK], F32, kind="ExternalOutput")
    xs = kb.sb([128, KT, TOK], F32, "xs")
    xv = xT.ap().rearrange("(kt p) t -> p kt t", p=128)
    for kt in range(KT):
        kb.dma("sp" if kt % 2 == 0 else "act", xs[:, kt, :], xv[:, kt, :], w=[xs])
    vt = []
    for i in range(5):
        t = kb.sb([128, KT], F32, f"vec{i}")
        kb.dma("sp", t[:], vecs.ap()[i], w=[t])
        vt.append(t)
    ones = kb.sb([128, 128], F32, "ones")
    kb.op("dve", lambda e: e.memset(ones[:], 1.0), w=[ones])
    hT = kb.sb([128, KT, TOK], BF16, "hT")
    emit_norm_mod(kb, xs, hT, vt[0], vt[1], vt[2], vt[3], vt[4], ones)
    emit_linear(kb, hT, W, ntiles, zT)
    kb.finish()
    return kb

import math

LTOT = 8448
TC = 256
NCH = LTOT // TC
NTD = 8


def build_k2a():
    kb = KB()
    uT = kb.dram("uT", [2, 128, LTOT], F32, kind="ExternalInput")
    Bblk = kb.dram("Bblk", [2, 2, 4, 128, 128], F32, kind="ExternalInput")
    Cblk = kb.dram("Cblk", [2, 2, 4, 128, 128], F32, kind="ExternalInput")
    lam = kb.dram("lam", [3, 128, NTD], F32, kind="ExternalInput")
    dvec = kb.dram("dvec", [128, 1], F32, kind="ExternalInput")
    yT = kb.dram("yT", [2, 128, LTOT], F32, kind="ExternalOutput")

    ubf = [kb.sb([128, LTOT], BF16, f"ubf{d}") for d in range(2)]
    for d in range(2):
        for c0 in range(0, LTOT, 2048):
            cn = min(2048, LTOT - c0)
            kb.dma("pool", ubf[d][:, c0:c0 + cn], uT.ap()[d, :, c0:c0 + cn], w=[ubf[d]])
    dv = kb.sb([128, 1], F32, "dv")
    kb.dma("sp", dv[:], dvec.ap(), w=[dv])
    Bb = kb.sb([128, 2, 2, 4, 128], BF16, "Bb")
    for d in range(2):
        for p in range(2):
            kb.dma("pool", Bb[:, d, p], Bblk.ap()[d, p].rearrange("q k n -> k q n"), w=[Bb])
    Cf = kb.sb([128, 2, 2, 4, 128], F32, "Cf")
    for d in range(2):
        for p in range(2):
            kb.dma("act", Cf[:, d, p], Cblk.ap()[d, p].rearrange("q k n -> k q n"), w=[Cf])
    Cb = kb.sb([128, 2, 2, 4, 128], BF16, "Cb")
    for d in range(2):
        kb.op("dve", lambda e, d=d: e.tensor_copy(out=Cb[:, d, 0], in_=Cf[:, d, 0]), r=[Cf], w=[Cb])
        kb.op("dve", lambda e, d=d: e.tensor_scalar(out=Cb[:, d, 1], in0=Cf[:, d, 1], scalar1=-1.0, scalar2=None, op0=ALU.mult),
              r=[Cf], w=[Cb])
    lr = kb.sb([128, NTD], F32, "lr")
    li = kb.sb([128, NTD], F32, "li")
    ldt = kb.sb([128, NTD], F32, "ldt")
    kb.dma("sp", lr[:], lam.ap()[0], w=[lr])
    kb.dma("sp", li[:], lam.ap()[1], w=[li])
    kb.dma("sp", ldt[:], lam.ap()[2], w=[ldt])

    nid = [0]

    def sm(name=None):
        nid[0] += 1
        return kb.sb([128, NTD], F32, name or f"sm{nid[0]}")

    def tt(out, a, b, op):
        kb.op("dve", lambda e: e.tensor_tensor(out=out[:], in0=a[:], in1=b[:], op=op), r=[a, b], w=[out])

    def ts(out, a, s1, op0, s2=None, op1=None):
        if op1 is None:
            kb.op("dve", lambda e: e.tensor_scalar(out=out[:], in0=a[:], scalar1=s1, scalar2=None, op0=op0), r=[a], w=[out])
        else:
            kb.op("dve", lambda e: e.tensor_scalar(out=out[:], in0=a[:], scalar1=s1, scalar2=s2, op0=op0, op1=op1), r=[a], w=[out])

    dt = sm("dt")
    kb.op("act", lambda e: e.activation(out=dt[:], in_=ldt[:], func=AF.Exp), r=[ldt], w=[dt])
    lrd = sm()
    tt(lrd, lr, dt, ALU.mult)
    mag = sm("mag")
    kb.op("act", lambda e: e.activation(out=mag[:], in_=lrd[:], func=AF.Exp), r=[lrd], w=[mag])
    ang = sm("ang")
    tt(ang, li, dt, ALU.mult)
    kf = sm()
    ts(kf, ang, 1.0 / (2 * math.pi), ALU.mult)
    ki = kb.sb([128, NTD], I32, "ki")
    kb.op("dve", lambda e: e.tensor_copy(out=ki[:], in_=kf[:]), r=[kf], w=[ki])
    kf2 = sm()
    kb.op("dve", lambda e: e.tensor_copy(out=kf2[:], in_=ki[:]), r=[ki], w=[kf2])
    C1 = 6.28125
    C2 = 2 * math.pi - C1
    r1 = sm()
    kb.op("dve", lambda e: e.scalar_tensor_tensor(out=r1[:], in0=kf2[:], scalar=-C1, in1=ang[:], op0=ALU.mult, op1=ALU.add),
          r=[kf2, ang], w=[r1])
    r2 = sm()
    kb.op("dve", lambda e: e.scalar_tensor_tensor(out=r2[:], in0=kf2[:], scalar=-C2, in1=r1[:], op0=ALU.mult, op1=ALU.add),
          r=[kf2, r1], w=[r2])
    xx = sm("xx")
    ts(xx, r2, 0.125, ALU.mult)
    x2 = sm("x2")
    tt(x2, xx, xx, ALU.mult)

    def horner(coefs):
        p = sm()
        ts(p, x2, -1.0 / coefs[-1], ALU.mult, 1.0, ALU.add)
        for cf in reversed(coefs[:-1]):
            q_ = sm()
            tt(q_, p, x2, ALU.mult)
            p = sm()
            ts(p, q_, -1.0 / cf, ALU.mult, 1.0, ALU.add)
        return p

    ps_ = horner([6.0, 20.0, 42.0, 72.0, 110.0, 156.0])
    sn = sm("sn")
    tt(sn, ps_, xx, ALU.mult)
    cs = horner([2.0, 12.0, 30.0, 56.0, 90.0, 132.0])
    for _ in range(3):
        c2 = sm(); s2 = sm(); sc_ = sm()
        tt(c2, cs, cs, ALU.mult)
        tt(s2, sn, sn, ALU.mult)
        tt(sc_, sn, cs, ALU.mult)
        cs = sm(); sn = sm()
        tt(cs, c2, s2, ALU.subtract)
        ts(sn, sc_, 2.0, ALU.mult)
    ab_re = sm("ab_re"); ab_im = sm("ab_im")
    tt(ab_re, mag, cs, ALU.mult)
    tt(ab_im, mag, sn, ALU.mult)
    den = sm(); t_a = sm(); t_b = sm()
    tt(t_a, lr, lr, ALU.mult)
    tt(t_b, li, li, ALU.mult)
    tt(den, t_a, t_b, ALU.add)
    rden = sm()
    kb.op("dve", lambda e: e.reciprocal(out=rden[:], in_=den[:]), r=[den], w=[rden])
    nr = sm()
    ts(nr, ab_re, -1.0, ALU.add)
    f_re = sm("f_re"); f_im = sm("f_im")
    u1 = sm(); u2 = sm(); u3 = sm()
    tt(u1, nr, lr, ALU.mult)
    tt(u2, ab_im, li, ALU.mult)
    tt(u3, u1, u2, ALU.add)
    tt(f_re, u3, rden, ALU.mult)
    v1 = sm(); v2 = sm(); v3 = sm()
    tt(v1, ab_im, lr, ALU.mult)
    tt(v2, nr, li, ALU.mult)
    tt(v3, v1, v2, ALU.subtract)
    tt(f_im, v3, rden, ALU.mult)
    nsn_unused = None

    Er = kb.sb([128, NTD, TC + 1], F32, "Er")
    Ei = kb.sb([128, NTD, TC + 1], F32, "Ei")
    kb.op("dve", lambda e: e.memset(Er[:, :, 0:1], 1.0), w=[Er])
    kb.op("dve", lambda e: e.memset(Ei[:, :, 0:1], 0.0), w=[Ei])
    pr, pi_ = cs, sn
    n = 1
    while n <= TC:
        m = min(n, TC + 1 - n)
        npi = sm()
        ts(npi, pi_, -1.0, ALU.mult)
        ta = kb.sb([128, NTD, m], F32, f"eta{n}")
        tb = kb.sb([128, NTD, m], F32, f"etb{n}")
        for td in range(NTD):
            kb.op("dve", lambda e, td=td: e.tensor_scalar(out=ta[:, td, :], in0=Er[:, td, 0:m], scalar1=pr[:, td:td + 1], scalar2=None, op0=ALU.mult),
                  r=[Er, pr], w=[ta])
            kb.op("dve", lambda e, td=td: e.scalar_tensor_tensor(out=Er[:, td, n:n + m], in0=Ei[:, td, 0:m], scalar=npi[:, td:td + 1], in1=ta[:, td, :],
                                                                 op0=ALU.mult, op1=ALU.add), r=[Ei, npi, ta], w=[Er])
            kb.op("dve", lambda e, td=td: e.tensor_scalar(out=tb[:, td, :], in0=Er[:, td, 0:m], scalar1=pi_[:, td:td + 1], scalar2=None, op0=ALU.mult),
                  r=[Er, pi_], w=[tb])
            kb.op("dve", lambda e, td=td: e.scalar_tensor_tensor(out=Ei[:, td, n:n + m], in0=Ei[:, td, 0:m], scalar=pr[:, td:td + 1], in1=tb[:, td, :],
                                                                 op0=ALU.mult, op1=ALU.add), r=[Ei, pr, tb], w=[Ei])
        c2 = sm(); s2 = sm(); sc_ = sm()
        tt(c2, pr, pr, ALU.mult)
        tt(s2, pi_, pi_, ALU.mult)
        tt(sc_, pr, pi_, ALU.mult)
        pr = sm(); pi_ = sm()
        tt(pr, c2, s2, ALU.subtract)
        ts(pi_, sc_, 2.0, ALU.mult)
        n *= 2
    Rr = kb.sb([128, NTD, TC], F32, "Rr")
    Ri = kb.sb([128, NTD, TC], F32, "Ri")
    rmul = kb.sb([128, NTD * TC], F32, "rmul")
    rmul3 = rmul[:].rearrange("p (n t) -> p n t", t=TC)
    onesT = kb.sb([128, TC], F32, "onesT")
    kb.op("dve", lambda e: e.memset(onesT[:], 1.0), w=[onesT])
    nf_re = sm()
    ts(nf_re, f_re, -1.0, ALU.mult)
    tr = kb.sb([128, NTD, TC], F32, "trtmp")
    for td in range(NTD):
        kb.op("dve", lambda e, td=td: e.tensor_scalar(out=tr[:, td, :], in0=Er[:, td, 0:TC], scalar1=f_re[:, td:td + 1], scalar2=None, op0=ALU.mult),
              r=[Er, f_re], w=[tr])
        kb.op("dve", lambda e, td=td: e.scalar_tensor_tensor(out=Rr[:, td, :], in0=Ei[:, td, 0:TC], scalar=f_im[:, td:td + 1], in1=tr[:, td, :],
                                                             op0=ALU.mult, op1=ALU.add), r=[Ei, f_im, tr], w=[Rr])
        kb.op("dve", lambda e, td=td: e.tensor_scalar(out=tr[:, td, :], in0=Er[:, td, 0:TC], scalar1=f_im[:, td:td + 1], scalar2=None, op0=ALU.mult),
              r=[Er, f_im], w=[tr])
        kb.op("dve", lambda e, td=td: e.scalar_tensor_tensor(out=Ri[:, td, :], in0=Ei[:, td, 0:TC], scalar=nf_re[:, td:td + 1], in1=tr[:, td, :],
                                                             op0=ALU.mult, op1=ALU.add), r=[Ei, nf_re, tr], w=[Ri])
        kb.op("dve", lambda e, td=td: e.tensor_scalar(out=rmul3[:, td, :], in0=onesT[:], scalar1=mag[:, td:td + 1], scalar2=None, op0=ALU.mult),
              r=[onesT, mag], w=[rmul])
    kb.op("dve", lambda e: e.memset(rmul3[:, :, 0:1], 0.0), w=[rmul])

    Q4 = 4 * TC
    pX = [kb.ps([128, Q4], F32, f"pX{d}") for d in range(2)]
    pY = [kb.ps([128, 512], F32, f"pY{d}") for d in range(2)]
    W = {}
    for nm in ("xre", "xim", "t1", "t2", "t3", "t4", "gr", "gi"):
        W[nm] = [kb.sb([128, Q4], F32, f"w_{nm}{i}") for i in range(2)]
    for nm in ("hr", "hi"):
        W[nm] = [kb.sb([128, Q4], BF16, f"w_{nm}{i}") for i in range(2)]
    yo = [[kb.sb([128, TC], F32, f"yo{d}_{i}") for i in range(2)] for d in range(2)]
    ufc = [kb.sb([128, TC], F32, f"ufc{i}") for i in range(2)]
    carry = [[kb.sb([128, 8], F32, f"carry{d}_{i}") for i in range(2)] for d in range(2)]
    for d in range(2):
        kb.op("dve", lambda e, d=d: e.memset(carry[d][0][:], 0.0), w=[carry[d][0]])
    ctmp = [kb.sb([128, 16], F32, f"ctmp{d}") for d in range(2)]

    def v3(t):
        return t[:].rearrange("p (q t) -> p q t", t=TC)

    def body(d, c):
        ops = []
        A = ops.append
        qs = slice(d * 4, d * 4 + 4)
        c0 = c * TC
        py = pY[d]
        px = pX[d]
        w = {k: v[d] for k, v in W.items()}
        if d == 0:
            A(lambda: kb.dma("sp", ufc[c % 2][:], uT.ap()[0, :, c0:c0 + TC], w=[ufc[c % 2]]))

        def mm_in(p):
            for q in range(4):
                kb.op("pe", lambda e, q=q: e.matmul(px[:, q * TC:(q + 1) * TC], lhsT=Bb[:, d, p, q, :], rhs=ubf[d][:, c0:c0 + TC], start=True, stop=True),
                      r=[Bb, ubf[d]], w=[px])
        A(lambda: mm_in(0))
        A(lambda: kb.op("act", lambda e: e.copy(out=w["xre"][:], in_=px[:]), r=[px], w=[w["xre"]]))
        A(lambda: mm_in(1))
        A(lambda: kb.op("act", lambda e: e.copy(out=w["xim"][:], in_=px[:]), r=[px], w=[w["xim"]]))
        A(lambda: kb.op("dve", lambda e: e.tensor_tensor(out=v3(w["t1"]), in0=v3(w["xre"]), in1=Rr[:, qs, :], op=ALU.mult), r=[w["xre"], Rr], w=[w["t1"]]))
        A(lambda: kb.op("dve", lambda e: e.tensor_tensor(out=v3(w["t2"]), in0=v3(w["xim"]), in1=Ri[:, qs, :], op=ALU.mult), r=[w["xim"], Ri], w=[w["t2"]]))
        A(lambda: kb.op("dve", lambda e: e.tensor_tensor(out=v3(w["t3"]), in0=v3(w["xre"]), in1=Ri[:, qs, :], op=ALU.mult), r=[w["xre"], Ri], w=[w["t3"]]))
        A(lambda: kb.op("dve", lambda e: e.tensor_tensor(out=v3(w["t4"]), in0=v3(w["xim"]), in1=Rr[:, qs, :], op=ALU.mult), r=[w["xim"], Rr], w=[w["t4"]]))
        cin = carry[d][c % 2]
        cout = carry[d][(c + 1) % 2]
        ct = ctmp[d]
        A(lambda: kb.op("dve", lambda e: e.tensor_tensor(out=ct[:, 0:4], in0=cin[:, 0:4], in1=mag[:, qs], op=ALU.mult), r=[cin, mag], w=[ct]))
        A(lambda: kb.op("dve", lambda e: e.tensor_tensor(out=ct[:, 4:8], in0=cin[:, 4:8], in1=mag[:, qs], op=ALU.mult), r=[cin, mag], w=[ct]))
        A(lambda: kb.op("dve", lambda e: e.tensor_tensor(out=w["t1"][:], in0=w["t1"][:], in1=w["t2"][:], op=ALU.subtract), r=[w["t1"], w["t2"]], w=[w["t1"]]))
        A(lambda: kb.op("dve", lambda e: e.tensor_tensor(out=w["t3"][:], in0=w["t3"][:], in1=w["t4"][:], op=ALU.add), r=[w["t3"], w["t4"]], w=[w["t3"]]))
        A(lambda: kb.op("dve", lambda e: e.tensor_tensor(out=v3(w["t1"])[:, :, 0], in0=v3(w["t1"])[:, :, 0], in1=ct[:, 0:4], op=ALU.add), r=[w["t1"], ct], w=[w["t1"]]))
        A(lambda: kb.op("dve", lambda e: e.tensor_tensor(out=v3(w["t3"])[:, :, 0], in0=v3(w["t3"])[:, :, 0], in1=ct[:, 4:8], op=ALU.add), r=[w["t3"], ct], w=[w["t3"]]))
        A(lambda: kb.op("dve", lambda e: e.tensor_tensor_scan(out=w["gr"][:], data0=rmul[:, d * Q4:(d + 1) * Q4], data1=w["t1"][:], initial=0.0,
                                                              op0=ALU.mult, op1=ALU.add), r=[rmul, w["t1"]], w=[w["gr"]]))
        A(lambda: kb.op("dve", lambda e: e.tensor_tensor_scan(out=w["gi"][:], data0=rmul[:, d * Q4:(d + 1) * Q4], data1=w["t3"][:], initial=0.0,
                                                              op0=ALU.mult, op1=ALU.add), r=[rmul, w["t3"]], w=[w["gi"]]))
        grl = v3(w["gr"])[:, :, TC - 1]
        gil = v3(w["gi"])[:, :, TC - 1]
        ETr = Er[:, qs, TC]
        ETi = Ei[:, qs, TC]
        A(lambda: kb.op("dve", lambda e: e.tensor_tensor(out=ct[:, 12:16], in0=grl, in1=ETi, op=ALU.mult), r=[w["gr"], Ei], w=[ct]))
        A(lambda: kb.op("dve", lambda e: e.tensor_tensor(out=cout[:, 0:4], in0=grl, in1=ETr, op=ALU.mult), r=[w["gr"], Er], w=[cout]))
        A(lambda: kb.op("dve", lambda e: e.tensor_tensor(out=ct[:, 8:12], in0=gil, in1=ETi, op=ALU.mult), r=[w["gi"], Ei], w=[ct]))
        A(lambda: kb.op("dve", lambda e: e.tensor_tensor(out=cout[:, 4:8], in0=gil, in1=ETr, op=ALU.mult), r=[w["gi"], Er], w=[cout]))
        A(lambda: kb.op("dve", lambda e: e.tensor_tensor(out=cout[:, 0:4], in0=cout[:, 0:4], in1=ct[:, 8:12], op=ALU.subtract), r=[cout, ct], w=[cout]))
        A(lambda: kb.op("dve", lambda e: e.tensor_tensor(out=cout[:, 4:8], in0=cout[:, 4:8], in1=ct[:, 12:16], op=ALU.add), r=[cout, ct], w=[cout]))
        A(lambda: kb.op("dve", lambda e: e.tensor_tensor(out=v3(w["t2"]), in0=v3(w["gr"]), in1=Er[:, qs, 0:TC], op=ALU.mult), r=[w["gr"], Er], w=[w["t2"]]))
        A(lambda: kb.op("dve", lambda e: e.tensor_tensor(out=v3(w["xim"]), in0=v3(w["gi"]), in1=Er[:, qs, 0:TC], op=ALU.mult), r=[w["gi"], Er], w=[w["xim"]]))
        A(lambda: kb.op("dve", lambda e: e.tensor_tensor(out=v3(w["t4"]), in0=v3(w["gi"]), in1=Ei[:, qs, 0:TC], op=ALU.mult), r=[w["gi"], Ei], w=[w["t4"]]))
        A(lambda: kb.op("dve", lambda e: e.tensor_tensor(out=v3(w["xre"]), in0=v3(w["gr"]), in1=Ei[:, qs, 0:TC], op=ALU.mult), r=[w["gr"], Ei], w=[w["xre"]]))
        A(lambda: kb.op("dve", lambda e: e.tensor_tensor(out=w["hr"][:], in0=w["t2"][:], in1=w["t4"][:], op=ALU.subtract), r=[w["t2"], w["t4"]], w=[w["hr"]]))
        A(lambda: kb.op("dve", lambda e: e.tensor_tensor(out=w["hi"][:], in0=w["xre"][:], in1=w["xim"][:], op=ALU.add), r=[w["xre"], w["xim"]], w=[w["hi"]]))

        def mm_out():
            for q in range(4):
                kb.op("pe", lambda e, q=q: e.matmul(py[:, :TC], lhsT=Cb[:, d, 0, q, :], rhs=w["hr"][:, q * TC:(q + 1) * TC], start=(q == 0), stop=False), r=[Cb, w["hr"]], w=[py])
                kb.op("pe", lambda e, q=q: e.matmul(py[:, :TC], lhsT=Cb[:, d, 1, q, :], rhs=w["hi"][:, q * TC:(q + 1) * TC], start=False, stop=(q == 3)), r=[Cb, w["hi"]], w=[py])
        A(mm_out)
        o = yo[d][c % 2]
        if d == 0:
            A(lambda: kb.op("dve", lambda e: e.scalar_tensor_tensor(out=o[:], in0=ufc[c % 2][:], scalar=dv[:, 0:1], in1=py[:, :TC], op0=ALU.mult, op1=ALU.add),
                            r=[ufc[c % 2], dv, py], w=[o]))
        else:
            A(lambda: kb.op("act", lambda e: e.copy(out=o[:], in_=py[:, :TC]), r=[py], w=[o]))
        A(lambda: kb.dma("sp", yT.ap()[d, :, c0:c0 + TC], o[:], r=[o], w=[yT]))
        return ops

    for c in range(NCH):
        l0 = body(0, c)
        l1 = body(1, c)
        for i in range(max(len(l0), len(l1))):
            if i < len(l0):
                l0[i]()
            if i < len(l1):
                l1[i]()
    kb.finish()
    return kb


LT = 8448
NC64 = LT // 64


def build_k2b():
    kb = KB()
    qk = kb.dram("qk", [2, 128, LT], F32, kind="ExternalInput")
    hw1 = kb.dram("hw1", [16, LT], F32, kind="ExternalInput")
    w2b = kb.dram("w2b", [16, 128], F32, kind="ExternalInput")
    nb = kb.dram("nb", [128, 1], F32, kind="ExternalInput")
    vtok = kb.dram("vtok", [64, NC64, 256], F32, kind="ExternalInput")
    cst = kb.dram("cst", [128, 512 + 64 + 128], F32, kind="ExternalInput")
    oT = kb.dram("oT", [2, 128, LT], F32, kind="ExternalOutput")

    vb = kb.sb([64, NC64, 256], BF16, "vb")
    for c0 in range(0, NC64, 8):
        c1 = min(NC64, c0 + 8)
        kb.dma("pool", vb[:, c0:c1, :], vtok.ap()[:, c0:c1, :], w=[vb])
    cs_ = kb.sb([128, 704], F32, "cs_")
    kb.dma("sp", cs_[:], cst.ap(), w=[cs_])
    identb = kb.sb([128, 128], BF16, "identb")
    kb.op("dve", lambda e: e.tensor_copy(out=identb[:], in_=cs_[:, 576:704]), r=[cs_], w=[identb])
    w2s = kb.sb([16, 128], F32, "w2s")
    kb.dma("sp", w2s[:], w2b.ap(), w=[w2s])
    bs_ = kb.sb([128, 1], F32, "bs_")
    kb.dma("sp", bs_[:], nb.ap(), w=[bs_])
    nbs = kb.sb([128, 1], F32, "nbs")
    kb.op("dve", lambda e: e.tensor_scalar(out=nbs[:], in0=bs_[:], scalar1=-1.0, scalar2=None, op0=ALU.mult), r=[bs_], w=[nbs])
    qe = kb.sb([128, LT], BF16, "qe")
    ke = kb.sb([128, LT], BF16, "ke")
    kl = kb.sb([128, LT], BF16, "kl")
    ebl = kb.sb([128, NC64], F32, "ebl")

    BL = 512
    nblk = (LT + BL - 1) // BL
    A = {}
    for nm in ("q", "k", "e1", "la", "bc", "eb", "enb", "kef"):
        A[nm] = [kb.sb([128, BL], F32, f"a_{nm}{i}") for i in range(2)]
    A["h"] = [kb.sb([16, BL], F32, f"a_h{i}") for i in range(2)]
    pso = [kb.ps([128, 512], F32, f"pso{i}") for i in range(2)]
    pg = pso
    scale = 128 ** -0.5
    for bi in range(nblk):
        c0 = bi * BL
        cn = min(BL, LT - c0)
        b = bi % 2
        a = {k: v[b] for k, v in A.items()}
        kb.dma("sp", a["q"][:, :cn], qk.ap()[0, :, c0:c0 + cn], w=[a["q"]])
        kb.dma("act", a["k"][:, :cn], qk.ap()[1, :, c0:c0 + cn], w=[a["k"]])
        kb.dma("sp", a["h"][:, :cn], hw1.ap()[:, c0:c0 + cn], w=[a["h"]])
        kb.op("pe", lambda e: e.matmul(pg[b][:, :cn], lhsT=w2s[:], rhs=a["h"][:, :cn], start=True, stop=True), r=[w2s, a["h"]], w=[pg[b]])
        kb.op("act", lambda e: e.activation(out=a["e1"][:, :cn], in_=pg[b][:, :cn], func=AF.Exp, bias=nbs[:, 0:1], scale=-1.0),
              r=[pg[b], nbs], w=[a["e1"]])
        kb.op("act", lambda e: e.activation(out=a["la"][:, :cn], in_=a["e1"][:, :cn], func=AF.Ln, bias=1.0), r=[a["e1"]], w=[a["la"]])
        kb.op("dve", lambda e: e.tensor_scalar(out=a["la"][:, :cn], in0=a["la"][:, :cn], scalar1=-1.0 / 16.0, scalar2=None, op0=ALU.mult),
              r=[a["la"]], w=[a["la"]])
        kb.op("dve", lambda e: e.tensor_tensor_scan(out=a["bc"][:, :cn], data0=cs_[:, 0:cn], data1=a["la"][:, :cn], initial=0.0,
                                                    op0=ALU.mult, op1=ALU.add), r=[cs_, a["la"]], w=[a["bc"]])
        kb.op("act", lambda e: e.activation(out=a["eb"][:, :cn], in_=a["bc"][:, :cn], func=AF.Exp), r=[a["bc"]], w=[a["eb"]])
        kb.op("act", lambda e: e.activation(out=a["enb"][:, :cn], in_=a["bc"][:, :cn], func=AF.Exp, scale=-1.0), r=[a["bc"]], w=[a["enb"]])
        kb.op("dve", lambda e: e.scalar_tensor_tensor(out=qe[:, c0:c0 + cn], in0=a["q"][:, :cn], scalar=scale, in1=a["eb"][:, :cn],
                                                      op0=ALU.mult, op1=ALU.mult), r=[a["q"], a["eb"]], w=[qe])
        kb.op("dve", lambda e: e.tensor_tensor(out=a["kef"][:, :cn], in0=a["k"][:, :cn], in1=a["enb"][:, :cn], op=ALU.mult),
              r=[a["k"], a["enb"]], w=[a["kef"]])
        kb.op("act", lambda e: e.copy(out=ke[:, c0:c0 + cn], in_=a["kef"][:, :cn]), r=[a["kef"]], w=[ke])
        nch = cn // 64
        ch0 = c0 // 64
        kb.op("dve", lambda e: e.tensor_copy(out=ebl[:, ch0:ch0 + nch],
                                             in_=a["eb"][:, :cn].rearrange("p (c j) -> p c j", j=64)[:, :, 63]), r=[a["eb"]], w=[ebl])
        for cc in range(nch):
            kb.op("dve", lambda e, cc=cc: e.tensor_scalar(out=kl[:, c0 + cc * 64:c0 + (cc + 1) * 64], in0=a["kef"][:, cc * 64:(cc + 1) * 64],
                                                          scalar1=ebl[:, ch0 + cc:ch0 + cc + 1], scalar2=None, op0=ALU.mult),
                  r=[a["kef"], ebl], w=[kl])

    S = [kb.sb([128, 256], F32, f"S{i}") for i in range(2)]
    Sb = [kb.sb([128, 256], BF16, f"Sb{i}") for i in range(2)]
    kb.op("dve", lambda e: e.memset(S[0][:], 0.0), w=[S[0]])
    kb.op("dve", lambda e: e.memset(Sb[0][:], 0.0), w=[Sb[0]])
    psc = [kb.ps([64, 512], F32, f"psc{i}") for i in range(2)]
    ptr = [kb.ps([64, 128], BF16, f"ptr{i}") for i in range(2)]
    pst = [kb.ps([128, 512], F32, f"pst{i}") for i in range(1)]
    pm = [kb.sb([64, 64], BF16, f"pm{i}") for i in range(2)]
    klT = [kb.sb([64, 128], BF16, f"klT{i}") for i in range(2)]
    GB = 8
    ost = [kb.sb([128, 2, GB * 64], F32, f"ost{i}") for i in range(2)]
    for c in range(NC64):
        b = c % 2
        cols = slice(c * 64, (c + 1) * 64)
        So, Sn = S[c % 2], S[(c + 1) % 2]
        Sbo, Sbn = Sb[c % 2], Sb[(c + 1) % 2]
        kb.op("pe", lambda e: e.matmul(psc[b][:, 0:64], lhsT=ke[:, cols], rhs=qe[:, cols], start=True, stop=True), r=[ke, qe], w=[psc[b]])
        kb.op("dve", lambda e: e.tensor_tensor(out=pm[b][:], in0=psc[b][:, 0:64], in1=cs_[0:64, 512:576], op=ALU.mult), r=[psc[b], cs_], w=[pm[b]])
        kb.op("pe", lambda e: e.transpose(ptr[b][:], kl[:, cols], identb[:]), r=[kl, identb], w=[ptr[b]])
        kb.op("act", lambda e: e.copy(out=klT[b][:], in_=ptr[b][:]), r=[ptr[b]], w=[klT[b]])
        for vt in range(2):
            kb.op("pe", lambda e, vt=vt: e.matmul(pso[b][:, vt * 64:(vt + 1) * 64], lhsT=vb[:, c, vt * 128:(vt + 1) * 128], rhs=pm[b][:], start=True, stop=False),
                  r=[vb, pm[b]], w=[pso[b]])
            kb.op("pe", lambda e, vt=vt: e.matmul(pso[b][:, vt * 64:(vt + 1) * 64], lhsT=Sbo[:, vt * 128:(vt + 1) * 128], rhs=qe[:, cols], start=False, stop=True),
                  r=[Sbo, qe], w=[pso[b]])
        o = ost[(c // GB) % 2]
        oc = (c % GB) * 64
        kb.op("act", lambda e: e.copy(out=o[:, :, oc:oc + 64], in_=pso[b][:, 0:128].rearrange("p (v i) -> p v i", v=2)), r=[pso[b]], w=[o])
        kb.op("pe", lambda e: e.matmul(pst[0][:, 0:256], lhsT=klT[b][:], rhs=vb[:, c, :], start=True, stop=True), r=[klT[b], vb], w=[pst[0]])
        kb.op("dve", lambda e: e.scalar_tensor_tensor(out=Sn[:], in0=So[:], scalar=ebl[:, c:c + 1], in1=pst[0][:, 0:256], op0=ALU.mult, op1=ALU.add),
              r=[So, ebl, pst[0]], w=[Sn])
        kb.op("act", lambda e: e.copy(out=Sbn[:], in_=Sn[:]), r=[Sn], w=[Sbn])
        if c % GB == GB - 1 or c == NC64 - 1:
            g0 = (c // GB) * GB * 64
            gn = (c % GB + 1) * 64
            for vt in range(2):
                kb.dma("sp", oT.ap()[vt, :, g0:g0 + gn], o[:, vt, 0:gn], r=[o], w=[oT])
    kb.finish()
    return kb


L = 8192
NBQ = 64


def build_k2c():
    kb = KB()
    qT = kb.dram("qT", [2, 128, L], F32, kind="ExternalInput")
    kT = kb.dram("kT", [2, 128, L], F32, kind="ExternalInput")
    cs = kb.dram("cs", [2, 128, L], F32, kind="ExternalInput")
    cx = kb.dram("cx", [2, 128, 256], F32, kind="ExternalInput")
    vtok = kb.dram("vtok", [128, 66, 64], F32, kind="ExternalInput")
    masks = kb.dram("masks", [2, 128, 128], F32, kind="ExternalInput")
    sink = kb.dram("sink", [64, 2], F32, kind="ExternalInput")
    yT = kb.dram("yT", [128, L], F32, kind="ExternalOutput")
    ycT = kb.dram("ycT", [128, 256], F32, kind="ExternalOutput")

    qr = kb.sb([128, L + 256], BF16, "qr")
    kr = kb.sb([128, L + 256], BF16, "kr")
    vb = kb.sb([128, 66, 64], BF16, "vb")
    kb.dma("pool", vb[:], vtok.ap(), w=[vb])
    mk = kb.sb([128, 2, 128], BF16, "mk")
    kb.dma("pool", mk[:], masks.ap().rearrange("m j i -> j m i"), w=[mk])
    onesb = kb.sb([128, 64], BF16, "onesb")
    kb.op("dve", lambda e: e.memset(onesb[:], 1.0), w=[onesb])
    sk = kb.sb([64, 2], F32, "sk")
    kb.dma("sp", sk[:], sink.ap(), w=[sk])
    es = kb.sb([64, 2], F32, "es")
    kb.op("act", lambda e: e.activation(out=es[:], in_=sk[:], func=AF.Exp), r=[sk], w=[es])
    kb.dma("pool", qr[:, L:L + 256], cx.ap()[0], w=[qr])
    kb.dma("pool", kr[:, L:L + 256], cx.ap()[1], w=[kr])
    RB = 1024
    bufs = {}
    for nm in ("a", "ap", "c", "s", "t1", "t2"):
        bufs[nm] = [kb.sb([128, RB], F32, f"r_{nm}{i}") for i in range(2)]
    it = 0
    for src, dst in ((qT, qr), (kT, kr)):
        for c0 in range(0, L, RB):
            b = it % 2
            it += 1
            a, ap_, c_, s_, t1, t2 = (bufs[nm][b] for nm in ("a", "ap", "c", "s", "t1", "t2"))
            kb.dma("sp", a[:], src.ap()[0, :, c0:c0 + RB], w=[a])
            kb.dma("act", ap_[:], src.ap()[1, :, c0:c0 + RB], w=[ap_])
            kb.dma("sp", c_[:], cs.ap()[0, :, c0:c0 + RB], w=[c_])
            kb.dma("act", s_[:], cs.ap()[1, :, c0:c0 + RB], w=[s_])
            kb.op("dve", lambda e: e.tensor_tensor(out=t1[:], in0=a[:], in1=c_[:], op=ALU.mult), r=[a, c_], w=[t1])
            kb.op("dve", lambda e: e.tensor_tensor(out=t2[:], in0=ap_[:], in1=s_[:], op=ALU.mult), r=[ap_, s_], w=[t2])
            kb.op("dve", lambda e: e.tensor_tensor(out=dst[:, c0:c0 + RB], in0=t1[:], in1=t2[:], op=ALU.add), r=[t1, t2], w=[dst])

    ps1 = [kb.ps([128, 512], F32, f"ps1_{i}") for i in range(2)]
    ps2 = [kb.ps([128, 512], F32, f"ps2_{i}") for i in range(2)]
    po = [kb.ps([64, 512], F32, f"po{i}") for i in range(2)]
    pb1 = [kb.sb([128, 512], BF16, f"pb1_{i}") for i in range(2)]
    pb2 = [kb.sb([128, 128], BF16, f"pb2_{i}") for i in range(2)]
    den = [kb.sb([64, 128], F32, f"den{i}") for i in range(2)]
    rden = [kb.sb([64, 128], F32, f"rden{i}") for i in range(2)]
    GB = 8
    ob = [kb.sb([64, 2, GB * 128], F32, f"ob{i}") for i in range(2)]
    it = 0
    nblocks = NBQ + 2
    for n in range(nblocks):
        isctx = n >= NBQ
        o = ob[(n // GB) % 2]
        for hh in range(2):
            b = it % 2
            it += 1
            hs = slice(hh * 64, hh * 64 + 64)
            if not isctx:
                qcols = slice(n * 128, (n + 1) * 128)
                tiles = []
                if n > 0:
                    tiles.append((0, n - 1, 0))
                tiles.append((1, n, None))
                if n < NBQ - 1:
                    tiles.append((2, n + 1, 1))
                tiles.append((3, 64, None))
                tiles.append((4, 65, None))
            else:
                qcols = slice(L + (n - NBQ) * 128, L + (n - NBQ + 1) * 128)
                tiles = [(3, 64, None), (4, 65, None)]
            p1, p2 = ps1[b], ps2[b]
            for slot, kt, m in tiles:
                dstp = p1[:, slot * 128:(slot + 1) * 128] if slot < 4 else p2[:, 0:128]
                pt = p1 if slot < 4 else p2
                kb.op("pe", lambda e: e.matmul(dstp, lhsT=kr[hs, kt * 128:(kt + 1) * 128], rhs=qr[hs, qcols], start=True, stop=True),
                      r=[kr, qr], w=[pt])
            slots = [t[0] for t in tiles if t[0] < 4]
            lo, hi = min(slots) * 128, (max(slots) + 1) * 128
            kb.op("act", lambda e: e.activation(out=pb1[b][:, lo:hi], in_=p1[:, lo:hi], func=AF.Exp, scale=0.125), r=[p1], w=[pb1[b]])
            kb.op("act", lambda e: e.activation(out=pb2[b][:], in_=p2[:, 0:128], func=AF.Exp, scale=0.125), r=[p2], w=[pb2[b]])
            for slot, kt, m in tiles:
                if m is not None:
                    kb.op("dve", lambda e: e.tensor_tensor(out=pb1[b][:, slot * 128:(slot + 1) * 128], in0=pb1[b][:, slot * 128:(slot + 1) * 128],
                                                           in1=mk[:, m, :], op=ALU.mult), r=[pb1[b], mk], w=[pb1[b]])
            pp = po[b]
            for ti, (slot, kt, m) in enumerate(tiles):
                src = pb1[b][:, slot * 128:(slot + 1) * 128] if slot < 4 else pb2[b][:]
                st = pb1[b] if slot < 4 else pb2[b]
                kb.op("pe", lambda e: e.matmul(pp[:, 0:128], lhsT=vb[:, kt, :], rhs=src, start=(ti == 0), stop=(ti == len(tiles) - 1)),
                      r=[vb, st], w=[pp])
            for ti, (slot, kt, m) in enumerate(tiles):
                src = pb1[b][:, slot * 128:(slot + 1) * 128] if slot < 4 else pb2[b][:]
                st = pb1[b] if slot < 4 else pb2[b]
                kb.op("pe", lambda e: e.matmul(pp[:, 128:256], lhsT=onesb[:], rhs=src, start=(ti == 0), stop=(ti == len(tiles) - 1)),
                      r=[onesb, st], w=[pp])
            kb.op("dve", lambda e: e.tensor_scalar(out=den[b][:], in0=pp[:, 128:256], scalar1=es[:, hh:hh + 1], scalar2=None, op0=ALU.add),
                  r=[pp, es], w=[den[b]])
            kb.op("dve", lambda e: e.reciprocal(out=rden[b][:], in_=den[b][:]), r=[den[b]], w=[rden[b]])
            oc = (n % GB) * 128
            kb.op("dve", lambda e: e.tensor_tensor(out=o[:, hh, oc:oc + 128], in0=pp[:, 0:128], in1=rden[b][:], op=ALU.mult),
                  r=[pp, rden[b]], w=[o])
        if n < NBQ and n % GB == GB - 1:
            g0 = (n // GB) * GB * 128
            for hh in range(2):
                kb.dma("sp", yT.ap()[hh * 64:(hh + 1) * 64, g0:g0 + GB * 128], o[:, hh, :], r=[o], w=[yT])
        if n == nblocks - 1:
            for hh in range(2):
                kb.dma("sp", ycT.ap()[hh * 64:(hh + 1) * 64, :], o[:, hh, 0:256], r=[o], w=[ycT])
    kb.finish()
    return kb


def build_k3a():
    kb = KB()
    ys = kb.dram("ys", [2, 1024, TOK], F32, kind="ExternalInput")
    og = kb.dram("og", [2, 1024, TOK], F32, kind="ExternalInput")
    ya = kb.dram("ya", [1024, TOK], F32, kind="ExternalInput")
    zr = kb.dram("zr", [1024, TOK], F32, kind="ExternalInput")
    zg = kb.dram("zg", [6144, TOK], F32, kind="ExternalInput")
    xT = kb.dram("xT", [D, TOK], F32, kind="ExternalInput")
    wglu = kb.dram("wglu", [1024, 1024], F32, kind="ExternalInput")
    wbr = kb.dram("wbr", [3, 1024, D], F32, kind="ExternalInput")
    wout = kb.dram("wout", [D, D], F32, kind="ExternalInput")
    vecs = kb.dram("vecs", [3, 128, KT], F32, kind="ExternalInput")
    xo = kb.dram("xo", [D, TOK], F32, kind="ExternalOutput")

    vt_ = []
    for i in range(3):
        t = kb.sb([128, KT], F32, f"vec{i}")
        kb.dma("sp", t[:], vecs.ap()[i], w=[t])
        vt_.append(t)
    gn, m2l, m2c = vt_
    ones = kb.sb([128, 128], F32, "ones")
    kb.op("dve", lambda e: e.memset(ones[:], 1.0), w=[ones])
    PS = [kb.ps([128, 512], F32, f"ps{i}") for i in range(6)]
    psi = [0]

    def nps():
        psi[0] += 1
        return PS[psi[0] % 6]

    W = {}

    def wk(nm, n=2):
        if nm not in W:
            W[nm] = [[kb.sb([128, 512], F32, f"w_{nm}{i}") for i in range(n)], 0]
        W[nm][1] += 1
        return W[nm][0][W[nm][1] % len(W[nm][0])]

    ys5 = kb.sb([128, 8, TOK], BF16, "ys5")
    ygla = kb.sb([128, 8, TOK], BF16, "ygla")
    yatt = kb.sb([128, 8, TOK], BF16, "yatt")
    for kt in range(8):
        kb.dma("pool", yatt[:, kt, :], ya.ap()[kt * 128:(kt + 1) * 128, :], w=[yatt])

    gf = kb.sb([128, 8, TOK], F32, "gf")
    gb = kb.sb([128, 8, TOK], BF16, "gb")
    for kt in range(8):
        for (c0, cn) in BLKS:
            a = wk("a"); b = wk("b"); y = wk("y"); t = wk("t"); s = wk("s")
            kb.dma("sp", a[:, :cn], ys.ap()[0, kt * 128:(kt + 1) * 128, c0:c0 + cn], w=[a])
            kb.dma("act", b[:, :cn], ys.ap()[1, kt * 128:(kt + 1) * 128, c0:c0 + cn], w=[b])
            kb.op("dve", lambda e: e.tensor_tensor(out=y[:, :cn], in0=a[:, :cn], in1=b[:, :cn], op=ALU.add), r=[a, b], w=[y])
            kb.op("dve", lambda e: e.tensor_tensor(out=t[:, :cn], in0=y[:, :cn], in1=y[:, :cn], op=ALU.mult), r=[y], w=[t])
            kb.op("dve", lambda e: e.tensor_scalar(out=t[:, :cn], in0=t[:, :cn], scalar1=0.044715, scalar2=1.0, op0=ALU.mult, op1=ALU.add), r=[t], w=[t])
            kb.op("dve", lambda e: e.tensor_tensor(out=t[:, :cn], in0=t[:, :cn], in1=y[:, :cn], op=ALU.mult), r=[t, y], w=[t])
            kb.op("act", lambda e: e.activation(out=s[:, :cn], in_=t[:, :cn], func=AF.Sigmoid, scale=1.5957691216057308), r=[t], w=[s])
            kb.op("dve", lambda e: e.tensor_tensor(out=gf[:, kt, c0:c0 + cn], in0=y[:, :cn], in1=s[:, :cn], op=ALU.mult), r=[y, s], w=[gf])
            kb.op("dve", lambda e: e.tensor_copy(out=gb[:, kt, c0:c0 + cn], in_=gf[:, kt, c0:c0 + cn]), r=[gf], w=[gb])
    wgs = [kb.sb([128, 8, 128], BF16, f"wg{i}") for i in range(2)]
    wgv = wglu.ap().rearrange("(kt p) n -> p kt n", p=128)
    for nt in range(8):
        wg = wgs[nt % 2]
        kb.dma("pool", wg[:], wgv[:, :, nt * 128:(nt + 1) * 128], w=[wg])
        for (c0, cn) in BLKS:
            ps = nps()
            for kt in range(8):
                kb.op("pe", lambda e, kt=kt: e.matmul(ps[:, :cn], lhsT=wg[:, kt, :], rhs=gb[:, kt, c0:c0 + cn], start=(kt == 0), stop=(kt == 7)),
                      r=[wg, gb], w=[ps])
            s = wk("s")
            kb.op("act", lambda e: e.activation(out=s[:, :cn], in_=ps[:, :cn], func=AF.Sigmoid), r=[ps], w=[s])
            kb.op("dve", lambda e: e.tensor_tensor(out=ys5[:, nt, c0:c0 + cn], in0=gf[:, nt, c0:c0 + cn], in1=s[:, :cn], op=ALU.mult), r=[gf, s], w=[ys5])

    of = gf
    for hd in range(4):
        for (c0, cn) in BLKS:
            ps = nps()
            for v in range(2):
                kt = hd * 2 + v
                a = wk("a"); b = wk("b"); sq = wk("y")
                kb.dma("sp", a[:, :cn], og.ap()[0, kt * 128:(kt + 1) * 128, c0:c0 + cn], w=[a])
                kb.dma("act", b[:, :cn], og.ap()[1, kt * 128:(kt + 1) * 128, c0:c0 + cn], w=[b])
                kb.op("dve", lambda e: e.tensor_tensor(out=of[:, kt, c0:c0 + cn], in0=a[:, :cn], in1=b[:, :cn], op=ALU.add), r=[a, b], w=[of])
                kb.op("act", lambda e: e.activation(out=sq[:, :cn], in_=of[:, kt, c0:c0 + cn], func=AF.Square), r=[of], w=[sq])
                kb.op("pe", lambda e: e.matmul(ps[:, :cn], lhsT=ones[:], rhs=sq[:, :cn], start=(v == 0), stop=(v == 1)), r=[ones, sq], w=[ps])
            sd = wk("t"); rs = wk("s", 3)
            kb.op("act", lambda e: e.activation(out=sd[:, :cn], in_=ps[:, :cn], func=AF.Sqrt, bias=EPS, scale=1.0 / 256), r=[ps], w=[sd])
            kb.op("dve", lambda e: e.reciprocal(out=rs[:, :cn], in_=sd[:, :cn]), r=[sd], w=[rs])
            for v in range(2):
                kt = hd * 2 + v
                r_ = wk("a"); sg = wk("b"); sr = wk("y"); yy = wk("t")
                kb.dma("sp", r_[:, :cn], zr.ap()[kt * 128:(kt + 1) * 128, c0:c0 + cn], w=[r_])
                kb.op("act", lambda e: e.activation(out=sg[:, :cn], in_=r_[:, :cn], func=AF.Sigmoid), r=[r_], w=[sg])
                kb.op("dve", lambda e: e.tensor_tensor(out=sr[:, :cn], in0=r_[:, :cn], in1=sg[:, :cn], op=ALU.mult), r=[r_, sg], w=[sr])
                kb.op("dve", lambda e: e.tensor_tensor(out=yy[:, :cn], in0=of[:, kt, c0:c0 + cn], in1=rs[:, :cn], op=ALU.mult), r=[of, rs], w=[yy])
                kb.op("dve", lambda e: e.scalar_tensor_tensor(out=ygla[:, kt, c0:c0 + cn], in0=yy[:, :cn], scalar=gn[:, kt:kt + 1], in1=sr[:, :cn],
                                                              op0=ALU.mult, op1=ALU.mult), r=[yy, gn, sr], w=[ygla])

    mb = kb.sb([128, KT, TOK], BF16, "mb")
    wbs = [[kb.sb([128, 8, 128], BF16, f"wb{b}_{i}") for i in range(2)] for b in range(3)]
    ysrc = [ys5, ygla, yatt]
    for nt in range(KT):
        wts = []
        for b in range(3):
            wt = wbs[b][nt % 2]
            kb.dma("pool", wt[:], wbr.ap()[b].rearrange("(kt p) n -> p kt n", p=128)[:, :, nt * 128:(nt + 1) * 128], w=[wt])
            wts.append(wt)
        for (c0, cn) in BLKS:
            pss = []
            for b in range(3):
                ps = nps()
                for kt in range(8):
                    kb.op("pe", lambda e, kt=kt, b=b: e.matmul(ps[:, :cn], lhsT=wts[b][:, kt, :], rhs=ysrc[b][:, kt, c0:c0 + cn], start=(kt == 0), stop=(kt == 7)),
                          r=[wts[b], ysrc[b]], w=[ps])
                pss.append(ps)
            acc = wk("acc")
            for b in range(3):
                gt = wk("a"); sg = wk("b"); tm = wk("y")
                kb.dma("sp" if b != 1 else "act", gt[:, :cn], zg.ap()[b * 2048 + nt * 128:b * 2048 + (nt + 1) * 128, c0:c0 + cn], w=[gt])
                kb.op("act", lambda e: e.activation(out=sg[:, :cn], in_=gt[:, :cn], func=AF.Sigmoid), r=[gt], w=[sg])
                if b == 0:
                    kb.op("dve", lambda e: e.tensor_tensor(out=acc[:, :cn], in0=pss[b][:, :cn], in1=sg[:, :cn], op=ALU.mult), r=[pss[b], sg], w=[acc])
                else:
                    kb.op("dve", lambda e: e.tensor_tensor(out=tm[:, :cn], in0=pss[b][:, :cn], in1=sg[:, :cn], op=ALU.mult), r=[pss[b], sg], w=[tm])
                    if b == 1:
                        kb.op("dve", lambda e: e.tensor_tensor(out=acc[:, :cn], in0=acc[:, :cn], in1=tm[:, :cn], op=ALU.add), r=[acc, tm], w=[acc])
                    else:
                        kb.op("dve", lambda e: e.tensor_tensor(out=mb[:, nt, c0:c0 + cn], in0=acc[:, :cn], in1=tm[:, :cn], op=ALU.add), r=[acc, tm], w=[mb])

    wos = [kb.sb([128, KT, 128], BF16, f"wo{i}") for i in range(2)]
    xbs = [kb.sb([128, TOK], F32, f"xb{i}") for i in range(2)]
    wov = wout.ap().rearrange("(kt p) n -> p kt n", p=128)
    for nt in range(KT):
        wo = wos[nt % 2]
        xb = xbs[nt % 2]
        kb.dma("pool", wo[:], wov[:, :, nt * 128:(nt + 1) * 128], w=[wo])
        kb.dma("sp", xb[:], xT.ap()[nt * 128:(nt + 1) * 128, :], w=[xb])
        for bi, (c0, cn) in enumerate(BLKS):
            ps = nps()
            for kt in range(KT):
                kb.op("pe", lambda e, kt=kt: e.matmul(ps[:, :cn], lhsT=wo[:, kt, :], rhs=mb[:, kt, c0:c0 + cn], start=(kt == 0), stop=(kt == KT - 1)),
                      r=[wo, mb], w=[ps])
            mm = m2c if bi == 2 else m2l
            kb.op("dve", lambda e: e.scalar_tensor_tensor(out=xb[:, c0:c0 + cn], in0=ps[:, :cn], scalar=mm[:, nt:nt + 1], in1=xb[:, c0:c0 + cn],
                                                          op0=ALU.mult, op1=ALU.add), r=[ps, mm, xb], w=[xb])
        kb.dma("act", xo.ap()[nt * 128:(nt + 1) * 128, :], xb[:], r=[xb], w=[xo])
    kb.finish()
    return kb


def build_k3b():
    kb = KB()
    xT = kb.dram("xT", [D, TOK], F32, kind="ExternalInput")
    vecs = kb.dram("vecs", [5, 128, KT], F32, kind="ExternalInput")
    rt = kb.dram("rt", [D, 16], F32, kind="ExternalInput")
    h2 = kb.dram("h2", [D, TOK], F32, kind="ExternalOutput")
    aff = kb.dram("aff", [16, TOK], F32, kind="ExternalOutput")
    xs = kb.sb([128, KT, TOK], F32, "xs")
    xv = xT.ap().rearrange("(kt p) t -> p kt t", p=128)
    for kt in range(KT):
        kb.dma("sp" if kt % 2 == 0 else "act", xs[:, kt, :], xv[:, kt, :], w=[xs])
    vt = []
    for i in range(5):
        t = kb.sb([128, KT], F32, f"vec{i}")
        kb.dma("sp", t[:], vecs.ap()[i], w=[t])
        vt.append(t)
    ones = kb.sb([128, 128], F32, "ones")
    kb.op("dve", lambda e: e.memset(ones[:], 1.0), w=[ones])
    hT = kb.sb([128, KT, TOK], F32, "hT")
    emit_norm_mod(kb, xs, hT, vt[0], vt[1], vt[2], vt[3], vt[4], ones)
    h2v = h2.ap().rearrange("(kt p) t -> p kt t", p=128)
    for kt in range(KT):
        kb.dma("sp" if kt % 2 == 0 else "act", h2v[:, kt, :], hT[:, kt, :], r=[hT], w=[h2])
    rs = kb.sb([128, KT, 16], F32, "rs")
    kb.dma("sp", rs[:], rt.ap().rearrange("(kt p) e -> p kt e", p=128), w=[rs])
    pl = [kb.ps([16, 512], F32, f"pl{i}") for i in range(2)]
    pz = [kb.ps([16, 512], F32, f"pz{i}") for i in range(2)]
    ex = kb.sb([16, TOK], F32, "ex")
    rz = kb.sb([16, TOK], F32, "rz")
    af = kb.sb([16, TOK], F32, "af")
    for bi, (c0, cn) in enumerate(BLKS):
        p = pl[bi % 2]
        for kt in range(KT):
            kb.op("pe", lambda e, kt=kt: e.matmul(p[:, :cn], lhsT=rs[:, kt, :], rhs=hT[:, kt, c0:c0 + cn], start=(kt == 0), stop=(kt == KT - 1)),
                  r=[rs, hT], w=[p])
        kb.op("act", lambda e: e.activation(out=ex[:, c0:c0 + cn], in_=p[:, :cn], func=AF.Exp), r=[p], w=[ex])
        z = pz[bi % 2]
        kb.op("pe", lambda e: e.matmul(z[:, :cn], lhsT=ones[0:16, 0:16], rhs=ex[:, c0:c0 + cn], start=True, stop=True), r=[ones, ex], w=[z])
        kb.op("dve", lambda e: e.reciprocal(out=rz[:, c0:c0 + cn], in_=z[:, :cn]), r=[z], w=[rz])
        kb.op("dve", lambda e: e.tensor_tensor(out=af[:, c0:c0 + cn], in0=ex[:, c0:c0 + cn], in1=rz[:, c0:c0 + cn], op=ALU.mult), r=[ex, rz], w=[af])
    kb.dma("sp", aff.ap(), af[:], r=[af], w=[aff])
    kb.finish()
    return kb


D = 2048
NIT = 34
BIG = 1.0e6
SLOTS = 1056
SBLK = [(0, 512), (512, 512), (1024, 32)]


def emit_route(kb, A, ntt, cap, nst, tri, ones, iota_s, tokid, tagp, P):
    def sm(shape, dt=F32, nm=""):
        return kb.sb(shape, dt, tagp + nm)
    lo = sm([128, 1], nm="lo"); hi = sm([128, 1], nm="hi"); mid = sm([128, 1], nm="mid")
    kb.op("dve", lambda e: e.memset(lo[:], 0.0), w=[lo])
    kb.op("dve", lambda e: e.memset(hi[:], 1.0), w=[hi])
    kb.op("dve", lambda e: e.memset(mid[:], 0.5), w=[mid])
    junk = sm([128, ntt], nm="junk"); cnt = sm([128, 1], nm="cnt"); ge = sm([128, 1], nm="ge")
    d1 = sm([128, 1], nm="d1"); d2 = sm([128, 1], nm="d2"); sm_ = sm([128, 1], nm="sm")
    pc = P[0]
    for it in range(NIT):
        kb.op("dve", lambda e: e.tensor_scalar(out=junk[:], in0=A[:], scalar1=mid[:, 0:1], scalar2=0.0, op0=ALU.is_gt, op1=ALU.add, accum_out=cnt[:, 0:1]),
              r=[A, mid], w=[junk, cnt])
        kb.op("pe", lambda e: e.matmul(pc[:, 0:1], lhsT=ones[:], rhs=cnt[:, 0:1], start=True, stop=True), r=[ones, cnt], w=[pc])
        kb.op("dve", lambda e: e.tensor_scalar(out=ge[:], in0=pc[:, 0:1], scalar1=float(cap) - 0.5, scalar2=None, op0=ALU.is_gt), r=[pc], w=[ge])
        kb.op("dve", lambda e: e.tensor_tensor(out=d1[:], in0=mid[:], in1=lo[:], op=ALU.subtract), r=[mid, lo], w=[d1])
        kb.op("dve", lambda e: e.tensor_tensor(out=d2[:], in0=hi[:], in1=mid[:], op=ALU.subtract), r=[hi, mid], w=[d2])
        kb.op("dve", lambda e: e.scalar_tensor_tensor(out=lo[:], in0=d1[:], scalar=ge[:, 0:1], in1=lo[:], op0=ALU.mult, op1=ALU.add), r=[d1, ge, lo], w=[lo])
        kb.op("dve", lambda e: e.scalar_tensor_tensor(out=hi[:], in0=d2[:], scalar=ge[:, 0:1], in1=mid[:], op0=ALU.mult, op1=ALU.add), r=[d2, ge, mid], w=[hi])
        kb.op("dve", lambda e: e.tensor_tensor(out=sm_[:], in0=lo[:], in1=hi[:], op=ALU.add), r=[lo, hi], w=[sm_])
        kb.op("dve", lambda e: e.tensor_scalar(out=mid[:], in0=sm_[:], scalar1=0.5, scalar2=None, op0=ALU.mult), r=[sm_], w=[mid])
    mask = sm([128, ntt], nm="mask")
    kb.op("dve", lambda e: e.tensor_scalar(out=mask[:], in0=A[:], scalar1=lo[:, 0:1], scalar2=None, op0=ALU.is_gt), r=[A, lo], w=[mask])
    pp = P[1]
    kb.op("pe", lambda e: e.matmul(pp[:, 0:ntt], lhsT=tri[:], rhs=mask[:], start=True, stop=True), r=[tri, mask], w=[pp])
    kb.op("pe", lambda e: e.matmul(pp[:, 128:128 + ntt], lhsT=ones[:], rhs=mask[:], start=True, stop=True), r=[ones, mask], w=[pp])
    tot = sm([128, ntt], nm="tot"); cum = sm([128, ntt], nm="cum"); pos = sm([128, ntt], nm="pos")
    onesr = sm([128, ntt], nm="onesr")
    kb.op("dve", lambda e: e.memset(onesr[:], 1.0), w=[onesr])
    kb.op("dve", lambda e: e.tensor_copy(out=tot[:], in_=pp[:, 128:128 + ntt]), r=[pp], w=[tot])
    kb.op("dve", lambda e: e.tensor_tensor_scan(out=cum[:], data0=onesr[:], data1=tot[:], initial=0.0, op0=ALU.mult, op1=ALU.add), r=[onesr, tot], w=[cum])
    kb.op("dve", lambda e: e.tensor_tensor(out=cum[:], in0=cum[:], in1=tot[:], op=ALU.subtract), r=[cum, tot], w=[cum])
    kb.op("dve", lambda e: e.tensor_tensor(out=pos[:], in0=pp[:, 0:ntt], in1=cum[:], op=ALU.add), r=[pp, cum], w=[pos])
    pen = sm([128, ntt], nm="pen"); posm = sm([128, ntt], nm="posm")
    kb.op("dve", lambda e: e.tensor_scalar(out=pen[:], in0=mask[:], scalar1=-BIG, scalar2=BIG, op0=ALU.mult, op1=ALU.add), r=[mask], w=[pen])
    kb.op("dve", lambda e: e.tensor_tensor(out=posm[:], in0=pos[:], in1=pen[:], op=ALU.add), r=[pos, pen], w=[posm])
    posc_ = sm([128, ntt], nm="poscl")
    kb.op("dve", lambda e: e.tensor_scalar(out=posc_[:], in0=posm[:], scalar1=float(cap), scalar2=None, op0=ALU.min), r=[posm], w=[posc_])
    posi = sm([128, ntt], I32, nm="posi")
    kb.op("dve", lambda e: e.tensor_copy(out=posi[:], in_=posc_[:]), r=[posc_], w=[posi])
    ns = nst * 128
    pidx = P[2]
    pgr = [P[3 + i] for i in range((ns + 511) // 512)]
    ohs = [sm([128, ns], nm=f"oh{i}") for i in range(2)]
    arep = [sm([128, 128], nm=f"arep{i}") for i in range(2)]
    for tt in range(ntt):
        oh = ohs[tt % 2]; ar = arep[tt % 2]
        kb.op("dve", lambda e: e.tensor_scalar(out=oh[:], in0=iota_s[:, 0:ns], scalar1=posm[:, tt:tt + 1], scalar2=None, op0=ALU.is_equal),
              r=[iota_s, posm], w=[oh])
        kb.op("act", lambda e: e.activation(out=ar[:], in_=ones[:], func=AF.Copy, scale=A[:, tt:tt + 1]), r=[ones, A], w=[ar])
        for st in range(nst):
            kb.op("pe", lambda e, st=st: e.matmul(pidx[:, st:st + 1], lhsT=oh[:, st * 128:(st + 1) * 128], rhs=tokid[:, tt:tt + 1],
                                                  start=(tt == 0 and st == 0), stop=(tt == ntt - 1 and st == nst - 1), skip_group_check=True), r=[oh, tokid], w=[pidx])
        for gi, pg in enumerate(pgr):
            n0 = gi * 512
            nn = min(512, ns - n0)
            kb.op("pe", lambda e, pg=pg: e.matmul(pg[:, 0:nn], lhsT=ar[:], rhs=oh[:, n0:n0 + nn], start=(tt == 0), stop=(tt == ntt - 1)),
                  r=[ar, oh], w=[pg])
    idxf = sm([128, nst], nm="idxf")
    idxi = sm([128, nst], I32, nm="idxi")
    kb.op("dve", lambda e: e.tensor_copy(out=idxf[:], in_=pidx[:, 0:nst]), r=[pidx], w=[idxf])
    kb.op("dve", lambda e: e.tensor_copy(out=idxi[:], in_=idxf[:]), r=[idxf], w=[idxi])
    grow = sm([128, ns], nm="grow")
    for gi, pg in enumerate(pgr):
        n0 = gi * 512
        nn = min(512, ns - n0)
        kb.op("act", lambda e, pg=pg: e.copy(out=grow[:, n0:n0 + nn], in_=pg[:, 0:nn]), r=[pg], w=[grow])
    return posi, idxi, grow


def build_k4(with_ffn=True):
    kb = KB()
    nc = kb.nc
    affl = kb.dram("affl", [2, 128, 64], F32, kind="ExternalInput")
    affc = kb.dram("affc", [2, 128, 2], F32, kind="ExternalInput")
    h2 = kb.dram("h2", [8192, D], F32, kind="ExternalInput")
    h2c = kb.dram("h2c", [256, D], F32, kind="ExternalInput")
    wg = kb.dram("wg", [2, D, D], F32, kind="ExternalInput")
    wu = kb.dram("wu", [2, D, D], F32, kind="ExternalInput")
    wd = kb.dram("wd", [2, D, D], F32, kind="ExternalInput")
    cst = kb.dram("cst", [128, 128 + 1024 + 64 + 128], F32, kind="ExternalInput")
    yeT = kb.dram("yeT", [2, D, SLOTS], F32, kind="ExternalOutput")
    posl = kb.dram("posl", [2, 128, 64], I32, kind="ExternalOutput")
    posc = kb.dram("posc", [2, 128, 2], I32, kind="ExternalOutput")
    dbg = kb.dram("dbg", [2, 128, 9], I32, kind="ExternalOutput")

    cs_ = kb.sb([128, 1344], F32, "cs_")
    kb.dma("sp", cs_[:], cst.ap(), w=[cs_])
    tri = kb.sb([128, 128], F32, "tri"); iota_s = kb.sb([128, 1024], F32, "iota_s"); tokid = kb.sb([128, 64], F32, "tokid")
    identb = kb.sb([128, 128], BF16, "identb"); ones = kb.sb([128, 128], F32, "ones")
    kb.op("dve", lambda e: e.tensor_copy(out=tri[:], in_=cs_[:, 0:128]), r=[cs_], w=[tri])
    kb.op("dve", lambda e: e.tensor_copy(out=iota_s[:], in_=cs_[:, 128:1152]), r=[cs_], w=[iota_s])
    kb.op("dve", lambda e: e.tensor_copy(out=tokid[:], in_=cs_[:, 1152:1216]), r=[cs_], w=[tokid])
    kb.op("dve", lambda e: e.tensor_copy(out=identb[:], in_=cs_[:, 1216:1344]), r=[cs_], w=[identb])
    kb.op("dve", lambda e: e.memset(ones[:], 1.0), w=[ones])

    P = [kb.ps([128, 512], F32, f"P{i}") for i in range(6)]
    xsT = kb.sb([128, 16, SLOTS], BF16, "xsT")
    actT = kb.sb([128, 16, SLOTS], BF16, "actT")
    xg = [kb.sb([128, D], F32, f"xg{i}") for i in range(2)]
    xgb = [kb.sb([128, D], BF16, f"xgb{i}") for i in range(2)]
    ptr = [kb.ps([128, 1024], BF16, f"ptr{i}") for i in range(2)]
    wgs = [kb.sb([128, 16, 128], BF16, f"wgs{i}") for i in range(2)]
    wus = [kb.sb([128, 16, 128], BF16, f"wus{i}") for i in range(2)]
    sg = [kb.sb([128, 512], F32, f"sg{i}") for i in range(2)]
    tq = [kb.sb([128, 512], F32, f"tq{i}") for i in range(2)]
    ob = [kb.sb([128, SLOTS], F32, f"ob{i}") for i in range(2)]
    for e2 in range(2):
        Al = kb.sb([128, 64], F32, f"Al{e2}")
        Ac = kb.sb([128, 2], F32, f"Ac{e2}")
        kb.dma("sp", Al[:], affl.ap()[e2], w=[Al])
        kb.dma("sp", Ac[:], affc.ap()[e2], w=[Ac])
        posi_l, idx_l, grow_l = emit_route(kb, Al, 64, 1024, 8, tri, ones, iota_s, tokid, f"rl{e2}_", P)
        posi_c, idx_c, grow_c = emit_route(kb, Ac, 2, 32, 1, tri, ones, iota_s, tokid, f"rc{e2}_", P)
        kb.dma("sp", posl.ap()[e2], posi_l[:], r=[posi_l], w=[posl])
        kb.dma("sp", posc.ap()[e2], posi_c[:], r=[posi_c], w=[posc])
        kb.dma("sp", dbg.ap()[e2, :, 0:8], idx_l[:], r=[idx_l], w=[dbg], allow_slow_non_contiguous=True)
        kb.dma("sp", dbg.ap()[e2, :, 8:9], idx_c[:], r=[idx_c], w=[dbg], allow_slow_non_contiguous=True)
        if not with_ffn:
            continue
        for st in range(9):
            b = st % 2
            rows = 128 if st < 8 else 32
            idx_ap = idx_l[:, st:st + 1] if st < 8 else idx_c[0:32, 0:1]
            src = h2.ap() if st < 8 else h2c.ap()
            need = kb._need([idx_l if st < 8 else idx_c], [xg[b]])
            kb._wait("pool", need)
            ins = nc.gpsimd.indirect_dma_start(out=xg[b][0:rows, :], out_offset=None, in_=src,
                                               in_offset=bass.IndirectOffsetOnAxis(ap=idx_ap, axis=0))
            t = xg[b]
            if t.dsem is None:
                t.dsem = kb.st.enter_context(nc.semaphore("d_" + t.name))
            t.dcount += 16
            ins.then_inc(t.dsem, 16)
            key = "d_" + t.name
            (idx_l if st < 8 else idx_c).r[key] = (t.dsem, t.dcount)
            t.w[key] = (t.dsem, t.dcount)
            kb.op("act", lambda e: e.copy(out=xgb[b][0:rows, :], in_=xg[b][0:rows, :]), r=[xg[b]], w=[xgb[b]])
            for half in range(2):
                pt = ptr[half]
                for k8 in range(8):
                    kt = half * 8 + k8
                    kb.op("pe", lambda e, kt=kt, k8=k8: e.transpose(pt[:, k8 * 128:k8 * 128 + rows], xgb[b][0:rows, kt * 128:(kt + 1) * 128], identb[0:rows, 0:rows]),
                          r=[xgb[b], identb], w=[pt])
                kb.op("dve", lambda e: e.tensor_copy(out=xsT[:, half * 8:(half + 1) * 8, st * 128:st * 128 + rows],
                                                     in_=pt[:].rearrange("p (k c) -> p k c", k=8)[:, :, 0:rows]), r=[pt], w=[xsT])
        PA = [P[0], P[1]]
        PU = [P[2], P[3]]
        wgv = wg.ap()[e2].rearrange("(kt p) n -> p kt n", p=128)
        wuv = wu.ap()[e2].rearrange("(kt p) n -> p kt n", p=128)
        wdv = wd.ap()[e2].rearrange("(kt p) n -> p kt n", p=128)
        n = 0
        for ft in range(16):
            wgt = wgs[ft % 2]; wut = wus[ft % 2]
            kb.dma("pool", wgt[:], wgv[:, :, ft * 128:(ft + 1) * 128], w=[wgt])
            kb.dma("pool", wut[:], wuv[:, :, ft * 128:(ft + 1) * 128], w=[wut])
            for (c0, cn) in SBLK:
                pA = PA[n % 2]; pU = PU[n % 2]; s_ = sg[n % 2]; t_ = tq[n % 2]
                n += 1
                for kt in range(16):
                    kb.op("pe", lambda e, kt=kt: e.matmul(pA[:, :cn], lhsT=wgt[:, kt, :], rhs=xsT[:, kt, c0:c0 + cn], start=(kt == 0), stop=(kt == 15)),
                          r=[wgt, xsT], w=[pA])
                for kt in range(16):
                    kb.op("pe", lambda e, kt=kt: e.matmul(pU[:, :cn], lhsT=wut[:, kt, :], rhs=xsT[:, kt, c0:c0 + cn], start=(kt == 0), stop=(kt == 15)),
                          r=[wut, xsT], w=[pU])
                kb.op("act", lambda e: e.activation(out=s_[:, :cn], in_=pA[:, :cn], func=AF.Sigmoid), r=[pA], w=[s_])
                kb.op("dve", lambda e: e.tensor_tensor(out=t_[:, :cn], in0=pA[:, :cn], in1=s_[:, :cn], op=ALU.mult), r=[pA, s_], w=[t_])
                kb.op("dve", lambda e: e.tensor_tensor(out=actT[:, ft, c0:c0 + cn], in0=pU[:, :cn], in1=t_[:, :cn], op=ALU.mult), r=[pU, t_], w=[actT])
        for dt_ in range(16):
            wdt = wgs[dt_ % 2]
            kb.dma("pool", wdt[:], wdv[:, :, dt_ * 128:(dt_ + 1) * 128], w=[wdt])
            o = ob[dt_ % 2]
            for (c0, cn) in SBLK:
                pA = PA[n % 2]
                n += 1
                for kt in range(16):
                    kb.op("pe", lambda e, kt=kt: e.matmul(pA[:, :cn], lhsT=wdt[:, kt, :], rhs=actT[:, kt, c0:c0 + cn], start=(kt == 0), stop=(kt == 15)),
                          r=[wdt, actT], w=[pA])
                if c0 < 1024:
                    kb.op("dve", lambda e: e.tensor_tensor(out=o[:, c0:c0 + cn], in0=pA[:, :cn], in1=grow_l[:, c0:c0 + cn], op=ALU.mult), r=[pA, grow_l], w=[o])
                else:
                    kb.op("dve", lambda e: e.tensor_tensor(out=o[:, c0:c0 + cn], in0=pA[:, :cn], in1=grow_c[:, 0:cn], op=ALU.mult), r=[pA, grow_c], w=[o])
            kb.dma("sp", yeT.ap()[e2, dt_ * 128:(dt_ + 1) * 128, :], o[:], r=[o], w=[yeT])
    kb.finish()
    return kb


D = 2048


def build_k5(final):
    kb = KB()
    nc = kb.nc
    x = kb.dram("x", [1056, D], F32, kind="ExternalInput")
    yl = [kb.dram(f"yl{e}", [1025, D], F32, kind="ExternalInput") for e in range(16)]
    yc = [kb.dram(f"yc{e}", [33, D], F32, kind="ExternalInput") for e in range(16)]
    pos = kb.dram("pos", [128, 9, 16], I32, kind="ExternalInput")
    rep = kb.dram("rep", [3, 128, D], F32, kind="ExternalInput")
    xo = kb.dram("xo", [1056, D], F32, kind="ExternalOutput")
    ps_ = kb.sb([128, 9, 16], I32, "pos_sb")
    kb.dma("sp", ps_[:], pos.ap(), w=[ps_])
    reps = []
    for i in range(3):
        t = kb.sb([128, D], F32, f"rep{i}")
        kb.dma("sp" if i % 2 == 0 else "act", t[:], rep.ap()[i], w=[t])
        reps.append(t)
    G = [kb.sb([128, D], F32, f"g{i}") for i in range(4)]
    ACC = [kb.sb([128, D], F32, f"acc{i}") for i in range(2)]
    XT = [kb.sb([128, D], F32, f"xt{i}") for i in range(2)]
    ss = [kb.sb([128, 1], F32, f"ss{i}") for i in range(2)]
    sd = [kb.sb([128, 1], F32, f"sd{i}") for i in range(2)]
    junk = kb.sb([128, D], F32, "junk")
    gi = 0
    for tl in range(9):
        rows = 128 if tl < 8 else 32
        r0 = tl * 128
        acc = ACC[tl % 2]; xt = XT[tl % 2]
        kb.dma("sp", xt[0:rows, :], x.ap()[r0:r0 + rows, :], w=[xt])
        for e in range(16):
            g = G[gi % 4] if e > 0 else acc
            gi += 1
            src = yl[e] if tl < 8 else yc[e]
            need = kb._need([ps_], [g])
            kb._wait("pool", need)
            ins = nc.gpsimd.indirect_dma_start(out=g[0:rows, :], out_offset=None, in_=src.ap(),
                                               in_offset=bass.IndirectOffsetOnAxis(ap=ps_[0:rows, tl, e:e + 1], axis=0))
            if g.dsem is None:
                g.dsem = kb.st.enter_context(nc.semaphore("d_" + g.name))
            g.dcount += 16
            ins.then_inc(g.dsem, 16)
            key = "d_" + g.name
            ps_.r[key] = (g.dsem, g.dcount)
            g.w[key] = (g.dsem, g.dcount)
            if e > 0:
                eng = "dve"
                kb.op(eng, lambda en: en.tensor_tensor(out=acc[0:rows, :], in0=acc[0:rows, :], in1=g[0:rows, :], op=ALU.add), r=[acc, g], w=[acc])
        m5 = reps[0] if tl < 8 else reps[1]
        kb.op("dve", lambda en: en.tensor_tensor(out=acc[0:rows, :], in0=acc[0:rows, :], in1=m5[0:rows, :], op=ALU.mult), r=[acc, m5], w=[acc])
        kb.op("dve", lambda en: en.tensor_tensor(out=xt[0:rows, :], in0=xt[0:rows, :], in1=acc[0:rows, :], op=ALU.add), r=[xt, acc], w=[xt])
        if final and tl < 8:
            s_ = ss[tl % 2]; d_ = sd[tl % 2]
            kb.op("act", lambda en: en.activation(out=junk[0:rows, :], in_=xt[0:rows, :], func=AF.Square), r=[xt], w=[junk])
            kb.op("dve", lambda en: en.tensor_scalar(out=junk[0:rows, :], in0=junk[0:rows, :], scalar1=1.0, scalar2=0.0, op0=ALU.mult, op1=ALU.add,
                                                     accum_out=s_[0:rows, 0:1]), r=[junk], w=[junk, s_])
            kb.op("act", lambda en: en.activation(out=d_[0:rows, :], in_=s_[0:rows, :], func=AF.Sqrt, bias=1e-6, scale=1.0 / D), r=[s_], w=[d_])
            kb.op("dve", lambda en: en.reciprocal(out=s_[0:rows, :], in_=d_[0:rows, :]), r=[d_], w=[s_])
            kb.op("dve", lambda en: en.scalar_tensor_tensor(out=xt[0:rows, :], in0=xt[0:rows, :], scalar=s_[0:rows, 0:1], in1=reps[2][0:rows, :],
                                                            op0=ALU.mult, op1=ALU.mult), r=[xt, s_, reps[2]], w=[xt])
        kb.dma("act", xo.ap()[r0:r0 + rows, :], xt[0:rows, :], r=[xt], w=[xo])
    kb.finish()
    return kb

import numpy as np
QA0 = 4096
def rope_tables():
    L = 8192
    row = np.repeat(np.arange(L // 64), 64).astype(np.float32)
    col = np.tile(np.arange(64), L // 64).astype(np.float32)
    inv = (np.float32(10000.0) ** (-np.arange(16, dtype=np.float32) / np.float32(16))).astype(np.float32)
    ang = np.concatenate([row[:, None] * inv, col[:, None] * inv], -1).astype(np.float32)
    c = np.cos(ang).astype(np.float32); s = np.sin(ang).astype(np.float32)
    cosT = np.concatenate([c, c], 1).T
    sinT = np.concatenate([-s, s], 1).T
    cs = np.stack([np.concatenate([cosT, cosT], 0), np.concatenate([sinT, sinT], 0)])
    return np.ascontiguousarray(cs.astype(np.float32))
def attn_masks():
    j = np.arange(128)[:, None]; i = np.arange(128)[None, :]
    return np.ascontiguousarray(np.stack([(j >= i), (j <= i)]).astype(np.float32))
def perm64(w, nh):
    sh = w.shape[:-1]
    w = w.reshape(sh + (nh, 2, 32))
    return w[..., ::-1, :].reshape(sh + (nh * 64,))
def attn_inputs(ci, q_all, k_all, v_all, qc_all, kc_all, vc_all, sink_l, cs, masks):
    kvh = ci // 2
    q = q_all[:, ci * 128:(ci + 1) * 128]
    qp = perm64(q, 2)
    k = k_all[:, kvh * 64:(kvh + 1) * 64]; kp = perm64(k, 1)
    kd = np.concatenate([k, k], 1); kpd = np.concatenate([kp, kp], 1)
    qT = np.ascontiguousarray(np.stack([q.T, qp.T])); kT = np.ascontiguousarray(np.stack([kd.T, kpd.T]))
    kc = kc_all[:, kvh * 64:(kvh + 1) * 64]
    cx = np.ascontiguousarray(np.stack([qc_all[:, ci * 128:(ci + 1) * 128].T, np.concatenate([kc, kc], 1).T]))
    v = np.concatenate([v_all[:, kvh * 64:(kvh + 1) * 64], vc_all[:, kvh * 64:(kvh + 1) * 64]], 0)
    vtok = np.ascontiguousarray(v.reshape(66, 128, 64).transpose(1, 0, 2))
    sk = np.ascontiguousarray(np.broadcast_to(sink_l[2 * ci:2 * ci + 2][None, :], (64, 2)).astype(np.float32))
    return {"qT": qT, "kT": kT, "cs": cs, "cx": cx, "vtok": vtok, "masks": masks, "sink": sk}
GQ0 = 1024; GK0 = 1536; GV0 = 2048; GR0 = 3072
def dir_order(a, dd):
    if dd == 0: return a
    return np.concatenate([a[:256][::-1], a[256:][::-1]], 0)
def gla_consts():
    c = np.zeros((128, 704), np.float32)
    m = np.ones(512, np.float32); m[::64] = 0
    c[:, :512] = m
    j = np.arange(64)[:, None]; i = np.arange(64)[None, :]
    c[:64, 512:576] = (j <= i)
    c[:, 576:704] = np.eye(128)
    return c
def gla_inputs(ci, l, qg, kg, vg, hw1_all, d, consts):
    hd = ci // 2; dd = ci % 2
    q = dir_order(qg[:, hd * 128:(hd + 1) * 128], dd); k = dir_order(kg[:, hd * 128:(hd + 1) * 128], dd)
    v = dir_order(vg[:, hd * 256:(hd + 1) * 256], dd)
    hw = dir_order(hw1_all[:, dd * 16:(dd + 1) * 16], dd)
    return {"qk": np.ascontiguousarray(np.stack([q.T, k.T]).astype(np.float32)), "hw1": np.ascontiguousarray(hw.T.astype(np.float32)),
            "w2b": np.ascontiguousarray(d["gla_w2"][l, dd][:, hd * 128:(hd + 1) * 128]),
            "nb": np.ascontiguousarray(d["gla_b"][l, dd, hd * 128:(hd + 1) * 128].reshape(128, 1)),
            "vtok": np.ascontiguousarray(v.reshape(132, 64, 256).transpose(1, 0, 2).astype(np.float32)), "cst": consts}
def moe_consts():
    c = np.zeros((128, 1344), np.float32)
    pj = np.arange(128)
    c[:, 0:128] = (pj[:, None] < pj[None, :])
    c[:, 128:1152] = np.arange(1024)[None, :]
    c[:, 1152:1216] = (np.arange(64)[None, :] * 128 + pj[:, None])
    c[:, 1216:1344] = np.eye(128)
    return c
def moe_inputs(ci, l, aff_lat, aff_ctx, h2_tok, d, consts):
    es = [2 * ci, 2 * ci + 1]
    affl = np.stack([aff_lat[:, e].reshape(64, 128).T for e in es]).astype(np.float32)
    affc = np.stack([aff_ctx[:, e].reshape(2, 128).T for e in es]).astype(np.float32)
    return {"affl": np.ascontiguousarray(affl), "affc": np.ascontiguousarray(affc), "h2": np.ascontiguousarray(h2_tok[:8192]), "h2c": np.ascontiguousarray(h2_tok[8192:]),
            "wg": np.ascontiguousarray(d["moe_w_gate"][l, es[0]:es[0] + 2]), "wu": np.ascontiguousarray(d["moe_w_up"][l, es[0]:es[0] + 2]),
            "wd": np.ascontiguousarray(d["moe_w_down"][l, es[0]:es[0] + 2]), "cst": consts}

QA0_ = 4096
GQ0_, GK0_, GV0_, GR0_ = 1024, 1536, 2048, 3072


def _pvec(v):
    return np.ascontiguousarray(np.asarray(v, np.float32).reshape(-1, 128).T)


def _build_wext(w_in, w1):
    qa = w_in[:, QA0_:QA0_ + 1024]
    ka = w_in[:, QA0_ + 1024:QA0_ + 1280]
    W = np.zeros((2048, NW), np.float32)
    W[:, :11776] = w_in
    W[:, 11776:12800] = perm64(qa, 16)
    W[:, 12800:13056] = perm64(ka, 4)
    W[:, 13056:13072] = w1[0]
    W[:, 13072:13088] = w1[1]
    return W


def _rev_cols(a):
    return np.concatenate([a[:, :256][:, ::-1], a[:, 256:][:, ::-1]], 1)


def _s5_inputs(l, gi, uall_T, p):
    uT = np.ascontiguousarray(np.stack([uall_T, _rev_cols(uall_T)]).astype(np.float32))
    Bblk = np.zeros((2, 2, 4, 128, 128), np.float32)
    Cblk = np.zeros((2, 2, 4, 128, 128), np.float32)
    lam = np.zeros((3, 128, 8), np.float32)
    for dd in range(2):
        for q in range(4):
            for g2 in range(2):
                gl = 2 * q + g2
                g = 8 * gi + gl
                for pi, (bn, cn) in enumerate((("s5_b_re", "s5_c_re"), ("s5_b_im", "s5_c_im"))):
                    Bblk[dd, pi, q, gl * 16:(gl + 1) * 16, g2 * 64:(g2 + 1) * 64] = p[bn][l, dd, g].T
                    Cblk[dd, pi, q, g2 * 64:(g2 + 1) * 64, gl * 16:(gl + 1) * 16] = p[cn][l, dd, g].T
                lam[0, g2 * 64:(g2 + 1) * 64, dd * 4 + q] = p["s5_lam_re"][l, dd, g]
                lam[1, g2 * 64:(g2 + 1) * 64, dd * 4 + q] = p["s5_lam_im"][l, dd, g]
                lam[2, g2 * 64:(g2 + 1) * 64, dd * 4 + q] = p["s5_log_dt"][l, dd, g]
    dvec = np.ascontiguousarray(p["s5_d"][l, gi * 128:(gi + 1) * 128].reshape(128, 1))
    return {"uT": uT, "Bblk": Bblk, "Cblk": Cblk, "lam": lam, "dvec": dvec}


def _run(kb, ims):
    res = run_bass_kernel_spmd(kb.nc, ims, core_ids=list(range(NCORES)))
    return res.results


def kernel(x, c, ctx, c_ctx, ada_w, ada_b, norm1_g, norm2_g, w_in, s5_lam_re, s5_lam_im, s5_log_dt,
           s5_b_re, s5_b_im, s5_c_re, s5_c_im, s5_d, s5_w_glu, gla_w1, gla_w2, gla_b, gla_norm_g,
           attn_sink, w_branch_s5, w_branch_gla, w_branch_attn, w_out, moe_router, moe_w_gate,
           moe_w_up, moe_w_down, final_g):
    p = {k: np.asarray(v, np.float32) for k, v in dict(
        s5_lam_re=s5_lam_re, s5_lam_im=s5_lam_im, s5_log_dt=s5_log_dt, s5_b_re=s5_b_re, s5_b_im=s5_b_im,
        s5_c_re=s5_c_re, s5_c_im=s5_c_im, s5_d=s5_d, gla_w2=gla_w2, gla_b=gla_b, moe_w_gate=moe_w_gate,
        moe_w_up=moe_w_up, moe_w_down=moe_w_down).items()}
    f32 = np.float32
    x_lat = np.asarray(x, f32)[0].copy()
    xc = np.asarray(ctx, f32)[0].copy()
    ada_w = np.asarray(ada_w, f32); ada_b = np.asarray(ada_b, f32)
    cT = np.stack([np.asarray(c, f32)[0], np.asarray(c_ctx, f32)], axis=1)
    cT = np.ascontiguousarray(cT.reshape(16, 128, 2).transpose(1, 0, 2))
    ims = []
    for i in range(NCORES):
        aw = np.ascontiguousarray(ada_w[:, :, i * 1536:(i + 1) * 1536])
        ab = np.ascontiguousarray(ada_b[:, i * 1536:(i + 1) * 1536].reshape(2, 12, 128).transpose(0, 2, 1))
        ims.append({"cT": cT, "adaw": aw, "adab": ab})
    r = _run(build_k0(), ims)
    mod = np.zeros((2, 12288, 2), f32)
    for i in range(NCORES):
        mod[:, i * 1536:(i + 1) * 1536, :] = r[i]["modT"].transpose(0, 2, 1, 3).reshape(2, 1536, 2)
    cs_tab = rope_tables(); amask = attn_masks(); gconst = gla_consts(); mconst = moe_consts()

    def shard_cols(A, ci):
        return np.ascontiguousarray(np.concatenate([A[:, 256 + ci * 1024:256 + (ci + 1) * 1024], A[:, ci * 32:(ci + 1) * 32]], 1))

    for l in range(2):
        m = mod[l].reshape(6, 2048, 2)
        W = _build_wext(np.asarray(w_in[l], f32), np.asarray(gla_w1[l], f32))
        vecs = np.stack([_pvec(norm1_g[l]), _pvec(m[1, :, 0]), _pvec(m[0, :, 0]), _pvec(m[1, :, 1]), _pvec(m[0, :, 1])])
        xTs = [np.ascontiguousarray(np.concatenate([x_lat[i * 1024:(i + 1) * 1024], xc[i * 32:(i + 1) * 32]], 0).T) for i in range(NCORES)]
        r = _run(build_k1(), [{"xT": xTs[i], "W": W, "vecs": vecs} for i in range(NCORES)])
        ZT = np.empty((NW, 8448), f32)
        for i in range(NCORES):
            z = r[i]["zT"]
            ZT[:, 256 + i * 1024:256 + (i + 1) * 1024] = z[:, :1024]
            ZT[:, i * 32:(i + 1) * 32] = z[:, 1024:]
        del r, W
        r = _run(build_k2a(), [_s5_inputs(l, gi, ZT[gi * 128:(gi + 1) * 128], p) for gi in range(NCORES)])
        YS = np.empty((2, 1024, 8448), f32)
        for gi in range(NCORES):
            YS[0, gi * 128:(gi + 1) * 128] = r[gi]["yT"][0]
            YS[1, gi * 128:(gi + 1) * 128] = _rev_cols(r[gi]["yT"][1])
        ims = []
        for ci in range(NCORES):
            hd, dd = ci // 2, ci % 2
            od = (lambda a: a) if dd == 0 else _rev_cols
            q = od(ZT[GQ0_ + hd * 128:GQ0_ + (hd + 1) * 128]); k = od(ZT[GK0_ + hd * 128:GK0_ + (hd + 1) * 128])
            v = od(ZT[GV0_ + hd * 256:GV0_ + (hd + 1) * 256])
            hw = od(ZT[13056 + dd * 16:13056 + (dd + 1) * 16])
            ims.append({"qk": np.ascontiguousarray(np.stack([q, k])), "hw1": np.ascontiguousarray(hw),
                        "w2b": np.ascontiguousarray(p["gla_w2"][l, dd][:, hd * 128:(hd + 1) * 128]),
                        "nb": np.ascontiguousarray(p["gla_b"][l, dd, hd * 128:(hd + 1) * 128].reshape(128, 1)),
                        "vtok": np.ascontiguousarray(v.T.reshape(132, 64, 256).transpose(1, 0, 2)), "cst": gconst})
        r = _run(build_k2b(), ims)
        OG = np.empty((2, 1024, 8448), f32)
        for ci in range(NCORES):
            hd, dd = ci // 2, ci % 2
            o = r[ci]["oT"].reshape(256, 8448)
            OG[dd, hd * 256:(hd + 1) * 256] = o if dd == 0 else _rev_cols(o)
        ims = []
        for ci in range(NCORES):
            kvh = ci // 2
            lat = slice(256, 8448); cx_ = slice(0, 256)
            q = ZT[QA0_ + ci * 128:QA0_ + (ci + 1) * 128]; qp = ZT[11776 + ci * 128:11776 + (ci + 1) * 128]
            k = ZT[QA0_ + 1024 + kvh * 64:QA0_ + 1024 + (kvh + 1) * 64]; kp = ZT[12800 + kvh * 64:12800 + (kvh + 1) * 64]
            v = ZT[QA0_ + 1280 + kvh * 64:QA0_ + 1280 + (kvh + 1) * 64]
            qT = np.ascontiguousarray(np.stack([q[:, lat], qp[:, lat]]))
            kT = np.ascontiguousarray(np.stack([np.concatenate([k[:, lat], k[:, lat]], 0), np.concatenate([kp[:, lat], kp[:, lat]], 0)]))
            cxx = np.ascontiguousarray(np.stack([q[:, cx_], np.concatenate([k[:, cx_], k[:, cx_]], 0)]))
            vt = np.concatenate([v[:, lat], v[:, cx_]], 1).T
            vtok = np.ascontiguousarray(vt.reshape(66, 128, 64).transpose(1, 0, 2))
            sk = np.ascontiguousarray(np.broadcast_to(np.asarray(attn_sink[l], f32)[2 * ci:2 * ci + 2][None, :], (64, 2)))
            ims.append({"qT": qT, "kT": kT, "cs": cs_tab, "cx": cxx, "vtok": vtok, "masks": amask, "sink": sk})
        r = _run(build_k2c(), ims)
        YA = np.empty((1024, 8448), f32)
        for ci in range(NCORES):
            YA[ci * 128:(ci + 1) * 128, 256:] = r[ci]["yT"]
            YA[ci * 128:(ci + 1) * 128, :256] = r[ci]["ycT"]
        gng = np.zeros(2048, f32); gng[:1024] = np.asarray(gla_norm_g[l], f32).reshape(-1)
        vecs = np.stack([_pvec(gng), _pvec(m[2, :, 0]), _pvec(m[2, :, 1])])
        wbr = np.ascontiguousarray(np.stack([np.asarray(w_branch_s5[l], f32), np.asarray(w_branch_gla[l], f32), np.asarray(w_branch_attn[l], f32)]))
        wgl = np.ascontiguousarray(np.asarray(s5_w_glu[l], f32)); wo_ = np.ascontiguousarray(np.asarray(w_out[l], f32))
        ims = []
        for ci in range(NCORES):
            ims.append({"ys": np.stack([shard_cols(YS[0], ci), shard_cols(YS[1], ci)]), "og": np.stack([shard_cols(OG[0], ci), shard_cols(OG[1], ci)]),
                        "ya": shard_cols(YA, ci), "zr": shard_cols(ZT[GR0_:GR0_ + 1024], ci), "zg": shard_cols(ZT[5632:11776], ci),
                        "xT": xTs[ci], "wglu": wgl, "wbr": wbr, "wout": wo_, "vecs": vecs})
        r = _run(build_k3a(), ims)
        xmidT = [r[ci]["xo"] for ci in range(NCORES)]
        del ims, YS, OG, YA, ZT
        vecs = np.stack([_pvec(norm2_g[l]), _pvec(m[4, :, 0]), _pvec(m[3, :, 0]), _pvec(m[4, :, 1]), _pvec(m[3, :, 1])])
        rt = np.ascontiguousarray(np.asarray(moe_router[l], f32))
        r = _run(build_k3b(), [{"xT": xmidT[ci], "vecs": vecs, "rt": rt} for ci in range(NCORES)])
        h2l = np.empty((8192, 2048), f32); h2c = np.empty((256, 2048), f32)
        affl = np.empty((8192, 16), f32); affc = np.empty((256, 16), f32)
        for ci in range(NCORES):
            h = r[ci]["h2"]; a = r[ci]["aff"]
            h2l[ci * 1024:(ci + 1) * 1024] = h[:, :1024].T; h2c[ci * 32:(ci + 1) * 32] = h[:, 1024:].T
            affl[ci * 1024:(ci + 1) * 1024] = a[:, :1024].T; affc[ci * 32:(ci + 1) * 32] = a[:, 1024:].T
        ims = []
        for ci in range(NCORES):
            es = [2 * ci, 2 * ci + 1]
            ims.append({"affl": np.ascontiguousarray(np.stack([affl[:, e].reshape(64, 128).T for e in es])),
                        "affc": np.ascontiguousarray(np.stack([affc[:, e].reshape(2, 128).T for e in es])),
                        "h2": h2l, "h2c": h2c,
                        "wg": np.ascontiguousarray(p["moe_w_gate"][l, es[0]:es[0] + 2]), "wu": np.ascontiguousarray(p["moe_w_up"][l, es[0]:es[0] + 2]),
                        "wd": np.ascontiguousarray(p["moe_w_down"][l, es[0]:es[0] + 2]), "cst": mconst})
        r = _run(build_k4(), ims)
        del ims
        yl = []; yc = []
        posl = np.empty((16, 128, 64), np.int32); posc = np.empty((16, 128, 2), np.int32)
        zrow = np.zeros((1, 2048), f32)
        for ci in range(NCORES):
            for e2 in range(2):
                e = 2 * ci + e2
                yt = r[ci]["yeT"][e2]
                yl.append(np.ascontiguousarray(np.concatenate([yt[:, :1024].T, zrow], 0)))
                yc.append(np.ascontiguousarray(np.concatenate([yt[:, 1024:1056].T, zrow], 0)))
                posl[e] = r[ci]["posl"][e2]; posc[e] = r[ci]["posc"][e2]
        rep = np.ascontiguousarray(np.stack([np.broadcast_to(m[5, :, 0], (128, 2048)), np.broadcast_to(m[5, :, 1], (128, 2048)),
                                             np.broadcast_to(np.asarray(final_g, f32), (128, 2048))]).astype(f32))
        ims = []
        for cj in range(NCORES):
            pos = np.full((128, 9, 16), 32, np.int32)
            pos[:, 0:8, :] = posl[:, :, 8 * cj:8 * cj + 8].transpose(1, 2, 0)
            pos[0:32, 8, :] = posc[:, (cj % 4) * 32:(cj % 4) * 32 + 32, cj // 4].T
            im = {"x": np.ascontiguousarray(xmidT[cj].T), "pos": np.ascontiguousarray(pos), "rep": rep}
            for e in range(16):
                im[f"yl{e}"] = yl[e]; im[f"yc{e}"] = yc[e]
            ims.append(im)
        r = _run(build_k5(l == 1), ims)
        del ims
        for cj in range(NCORES):
            xo = r[cj]["xo"]
            x_lat[cj * 1024:(cj + 1) * 1024] = xo[:1024]
            xc[cj * 32:(cj + 1) * 32] = xo[1024:]
    return x_lat.reshape(1, 8192, 2048).astype(np.float32)
```

```python
import numpy as np
import ml_dtypes
from contextlib import ExitStack
import concourse.bass as bass
import concourse.mybir as mybir
from concourse.bass_utils import run_bass_kernel_spmd

F32 = mybir.dt.float32
BF16 = mybir.dt.bfloat16
I32 = mybir.dt.int32
U32 = mybir.dt.uint32
AF = mybir.ActivationFunctionType
ALU = mybir.AluOpType
AX = mybir.AxisListType
NPBF16 = ml_dtypes.bfloat16
NCORES = 8


class T:
    def __init__(self, name, h):
        self.name = name
        self.h = h
        self.w = {}
        self.r = {}
        self.gen_need = {}
        self.dsem = None
        self.dcount = 0

    def __getitem__(self, idx):
        return self.h[idx]

    def ap(self):
        return self.h.ap() if hasattr(self.h, "ap") else self.h[:]


class KB:
    def __init__(self, same_engine_sync=True):
        self.nc = bass.Bass("TRN2", target_bir_lowering=False)
        nc = self.nc
        self.st = ExitStack()
        self.E = {"pe": nc.tensor, "act": nc.scalar, "dve": nc.vector, "pool": nc.gpsimd, "sp": nc.sync}
        self.sem = {}
        self.cnt = {}
        self.known = {}
        for e in self.E:
            self.sem[e] = self.st.enter_context(nc.semaphore("s_" + e))
            self.cnt[e] = 0
            self.known[e] = {}
        self.qsem = {}
        self.qcnt = {}
        for q in ("sp", "act", "pool"):
            self.qsem[q] = self.st.enter_context(nc.semaphore("q_" + q))
            self.qcnt[q] = 0
        self.ses = same_engine_sync
        self.outs = []
        self.nid = 0

    def sb(self, shape, dt=F32, name=None):
        self.nid += 1
        name = name or f"t{self.nid}"
        h = self.st.enter_context(self.nc.sbuf_tensor(name, list(shape), dt))
        return T(name, h)

    def ps(self, shape, dt=F32, name=None):
        self.nid += 1
        name = name or f"p{self.nid}"
        h = self.st.enter_context(self.nc.psum_tensor(name, list(shape), dt))
        return T(name, h)

    def dram(self, name, shape, dt=F32, kind="Internal"):
        h = self.nc.dram_tensor(name, list(shape), dt, kind=kind)
        t = T(name, h)
        if kind == "ExternalOutput":
            self.outs.append(t)
        return t

    def _need(self, r, w, dkey=None):
        need = {}

        def add(key, sem, val):
            if key not in need or need[key][1] < val:
                need[key] = (sem, val)

        for t in r:
            for k, (s, v) in t.w.items():
                add(k, s, v)
        for t in w:
            for k, (s, v) in t.w.items():
                if dkey is not None and k == dkey:
                    continue
                add(k, s, v)
            for k, (s, v) in t.r.items():
                add(k, s, v)
        return need

    def _wait(self, e, need):
        eng = self.E[e]
        for key, (sem, val) in need.items():
            if self.known[e].get(key, 0) >= val:
                continue
            if key == e and (not self.ses or e == "pe"):
                continue
            eng.wait_ge(sem, val)
            self.known[e][key] = val

    def op(self, e, fn, r=(), w=()):
        need = self._need(r, w)
        self._wait(e, need)
        ins = fn(self.E[e])
        self.cnt[e] += 1
        ins.then_inc(self.sem[e], 1)
        ev = (self.sem[e], self.cnt[e])
        for t in r:
            t.r[e] = ev
        for t in w:
            t.w[e] = ev
        return ins

    def dma(self, q, out_ap, in_ap, r=(), w=(), **kw):
        wt = w[0] if w else None
        dkey = None
        if wt is not None and wt.dsem is None:
            wt.dsem = self.st.enter_context(self.nc.semaphore("d_" + wt.name))
        if wt is not None:
            dkey = "d_" + wt.name
        need = self._need(r, w, dkey)
        self._wait(q, need)
        ins = self.E[q].dma_start(out=out_ap, in_=in_ap, **kw)
        if wt is not None:
            wt.dcount += 16
            sem, val, key = wt.dsem, wt.dcount, dkey
        else:
            self.qcnt[q] += 16
            sem, val, key = self.qsem[q], self.qcnt[q], "q_" + q
        ins.then_inc(sem, 16)
        for t in r:
            t.r[key] = (sem, val)
        for t in w:
            t.w[key] = (sem, val)
        return ins

    def finish(self):
        need = {}
        for t in self.outs:
            for k, (s, v) in t.w.items():
                if k not in need or need[k][1] < v:
                    need[k] = (s, v)
        self._wait("sp", need)
        for q in self.qsem:
            if self.qcnt[q]:
                self.E["sp"].wait_ge(self.qsem[q], self.qcnt[q])
        return self.nc


def run(kb, in_maps, trace=False):
    res = run_bass_kernel_spmd(kb.nc, in_maps, core_ids=list(range(len(in_maps))), trace=trace)
    return res


D = 2048
KT = 16
NW = 13184
TOK = 1056
BLKS = [(0, 512), (512, 512), (1024, 32)]
EPS = 1e-6


def build_k0():
    kb = KB()
    cT = kb.dram("cT", [128, KT, 2], F32, kind="ExternalInput")
    adaw = kb.dram("adaw", [2, D, 1536], F32, kind="ExternalInput")
    adab = kb.dram("adab", [2, 128, 12], F32, kind="ExternalInput")
    modT = kb.dram("modT", [2, 128, 12, 2], F32, kind="ExternalOutput")
    c_sb = kb.sb([128, KT, 2], F32, "c_sb")
    sc = kb.sb([128, KT, 2], F32, "sc")
    kb.dma("sp", c_sb[:], cT.ap(), w=[c_sb])
    kb.op("act", lambda e: e.activation(out=sc[:], in_=c_sb[:], func=AF.Silu), r=[c_sb], w=[sc])
    wts = [kb.sb([128, KT, 1536], F32, f"w{l}") for l in range(2)]
    for l in range(2):
        src = adaw.ap()[l].rearrange("(kt p) n -> p kt n", p=128)
        for kt in range(KT):
            kb.dma("sp" if kt % 2 == 0 else "act", wts[l][:, kt, :], src[:, kt, :], w=[wts[l]])
    for l in range(2):
        b_sb = kb.sb([128, 12], F32, f"b{l}")
        kb.dma("sp", b_sb[:], adab.ap()[l], w=[b_sb])
        pt = kb.ps([128, 12, 2], F32, f"pm{l}")
        for j in range(12):
            for kt in range(KT):
                kb.op("pe", lambda e, j=j, kt=kt: e.matmul(pt[:, j, :], lhsT=wts[l][:, kt, j * 128:(j + 1) * 128],
                                                            rhs=sc[:, kt, :], start=(kt == 0), stop=(kt == KT - 1)),
                      r=[wts[l], sc], w=[pt])
        o = kb.sb([128, 12, 2], F32, f"o{l}")
        for col in range(2):
            kb.op("dve", lambda e, col=col: e.tensor_tensor(out=o[:, :, col], in0=pt[:, :, col], in1=b_sb[:], op=ALU.add),
                  r=[pt, b_sb], w=[o])
        kb.dma("sp", modT.ap()[l], o[:], r=[o], w=[modT])
    kb.finish()
    return kb


def emit_norm_mod(kb, xs, hT, g_sb, sc_l, sh_l, sc_c, sh_c, ones, tagp=""):
    A_l = kb.sb([128, KT], F32, tagp + "A_l")
    A_c = kb.sb([128, KT], F32, tagp + "A_c")
    kb.op("dve", lambda e: e.scalar_tensor_tensor(out=A_l[:], in0=sc_l[:], scalar=1.0, in1=g_sb[:], op0=ALU.add, op1=ALU.mult),
          r=[sc_l, g_sb], w=[A_l])
    kb.op("dve", lambda e: e.scalar_tensor_tensor(out=A_c[:], in0=sc_c[:], scalar=1.0, in1=g_sb[:], op0=ALU.add, op1=ALU.mult),
          r=[sc_c, g_sb], w=[A_c])
    rstd = kb.sb([128, TOK], F32, tagp + "rstd")
    sqs = [kb.sb([128, 512], F32, tagp + f"sq{i}") for i in range(2)]
    pss = [kb.ps([128, 512], F32, tagp + f"pss{i}") for i in range(2)]
    n = 0
    for bi, (c0, cn) in enumerate(BLKS):
        ps = pss[bi % 2]
        for kt in range(KT):
            sq = sqs[n % 2]
            n += 1
            kb.op("act", lambda e, kt=kt, sq=sq: e.activation(out=sq[:, :cn], in_=xs[:, kt, c0:c0 + cn], func=AF.Square),
                  r=[xs], w=[sq])
            kb.op("pe", lambda e, kt=kt, sq=sq: e.matmul(ps[:, :cn], lhsT=ones[:], rhs=sq[:, :cn], start=(kt == 0), stop=(kt == KT - 1)),
                  r=[ones, sq], w=[ps])
        sd = sqs[n % 2]
        n += 1
        kb.op("act", lambda e: e.activation(out=sd[:, :cn], in_=ps[:, :cn], func=AF.Sqrt, bias=EPS, scale=1.0 / D),
              r=[ps], w=[sd])
        kb.op("dve", lambda e: e.reciprocal(out=rstd[:, c0:c0 + cn], in_=sd[:, :cn]), r=[sd], w=[rstd])
    tmps = [kb.sb([128, 512], F32, tagp + f"tmp{i}") for i in range(2)]
    n = 0
    for bi, (c0, cn) in enumerate(BLKS):
        A = A_c if bi == 2 else A_l
        Bt = sh_c if bi == 2 else sh_l
        for kt in range(KT):
            tmp = tmps[n % 2]
            n += 1
            kb.op("dve", lambda e, kt=kt, tmp=tmp: e.tensor_tensor(out=tmp[:, :cn], in0=xs[:, kt, c0:c0 + cn], in1=rstd[:, c0:c0 + cn], op=ALU.mult),
                  r=[xs, rstd], w=[tmp])
            kb.op("act", lambda e, kt=kt, tmp=tmp, A=A, Bt=Bt: e.activation(out=hT[:, kt, c0:c0 + cn], in_=tmp[:, :cn], func=AF.Identity,
                                                                    bias=Bt[:, kt:kt + 1], scale=A[:, kt:kt + 1]),
                  r=[tmp, A, Bt], w=[hT])


def emit_linear(kb, hT, W, ntiles, out_dram, kt_n=KT, tagp="", evac=None):
    wbs = [kb.sb([128, kt_n, 128], BF16, tagp + f"wb{i}") for i in range(3)]
    pps = [kb.ps([128, 512], F32, tagp + f"pp{i}") for i in range(4)]
    obs = [kb.sb([128, TOK], F32, tagp + f"ob{i}") for i in range(2)]
    Wv = W.ap().rearrange("(kt p) n -> p kt n", p=128)
    pi = 0
    for nt in range(ntiles):
        wb = wbs[nt % 3]
        kb.dma("pool", wb[:], Wv[:, :, nt * 128:(nt + 1) * 128], w=[wb])
        ob = obs[nt % 2]
        for bi, (c0, cn) in enumerate(BLKS):
            pp = pps[pi % 4]
            pi += 1
            for kt in range(kt_n):
                kb.op("pe", lambda e, kt=kt, pp=pp, wb=wb: e.matmul(pp[:, :cn], lhsT=wb[:, kt, :], rhs=hT[:, kt, c0:c0 + cn],
                                                             start=(kt == 0), stop=(kt == kt_n - 1)),
                      r=[wb, hT], w=[pp])
            eng = "act" if pi % 2 == 0 else "dve"
            if eng == "act":
                kb.op("act", lambda e, pp=pp, ob=ob: e.copy(out=ob[:, c0:c0 + cn], in_=pp[:, :cn]), r=[pp], w=[ob])
            else:
                kb.op("dve", lambda e, pp=pp, ob=ob: e.tensor_copy(out=ob[:, c0:c0 + cn], in_=pp[:, :cn]), r=[pp], w=[ob])
        kb.dma("sp", out_dram.ap()[nt * 128:(nt + 1) * 128, :], ob[:], r=[ob], w=[out_dram])


def build_k1(ntiles=NW // 128):
    kb = KB()
    xT = kb.dram("xT", [D, TOK], F32, kind="ExternalInput")
    W = kb.dram("W", [D, ntiles * 128], F32, kind="ExternalInput")
    vecs = kb.dram("vecs", [5, 128, KT], F32, kind="ExternalInput")
    zT = kb.dram("zT", [ntiles * 128, TOK], F32, kind="ExternalOutput")
    xs = kb.sb([128, KT, TOK], F32, "xs")
    xv = xT.ap().rearrange("(kt p) t -> p kt t", p=128)
    for kt in range(KT):
        kb.dma("sp" if kt % 2 == 0 else "act", xs[:, kt, :], xv[:, kt, :], w=[xs])
    vt = []
    for i in range(5):
        t = kb.sb([128, KT], F32, f"vec{i}")
        kb.dma("sp", t[:], vecs.ap()[i], w=[t])
        vt.append(t)
    ones = kb.sb([128, 128], F32, "ones")
    kb.op("dve", lambda e: e.memset(ones[:], 1.0), w=[ones])
    hT = kb.sb([128, KT, TOK], BF16, "hT")
    emit_norm_mod(kb, xs, hT, vt[0], vt[1], vt[2], vt[3], vt[4], ones)
    emit_linear(kb, hT, W, ntiles, zT)
    kb.finish()
    return kb

import math

LTOT = 8448
TC = 256
NCH = LTOT // TC
NTD = 8


def build_k2a():
    kb = KB()
    uT = kb.dram("uT", [2, 128, LTOT], F32, kind="ExternalInput")
    Bblk = kb.dram("Bblk", [2, 2, 4, 128, 128], F32, kind="ExternalInput")
    Cblk = kb.dram("Cblk", [2, 2, 4, 128, 128], F32, kind="ExternalInput")
    lam = kb.dram("lam", [3, 128, NTD], F32, kind="ExternalInput")
    dvec = kb.dram("dvec", [128, 1], F32, kind="ExternalInput")
    yT = kb.dram("yT", [2, 128, LTOT], F32, kind="ExternalOutput")

    ubf = [kb.sb([128, LTOT], BF16, f"ubf{d}") for d in range(2)]
    for d in range(2):
        for c0 in range(0, LTOT, 2048):
            cn = min(2048, LTOT - c0)
            kb.dma("pool", ubf[d][:, c0:c0 + cn], uT.ap()[d, :, c0:c0 + cn], w=[ubf[d]])
    dv = kb.sb([128, 1], F32, "dv")
    kb.dma("sp", dv[:], dvec.ap(), w=[dv])
    Bb = kb.sb([128, 2, 2, 4, 128], BF16, "Bb")
    for d in range(2):
        for p in range(2):
            kb.dma("pool", Bb[:, d, p], Bblk.ap()[d, p].rearrange("q k n -> k q n"), w=[Bb])
    Cf = kb.sb([128, 2, 2, 4, 128], F32, "Cf")
    for d in range(2):
        for p in range(2):
            kb.dma("act", Cf[:, d, p], Cblk.ap()[d, p].rearrange("q k n -> k q n"), w=[Cf])
    Cb = kb.sb([128, 2, 2, 4, 128], BF16, "Cb")
    Cn = kb.sb([128, 2, 4, 128], BF16, "Cn")
    for d in range(2):
        kb.op("dve", lambda e, d=d: e.tensor_scalar(out=Cn[:, d], in0=Cf[:, d, 0], scalar1=-1.0, scalar2=None, op0=ALU.mult), r=[Cf], w=[Cn])
        kb.op("dve", lambda e, d=d: e.tensor_copy(out=Cb[:, d, 0], in_=Cf[:, d, 0]), r=[Cf], w=[Cb])
        kb.op("dve", lambda e, d=d: e.tensor_scalar(out=Cb[:, d, 1], in0=Cf[:, d, 1], scalar1=-1.0, scalar2=None, op0=ALU.mult),
              r=[Cf], w=[Cb])
    lr = kb.sb([128, NTD], F32, "lr")
    li = kb.sb([128, NTD], F32, "li")
    ldt = kb.sb([128, NTD], F32, "ldt")
    kb.dma("sp", lr[:], lam.ap()[0], w=[lr])
    kb.dma("sp", li[:], lam.ap()[1], w=[li])
    kb.dma("sp", ldt[:], lam.ap()[2], w=[ldt])

    nid = [0]

    def sm(name=None):
        nid[0] += 1
        return kb.sb([128, NTD], F32, name or f"sm{nid[0]}")

    def tt(out, a, b, op):
        kb.op("dve", lambda e: e.tensor_tensor(out=out[:], in0=a[:], in1=b[:], op=op), r=[a, b], w=[out])

    def ts(out, a, s1, op0, s2=None, op1=None):
        if op1 is None:
            kb.op("dve", lambda e: e.tensor_scalar(out=out[:], in0=a[:], scalar1=s1, scalar2=None, op0=op0), r=[a], w=[out])
        else:
            kb.op("dve", lambda e: e.tensor_scalar(out=out[:], in0=a[:], scalar1=s1, scalar2=s2, op0=op0, op1=op1), r=[a], w=[out])

    dt = sm("dt")
    kb.op("act", lambda e: e.activation(out=dt[:], in_=ldt[:], func=AF.Exp), r=[ldt], w=[dt])
    lrd = sm()
    tt(lrd, lr, dt, ALU.mult)
    mag = sm("mag")
    kb.op("act", lambda e: e.activation(out=mag[:], in_=lrd[:], func=AF.Exp), r=[lrd], w=[mag])
    ang = sm("ang")
    tt(ang, li, dt, ALU.mult)
    kf = sm()
    ts(kf, ang, 1.0 / (2 * math.pi), ALU.mult)
    ki = kb.sb([128, NTD], I32, "ki")
    kb.op("dve", lambda e: e.tensor_copy(out=ki[:], in_=kf[:]), r=[kf], w=[ki])
    kf2 = sm()
    kb.op("dve", lambda e: e.tensor_copy(out=kf2[:], in_=ki[:]), r=[ki], w=[kf2])
    C1 = 6.28125
    C2 = 2 * math.pi - C1
    r1 = sm()
    kb.op("dve", lambda e: e.scalar_tensor_tensor(out=r1[:], in0=kf2[:], scalar=-C1, in1=ang[:], op0=ALU.mult, op1=ALU.add),
          r=[kf2, ang], w=[r1])
    r2 = sm()
    kb.op("dve", lambda e: e.scalar_tensor_tensor(out=r2[:], in0=kf2[:], scalar=-C2, in1=r1[:], op0=ALU.mult, op1=ALU.add),
          r=[kf2, r1], w=[r2])
    xx = sm("xx")
    ts(xx, r2, 0.125, ALU.mult)
    x2 = sm("x2")
    tt(x2, xx, xx, ALU.mult)

    def horner(coefs):
        p = sm()
        ts(p, x2, -1.0 / coefs[-1], ALU.mult, 1.0, ALU.add)
        for cf in reversed(coefs[:-1]):
            q_ = sm()
            tt(q_, p, x2, ALU.mult)
            p = sm()
            ts(p, q_, -1.0 / cf, ALU.mult, 1.0, ALU.add)
        return p

    ps_ = horner([6.0, 20.0, 42.0, 72.0, 110.0, 156.0])
    sn = sm("sn")
    tt(sn, ps_, xx, ALU.mult)
    cs = horner([2.0, 12.0, 30.0, 56.0, 90.0, 132.0])
    for _ in range(3):
        c2 = sm(); s2 = sm(); sc_ = sm()
        tt(c2, cs, cs, ALU.mult)
        tt(s2, sn, sn, ALU.mult)
        tt(sc_, sn, cs, ALU.mult)
        cs = sm(); sn = sm()
        tt(cs, c2, s2, ALU.subtract)
        ts(sn, sc_, 2.0, ALU.mult)
    ab_re = sm("ab_re"); ab_im = sm("ab_im")
    tt(ab_re, mag, cs, ALU.mult)
    tt(ab_im, mag, sn, ALU.mult)
    den = sm(); t_a = sm(); t_b = sm()
    tt(t_a, lr, lr, ALU.mult)
    tt(t_b, li, li, ALU.mult)
    tt(den, t_a, t_b, ALU.add)
    rden = sm()
    kb.op("dve", lambda e: e.reciprocal(out=rden[:], in_=den[:]), r=[den], w=[rden])
    nr = sm()
    ts(nr, ab_re, -1.0, ALU.add)
    f_re = sm("f_re"); f_im = sm("f_im")
    u1 = sm(); u2 = sm(); u3 = sm()
    tt(u1, nr, lr, ALU.mult)
    tt(u2, ab_im, li, ALU.mult)
    tt(u3, u1, u2, ALU.add)
    tt(f_re, u3, rden, ALU.mult)
    v1 = sm(); v2 = sm(); v3 = sm()
    tt(v1, ab_im, lr, ALU.mult)
    tt(v2, nr, li, ALU.mult)
    tt(v3, v1, v2, ALU.subtract)
    tt(f_im, v3, rden, ALU.mult)
    nsn_unused = None

    Er = kb.sb([128, NTD, TC + 1], F32, "Er")
    Ei = kb.sb([128, NTD, TC + 1], F32, "Ei")
    kb.op("dve", lambda e: e.memset(Er[:, :, 0:1], 1.0), w=[Er])
    kb.op("dve", lambda e: e.memset(Ei[:, :, 0:1], 0.0), w=[Ei])
    pr, pi_ = cs, sn
    n = 1
    while n <= TC:
        m = min(n, TC + 1 - n)
        npi = sm()
        ts(npi, pi_, -1.0, ALU.mult)
        ta = kb.sb([128, NTD, m], F32, f"eta{n}")
        tb = kb.sb([128, NTD, m], F32, f"etb{n}")
        for td in range(NTD):
            kb.op("dve", lambda e, td=td: e.tensor_scalar(out=ta[:, td, :], in0=Er[:, td, 0:m], scalar1=pr[:, td:td + 1], scalar2=None, op0=ALU.mult),
                  r=[Er, pr], w=[ta])
            kb.op("dve", lambda e, td=td: e.scalar_tensor_tensor(out=Er[:, td, n:n + m], in0=Ei[:, td, 0:m], scalar=npi[:, td:td + 1], in1=ta[:, td, :],
                                                                 op0=ALU.mult, op1=ALU.add), r=[Ei, npi, ta], w=[Er])
            kb.op("dve", lambda e, td=td: e.tensor_scalar(out=tb[:, td, :], in0=Er[:, td, 0:m], scalar1=pi_[:, td:td + 1], scalar2=None, op0=ALU.mult),
                  r=[Er, pi_], w=[tb])
            kb.op("dve", lambda e, td=td: e.scalar_tensor_tensor(out=Ei[:, td, n:n + m], in0=Ei[:, td, 0:m], scalar=pr[:, td:td + 1], in1=tb[:, td, :],
                                                                 op0=ALU.mult, op1=ALU.add), r=[Ei, pr, tb], w=[Ei])
        c2 = sm(); s2 = sm(); sc_ = sm()
        tt(c2, pr, pr, ALU.mult)
        tt(s2, pi_, pi_, ALU.mult)
        tt(sc_, pr, pi_, ALU.mult)
        pr = sm(); pi_ = sm()
        tt(pr, c2, s2, ALU.subtract)
        ts(pi_, sc_, 2.0, ALU.mult)
        n *= 2
    Rr = kb.sb([128, NTD, TC], F32, "Rr")
    Ri = kb.sb([128, NTD, TC], F32, "Ri")
    rmul = kb.sb([128, NTD * TC], F32, "rmul")
    rmul3 = rmul[:].rearrange("p (n t) -> p n t", t=TC)
    onesT = kb.sb([128, TC], F32, "onesT")
    kb.op("dve", lambda e: e.memset(onesT[:], 1.0), w=[onesT])
    nf_re = sm()
    ts(nf_re, f_re, -1.0, ALU.mult)
    tr = kb.sb([128, NTD, TC], F32, "trtmp")
    for td in range(NTD):
        kb.op("dve", lambda e, td=td: e.tensor_scalar(out=tr[:, td, :], in0=Er[:, td, 0:TC], scalar1=f_re[:, td:td + 1], scalar2=None, op0=ALU.mult),
              r=[Er, f_re], w=[tr])
        kb.op("dve", lambda e, td=td: e.scalar_tensor_tensor(out=Rr[:, td, :], in0=Ei[:, td, 0:TC], scalar=f_im[:, td:td + 1], in1=tr[:, td, :],
                                                             op0=ALU.mult, op1=ALU.add), r=[Ei, f_im, tr], w=[Rr])
        kb.op("dve", lambda e, td=td: e.tensor_scalar(out=tr[:, td, :], in0=Er[:, td, 0:TC], scalar1=f_im[:, td:td + 1], scalar2=None, op0=ALU.mult),
              r=[Er, f_im], w=[tr])
        kb.op("dve", lambda e, td=td: e.scalar_tensor_tensor(out=Ri[:, td, :], in0=Ei[:, td, 0:TC], scalar=nf_re[:, td:td + 1], in1=tr[:, td, :],
                                                             op0=ALU.mult, op1=ALU.add), r=[Ei, nf_re, tr], w=[Ri])
        kb.op("dve", lambda e, td=td: e.tensor_scalar(out=rmul3[:, td, :], in0=onesT[:], scalar1=mag[:, td:td + 1], scalar2=None, op0=ALU.mult),
              r=[onesT, mag], w=[rmul])
    kb.op("dve", lambda e: e.memset(rmul3[:, :, 0:1], 0.0), w=[rmul])

    Q4 = 4 * TC
    pX = [kb.ps([128, Q4], F32, f"pX{d}") for d in range(2)]
    pY = [kb.ps([128, 512], F32, f"pY{d}") for d in range(2)]
    W = {}
    for nm in ("xre", "xim", "t1", "t2", "t3", "t4", "gr", "gi"):
        W[nm] = [kb.sb([128, Q4], F32, f"w_{nm}{i}") for i in range(2)]
    for nm in ("b1", "b2", "b3", "b4"):
        W[nm] = [kb.sb([128, Q4], BF16, f"w_{nm}{i}") for i in range(2)]
    yo = [[kb.sb([128, TC], F32, f"yo{d}_{i}") for i in range(2)] for d in range(2)]
    ufc = [kb.sb([128, TC], F32, f"ufc{i}") for i in range(2)]
    carry = [[kb.sb([128, 8], F32, f"carry{d}_{i}") for i in range(2)] for d in range(2)]
    for d in range(2):
        kb.op("dve", lambda e, d=d: e.memset(carry[d][0][:], 0.0), w=[carry[d][0]])
    ctmp = [kb.sb([128, 16], F32, f"ctmp{d}") for d in range(2)]

    def v3(t):
        return t[:].rearrange("p (q t) -> p q t", t=TC)

    def body(d, c):
        ops = []
        A = ops.append
        qs = slice(d * 4, d * 4 + 4)
        c0 = c * TC
        py = pY[d]
        px = pX[d]
        w = {k: v[d] for k, v in W.items()}
        if d == 0:
            A(lambda: kb.dma("sp", ufc[c % 2][:], uT.ap()[0, :, c0:c0 + TC], w=[ufc[c % 2]]))

        def mm_in(p):
            for q in range(4):
                kb.op("pe", lambda e, q=q: e.matmul(px[:, q * TC:(q + 1) * TC], lhsT=Bb[:, d, p, q, :], rhs=ubf[d][:, c0:c0 + TC], start=True, stop=True),
                      r=[Bb, ubf[d]], w=[px])
        A(lambda: mm_in(0))
        A(lambda: kb.op("act", lambda e: e.copy(out=w["xre"][:], in_=px[:]), r=[px], w=[w["xre"]]))
        A(lambda: mm_in(1))
        A(lambda: kb.op("act", lambda e: e.copy(out=w["xim"][:], in_=px[:]), r=[px], w=[w["xim"]]))
        A(lambda: kb.op("dve", lambda e: e.tensor_tensor(out=v3(w["t1"]), in0=v3(w["xre"]), in1=Rr[:, qs, :], op=ALU.mult), r=[w["xre"], Rr], w=[w["t1"]]))
        A(lambda: kb.op("dve", lambda e: e.tensor_tensor(out=v3(w["t2"]), in0=v3(w["xim"]), in1=Ri[:, qs, :], op=ALU.mult), r=[w["xim"], Ri], w=[w["t2"]]))
        A(lambda: kb.op("dve", lambda e: e.tensor_tensor(out=v3(w["t3"]), in0=v3(w["xre"]), in1=Ri[:, qs, :], op=ALU.mult), r=[w["xre"], Ri], w=[w["t3"]]))
        A(lambda: kb.op("dve", lambda e: e.tensor_tensor(out=v3(w["t4"]), in0=v3(w["xim"]), in1=Rr[:, qs, :], op=ALU.mult), r=[w["xim"], Rr], w=[w["t4"]]))
        cin = carry[d][c % 2]
        cout = carry[d][(c + 1) % 2]
        ct = ctmp[d]
        A(lambda: kb.op("dve", lambda e: e.tensor_tensor(out=ct[:, 0:4], in0=cin[:, 0:4], in1=mag[:, qs], op=ALU.mult), r=[cin, mag], w=[ct]))
        A(lambda: kb.op("dve", lambda e: e.tensor_tensor(out=ct[:, 4:8], in0=cin[:, 4:8], in1=mag[:, qs], op=ALU.mult), r=[cin, mag], w=[ct]))
        A(lambda: kb.op("dve", lambda e: e.tensor_tensor(out=w["t1"][:], in0=w["t1"][:], in1=w["t2"][:], op=ALU.subtract), r=[w["t1"], w["t2"]], w=[w["t1"]]))
        A(lambda: kb.op("dve", lambda e: e.tensor_tensor(out=w["t3"][:], in0=w["t3"][:], in1=w["t4"][:], op=ALU.add), r=[w["t3"], w["t4"]], w=[w["t3"]]))
        A(lambda: kb.op("dve", lambda e: e.tensor_tensor(out=v3(w["t1"])[:, :, 0], in0=v3(w["t1"])[:, :, 0], in1=ct[:, 0:4], op=ALU.add), r=[w["t1"], ct], w=[w["t1"]]))
        A(lambda: kb.op("dve", lambda e: e.tensor_tensor(out=v3(w["t3"])[:, :, 0], in0=v3(w["t3"])[:, :, 0], in1=ct[:, 4:8], op=ALU.add), r=[w["t3"], ct], w=[w["t3"]]))
        A(lambda: kb.op("dve", lambda e: e.tensor_tensor_scan(out=w["gr"][:], data0=rmul[:, d * Q4:(d + 1) * Q4], data1=w["t1"][:], initial=0.0,
                                                              op0=ALU.mult, op1=ALU.add), r=[rmul, w["t1"]], w=[w["gr"]]))
        A(lambda: kb.op("dve", lambda e: e.tensor_tensor_scan(out=w["gi"][:], data0=rmul[:, d * Q4:(d + 1) * Q4], data1=w["t3"][:], initial=0.0,
                                                              op0=ALU.mult, op1=ALU.add), r=[rmul, w["t3"]], w=[w["gi"]]))
        grl = v3(w["gr"])[:, :, TC - 1]
        gil = v3(w["gi"])[:, :, TC - 1]
        ETr = Er[:, qs, TC]
        ETi = Ei[:, qs, TC]
        A(lambda: kb.op("dve", lambda e: e.tensor_tensor(out=ct[:, 12:16], in0=grl, in1=ETi, op=ALU.mult), r=[w["gr"], Ei], w=[ct]))
        A(lambda: kb.op("dve", lambda e: e.tensor_tensor(out=cout[:, 0:4], in0=grl, in1=ETr, op=ALU.mult), r=[w["gr"], Er], w=[cout]))
        A(lambda: kb.op("dve", lambda e: e.tensor_tensor(out=ct[:, 8:12], in0=gil, in1=ETi, op=ALU.mult), r=[w["gi"], Ei], w=[ct]))
        A(lambda: kb.op("dve", lambda e: e.tensor_tensor(out=cout[:, 4:8], in0=gil, in1=ETr, op=ALU.mult), r=[w["gi"], Er], w=[cout]))
        A(lambda: kb.op("dve", lambda e: e.tensor_tensor(out=cout[:, 0:4], in0=cout[:, 0:4], in1=ct[:, 8:12], op=ALU.subtract), r=[cout, ct], w=[cout]))
        A(lambda: kb.op("dve", lambda e: e.tensor_tensor(out=cout[:, 4:8], in0=cout[:, 4:8], in1=ct[:, 12:16], op=ALU.add), r=[cout, ct], w=[cout]))
        A(lambda: kb.op("dve", lambda e: e.tensor_tensor(out=v3(w["b1"]), in0=v3(w["gr"]), in1=Er[:, qs, 0:TC], op=ALU.mult), r=[w["gr"], Er], w=[w["b1"]]))
        A(lambda: kb.op("dve", lambda e: e.tensor_tensor(out=v3(w["b2"]), in0=v3(w["gi"]), in1=Ei[:, qs, 0:TC], op=ALU.mult), r=[w["gi"], Ei], w=[w["b2"]]))
        A(lambda: kb.op("dve", lambda e: e.tensor_tensor(out=v3(w["b3"]), in0=v3(w["gr"]), in1=Ei[:, qs, 0:TC], op=ALU.mult), r=[w["gr"], Ei], w=[w["b3"]]))
        A(lambda: kb.op("dve", lambda e: e.tensor_tensor(out=v3(w["b4"]), in0=v3(w["gi"]), in1=Er[:, qs, 0:TC], op=ALU.mult), r=[w["gi"], Er], w=[w["b4"]]))

        def mm_out():
            n = 0
            for q in range(4):
                for (lh, src) in ((Cb[:, d, 0, q, :], "b1"), (Cn[:, d, q, :], "b2"), (Cb[:, d, 1, q, :], "b3"), (Cb[:, d, 1, q, :], "b4")):
                    kb.op("pe", lambda e, lh=lh, src=src, q=q, n=n: e.matmul(py[:, :TC], lhsT=lh, rhs=w[src][:, q * TC:(q + 1) * TC], start=(n == 0), stop=(n == 15)),
                          r=[Cb, Cn, w[src]], w=[py])
                    n += 1
        A(mm_out)
        o = yo[d][c % 2]
        if d == 0:
            A(lambda: kb.op("dve", lambda e: e.scalar_tensor_tensor(out=o[:], in0=ufc[c % 2][:], scalar=dv[:, 0:1], in1=py[:, :TC], op0=ALU.mult, op1=ALU.add),
                            r=[ufc[c % 2], dv, py], w=[o]))
        else:
            A(lambda: kb.op("act", lambda e: e.copy(out=o[:], in_=py[:, :TC]), r=[py], w=[o]))
        A(lambda: kb.dma("sp", yT.ap()[d, :, c0:c0 + TC], o[:], r=[o], w=[yT]))
        return ops

    for c in range(NCH):
        l0 = body(0, c)
        l1 = body(1, c)
        for i in range(max(len(l0), len(l1))):
            if i < len(l0):
                l0[i]()
            if i < len(l1):
                l1[i]()
    kb.finish()
    return kb


LT = 8448
NC64 = LT // 64


def build_k2b():
    kb = KB()
    qk = kb.dram("qk", [2, 128, LT], F32, kind="ExternalInput")
    hw1 = kb.dram("hw1", [16, LT], F32, kind="ExternalInput")
    w2b = kb.dram("w2b", [16, 128], F32, kind="ExternalInput")
    nb = kb.dram("nb", [128, 1], F32, kind="ExternalInput")
    vtok = kb.dram("vtok", [64, NC64, 256], F32, kind="ExternalInput")
    cst = kb.dram("cst", [128, 512 + 64 + 128], F32, kind="ExternalInput")
    oT = kb.dram("oT", [2, 128, LT], F32, kind="ExternalOutput")

    vb = kb.sb([64, NC64, 256], BF16, "vb")
    for c0 in range(0, NC64, 8):
        c1 = min(NC64, c0 + 8)
        kb.dma("pool", vb[:, c0:c1, :], vtok.ap()[:, c0:c1, :], w=[vb])
    cs_ = kb.sb([128, 704], F32, "cs_")
    kb.dma("sp", cs_[:], cst.ap(), w=[cs_])
    identb = kb.sb([128, 128], BF16, "identb")
    kb.op("dve", lambda e: e.tensor_copy(out=identb[:], in_=cs_[:, 576:704]), r=[cs_], w=[identb])
    w2s = kb.sb([16, 128], F32, "w2s")
    kb.dma("sp", w2s[:], w2b.ap(), w=[w2s])
    bs_ = kb.sb([128, 1], F32, "bs_")
    kb.dma("sp", bs_[:], nb.ap(), w=[bs_])
    nbs = kb.sb([128, 1], F32, "nbs")
    kb.op("dve", lambda e: e.tensor_scalar(out=nbs[:], in0=bs_[:], scalar1=-1.0, scalar2=None, op0=ALU.mult), r=[bs_], w=[nbs])
    qe = kb.sb([128, LT], BF16, "qe")
    ke = kb.sb([128, LT], BF16, "ke")
    kl = kb.sb([128, LT], BF16, "kl")
    ebl = kb.sb([128, NC64], F32, "ebl")

    BL = 512
    nblk = (LT + BL - 1) // BL
    A = {}
    for nm in ("q", "k", "e1", "la", "bc", "eb", "enb", "kef"):
        A[nm] = [kb.sb([128, BL], F32, f"a_{nm}{i}") for i in range(2)]
    A["h"] = [kb.sb([16, BL], F32, f"a_h{i}") for i in range(2)]
    pso = [kb.ps([128, 512], F32, f"pso{i}") for i in range(2)]
    pg = pso
    scale = 128 ** -0.5
    for bi in range(nblk):
        c0 = bi * BL
        cn = min(BL, LT - c0)
        b = bi % 2
        a = {k: v[b] for k, v in A.items()}
        kb.dma("sp", a["q"][:, :cn], qk.ap()[0, :, c0:c0 + cn], w=[a["q"]])
        kb.dma("act", a["k"][:, :cn], qk.ap()[1, :, c0:c0 + cn], w=[a["k"]])
        kb.dma("sp", a["h"][:, :cn], hw1.ap()[:, c0:c0 + cn], w=[a["h"]])
        kb.op("pe", lambda e: e.matmul(pg[b][:, :cn], lhsT=w2s[:], rhs=a["h"][:, :cn], start=True, stop=True), r=[w2s, a["h"]], w=[pg[b]])
        kb.op("act", lambda e: e.activation(out=a["e1"][:, :cn], in_=pg[b][:, :cn], func=AF.Exp, bias=nbs[:, 0:1], scale=-1.0),
              r=[pg[b], nbs], w=[a["e1"]])
        kb.op("act", lambda e: e.activation(out=a["la"][:, :cn], in_=a["e1"][:, :cn], func=AF.Ln, bias=1.0), r=[a["e1"]], w=[a["la"]])
        kb.op("dve", lambda e: e.tensor_scalar(out=a["la"][:, :cn], in0=a["la"][:, :cn], scalar1=-1.0 / 16.0, scalar2=None, op0=ALU.mult),
              r=[a["la"]], w=[a["la"]])
        kb.op("dve", lambda e: e.tensor_tensor_scan(out=a["bc"][:, :cn], data0=cs_[:, 0:cn], data1=a["la"][:, :cn], initial=0.0,
                                                    op0=ALU.mult, op1=ALU.add), r=[cs_, a["la"]], w=[a["bc"]])
        kb.op("act", lambda e: e.activation(out=a["eb"][:, :cn], in_=a["bc"][:, :cn], func=AF.Exp), r=[a["bc"]], w=[a["eb"]])
        kb.op("act", lambda e: e.activation(out=a["enb"][:, :cn], in_=a["bc"][:, :cn], func=AF.Exp, scale=-1.0), r=[a["bc"]], w=[a["enb"]])
        kb.op("dve", lambda e: e.scalar_tensor_tensor(out=qe[:, c0:c0 + cn], in0=a["q"][:, :cn], scalar=scale, in1=a["eb"][:, :cn],
                                                      op0=ALU.mult, op1=ALU.mult), r=[a["q"], a["eb"]], w=[qe])
        kb.op("dve", lambda e: e.tensor_tensor(out=a["kef"][:, :cn], in0=a["k"][:, :cn], in1=a["enb"][:, :cn], op=ALU.mult),
              r=[a["k"], a["enb"]], w=[a["kef"]])
        kb.op("act", lambda e: e.copy(out=ke[:, c0:c0 + cn], in_=a["kef"][:, :cn]), r=[a["kef"]], w=[ke])
        nch = cn // 64
        ch0 = c0 // 64
        kb.op("dve", lambda e: e.tensor_copy(out=ebl[:, ch0:ch0 + nch],
                                             in_=a["eb"][:, :cn].rearrange("p (c j) -> p c j", j=64)[:, :, 63]), r=[a["eb"]], w=[ebl])
        for cc in range(nch):
            kb.op("dve", lambda e, cc=cc: e.tensor_scalar(out=kl[:, c0 + cc * 64:c0 + (cc + 1) * 64], in0=a["kef"][:, cc * 64:(cc + 1) * 64],
                                                          scalar1=ebl[:, ch0 + cc:ch0 + cc + 1], scalar2=None, op0=ALU.mult),
                  r=[a["kef"], ebl], w=[kl])

    S = [kb.sb([128, 256], F32, f"S{i}") for i in range(2)]
    Sb = [kb.sb([128, 256], BF16, f"Sb{i}") for i in range(2)]
    kb.op("dve", lambda e: e.memset(S[0][:], 0.0), w=[S[0]])
    kb.op("dve", lambda e: e.memset(Sb[0][:], 0.0), w=[Sb[0]])
    psc = [kb.ps([64, 512], F32, f"psc{i}") for i in range(2)]
    ptr = [kb.ps([64, 128], BF16, f"ptr{i}") for i in range(2)]
    pst = [kb.ps([128, 512], F32, f"pst{i}") for i in range(1)]
    pm = [kb.sb([64, 64], BF16, f"pm{i}") for i in range(2)]
    klT = [kb.sb([64, 128], BF16, f"klT{i}") for i in range(2)]
    GB = 8
    ost = [kb.sb([128, 2, GB * 64], F32, f"ost{i}") for i in range(2)]
    for c in range(NC64):
        b = c % 2
        cols = slice(c * 64, (c + 1) * 64)
        So, Sn = S[c % 2], S[(c + 1) % 2]
        Sbo, Sbn = Sb[c % 2], Sb[(c + 1) % 2]
        kb.op("pe", lambda e: e.matmul(psc[b][:, 0:64], lhsT=ke[:, cols], rhs=qe[:, cols], start=True, stop=True), r=[ke, qe], w=[psc[b]])
        kb.op("dve", lambda e: e.tensor_tensor(out=pm[b][:], in0=psc[b][:, 0:64], in1=cs_[0:64, 512:576], op=ALU.mult), r=[psc[b], cs_], w=[pm[b]])
        kb.op("pe", lambda e: e.transpose(ptr[b][:], kl[:, cols], identb[:]), r=[kl, identb], w=[ptr[b]])
        kb.op("act", lambda e: e.copy(out=klT[b][:], in_=ptr[b][:]), r=[ptr[b]], w=[klT[b]])
        for vt in range(2):
            kb.op("pe", lambda e, vt=vt: e.matmul(pso[b][:, vt * 64:(vt + 1) * 64], lhsT=vb[:, c, vt * 128:(vt + 1) * 128], rhs=pm[b][:], start=True, stop=False),
                  r=[vb, pm[b]], w=[pso[b]])
            kb.op("pe", lambda e, vt=vt: e.matmul(pso[b][:, vt * 64:(vt + 1) * 64], lhsT=Sbo[:, vt * 128:(vt + 1) * 128], rhs=qe[:, cols], start=False, stop=True),
                  r=[Sbo, qe], w=[pso[b]])
        o = ost[(c // GB) % 2]
        oc = (c % GB) * 64
        kb.op("act", lambda e: e.copy(out=o[:, :, oc:oc + 64], in_=pso[b][:, 0:128].rearrange("p (v i) -> p v i", v=2)), r=[pso[b]], w=[o])
        kb.op("pe", lambda e: e.matmul(pst[0][:, 0:256], lhsT=klT[b][:], rhs=vb[:, c, :], start=True, stop=True), r=[klT[b], vb], w=[pst[0]])
        kb.op("dve", lambda e: e.scalar_tensor_tensor(out=Sn[:], in0=So[:], scalar=ebl[:, c:c + 1], in1=pst[0][:, 0:256], op0=ALU.mult, op1=ALU.add),
              r=[So, ebl, pst[0]], w=[Sn])
        kb.op("act", lambda e: e.copy(out=Sbn[:], in_=Sn[:]), r=[Sn], w=[Sbn])
        if c % GB == GB - 1 or c == NC64 - 1:
            g0 = (c // GB) * GB * 64
            gn = (c % GB + 1) * 64
            for vt in range(2):
                kb.dma("sp", oT.ap()[vt, :, g0:g0 + gn], o[:, vt, 0:gn], r=[o], w=[oT])
    kb.finish()
    return kb


L = 8192
NBQ = 64


def build_k2c():
    kb = KB()
    qT = kb.dram("qT", [2, 128, L], F32, kind="ExternalInput")
    kT = kb.dram("kT", [2, 128, L], F32, kind="ExternalInput")
    cs = kb.dram("cs", [2, 128, L], F32, kind="ExternalInput")
    cx = kb.dram("cx", [2, 128, 256], F32, kind="ExternalInput")
    vtok = kb.dram("vtok", [128, 66, 64], F32, kind="ExternalInput")
    masks = kb.dram("masks", [2, 128, 128], F32, kind="ExternalInput")
    sink = kb.dram("sink", [64, 2], F32, kind="ExternalInput")
    yT = kb.dram("yT", [128, L], F32, kind="ExternalOutput")
    ycT = kb.dram("ycT", [128, 256], F32, kind="ExternalOutput")

    qr = kb.sb([128, L + 256], BF16, "qr")
    kr = kb.sb([128, L + 256], BF16, "kr")
    vb = kb.sb([128, 66, 64], BF16, "vb")
    kb.dma("pool", vb[:], vtok.ap(), w=[vb])
    mk = kb.sb([128, 2, 128], BF16, "mk")
    kb.dma("pool", mk[:], masks.ap().rearrange("m j i -> j m i"), w=[mk])
    onesb = kb.sb([128, 64], BF16, "onesb")
    kb.op("dve", lambda e: e.memset(onesb[:], 1.0), w=[onesb])
    sk = kb.sb([64, 2], F32, "sk")
    kb.dma("sp", sk[:], sink.ap(), w=[sk])
    es = kb.sb([64, 2], F32, "es")
    kb.op("act", lambda e: e.activation(out=es[:], in_=sk[:], func=AF.Exp), r=[sk], w=[es])
    kb.dma("pool", qr[:, L:L + 256], cx.ap()[0], w=[qr])
    kb.dma("pool", kr[:, L:L + 256], cx.ap()[1], w=[kr])
    RB = 1024
    bufs = {}
    for nm in ("a", "ap", "c", "s", "t1", "t2"):
        bufs[nm] = [kb.sb([128, RB], F32, f"r_{nm}{i}") for i in range(2)]
    it = 0
    for src, dst in ((qT, qr), (kT, kr)):
        for c0 in range(0, L, RB):
            b = it % 2
            it += 1
            a, ap_, c_, s_, t1, t2 = (bufs[nm][b] for nm in ("a", "ap", "c", "s", "t1", "t2"))
            kb.dma("sp", a[:], src.ap()[0, :, c0:c0 + RB], w=[a])
            kb.dma("act", ap_[:], src.ap()[1, :, c0:c0 + RB], w=[ap_])
            kb.dma("sp", c_[:], cs.ap()[0, :, c0:c0 + RB], w=[c_])
            kb.dma("act", s_[:], cs.ap()[1, :, c0:c0 + RB], w=[s_])
            kb.op("dve", lambda e: e.tensor_tensor(out=t1[:], in0=a[:], in1=c_[:], op=ALU.mult), r=[a, c_], w=[t1])
            kb.op("dve", lambda e: e.tensor_tensor(out=t2[:], in0=ap_[:], in1=s_[:], op=ALU.mult), r=[ap_, s_], w=[t2])
            kb.op("dve", lambda e: e.tensor_tensor(out=dst[:, c0:c0 + RB], in0=t1[:], in1=t2[:], op=ALU.add), r=[t1, t2], w=[dst])

    ps1 = [kb.ps([128, 512], F32, f"ps1_{i}") for i in range(2)]
    ps2 = [kb.ps([128, 512], F32, f"ps2_{i}") for i in range(2)]
    po = [kb.ps([64, 512], F32, f"po{i}") for i in range(2)]
    pb1 = [kb.sb([128, 512], BF16, f"pb1_{i}") for i in range(2)]
    pb2 = [kb.sb([128, 128], BF16, f"pb2_{i}") for i in range(2)]
    den = [kb.sb([64, 128], F32, f"den{i}") for i in range(2)]
    rden = [kb.sb([64, 128], F32, f"rden{i}") for i in range(2)]
    GB = 8
    ob = [kb.sb([64, 2, GB * 128], F32, f"ob{i}") for i in range(2)]
    it = 0
    nblocks = NBQ + 2
    for n in range(nblocks):
        isctx = n >= NBQ
        o = ob[(n // GB) % 2]
        for hh in range(2):
            b = it % 2
            it += 1
            hs = slice(hh * 64, hh * 64 + 64)
            if not isctx:
                qcols = slice(n * 128, (n + 1) * 128)
                tiles = []
                if n > 0:
                    tiles.append((0, n - 1, 0))
                tiles.append((1, n, None))
                if n < NBQ - 1:
                    tiles.append((2, n + 1, 1))
                tiles.append((3, 64, None))
                tiles.append((4, 65, None))
            else:
                qcols = slice(L + (n - NBQ) * 128, L + (n - NBQ + 1) * 128)
                tiles = [(3, 64, None), (4, 65, None)]
            p1, p2 = ps1[b], ps2[b]
            for slot, kt, m in tiles:
                dstp = p1[:, slot * 128:(slot + 1) * 128] if slot < 4 else p2[:, 0:128]
                pt = p1 if slot < 4 else p2
                kb.op("pe", lambda e: e.matmul(dstp, lhsT=kr[hs, kt * 128:(kt + 1) * 128], rhs=qr[hs, qcols], start=True, stop=True),
                      r=[kr, qr], w=[pt])
            slots = [t[0] for t in tiles if t[0] < 4]
            lo, hi = min(slots) * 128, (max(slots) + 1) * 128
            kb.op("act", lambda e: e.activation(out=pb1[b][:, lo:hi], in_=p1[:, lo:hi], func=AF.Exp, scale=0.125), r=[p1], w=[pb1[b]])
            kb.op("act", lambda e: e.activation(out=pb2[b][:], in_=p2[:, 0:128], func=AF.Exp, scale=0.125), r=[p2], w=[pb2[b]])
            for slot, kt, m in tiles:
                if m is not None:
                    kb.op("dve", lambda e: e.tensor_tensor(out=pb1[b][:, slot * 128:(slot + 1) * 128], in0=pb1[b][:, slot * 128:(slot + 1) * 128],
                                                           in1=mk[:, m, :], op=ALU.mult), r=[pb1[b], mk], w=[pb1[b]])
            pp = po[b]
            for ti, (slot, kt, m) in enumerate(tiles):
                src = pb1[b][:, slot * 128:(slot + 1) * 128] if slot < 4 else pb2[b][:]
                st = pb1[b] if slot < 4 else pb2[b]
                kb.op("pe", lambda e: e.matmul(pp[:, 0:128], lhsT=vb[:, kt, :], rhs=src, start=(ti == 0), stop=(ti == len(tiles) - 1)),
                      r=[vb, st], w=[pp])
            for ti, (slot, kt, m) in enumerate(tiles):
                src = pb1[b][:, slot * 128:(slot + 1) * 128] if slot < 4 else pb2[b][:]
                st = pb1[b] if slot < 4 else pb2[b]
                kb.op("pe", lambda e: e.matmul(pp[:, 128:256], lhsT=onesb[:], rhs=src, start=(ti == 0), stop=(ti == len(tiles) - 1)),
                      r=[onesb, st], w=[pp])
            kb.op("dve", lambda e: e.tensor_scalar(out=den[b][:], in0=pp[:, 128:256], scalar1=es[:, hh:hh + 1], scalar2=None, op0=ALU.add),
                  r=[pp, es], w=[den[b]])
            kb.op("dve", lambda e: e.reciprocal(out=rden[b][:], in_=den[b][:]), r=[den[b]], w=[rden[b]])
            oc = (n % GB) * 128
            kb.op("dve", lambda e: e.tensor_tensor(out=o[:, hh, oc:oc + 128], in0=pp[:, 0:128], in1=rden[b][:], op=ALU.mult),
                  r=[pp, rden[b]], w=[o])
        if n < NBQ and n % GB == GB - 1:
            g0 = (n // GB) * GB * 128
            for hh in range(2):
                kb.dma("sp", yT.ap()[hh * 64:(hh + 1) * 64, g0:g0 + GB * 128], o[:, hh, :], r=[o], w=[yT])
        if n == nblocks - 1:
            for hh in range(2):
                kb.dma("sp", ycT.ap()[hh * 64:(hh + 1) * 64, :], o[:, hh, 0:256], r=[o], w=[ycT])
    kb.finish()
    return kb


def build_k3a():
    kb = KB()
    ys = kb.dram("ys", [2, 1024, TOK], F32, kind="ExternalInput")
    og = kb.dram("og", [2, 1024, TOK], F32, kind="ExternalInput")
    ya = kb.dram("ya", [1024, TOK], F32, kind="ExternalInput")
    zr = kb.dram("zr", [1024, TOK], F32, kind="ExternalInput")
    zg = kb.dram("zg", [6144, TOK], F32, kind="ExternalInput")
    xT = kb.dram("xT", [D, TOK], F32, kind="ExternalInput")
    wglu = kb.dram("wglu", [1024, 1024], F32, kind="ExternalInput")
    wbr = kb.dram("wbr", [3, 1024, D], F32, kind="ExternalInput")
    wout = kb.dram("wout", [D, D], F32, kind="ExternalInput")
    vecs = kb.dram("vecs", [3, 128, KT], F32, kind="ExternalInput")
    xo = kb.dram("xo", [D, TOK], F32, kind="ExternalOutput")

    vt_ = []
    for i in range(3):
        t = kb.sb([128, KT], F32, f"vec{i}")
        kb.dma("sp", t[:], vecs.ap()[i], w=[t])
        vt_.append(t)
    gn, m2l, m2c = vt_
    ones = kb.sb([128, 128], F32, "ones")
    kb.op("dve", lambda e: e.memset(ones[:], 1.0), w=[ones])
    PS = [kb.ps([128, 512], F32, f"ps{i}") for i in range(6)]
    psi = [0]

    def nps():
        psi[0] += 1
        return PS[psi[0] % 6]

    W = {}

    def wk(nm, n=2):
        if nm not in W:
            W[nm] = [[kb.sb([128, 512], F32, f"w_{nm}{i}") for i in range(n)], 0]
        W[nm][1] += 1
        return W[nm][0][W[nm][1] % len(W[nm][0])]

    ys5 = kb.sb([128, 8, TOK], BF16, "ys5")
    ygla = kb.sb([128, 8, TOK], BF16, "ygla")
    yatt = kb.sb([128, 8, TOK], BF16, "yatt")
    for kt in range(8):
        kb.dma("pool", yatt[:, kt, :], ya.ap()[kt * 128:(kt + 1) * 128, :], w=[yatt])

    gf = kb.sb([128, 8, TOK], F32, "gf")
    gb = kb.sb([128, 8, TOK], BF16, "gb")
    for kt in range(8):
        for (c0, cn) in BLKS:
            a = wk("a"); b = wk("b"); y = wk("y"); t = wk("t"); s = wk("s")
            kb.dma("sp", a[:, :cn], ys.ap()[0, kt * 128:(kt + 1) * 128, c0:c0 + cn], w=[a])
            kb.dma("act", b[:, :cn], ys.ap()[1, kt * 128:(kt + 1) * 128, c0:c0 + cn], w=[b])
            kb.op("dve", lambda e: e.tensor_tensor(out=y[:, :cn], in0=a[:, :cn], in1=b[:, :cn], op=ALU.add), r=[a, b], w=[y])
            kb.op("dve", lambda e: e.tensor_tensor(out=t[:, :cn], in0=y[:, :cn], in1=y[:, :cn], op=ALU.mult), r=[y], w=[t])
            kb.op("dve", lambda e: e.tensor_scalar(out=t[:, :cn], in0=t[:, :cn], scalar1=0.044715, scalar2=1.0, op0=ALU.mult, op1=ALU.add), r=[t], w=[t])
            kb.op("dve", lambda e: e.tensor_tensor(out=t[:, :cn], in0=t[:, :cn], in1=y[:, :cn], op=ALU.mult), r=[t, y], w=[t])
            kb.op("act", lambda e: e.activation(out=s[:, :cn], in_=t[:, :cn], func=AF.Sigmoid, scale=1.5957691216057308), r=[t], w=[s])
            kb.op("dve", lambda e: e.tensor_tensor(out=gf[:, kt, c0:c0 + cn], in0=y[:, :cn], in1=s[:, :cn], op=ALU.mult), r=[y, s], w=[gf])
            kb.op("dve", lambda e: e.tensor_copy(out=gb[:, kt, c0:c0 + cn], in_=gf[:, kt, c0:c0 + cn]), r=[gf], w=[gb])
    wgs = [kb.sb([128, 8, 128], BF16, f"wg{i}") for i in range(2)]
    wgv = wglu.ap().rearrange("(kt p) n -> p kt n", p=128)
    for nt in range(8):
        wg = wgs[nt % 2]
        kb.dma("pool", wg[:], wgv[:, :, nt * 128:(nt + 1) * 128], w=[wg])
        for (c0, cn) in BLKS:
            ps = nps()
            for kt in range(8):
                kb.op("pe", lambda e, kt=kt: e.matmul(ps[:, :cn], lhsT=wg[:, kt, :], rhs=gb[:, kt, c0:c0 + cn], start=(kt == 0), stop=(kt == 7)),
                      r=[wg, gb], w=[ps])
            s = wk("s")
            kb.op("act", lambda e: e.activation(out=s[:, :cn], in_=ps[:, :cn], func=AF.Sigmoid), r=[ps], w=[s])
            kb.op("dve", lambda e: e.tensor_tensor(out=ys5[:, nt, c0:c0 + cn], in0=gf[:, nt, c0:c0 + cn], in1=s[:, :cn], op=ALU.mult), r=[gf, s], w=[ys5])

    of = gf
    for hd in range(4):
        for (c0, cn) in BLKS:
            ps = nps()
            for v in range(2):
                kt = hd * 2 + v
                a = wk("a"); b = wk("b"); sq = wk("y")
                kb.dma("sp", a[:, :cn], og.ap()[0, kt * 128:(kt + 1) * 128, c0:c0 + cn], w=[a])
                kb.dma("act", b[:, :cn], og.ap()[1, kt * 128:(kt + 1) * 128, c0:c0 + cn], w=[b])
                kb.op("dve", lambda e: e.tensor_tensor(out=of[:, kt, c0:c0 + cn], in0=a[:, :cn], in1=b[:, :cn], op=ALU.add), r=[a, b], w=[of])
                kb.op("act", lambda e: e.activation(out=sq[:, :cn], in_=of[:, kt, c0:c0 + cn], func=AF.Square), r=[of], w=[sq])
                kb.op("pe", lambda e: e.matmul(ps[:, :cn], lhsT=ones[:], rhs=sq[:, :cn], start=(v == 0), stop=(v == 1)), r=[ones, sq], w=[ps])
            sd = wk("t"); rs = wk("s", 3)
            kb.op("act", lambda e: e.activation(out=sd[:, :cn], in_=ps[:, :cn], func=AF.Sqrt, bias=EPS, scale=1.0 / 256), r=[ps], w=[sd])
            kb.op("dve", lambda e: e.reciprocal(out=rs[:, :cn], in_=sd[:, :cn]), r=[sd], w=[rs])
            for v in range(2):
                kt = hd * 2 + v
                r_ = wk("a"); sg = wk("b"); sr = wk("y"); yy = wk("t")
                kb.dma("sp", r_[:, :cn], zr.ap()[kt * 128:(kt + 1) * 128, c0:c0 + cn], w=[r_])
                kb.op("act", lambda e: e.activation(out=sg[:, :cn], in_=r_[:, :cn], func=AF.Sigmoid), r=[r_], w=[sg])
                kb.op("dve", lambda e: e.tensor_tensor(out=sr[:, :cn], in0=r_[:, :cn], in1=sg[:, :cn], op=ALU.mult), r=[r_, sg], w=[sr])
                kb.op("dve", lambda e: e.tensor_tensor(out=yy[:, :cn], in0=of[:, kt, c0:c0 + cn], in1=rs[:, :cn], op=ALU.mult), r=[of, rs], w=[yy])
                kb.op("dve", lambda e: e.scalar_tensor_tensor(out=ygla[:, kt, c0:c0 + cn], in0=yy[:, :cn], scalar=gn[:, kt:kt + 1], in1=sr[:, :cn],
                                                              op0=ALU.mult, op1=ALU.mult), r=[yy, gn, sr], w=[ygla])

    mb = kb.sb([128, KT, TOK], BF16, "mb")
    wbs = [[kb.sb([128, 8, 128], BF16, f"wb{b}_{i}") for i in range(2)] for b in range(3)]
    ysrc = [ys5, ygla, yatt]
    for nt in range(KT):
        wts = []
        for b in range(3):
            wt = wbs[b][nt % 2]
            kb.dma("pool", wt[:], wbr.ap()[b].rearrange("(kt p) n -> p kt n", p=128)[:, :, nt * 128:(nt + 1) * 128], w=[wt])
            wts.append(wt)
        for (c0, cn) in BLKS:
            pss = []
            for b in range(3):
                ps = nps()
                for kt in range(8):
                    kb.op("pe", lambda e, kt=kt, b=b: e.matmul(ps[:, :cn], lhsT=wts[b][:, kt, :], rhs=ysrc[b][:, kt, c0:c0 + cn], start=(kt == 0), stop=(kt == 7)),
                          r=[wts[b], ysrc[b]], w=[ps])
                pss.append(ps)
            acc = wk("acc")
            for b in range(3):
                gt = wk("a"); sg = wk("b"); tm = wk("y")
                kb.dma("sp" if b != 1 else "act", gt[:, :cn], zg.ap()[b * 2048 + nt * 128:b * 2048 + (nt + 1) * 128, c0:c0 + cn], w=[gt])
                kb.op("act", lambda e: e.activation(out=sg[:, :cn], in_=gt[:, :cn], func=AF.Sigmoid), r=[gt], w=[sg])
                if b == 0:
                    kb.op("dve", lambda e: e.tensor_tensor(out=acc[:, :cn], in0=pss[b][:, :cn], in1=sg[:, :cn], op=ALU.mult), r=[pss[b], sg], w=[acc])
                else:
                    kb.op("dve", lambda e: e.tensor_tensor(out=tm[:, :cn], in0=pss[b][:, :cn], in1=sg[:, :cn], op=ALU.mult), r=[pss[b], sg], w=[tm])
                    if b == 1:
                        kb.op("dve", lambda e: e.tensor_tensor(out=acc[:, :cn], in0=acc[:, :cn], in1=tm[:, :cn], op=ALU.add), r=[acc, tm], w=[acc])
                    else:
                        kb.op("dve", lambda e: e.tensor_tensor(out=mb[:, nt, c0:c0 + cn], in0=acc[:, :cn], in1=tm[:, :cn], op=ALU.add), r=[acc, tm], w=[mb])

    wos = [kb.sb([128, KT, 128], BF16, f"wo{i}") for i in range(2)]
    xbs = [kb.sb([128, TOK], F32, f"xb{i}") for i in range(2)]
    wov = wout.ap().rearrange("(kt p) n -> p kt n", p=128)
    for nt in range(KT):
        wo = wos[nt % 2]
        xb = xbs[nt % 2]
        kb.dma("pool", wo[:], wov[:, :, nt * 128:(nt + 1) * 128], w=[wo])
        kb.dma("sp", xb[:], xT.ap()[nt * 128:(nt + 1) * 128, :], w=[xb])
        for bi, (c0, cn) in enumerate(BLKS):
            ps = nps()
            for kt in range(KT):
                kb.op("pe", lambda e, kt=kt: e.matmul(ps[:, :cn], lhsT=wo[:, kt, :], rhs=mb[:, kt, c0:c0 + cn], start=(kt == 0), stop=(kt == KT - 1)),
                      r=[wo, mb], w=[ps])
            mm = m2c if bi == 2 else m2l
            kb.op("dve", lambda e: e.scalar_tensor_tensor(out=xb[:, c0:c0 + cn], in0=ps[:, :cn], scalar=mm[:, nt:nt + 1], in1=xb[:, c0:c0 + cn],
                                                          op0=ALU.mult, op1=ALU.add), r=[ps, mm, xb], w=[xb])
        kb.dma("act", xo.ap()[nt * 128:(nt + 1) * 128, :], xb[:], r=[xb], w=[xo])
    kb.finish()
    return kb


def build_k3b():
    kb = KB()
    xT = kb.dram("xT", [D, TOK], F32, kind="ExternalInput")
    vecs = kb.dram("vecs", [5, 128, KT], F32, kind="ExternalInput")
    rt = kb.dram("rt", [D, 16], F32, kind="ExternalInput")
    h2 = kb.dram("h2", [D, TOK], F32, kind="ExternalOutput")
    aff = kb.dram("aff", [16, TOK], F32, kind="ExternalOutput")
    xs = kb.sb([128, KT, TOK], F32, "xs")
    xv = xT.ap().rearrange("(kt p) t -> p kt t", p=128)
    for kt in range(KT):
        kb.dma("sp" if kt % 2 == 0 else "act", xs[:, kt, :], xv[:, kt, :], w=[xs])
    vt = []
    for i in range(5):
        t = kb.sb([128, KT], F32, f"vec{i}")
        kb.dma("sp", t[:], vecs.ap()[i], w=[t])
        vt.append(t)
    ones = kb.sb([128, 128], F32, "ones")
    kb.op("dve", lambda e: e.memset(ones[:], 1.0), w=[ones])
    hT = kb.sb([128, KT, TOK], F32, "hT")
    emit_norm_mod(kb, xs, hT, vt[0], vt[1], vt[2], vt[3], vt[4], ones)
    h2v = h2.ap().rearrange("(kt p) t -> p kt t", p=128)
    for kt in range(KT):
        kb.dma("sp" if kt % 2 == 0 else "act", h2v[:, kt, :], hT[:, kt, :], r=[hT], w=[h2])
    rs = kb.sb([128, KT, 16], F32, "rs")
    kb.dma("sp", rs[:], rt.ap().rearrange("(kt p) e -> p kt e", p=128), w=[rs])
    pl = [kb.ps([16, 512], F32, f"pl{i}") for i in range(2)]
    pz = [kb.ps([16, 512], F32, f"pz{i}") for i in range(2)]
    ex = kb.sb([16, TOK], F32, "ex")
    rz = kb.sb([16, TOK], F32, "rz")
    af = kb.sb([16, TOK], F32, "af")
    for bi, (c0, cn) in enumerate(BLKS):
        p = pl[bi % 2]
        for kt in range(KT):
            kb.op("pe", lambda e, kt=kt: e.matmul(p[:, :cn], lhsT=rs[:, kt, :], rhs=hT[:, kt, c0:c0 + cn], start=(kt == 0), stop=(kt == KT - 1)),
                  r=[rs, hT], w=[p])
        kb.op("act", lambda e: e.activation(out=ex[:, c0:c0 + cn], in_=p[:, :cn], func=AF.Exp), r=[p], w=[ex])
        z = pz[bi % 2]
        kb.op("pe", lambda e: e.matmul(z[:, :cn], lhsT=ones[0:16, 0:16], rhs=ex[:, c0:c0 + cn], start=True, stop=True), r=[ones, ex], w=[z])
        kb.op("dve", lambda e: e.reciprocal(out=rz[:, c0:c0 + cn], in_=z[:, :cn]), r=[z], w=[rz])
        kb.op("dve", lambda e: e.tensor_tensor(out=af[:, c0:c0 + cn], in0=ex[:, c0:c0 + cn], in1=rz[:, c0:c0 + cn], op=ALU.mult), r=[ex, rz], w=[af])
    kb.dma("sp", aff.ap(), af[:], r=[af], w=[aff])
    kb.finish()
    return kb


D = 2048
NIT = 34
BIG = 1.0e6
SLOTS = 1056
SBLK = [(0, 512), (512, 512), (1024, 32)]


def make_route(kb, A, ntt, cap, nst, tri, ones, iota_s, tokid, tagp, Rb, G, rcol):
    def sm(shape, dt=F32, nm=""):
        return kb.sb(shape, dt, tagp + nm)
    lo = sm([128, 1], nm="lo"); hi = sm([128, 1], nm="hi"); mid = sm([128, 1], nm="mid")
    junk = sm([128, ntt], nm="junk"); cnt = sm([128, 1], nm="cnt"); ge = sm([128, 1], nm="ge")
    d1 = sm([128, 1], nm="d1"); d2 = sm([128, 1], nm="d2"); sm_ = sm([128, 1], nm="sm")
    pc = Rb[:, rcol:rcol + 1]
    bis = []
    B = bis.append
    B(lambda: kb.op("dve", lambda e: e.memset(lo[:], 0.0), w=[lo]))
    B(lambda: kb.op("dve", lambda e: e.memset(hi[:], 1.0), w=[hi]))
    B(lambda: kb.op("dve", lambda e: e.memset(mid[:], 0.5), w=[mid]))
    for it in range(NIT):
        B(lambda: kb.op("dve", lambda e: e.tensor_scalar(out=junk[:], in0=A[:], scalar1=mid[:, 0:1], scalar2=0.0, op0=ALU.is_gt, op1=ALU.add, accum_out=cnt[:, 0:1]),
                        r=[A, mid], w=[junk, cnt]))
        B(lambda: kb.op("pe", lambda e: e.matmul(pc, lhsT=ones[:], rhs=cnt[:, 0:1], start=True, stop=True), r=[ones, cnt], w=[Rb]))
        B(lambda: kb.op("dve", lambda e: e.tensor_scalar(out=ge[:], in0=pc, scalar1=float(cap) - 0.5, scalar2=None, op0=ALU.is_gt), r=[Rb], w=[ge]))
        B(lambda: kb.op("dve", lambda e: e.tensor_tensor(out=d1[:], in0=mid[:], in1=lo[:], op=ALU.subtract), r=[mid, lo], w=[d1]))
        B(lambda: kb.op("dve", lambda e: e.tensor_tensor(out=d2[:], in0=hi[:], in1=mid[:], op=ALU.subtract), r=[hi, mid], w=[d2]))
        B(lambda: kb.op("dve", lambda e: e.scalar_tensor_tensor(out=lo[:], in0=d1[:], scalar=ge[:, 0:1], in1=lo[:], op0=ALU.mult, op1=ALU.add), r=[d1, ge, lo], w=[lo]))
        B(lambda: kb.op("dve", lambda e: e.scalar_tensor_tensor(out=hi[:], in0=d2[:], scalar=ge[:, 0:1], in1=mid[:], op0=ALU.mult, op1=ALU.add), r=[d2, ge, mid], w=[hi]))
        B(lambda: kb.op("dve", lambda e: e.tensor_tensor(out=sm_[:], in0=lo[:], in1=hi[:], op=ALU.add), r=[lo, hi], w=[sm_]))
        B(lambda: kb.op("dve", lambda e: e.tensor_scalar(out=mid[:], in0=sm_[:], scalar1=0.5, scalar2=None, op0=ALU.mult), r=[sm_], w=[mid]))
    rest = []
    R = rest.append
    mask = sm([128, ntt], nm="mask")
    R(lambda: kb.op("dve", lambda e: e.tensor_scalar(out=mask[:], in0=A[:], scalar1=lo[:, 0:1], scalar2=None, op0=ALU.is_gt), r=[A, lo], w=[mask]))
    ppre = Rb[:, 16:16 + ntt]
    ptot = Rb[:, 128:128 + ntt]
    R(lambda: kb.op("pe", lambda e: e.matmul(ppre, lhsT=tri[:], rhs=mask[:], start=True, stop=True), r=[tri, mask], w=[Rb]))
    R(lambda: kb.op("pe", lambda e: e.matmul(ptot, lhsT=ones[:], rhs=mask[:], start=True, stop=True), r=[ones, mask], w=[Rb]))
    tot = sm([128, ntt], nm="tot"); cum = sm([128, ntt], nm="cum"); pos = sm([128, ntt], nm="pos")
    onesr = sm([128, ntt], nm="onesr")
    R(lambda: kb.op("dve", lambda e: e.memset(onesr[:], 1.0), w=[onesr]))
    R(lambda: kb.op("dve", lambda e: e.tensor_copy(out=tot[:], in_=ptot), r=[Rb], w=[tot]))
    R(lambda: kb.op("dve", lambda e: e.tensor_tensor_scan(out=cum[:], data0=onesr[:], data1=tot[:], initial=0.0, op0=ALU.mult, op1=ALU.add), r=[onesr, tot], w=[cum]))
    R(lambda: kb.op("dve", lambda e: e.tensor_tensor(out=cum[:], in0=cum[:], in1=tot[:], op=ALU.subtract), r=[cum, tot], w=[cum]))
    R(lambda: kb.op("dve", lambda e: e.tensor_tensor(out=pos[:], in0=ppre, in1=cum[:], op=ALU.add), r=[Rb, cum], w=[pos]))
    pen = sm([128, ntt], nm="pen"); posm = sm([128, ntt], nm="posm")
    R(lambda: kb.op("dve", lambda e: e.tensor_scalar(out=pen[:], in0=mask[:], scalar1=-BIG, scalar2=BIG, op0=ALU.mult, op1=ALU.add), r=[mask], w=[pen]))
    R(lambda: kb.op("dve", lambda e: e.tensor_tensor(out=posm[:], in0=pos[:], in1=pen[:], op=ALU.add), r=[pos, pen], w=[posm]))
    posc_ = sm([128, ntt], nm="poscl")
    R(lambda: kb.op("dve", lambda e: e.tensor_scalar(out=posc_[:], in0=posm[:], scalar1=float(cap), scalar2=None, op0=ALU.min), r=[posm], w=[posc_]))
    posi = sm([128, ntt], I32, nm="posi")
    R(lambda: kb.op("dve", lambda e: e.tensor_copy(out=posi[:], in_=posc_[:]), r=[posc_], w=[posi]))
    ns = nst * 128
    pgr = [G[i] for i in range((ns + 511) // 512)]
    ohs = [sm([128, ns], nm=f"oh{i}") for i in range(2)]
    arep = [sm([128, 128], nm=f"arep{i}") for i in range(2)]
    for tt in range(ntt):
        oh = ohs[tt % 2]; ar = arep[tt % 2]
        R(lambda tt=tt, oh=oh: kb.op("dve", lambda e: e.tensor_scalar(out=oh[:], in0=iota_s[:, 0:ns], scalar1=posm[:, tt:tt + 1], scalar2=None, op0=ALU.is_equal),
                                     r=[iota_s, posm], w=[oh]))
        R(lambda tt=tt, ar=ar: kb.op("act", lambda e: e.activation(out=ar[:], in_=ones[:], func=AF.Copy, scale=A[:, tt:tt + 1]), r=[ones, A], w=[ar]))

        def mm(tt=tt, oh=oh, ar=ar):
            for st in range(nst):
                kb.op("pe", lambda e, st=st: e.matmul(Rb[:, 256 + st:256 + st + 1], lhsT=oh[:, st * 128:(st + 1) * 128], rhs=tokid[:, tt:tt + 1],
                                                      start=(tt == 0 and st == 0), stop=(tt == ntt - 1 and st == nst - 1), skip_group_check=True), r=[oh, tokid], w=[Rb])
            for gi, pg in enumerate(pgr):
                n0 = gi * 512
                nn = min(512, ns - n0)
                kb.op("pe", lambda e, pg=pg, n0=n0, nn=nn: e.matmul(pg[:, 0:nn], lhsT=ar[:], rhs=oh[:, n0:n0 + nn], start=(tt == 0), stop=(tt == ntt - 1)),
                      r=[ar, oh], w=[pg])
        R(mm)
    idxf = sm([128, nst], nm="idxf")
    idxi = sm([128, nst], I32, nm="idxi")
    R(lambda: kb.op("dve", lambda e: e.tensor_copy(out=idxf[:], in_=Rb[:, 256:256 + nst]), r=[Rb], w=[idxf]))
    R(lambda: kb.op("dve", lambda e: e.tensor_copy(out=idxi[:], in_=idxf[:]), r=[idxf], w=[idxi]))
    grow = sm([128, ns], nm="grow")
    for gi, pg in enumerate(pgr):
        n0 = gi * 512
        nn = min(512, ns - n0)
        R(lambda pg=pg, n0=n0, nn=nn: kb.op("act", lambda e: e.copy(out=grow[:, n0:n0 + nn], in_=pg[:, 0:nn]), r=[pg], w=[grow]))
    return bis, rest, {"posi": posi, "idxi": idxi, "grow": grow}


def interleave(lists, weights=None):
    n = [len(l) for l in lists]
    pos = [0] * len(lists)
    total = max(n) if n else 0
    for step in range(total):
        for i, l in enumerate(lists):
            tgt = ((step + 1) * n[i] + total - 1) // total
            while pos[i] < min(tgt, n[i]):
                l[pos[i]]()
                pos[i] += 1


def build_k4(with_ffn=True):
    kb = KB()
    nc = kb.nc
    affl = kb.dram("affl", [2, 128, 64], F32, kind="ExternalInput")
    affc = kb.dram("affc", [2, 128, 2], F32, kind="ExternalInput")
    h2 = kb.dram("h2", [8192, D], F32, kind="ExternalInput")
    h2c = kb.dram("h2c", [256, D], F32, kind="ExternalInput")
    wg = kb.dram("wg", [2, D, D], F32, kind="ExternalInput")
    wu = kb.dram("wu", [2, D, D], F32, kind="ExternalInput")
    wd = kb.dram("wd", [2, D, D], F32, kind="ExternalInput")
    cst = kb.dram("cst", [128, 128 + 1024 + 64 + 128], F32, kind="ExternalInput")
    yeT = kb.dram("yeT", [2, D, SLOTS], F32, kind="ExternalOutput")
    posl = kb.dram("posl", [2, 128, 64], I32, kind="ExternalOutput")
    posc = kb.dram("posc", [2, 128, 2], I32, kind="ExternalOutput")
    dbg = kb.dram("dbg", [2, 128, 9], I32, kind="ExternalOutput")

    cs_ = kb.sb([128, 1344], F32, "cs_")
    kb.dma("sp", cs_[:], cst.ap(), w=[cs_])
    tri = kb.sb([128, 128], F32, "tri"); iota_s = kb.sb([128, 1024], F32, "iota_s"); tokid = kb.sb([128, 64], F32, "tokid")
    identb = kb.sb([128, 128], BF16, "identb"); ones = kb.sb([128, 128], F32, "ones")
    kb.op("dve", lambda e: e.tensor_copy(out=tri[:], in_=cs_[:, 0:128]), r=[cs_], w=[tri])
    kb.op("dve", lambda e: e.tensor_copy(out=iota_s[:], in_=cs_[:, 128:1152]), r=[cs_], w=[iota_s])
    kb.op("dve", lambda e: e.tensor_copy(out=tokid[:], in_=cs_[:, 1152:1216]), r=[cs_], w=[tokid])
    kb.op("dve", lambda e: e.tensor_copy(out=identb[:], in_=cs_[:, 1216:1344]), r=[cs_], w=[identb])
    kb.op("dve", lambda e: e.memset(ones[:], 1.0), w=[ones])

    Rb = [kb.ps([128, 512], F32, f"Rb{i}") for i in range(1)]
    G = [kb.ps([128, 512], F32, f"G{i}") for i in range(2)]
    F = [kb.ps([128, 512], F32, f"F{i}") for i in range(4)]
    ptr = kb.ps([128, 1024], BF16, "ptr")
    xsT = kb.sb([128, 16, SLOTS], BF16, "xsT")
    actT = kb.sb([128, 16, SLOTS], BF16, "actT")
    xg = [kb.sb([128, D], F32, f"xg{i}") for i in range(2)]
    xgb = [kb.sb([128, D], BF16, f"xgb{i}") for i in range(2)]
    wgs = [kb.sb([128, 16, 128], BF16, f"wgs{i}") for i in range(2)]
    wus = [kb.sb([128, 16, 128], BF16, f"wus{i}") for i in range(2)]
    sg = [kb.sb([128, 512], F32, f"sg{i}") for i in range(2)]
    tq = [kb.sb([128, 512], F32, f"tq{i}") for i in range(2)]
    ob = [kb.sb([128, SLOTS], F32, f"ob{i}") for i in range(2)]

    routes = {}
    for e2 in range(2):
        Al = kb.sb([128, 64], F32, f"Al{e2}")
        Ac = kb.sb([128, 2], F32, f"Ac{e2}")
        kb.dma("sp", Al[:], affl.ap()[e2], w=[Al])
        kb.dma("sp", Ac[:], affc.ap()[e2], w=[Ac])
        routes[(e2, 0)] = make_route(kb, Al, 64, 1024, 8, tri, ones, iota_s, tokid, f"rl{e2}_", Rb[0], G, e2 * 2)
        routes[(e2, 1)] = make_route(kb, Ac, 2, 32, 1, tri, ones, iota_s, tokid, f"rc{e2}_", Rb[0], G, e2 * 2 + 1)

    def outputs(e2):
        ol, oc = routes[(e2, 0)][2], routes[(e2, 1)][2]
        kb.dma("sp", posl.ap()[e2], ol["posi"][:], r=[ol["posi"]], w=[posl])
        kb.dma("sp", posc.ap()[e2], oc["posi"][:], r=[oc["posi"]], w=[posc])
        kb.dma("sp", dbg.ap()[e2, :, 0:8], ol["idxi"][:], r=[ol["idxi"]], w=[dbg], allow_slow_non_contiguous=True)
        kb.dma("sp", dbg.ap()[e2, :, 8:9], oc["idxi"][:], r=[oc["idxi"]], w=[dbg], allow_slow_non_contiguous=True)

    def ffn_ops(e2):
        ops = []
        Aop = ops.append
        idx_l, idx_c = routes[(e2, 0)][2]["idxi"], routes[(e2, 1)][2]["idxi"]
        grow_l, grow_c = routes[(e2, 0)][2]["grow"], routes[(e2, 1)][2]["grow"]
        for st in range(9):
            b = st % 2
            rows = 128 if st < 8 else 32

            def gather(st=st, b=b, rows=rows):
                idx_t = idx_l if st < 8 else idx_c
                idx_ap = idx_l[:, st:st + 1] if st < 8 else idx_c[0:32, 0:1]
                src = h2.ap() if st < 8 else h2c.ap()
                need = kb._need([idx_t], [xg[b]])
                kb._wait("pool", need)
                ins = nc.gpsimd.indirect_dma_start(out=xg[b][0:rows, :], out_offset=None, in_=src,
                                                   in_offset=bass.IndirectOffsetOnAxis(ap=idx_ap, axis=0))
                t = xg[b]
                if t.dsem is None:
                    t.dsem = kb.st.enter_context(nc.semaphore("d_" + t.name))
                t.dcount += 16
                ins.then_inc(t.dsem, 16)
                key = "d_" + t.name
                idx_t.r[key] = (t.dsem, t.dcount)
                t.w[key] = (t.dsem, t.dcount)
            Aop(gather)
            Aop(lambda b=b, rows=rows: kb.op("act", lambda e: e.copy(out=xgb[b][0:rows, :], in_=xg[b][0:rows, :]), r=[xg[b]], w=[xgb[b]]))
            for half in range(2):
                def tr(half=half, b=b, rows=rows, st=st):
                    for k8 in range(8):
                        kt = half * 8 + k8
                        kb.op("pe", lambda e, kt=kt, k8=k8: e.transpose(ptr[:, k8 * 128:k8 * 128 + rows], xgb[b][0:rows, kt * 128:(kt + 1) * 128], identb[0:rows, 0:rows]),
                              r=[xgb[b], identb], w=[ptr])
                    kb.op("dve", lambda e: e.tensor_copy(out=xsT[:, half * 8:(half + 1) * 8, st * 128:st * 128 + rows],
                                                         in_=ptr[:].rearrange("p (k c) -> p k c", k=8)[:, :, 0:rows]), r=[ptr], w=[xsT])
                Aop(tr)
        PA = [F[0], F[1]]
        PU = [F[2], F[3]]
        wgv = wg.ap()[e2].rearrange("(kt p) n -> p kt n", p=128)
        wuv = wu.ap()[e2].rearrange("(kt p) n -> p kt n", p=128)
        wdv = wd.ap()[e2].rearrange("(kt p) n -> p kt n", p=128)
        n = 0
        for ft in range(16):
            wgt = wgs[ft % 2]; wut = wus[ft % 2]
            Aop(lambda ft=ft, wgt=wgt, wut=wut: (kb.dma("pool", wgt[:], wgv[:, :, ft * 128:(ft + 1) * 128], w=[wgt]),
                                                  kb.dma("pool", wut[:], wuv[:, :, ft * 128:(ft + 1) * 128], w=[wut])))
            for (c0, cn) in SBLK:
                pA = PA[n % 2]; pU = PU[n % 2]; s_ = sg[n % 2]; t_ = tq[n % 2]
                n += 1

                def blk(ft=ft, wgt=wgt, wut=wut, pA=pA, pU=pU, s_=s_, t_=t_, c0=c0, cn=cn):
                    for kt in range(16):
                        kb.op("pe", lambda e, kt=kt: e.matmul(pA[:, :cn], lhsT=wgt[:, kt, :], rhs=xsT[:, kt, c0:c0 + cn], start=(kt == 0), stop=(kt == 15)),
                              r=[wgt, xsT], w=[pA])
                    for kt in range(16):
                        kb.op("pe", lambda e, kt=kt: e.matmul(pU[:, :cn], lhsT=wut[:, kt, :], rhs=xsT[:, kt, c0:c0 + cn], start=(kt == 0), stop=(kt == 15)),
                              r=[wut, xsT], w=[pU])
                    kb.op("act", lambda e: e.activation(out=s_[:, :cn], in_=pA[:, :cn], func=AF.Sigmoid), r=[pA], w=[s_])
                    kb.op("dve", lambda e: e.tensor_tensor(out=t_[:, :cn], in0=pA[:, :cn], in1=s_[:, :cn], op=ALU.mult), r=[pA, s_], w=[t_])
                    kb.op("dve", lambda e: e.tensor_tensor(out=actT[:, ft, c0:c0 + cn], in0=pU[:, :cn], in1=t_[:, :cn], op=ALU.mult), r=[pU, t_], w=[actT])
                Aop(blk)
        for dt_ in range(16):
            wdt = wgs[dt_ % 2]
            o = ob[dt_ % 2]
            Aop(lambda dt_=dt_, wdt=wdt: kb.dma("pool", wdt[:], wdv[:, :, dt_ * 128:(dt_ + 1) * 128], w=[wdt]))
            for (c0, cn) in SBLK:
                pA = PA[n % 2]
                n += 1

                def dblk(dt_=dt_, wdt=wdt, o=o, pA=pA, c0=c0, cn=cn):
                    for kt in range(16):
                        kb.op("pe", lambda e, kt=kt: e.matmul(pA[:, :cn], lhsT=wdt[:, kt, :], rhs=actT[:, kt, c0:c0 + cn], start=(kt == 0), stop=(kt == 15)),
                              r=[wdt, actT], w=[pA])
                    if c0 < 1024:
                        kb.op("dve", lambda e: e.tensor_tensor(out=o[:, c0:c0 + cn], in0=pA[:, :cn], in1=grow_l[:, c0:c0 + cn], op=ALU.mult), r=[pA, grow_l], w=[o])
                    else:
                        kb.op("dve", lambda e: e.tensor_tensor(out=o[:, c0:c0 + cn], in0=pA[:, :cn], in1=grow_c[:, 0:cn], op=ALU.mult), r=[pA, grow_c], w=[o])
                Aop(dblk)
            Aop(lambda dt_=dt_, o=o: kb.dma("sp", yeT.ap()[e2, dt_ * 128:(dt_ + 1) * 128, :], o[:], r=[o], w=[yeT]))
        return ops

    interleave([routes[k][0] for k in ((0, 0), (0, 1), (1, 0), (1, 1))])
    for th in routes[(0, 0)][1]:
        th()
    for th in routes[(0, 1)][1]:
        th()
    outputs(0)
    if with_ffn:
        interleave([routes[(1, 0)][1] + routes[(1, 1)][1], ffn_ops(0)])
    else:
        for th in routes[(1, 0)][1] + routes[(1, 1)][1]:
            th()
    outputs(1)
    if with_ffn:
        for th in ffn_ops(1):
            th()
    kb.finish()
    return kb


D = 2048


def build_k5(final):
    kb = KB()
    nc = kb.nc
    x = kb.dram("x", [1056, D], F32, kind="ExternalInput")
    yl = [kb.dram(f"yl{e}", [1025, D], F32, kind="ExternalInput") for e in range(16)]
    yc = [kb.dram(f"yc{e}", [33, D], F32, kind="ExternalInput") for e in range(16)]
    pos = kb.dram("pos", [128, 9, 16], I32, kind="ExternalInput")
    rep = kb.dram("rep", [3, 128, D], F32, kind="ExternalInput")
    xo = kb.dram("xo", [1056, D], F32, kind="ExternalOutput")
    ps_ = kb.sb([128, 9, 16], I32, "pos_sb")
    kb.dma("sp", ps_[:], pos.ap(), w=[ps_])
    reps = []
    for i in range(3):
        t = kb.sb([128, D], F32, f"rep{i}")
        kb.dma("sp" if i % 2 == 0 else "act", t[:], rep.ap()[i], w=[t])
        reps.append(t)
    G = [kb.sb([128, D], F32, f"g{i}") for i in range(4)]
    ACC = [kb.sb([128, D], F32, f"acc{i}") for i in range(2)]
    XT = [kb.sb([128, D], F32, f"xt{i}") for i in range(2)]
    ss = [kb.sb([128, 1], F32, f"ss{i}") for i in range(2)]
    sd = [kb.sb([128, 1], F32, f"sd{i}") for i in range(2)]
    junk = kb.sb([128, D], F32, "junk")
    gi = 0
    for tl in range(9):
        rows = 128 if tl < 8 else 32
        r0 = tl * 128
        acc = ACC[tl % 2]; xt = XT[tl % 2]
        kb.dma("sp", xt[0:rows, :], x.ap()[r0:r0 + rows, :], w=[xt])
        for e in range(16):
            g = G[gi % 4] if e > 0 else acc
            gi += 1
            src = yl[e] if tl < 8 else yc[e]
            need = kb._need([ps_], [g])
            kb._wait("pool", need)
            ins = nc.gpsimd.indirect_dma_start(out=g[0:rows, :], out_offset=None, in_=src.ap(),
                                               in_offset=bass.IndirectOffsetOnAxis(ap=ps_[0:rows, tl, e:e + 1], axis=0))
            if g.dsem is None:
                g.dsem = kb.st.enter_context(nc.semaphore("d_" + g.name))
            g.dcount += 16
            ins.then_inc(g.dsem, 16)
            key = "d_" + g.name
            ps_.r[key] = (g.dsem, g.dcount)
            g.w[key] = (g.dsem, g.dcount)
            if e > 0:
                eng = "dve"
                kb.op(eng, lambda en: en.tensor_tensor(out=acc[0:rows, :], in0=acc[0:rows, :], in1=g[0:rows, :], op=ALU.add), r=[acc, g], w=[acc])
        m5 = reps[0] if tl < 8 else reps[1]
        kb.op("dve", lambda en: en.tensor_tensor(out=acc[0:rows, :], in0=acc[0:rows, :], in1=m5[0:rows, :], op=ALU.mult), r=[acc, m5], w=[acc])
        kb.op("dve", lambda en: en.tensor_tensor(out=xt[0:rows, :], in0=xt[0:rows, :], in1=acc[0:rows, :], op=ALU.add), r=[xt, acc], w=[xt])
        if final and tl < 8:
            s_ = ss[tl % 2]; d_ = sd[tl % 2]
            kb.op("act", lambda en: en.activation(out=junk[0:rows, :], in_=xt[0:rows, :], func=AF.Square), r=[xt], w=[junk])
            kb.op("dve", lambda en: en.tensor_scalar(out=junk[0:rows, :], in0=junk[0:rows, :], scalar1=1.0, scalar2=0.0, op0=ALU.mult, op1=ALU.add,
                                                     accum_out=s_[0:rows, 0:1]), r=[junk], w=[junk, s_])
            kb.op("act", lambda en: en.activation(out=d_[0:rows, :], in_=s_[0:rows, :], func=AF.Sqrt, bias=1e-6, scale=1.0 / D), r=[s_], w=[d_])
            kb.op("dve", lambda en: en.reciprocal(out=s_[0:rows, :], in_=d_[0:rows, :]), r=[d_], w=[s_])
            kb.op("dve", lambda en: en.scalar_tensor_tensor(out=xt[0:rows, :], in0=xt[0:rows, :], scalar=s_[0:rows, 0:1], in1=reps[2][0:rows, :],
                                                            op0=ALU.mult, op1=ALU.mult), r=[xt, s_, reps[2]], w=[xt])
        kb.dma("act", xo.ap()[r0:r0 + rows, :], xt[0:rows, :], r=[xt], w=[xo])
    kb.finish()
    return kb

import numpy as np
QA0 = 4096
def rope_tables():
    L = 8192
    row = np.repeat(np.arange(L // 64), 64).astype(np.float32)
    col = np.tile(np.arange(64), L // 64).astype(np.float32)
    inv = (np.float32(10000.0) ** (-np.arange(16, dtype=np.float32) / np.float32(16))).astype(np.float32)
    ang = np.concatenate([row[:, None] * inv, col[:, None] * inv], -1).astype(np.float32)
    c = np.cos(ang).astype(np.float32); s = np.sin(ang).astype(np.float32)
    cosT = np.concatenate([c, c], 1).T
    sinT = np.concatenate([-s, s], 1).T
    cs = np.stack([np.concatenate([cosT, cosT], 0), np.concatenate([sinT, sinT], 0)])
    return np.ascontiguousarray(cs.astype(np.float32))
def attn_masks():
    j = np.arange(128)[:, None]; i = np.arange(128)[None, :]
    return np.ascontiguousarray(np.stack([(j >= i), (j <= i)]).astype(np.float32))
def perm64(w, nh):
    sh = w.shape[:-1]
    w = w.reshape(sh + (nh, 2, 32))
    return w[..., ::-1, :].reshape(sh + (nh * 64,))
def attn_inputs(ci, q_all, k_all, v_all, qc_all, kc_all, vc_all, sink_l, cs, masks):
    kvh = ci // 2
    q = q_all[:, ci * 128:(ci + 1) * 128]
    qp = perm64(q, 2)
    k = k_all[:, kvh * 64:(kvh + 1) * 64]; kp = perm64(k, 1)
    kd = np.concatenate([k, k], 1); kpd = np.concatenate([kp, kp], 1)
    qT = np.ascontiguousarray(np.stack([q.T, qp.T])); kT = np.ascontiguousarray(np.stack([kd.T, kpd.T]))
    kc = kc_all[:, kvh * 64:(kvh + 1) * 64]
    cx = np.ascontiguousarray(np.stack([qc_all[:, ci * 128:(ci + 1) * 128].T, np.concatenate([kc, kc], 1).T]))
    v = np.concatenate([v_all[:, kvh * 64:(kvh + 1) * 64], vc_all[:, kvh * 64:(kvh + 1) * 64]], 0)
    vtok = np.ascontiguousarray(v.reshape(66, 128, 64).transpose(1, 0, 2))
    sk = np.ascontiguousarray(np.broadcast_to(sink_l[2 * ci:2 * ci + 2][None, :], (64, 2)).astype(np.float32))
    return {"qT": qT, "kT": kT, "cs": cs, "cx": cx, "vtok": vtok, "masks": masks, "sink": sk}
GQ0 = 1024; GK0 = 1536; GV0 = 2048; GR0 = 3072
def dir_order(a, dd):
    if dd == 0: return a
    return np.concatenate([a[:256][::-1], a[256:][::-1]], 0)
def gla_consts():
    c = np.zeros((128, 704), np.float32)
    m = np.ones(512, np.float32); m[::64] = 0
    c[:, :512] = m
    j = np.arange(64)[:, None]; i = np.arange(64)[None, :]
    c[:64, 512:576] = (j <= i)
    c[:, 576:704] = np.eye(128)
    return c
def gla_inputs(ci, l, qg, kg, vg, hw1_all, d, consts):
    hd = ci // 2; dd = ci % 2
    q = dir_order(qg[:, hd * 128:(hd + 1) * 128], dd); k = dir_order(kg[:, hd * 128:(hd + 1) * 128], dd)
    v = dir_order(vg[:, hd * 256:(hd + 1) * 256], dd)
    hw = dir_order(hw1_all[:, dd * 16:(dd + 1) * 16], dd)
    return {"qk": np.ascontiguousarray(np.stack([q.T, k.T]).astype(np.float32)), "hw1": np.ascontiguousarray(hw.T.astype(np.float32)),
            "w2b": np.ascontiguousarray(d["gla_w2"][l, dd][:, hd * 128:(hd + 1) * 128]),
            "nb": np.ascontiguousarray(d["gla_b"][l, dd, hd * 128:(hd + 1) * 128].reshape(128, 1)),
            "vtok": np.ascontiguousarray(v.reshape(132, 64, 256).transpose(1, 0, 2).astype(np.float32)), "cst": consts}
def moe_consts():
    c = np.zeros((128, 1344), np.float32)
    pj = np.arange(128)
    c[:, 0:128] = (pj[:, None] < pj[None, :])
    c[:, 128:1152] = np.arange(1024)[None, :]
    c[:, 1152:1216] = (np.arange(64)[None, :] * 128 + pj[:, None])
    c[:, 1216:1344] = np.eye(128)
    return c
def moe_inputs(ci, l, aff_lat, aff_ctx, h2_tok, d, consts):
    es = [2 * ci, 2 * ci + 1]
    affl = np.stack([aff_lat[:, e].reshape(64, 128).T for e in es]).astype(np.float32)
    affc = np.stack([aff_ctx[:, e].reshape(2, 128).T for e in es]).astype(np.float32)
    return {"affl": np.ascontiguousarray(affl), "affc": np.ascontiguousarray(affc), "h2": np.ascontiguousarray(h2_tok[:8192]), "h2c": np.ascontiguousarray(h2_tok[8192:]),
            "wg": np.ascontiguousarray(d["moe_w_gate"][l, es[0]:es[0] + 2]), "wu": np.ascontiguousarray(d["moe_w_up"][l, es[0]:es[0] + 2]),
            "wd": np.ascontiguousarray(d["moe_w_down"][l, es[0]:es[0] + 2]), "cst": consts}

QA0_ = 4096
GQ0_, GK0_, GV0_, GR0_ = 1024, 1536, 2048, 3072


def _pvec(v):
    return np.ascontiguousarray(np.asarray(v, np.float32).reshape(-1, 128).T)


def _build_wext(w_in, w1):
    qa = w_in[:, QA0_:QA0_ + 1024]
    ka = w_in[:, QA0_ + 1024:QA0_ + 1280]
    W = np.zeros((2048, NW), np.float32)
    W[:, :11776] = w_in
    W[:, 11776:12800] = perm64(qa, 16)
    W[:, 12800:13056] = perm64(ka, 4)
    W[:, 13056:13072] = w1[0]
    W[:, 13072:13088] = w1[1]
    return W


def _rev_cols(a):
    return np.concatenate([a[:, :256][:, ::-1], a[:, 256:][:, ::-1]], 1)


def _s5_inputs(l, gi, uall_T, p):
    uT = np.ascontiguousarray(np.stack([uall_T, _rev_cols(uall_T)]).astype(np.float32))
    Bblk = np.zeros((2, 2, 4, 128, 128), np.float32)
    Cblk = np.zeros((2, 2, 4, 128, 128), np.float32)
    lam = np.zeros((3, 128, 8), np.float32)
    for dd in range(2):
        for q in range(4):
            for g2 in range(2):
                gl = 2 * q + g2
                g = 8 * gi + gl
                for pi, (bn, cn) in enumerate((("s5_b_re", "s5_c_re"), ("s5_b_im", "s5_c_im"))):
                    Bblk[dd, pi, q, gl * 16:(gl + 1) * 16, g2 * 64:(g2 + 1) * 64] = p[bn][l, dd, g].T
                    Cblk[dd, pi, q, g2 * 64:(g2 + 1) * 64, gl * 16:(gl + 1) * 16] = p[cn][l, dd, g].T
                lam[0, g2 * 64:(g2 + 1) * 64, dd * 4 + q] = p["s5_lam_re"][l, dd, g]
                lam[1, g2 * 64:(g2 + 1) * 64, dd * 4 + q] = p["s5_lam_im"][l, dd, g]
                lam[2, g2 * 64:(g2 + 1) * 64, dd * 4 + q] = p["s5_log_dt"][l, dd, g]
    dvec = np.ascontiguousarray(p["s5_d"][l, gi * 128:(gi + 1) * 128].reshape(128, 1))
    return {"uT": uT, "Bblk": Bblk, "Cblk": Cblk, "lam": lam, "dvec": dvec}


def _run(kb, ims):
    res = run_bass_kernel_spmd(kb.nc, ims, core_ids=list(range(NCORES)))
    return res.results


def kernel(x, c, ctx, c_ctx, ada_w, ada_b, norm1_g, norm2_g, w_in, s5_lam_re, s5_lam_im, s5_log_dt,
           s5_b_re, s5_b_im, s5_c_re, s5_c_im, s5_d, s5_w_glu, gla_w1, gla_w2, gla_b, gla_norm_g,
           attn_sink, w_branch_s5, w_branch_gla, w_branch_attn, w_out, moe_router, moe_w_gate,
           moe_w_up, moe_w_down, final_g):
    p = {k: np.asarray(v, np.float32) for k, v in dict(
        s5_lam_re=s5_lam_re, s5_lam_im=s5_lam_im, s5_log_dt=s5_log_dt, s5_b_re=s5_b_re, s5_b_im=s5_b_im,
        s5_c_re=s5_c_re, s5_c_im=s5_c_im, s5_d=s5_d, gla_w2=gla_w2, gla_b=gla_b, moe_w_gate=moe_w_gate,
        moe_w_up=moe_w_up, moe_w_down=moe_w_down).items()}
    f32 = np.float32
    x_lat = np.asarray(x, f32)[0].copy()
    xc = np.asarray(ctx, f32)[0].copy()
    ada_w = np.asarray(ada_w, f32); ada_b = np.asarray(ada_b, f32)
    cT = np.stack([np.asarray(c, f32)[0], np.asarray(c_ctx, f32)], axis=1)
    cT = np.ascontiguousarray(cT.reshape(16, 128, 2).transpose(1, 0, 2))
    ims = []
    for i in range(NCORES):
        aw = np.ascontiguousarray(ada_w[:, :, i * 1536:(i + 1) * 1536])
        ab = np.ascontiguousarray(ada_b[:, i * 1536:(i + 1) * 1536].reshape(2, 12, 128).transpose(0, 2, 1))
        ims.append({"cT": cT, "adaw": aw, "adab": ab})
    r = _run(build_k0(), ims)
    mod = np.zeros((2, 12288, 2), f32)
    for i in range(NCORES):
        mod[:, i * 1536:(i + 1) * 1536, :] = r[i]["modT"].transpose(0, 2, 1, 3).reshape(2, 1536, 2)
    cs_tab = rope_tables(); amask = attn_masks(); gconst = gla_consts(); mconst = moe_consts()

    def shard_cols(A, ci):
        return np.ascontiguousarray(np.concatenate([A[:, 256 + ci * 1024:256 + (ci + 1) * 1024], A[:, ci * 32:(ci + 1) * 32]], 1))

    for l in range(2):
        m = mod[l].reshape(6, 2048, 2)
        W = _build_wext(np.asarray(w_in[l], f32), np.asarray(gla_w1[l], f32))
        vecs = np.stack([_pvec(norm1_g[l]), _pvec(m[1, :, 0]), _pvec(m[0, :, 0]), _pvec(m[1, :, 1]), _pvec(m[0, :, 1])])
        xTs = [np.ascontiguousarray(np.concatenate([x_lat[i * 1024:(i + 1) * 1024], xc[i * 32:(i + 1) * 32]], 0).T) for i in range(NCORES)]
        r = _run(build_k1(), [{"xT": xTs[i], "W": W, "vecs": vecs} for i in range(NCORES)])
        ZT = np.empty((NW, 8448), f32)
        for i in range(NCORES):
            z = r[i]["zT"]
            ZT[:, 256 + i * 1024:256 + (i + 1) * 1024] = z[:, :1024]
            ZT[:, i * 32:(i + 1) * 32] = z[:, 1024:]
        del r, W
        r = _run(build_k2a(), [_s5_inputs(l, gi, ZT[gi * 128:(gi + 1) * 128], p) for gi in range(NCORES)])
        YS = np.empty((2, 1024, 8448), f32)
        for gi in range(NCORES):
            YS[0, gi * 128:(gi + 1) * 128] = r[gi]["yT"][0]
            YS[1, gi * 128:(gi + 1) * 128] = _rev_cols(r[gi]["yT"][1])
        ims = []
        for ci in range(NCORES):
            hd, dd = ci // 2, ci % 2
            od = (lambda a: a) if dd == 0 else _rev_cols
            q = od(ZT[GQ0_ + hd * 128:GQ0_ + (hd + 1) * 128]); k = od(ZT[GK0_ + hd * 128:GK0_ + (hd + 1) * 128])
            v = od(ZT[GV0_ + hd * 256:GV0_ + (hd + 1) * 256])
            hw = od(ZT[13056 + dd * 16:13056 + (dd + 1) * 16])
            ims.append({"qk": np.ascontiguousarray(np.stack([q, k])), "hw1": np.ascontiguousarray(hw),
                        "w2b": np.ascontiguousarray(p["gla_w2"][l, dd][:, hd * 128:(hd + 1) * 128]),
                        "nb": np.ascontiguousarray(p["gla_b"][l, dd, hd * 128:(hd + 1) * 128].reshape(128, 1)),
                        "vtok": np.ascontiguousarray(v.T.reshape(132, 64, 256).transpose(1, 0, 2)), "cst": gconst})
        r = _run(build_k2b(), ims)
        OG = np.empty((2, 1024, 8448), f32)
        for ci in range(NCORES):
            hd, dd = ci // 2, ci % 2
            o = r[ci]["oT"].reshape(256, 8448)
            OG[dd, hd * 256:(hd + 1) * 256] = o if dd == 0 else _rev_cols(o)
        ims = []
        for ci in range(NCORES):
            kvh = ci // 2
            lat = slice(256, 8448); cx_ = slice(0, 256)
            q = ZT[QA0_ + ci * 128:QA0_ + (ci + 1) * 128]; qp = ZT[11776 + ci * 128:11776 + (ci + 1) * 128]
            k = ZT[QA0_ + 1024 + kvh * 64:QA0_ + 1024 + (kvh + 1) * 64]; kp = ZT[12800 + kvh * 64:12800 + (kvh + 1) * 64]
            v = ZT[QA0_ + 1280 + kvh * 64:QA0_ + 1280 + (kvh + 1) * 64]
            qT = np.ascontiguousarray(np.stack([q[:, lat], qp[:, lat]]))
            kT = np.ascontiguousarray(np.stack([np.concatenate([k[:, lat], k[:, lat]], 0), np.concatenate([kp[:, lat], kp[:, lat]], 0)]))
            cxx = np.ascontiguousarray(np.stack([q[:, cx_], np.concatenate([k[:, cx_], k[:, cx_]], 0)]))
            vt = np.concatenate([v[:, lat], v[:, cx_]], 1).T
            vtok = np.ascontiguousarray(vt.reshape(66, 128, 64).transpose(1, 0, 2))
            sk = np.ascontiguousarray(np.broadcast_to(np.asarray(attn_sink[l], f32)[2 * ci:2 * ci + 2][None, :], (64, 2)))
            ims.append({"qT": qT, "kT": kT, "cs": cs_tab, "cx": cxx, "vtok": vtok, "masks": amask, "sink": sk})
        r = _run(build_k2c(), ims)
        YA = np.empty((1024, 8448), f32)
        for ci in range(NCORES):
            YA[ci * 128:(ci + 1) * 128, 256:] = r[ci]["yT"]
            YA[ci * 128:(ci + 1) * 128, :256] = r[ci]["ycT"]
        gng = np.zeros(2048, f32); gng[:1024] = np.asarray(gla_norm_g[l], f32).reshape(-1)
        vecs = np.stack([_pvec(gng), _pvec(m[2, :, 0]), _pvec(m[2, :, 1])])
        wbr = np.ascontiguousarray(np.stack([np.asarray(w_branch_s5[l], f32), np.asarray(w_branch_gla[l], f32), np.asarray(w_branch_attn[l], f32)]))
        wgl = np.ascontiguousarray(np.asarray(s5_w_glu[l], f32)); wo_ = np.ascontiguousarray(np.asarray(w_out[l], f32))
        ims = []
        for ci in range(NCORES):
            ims.append({"ys": np.stack([shard_cols(YS[0], ci), shard_cols(YS[1], ci)]), "og": np.stack([shard_cols(OG[0], ci), shard_cols(OG[1], ci)]),
                        "ya": shard_cols(YA, ci), "zr": shard_cols(ZT[GR0_:GR0_ + 1024], ci), "zg": shard_cols(ZT[5632:11776], ci),
                        "xT": xTs[ci], "wglu": wgl, "wbr": wbr, "wout": wo_, "vecs": vecs})
        r = _run(build_k3a(), ims)
        xmidT = [r[ci]["xo"] for ci in range(NCORES)]
        del ims, YS, OG, YA, ZT
        vecs = np.stack([_pvec(norm2_g[l]), _pvec(m[4, :, 0]), _pvec(m[3, :, 0]), _pvec(m[4, :, 1]), _pvec(m[3, :, 1])])
        rt = np.ascontiguousarray(np.asarray(moe_router[l], f32))
        r = _run(build_k3b(), [{"xT": xmidT[ci], "vecs": vecs, "rt": rt} for ci in range(NCORES)])
        h2l = np.empty((8192, 2048), f32); h2c = np.empty((256, 2048), f32)
        affl = np.empty((8192, 16), f32); affc = np.empty((256, 16), f32)
        for ci in range(NCORES):
            h = r[ci]["h2"]; a = r[ci]["aff"]
            h2l[ci * 1024:(ci + 1) * 1024] = h[:, :1024].T; h2c[ci * 32:(ci + 1) * 32] = h[:, 1024:].T
            affl[ci * 1024:(ci + 1) * 1024] = a[:, :1024].T; affc[ci * 32:(ci + 1) * 32] = a[:, 1024:].T
        ims = []
        for ci in range(NCORES):
            es = [2 * ci, 2 * ci + 1]
            ims.append({"affl": np.ascontiguousarray(np.stack([affl[:, e].reshape(64, 128).T for e in es])),
                        "affc": np.ascontiguousarray(np.stack([affc[:, e].reshape(2, 128).T for e in es])),
                        "h2": h2l, "h2c": h2c,
                        "wg": np.ascontiguousarray(p["moe_w_gate"][l, es[0]:es[0] + 2]), "wu": np.ascontiguousarray(p["moe_w_up"][l, es[0]:es[0] + 2]),
                        "wd": np.ascontiguousarray(p["moe_w_down"][l, es[0]:es[0] + 2]), "cst": mconst})
        r = _run(build_k4(), ims)
        del ims
        yl = []; yc = []
        posl = np.empty((16, 128, 64), np.int32); posc = np.empty((16, 128, 2), np.int32)
        zrow = np.zeros((1, 2048), f32)
        for ci in range(NCORES):
            for e2 in range(2):
                e = 2 * ci + e2
                yt = r[ci]["yeT"][e2]
                yl.append(np.ascontiguousarray(np.concatenate([yt[:, :1024].T, zrow], 0)))
                yc.append(np.ascontiguousarray(np.concatenate([yt[:, 1024:1056].T, zrow], 0)))
                posl[e] = r[ci]["posl"][e2]; posc[e] = r[ci]["posc"][e2]
        rep = np.ascontiguousarray(np.stack([np.broadcast_to(m[5, :, 0], (128, 2048)), np.broadcast_to(m[5, :, 1], (128, 2048)),
                                             np.broadcast_to(np.asarray(final_g, f32), (128, 2048))]).astype(f32))
        ims = []
        for cj in range(NCORES):
            pos = np.full((128, 9, 16), 32, np.int32)
            pos[:, 0:8, :] = posl[:, :, 8 * cj:8 * cj + 8].transpose(1, 2, 0)
            pos[0:32, 8, :] = posc[:, (cj % 4) * 32:(cj % 4) * 32 + 32, cj // 4].T
            im = {"x": np.ascontiguousarray(xmidT[cj].T), "pos": np.ascontiguousarray(pos), "rep": rep}
            for e in range(16):
                im[f"yl{e}"] = yl[e]; im[f"yc{e}"] = yc[e]
            ims.append(im)
        r = _run(build_k5(l == 1), ims)
        del ims
        for cj in range(NCORES):
            xo = r[cj]["xo"]
            x_lat[cj * 1024:(cj + 1) * 1024] = xo[:1024]
            xc[cj * 32:(cj + 1) * 32] = xo[1024:]
    return x_lat.reshape(1, 8192, 2048).astype(np.float32)
```

```python
import numpy as np
import ml_dtypes
from contextlib import ExitStack
import concourse.bass as bass
import concourse.mybir as mybir
from concourse.bass_utils import run_bass_kernel_spmd

F32 = mybir.dt.float32
BF16 = mybir.dt.bfloat16
I32 = mybir.dt.int32
U32 = mybir.dt.uint32
AF = mybir.ActivationFunctionType
ALU = mybir.AluOpType
AX = mybir.AxisListType
NPBF16 = ml_dtypes.bfloat16
NCORES = 8


class T:
    def __init__(self, name, h):
        self.name = name
        self.h = h
        self.w = {}
        self.r = {}
        self.gen_need = {}
        self.dsem = None
        self.dcount = 0

    def __getitem__(self, idx):
        return self.h[idx]

    def ap(self):
        return self.h.ap() if hasattr(self.h, "ap") else self.h[:]


class KB:
    def __init__(self, same_engine_sync=True):
        self.nc = bass.Bass("TRN2", target_bir_lowering=False)
        nc = self.nc
        self.st = ExitStack()
        self.E = {"pe": nc.tensor, "act": nc.scalar, "dve": nc.vector, "pool": nc.gpsimd, "sp": nc.sync}
        self.sem = {}
        self.cnt = {}
        self.known = {}
        for e in self.E:
            self.sem[e] = self.st.enter_context(nc.semaphore("s_" + e))
            self.cnt[e] = 0
            self.known[e] = {}
        self.qsem = {}
        self.qcnt = {}
        for q in ("sp", "act", "pool"):
            self.qsem[q] = self.st.enter_context(nc.semaphore("q_" + q))
            self.qcnt[q] = 0
        self.ses = same_engine_sync
        self.outs = []
        self.nid = 0

    def sb(self, shape, dt=F32, name=None):
        self.nid += 1
        name = name or f"t{self.nid}"
        h = self.st.enter_context(self.nc.sbuf_tensor(name, list(shape), dt))
        return T(name, h)

    def ps(self, shape, dt=F32, name=None):
        self.nid += 1
        name = name or f"p{self.nid}"
        h = self.st.enter_context(self.nc.psum_tensor(name, list(shape), dt))
        return T(name, h)

    def dram(self, name, shape, dt=F32, kind="Internal"):
        h = self.nc.dram_tensor(name, list(shape), dt, kind=kind)
        t = T(name, h)
        if kind == "ExternalOutput":
            self.outs.append(t)
        return t

    def _need(self, r, w, dkey=None):
        need = {}

        def add(key, sem, val):
            if key not in need or need[key][1] < val:
                need[key] = (sem, val)

        for t in r:
            for k, (s, v) in t.w.items():
                add(k, s, v)
        for t in w:
            for k, (s, v) in t.w.items():
                if dkey is not None and k == dkey:
                    continue
                add(k, s, v)
            for k, (s, v) in t.r.items():
                add(k, s, v)
        return need

    def _wait(self, e, need):
        eng = self.E[e]
        for key, (sem, val) in need.items():
            if self.known[e].get(key, 0) >= val:
                continue
            if key == e and (not self.ses or e == "pe"):
                continue
            eng.wait_ge(sem, val)
            self.known[e][key] = val

    def op(self, e, fn, r=(), w=()):
        need = self._need(r, w)
        self._wait(e, need)
        ins = fn(self.E[e])
        self.cnt[e] += 1
        ins.then_inc(self.sem[e], 1)
        ev = (self.sem[e], self.cnt[e])
        for t in r:
            t.r[e] = ev
        for t in w:
            t.w[e] = ev
        return ins

    def dma(self, q, out_ap, in_ap, r=(), w=(), **kw):
        wt = w[0] if w else None
        dkey = None
        if wt is not None and wt.dsem is None:
            wt.dsem = self.st.enter_context(self.nc.semaphore("d_" + wt.name))
        if wt is not None:
            dkey = "d_" + wt.name
        need = self._need(r, w, dkey)
        self._wait(q, need)
        ins = self.E[q].dma_start(out=out_ap, in_=in_ap, **kw)
        if wt is not None:
            wt.dcount += 16
            sem, val, key = wt.dsem, wt.dcount, dkey
        else:
            self.qcnt[q] += 16
            sem, val, key = self.qsem[q], self.qcnt[q], "q_" + q
        ins.then_inc(sem, 16)
        for t in r:
            t.r[key] = (sem, val)
        for t in w:
            t.w[key] = (sem, val)
        return ins

    def finish(self):
        need = {}
        for t in self.outs:
            for k, (s, v) in t.w.items():
                if k not in need or need[k][1] < v:
                    need[k] = (s, v)
        self._wait("sp", need)
        for q in self.qsem:
            if self.qcnt[q]:
                self.E["sp"].wait_ge(self.qsem[q], self.qcnt[q])
        return self.nc


def run(kb, in_maps, trace=False):
    res = run_bass_kernel_spmd(kb.nc, in_maps, core_ids=list(range(len(in_maps))), trace=trace)
    return res


D = 2048
KT = 16
NW = 13184
TOK = 1056
BLKS = [(0, 512), (512, 512), (1024, 32)]
EPS = 1e-6


def build_k0():
    kb = KB()
    cT = kb.dram("cT", [128, KT, 2], F32, kind="ExternalInput")
    adaw = kb.dram("adaw", [2, D, 1536], F32, kind="ExternalInput")
    adab = kb.dram("adab", [2, 128, 12], F32, kind="ExternalInput")
    modT = kb.dram("modT", [2, 128, 12, 2], F32, kind="ExternalOutput")
    c_sb = kb.sb([128, KT, 2], F32, "c_sb")
    sc = kb.sb([128, KT, 2], F32, "sc")
    kb.dma("sp", c_sb[:], cT.ap(), w=[c_sb])
    kb.op("act", lambda e: e.activation(out=sc[:], in_=c_sb[:], func=AF.Silu), r=[c_sb], w=[sc])
    wts = [kb.sb([128, KT, 1536], F32, f"w{l}") for l in range(2)]
    for l in range(2):
        src = adaw.ap()[l].rearrange("(kt p) n -> p kt n", p=128)
        for kt in range(KT):
            kb.dma("sp" if kt % 2 == 0 else "act", wts[l][:, kt, :], src[:, kt, :], w=[wts[l]])
    for l in range(2):
        b_sb = kb.sb([128, 12], F32, f"b{l}")
        kb.dma("sp", b_sb[:], adab.ap()[l], w=[b_sb])
        pt = kb.ps([128, 12, 2], F32, f"pm{l}")
        for j in range(12):
            for kt in range(KT):
                kb.op("pe", lambda e, j=j, kt=kt: e.matmul(pt[:, j, :], lhsT=wts[l][:, kt, j * 128:(j + 1) * 128],
                                                            rhs=sc[:, kt, :], start=(kt == 0), stop=(kt == KT - 1)),
                      r=[wts[l], sc], w=[pt])
        o = kb.sb([128, 12, 2], F32, f"o{l}")
        for col in range(2):
            kb.op("dve", lambda e, col=col: e.tensor_tensor(out=o[:, :, col], in0=pt[:, :, col], in1=b_sb[:], op=ALU.add),
                  r=[pt, b_sb], w=[o])
        kb.dma("sp", modT.ap()[l], o[:], r=[o], w=[modT])
    kb.finish()
    return kb


def emit_norm_mod(kb, xs, hT, g_sb, sc_l, sh_l, sc_c, sh_c, ones, tagp=""):
    A_l = kb.sb([128, KT], F32, tagp + "A_l")
    A_c = kb.sb([128, KT], F32, tagp + "A_c")
    kb.op("dve", lambda e: e.scalar_tensor_tensor(out=A_l[:], in0=sc_l[:], scalar=1.0, in1=g_sb[:], op0=ALU.add, op1=ALU.mult),
          r=[sc_l, g_sb], w=[A_l])
    kb.op("dve", lambda e: e.scalar_tensor_tensor(out=A_c[:], in0=sc_c[:], scalar=1.0, in1=g_sb[:], op0=ALU.add, op1=ALU.mult),
          r=[sc_c, g_sb], w=[A_c])
    rstd = kb.sb([128, TOK], F32, tagp + "rstd")
    sqs = [kb.sb([128, 512], F32, tagp + f"sq{i}") for i in range(2)]
    pss = [kb.ps([128, 512], F32, tagp + f"pss{i}") for i in range(2)]
    n = 0
    for bi, (c0, cn) in enumerate(BLKS):
        ps = pss[bi % 2]
        for kt in range(KT):
            sq = sqs[n % 2]
            n += 1
            kb.op("act", lambda e, kt=kt, sq=sq: e.activation(out=sq[:, :cn], in_=xs[:, kt, c0:c0 + cn], func=AF.Square),
                  r=[xs], w=[sq])
            kb.op("pe", lambda e, kt=kt, sq=sq: e.matmul(ps[:, :cn], lhsT=ones[:], rhs=sq[:, :cn], start=(kt == 0), stop=(kt == KT - 1)),
                  r=[ones, sq], w=[ps])
        sd = sqs[n % 2]
        n += 1
        kb.op("act", lambda e: e.activation(out=sd[:, :cn], in_=ps[:, :cn], func=AF.Sqrt, bias=EPS, scale=1.0 / D),
              r=[ps], w=[sd])
        kb.op("dve", lambda e: e.reciprocal(out=rstd[:, c0:c0 + cn], in_=sd[:, :cn]), r=[sd], w=[rstd])
    tmps = [kb.sb([128, 512], F32, tagp + f"tmp{i}") for i in range(2)]
    n = 0
    for bi, (c0, cn) in enumerate(BLKS):
        A = A_c if bi == 2 else A_l
        Bt = sh_c if bi == 2 else sh_l
        for kt in range(KT):
            tmp = tmps[n % 2]
            n += 1
            kb.op("dve", lambda e, kt=kt, tmp=tmp: e.tensor_tensor(out=tmp[:, :cn], in0=xs[:, kt, c0:c0 + cn], in1=rstd[:, c0:c0 + cn], op=ALU.mult),
                  r=[xs, rstd], w=[tmp])
            kb.op("act", lambda e, kt=kt, tmp=tmp, A=A, Bt=Bt: e.activation(out=hT[:, kt, c0:c0 + cn], in_=tmp[:, :cn], func=AF.Identity,
                                                                    bias=Bt[:, kt:kt + 1], scale=A[:, kt:kt + 1]),
                  r=[tmp, A, Bt], w=[hT])


def emit_linear(kb, hT, W, ntiles, out_dram, kt_n=KT, tagp="", evac=None):
    wbs = [kb.sb([128, kt_n, 128], BF16, tagp + f"wb{i}") for i in range(3)]
    pps = [kb.ps([128, 512], F32, tagp + f"pp{i}") for i in range(4)]
    obs = [kb.sb([128, TOK], F32, tagp + f"ob{i}") for i in range(2)]
    Wv = W.ap().rearrange("(kt p) n -> p kt n", p=128)
    pi = 0
    for nt in range(ntiles):
        wb = wbs[nt % 3]
        kb.dma("pool", wb[:], Wv[:, :, nt * 128:(nt + 1) * 128], w=[wb])
        ob = obs[nt % 2]
        for bi, (c0, cn) in enumerate(BLKS):
            pp = pps[pi % 4]
            pi += 1
            for kt in range(kt_n):
                kb.op("pe", lambda e, kt=kt, pp=pp, wb=wb: e.matmul(pp[:, :cn], lhsT=wb[:, kt, :], rhs=hT[:, kt, c0:c0 + cn],
                                                             start=(kt == 0), stop=(kt == kt_n - 1)),
                      r=[wb, hT], w=[pp])
            eng = "act" if pi % 2 == 0 else "dve"
            if eng == "act":
                kb.op("act", lambda e, pp=pp, ob=ob: e.copy(out=ob[:, c0:c0 + cn], in_=pp[:, :cn]), r=[pp], w=[ob])
            else:
                kb.op("dve", lambda e, pp=pp, ob=ob: e.tensor_copy(out=ob[:, c0:c0 + cn], in_=pp[:, :cn]), r=[pp], w=[ob])
        kb.dma("sp", out_dram.ap()[nt * 128:(nt + 1) * 128, :], ob[:], r=[ob], w=[out_dram])


def build_k1(ntiles=NW // 128):
    kb = KB()
    xT = kb.dram("xT", [D, TOK], F32, kind="ExternalInput")
    W = kb.dram("W", [D, ntiles * 128], F32, kind="ExternalInput")
    vecs = kb.dram("vecs", [5, 128, KT], F32, kind="ExternalInput")
    zT = kb.dram("zT", [ntiles * 128, TOK], F32, kind="ExternalOutput")
    xs = kb.sb([128, KT, TOK], F32, "xs")
    xv = xT.ap().rearrange("(kt p) t -> p kt t", p=128)
    for kt in range(KT):
        kb.dma("sp" if kt % 2 == 0 else "act", xs[:, kt, :], xv[:, kt, :], w=[xs])
    vt = []
    for i in range(5):
        t = kb.sb([128, KT], F32, f"vec{i}")
        kb.dma("sp", t[:], vecs.ap()[i], w=[t])
        vt.append(t)
    ones = kb.sb([128, 128], F32, "ones")
    kb.op("dve", lambda e: e.memset(ones[:], 1.0), w=[ones])
    hT = kb.sb([128, KT, TOK], BF16, "hT")
    emit_norm_mod(kb, xs, hT, vt[0], vt[1], vt[2], vt[3], vt[4], ones)
    emit_linear(kb, hT, W, ntiles, zT)
    kb.finish()
    return kb

import math

LTOT = 8448
TC = 256
NCH = LTOT // TC
NTD = 8


def build_k2a():
    kb = KB()
    uT = kb.dram("uT", [2, 128, LTOT], F32, kind="ExternalInput")
    Bblk = kb.dram("Bblk", [2, 2, 4, 128, 128], F32, kind="ExternalInput")
    Cblk = kb.dram("Cblk", [2, 2, 4, 128, 128], F32, kind="ExternalInput")
    lam = kb.dram("lam", [3, 128, NTD], F32, kind="ExternalInput")
    dvec = kb.dram("dvec", [128, 1], F32, kind="ExternalInput")
    yT = kb.dram("yT", [2, 128, LTOT], F32, kind="ExternalOutput")

    ubf = [kb.sb([128, LTOT], BF16, f"ubf{d}") for d in range(2)]
    for d in range(2):
        for c0 in range(0, LTOT, 2048):
            cn = min(2048, LTOT - c0)
            kb.dma("pool", ubf[d][:, c0:c0 + cn], uT.ap()[d, :, c0:c0 + cn], w=[ubf[d]])
    dv = kb.sb([128, 1], F32, "dv")
    kb.dma("sp", dv[:], dvec.ap(), w=[dv])
    Bb = kb.sb([128, 2, 2, 4, 128], BF16, "Bb")
    for d in range(2):
        for p in range(2):
            kb.dma("pool", Bb[:, d, p], Bblk.ap()[d, p].rearrange("q k n -> k q n"), w=[Bb])
    Cf = kb.sb([128, 2, 2, 4, 128], F32, "Cf")
    for d in range(2):
        for p in range(2):
            kb.dma("act", Cf[:, d, p], Cblk.ap()[d, p].rearrange("q k n -> k q n"), w=[Cf])
    Cb = kb.sb([128, 2, 2, 4, 128], BF16, "Cb")
    Cn = kb.sb([128, 2, 4, 128], BF16, "Cn")
    for d in range(2):
        kb.op("dve", lambda e, d=d: e.tensor_scalar(out=Cn[:, d], in0=Cf[:, d, 0], scalar1=-1.0, scalar2=None, op0=ALU.mult), r=[Cf], w=[Cn])
        kb.op("dve", lambda e, d=d: e.tensor_copy(out=Cb[:, d, 0], in_=Cf[:, d, 0]), r=[Cf], w=[Cb])
        kb.op("dve", lambda e, d=d: e.tensor_scalar(out=Cb[:, d, 1], in0=Cf[:, d, 1], scalar1=-1.0, scalar2=None, op0=ALU.mult),
              r=[Cf], w=[Cb])
    lr = kb.sb([128, NTD], F32, "lr")
    li = kb.sb([128, NTD], F32, "li")
    ldt = kb.sb([128, NTD], F32, "ldt")
    kb.dma("sp", lr[:], lam.ap()[0], w=[lr])
    kb.dma("sp", li[:], lam.ap()[1], w=[li])
    kb.dma("sp", ldt[:], lam.ap()[2], w=[ldt])

    nid = [0]

    def sm(name=None):
        nid[0] += 1
        return kb.sb([128, NTD], F32, name or f"sm{nid[0]}")

    def tt(out, a, b, op):
        kb.op("dve", lambda e: e.tensor_tensor(out=out[:], in0=a[:], in1=b[:], op=op), r=[a, b], w=[out])

    def ts(out, a, s1, op0, s2=None, op1=None):
        if op1 is None:
            kb.op("dve", lambda e: e.tensor_scalar(out=out[:], in0=a[:], scalar1=s1, scalar2=None, op0=op0), r=[a], w=[out])
        else:
            kb.op("dve", lambda e: e.tensor_scalar(out=out[:], in0=a[:], scalar1=s1, scalar2=s2, op0=op0, op1=op1), r=[a], w=[out])

    dt = sm("dt")
    kb.op("act", lambda e: e.activation(out=dt[:], in_=ldt[:], func=AF.Exp), r=[ldt], w=[dt])
    lrd = sm()
    tt(lrd, lr, dt, ALU.mult)
    mag = sm("mag")
    kb.op("act", lambda e: e.activation(out=mag[:], in_=lrd[:], func=AF.Exp), r=[lrd], w=[mag])
    ang = sm("ang")
    tt(ang, li, dt, ALU.mult)
    kf = sm()
    ts(kf, ang, 1.0 / (2 * math.pi), ALU.mult)
    ki = kb.sb([128, NTD], I32, "ki")
    kb.op("dve", lambda e: e.tensor_copy(out=ki[:], in_=kf[:]), r=[kf], w=[ki])
    kf2 = sm()
    kb.op("dve", lambda e: e.tensor_copy(out=kf2[:], in_=ki[:]), r=[ki], w=[kf2])
    C1 = 6.28125
    C2 = 2 * math.pi - C1
    r1 = sm()
    kb.op("dve", lambda e: e.scalar_tensor_tensor(out=r1[:], in0=kf2[:], scalar=-C1, in1=ang[:], op0=ALU.mult, op1=ALU.add),
          r=[kf2, ang], w=[r1])
    r2 = sm()
    kb.op("dve", lambda e: e.scalar_tensor_tensor(out=r2[:], in0=kf2[:], scalar=-C2, in1=r1[:], op0=ALU.mult, op1=ALU.add),
          r=[kf2, r1], w=[r2])
    xx = sm("xx")
    ts(xx, r2, 0.125, ALU.mult)
    x2 = sm("x2")
    tt(x2, xx, xx, ALU.mult)

    def horner(coefs):
        p = sm()
        ts(p, x2, -1.0 / coefs[-1], ALU.mult, 1.0, ALU.add)
        for cf in reversed(coefs[:-1]):
            q_ = sm()
            tt(q_, p, x2, ALU.mult)
            p = sm()
            ts(p, q_, -1.0 / cf, ALU.mult, 1.0, ALU.add)
        return p

    ps_ = horner([6.0, 20.0, 42.0, 72.0, 110.0, 156.0])
    sn = sm("sn")
    tt(sn, ps_, xx, ALU.mult)
    cs = horner([2.0, 12.0, 30.0, 56.0, 90.0, 132.0])
    for _ in range(3):
        c2 = sm(); s2 = sm(); sc_ = sm()
        tt(c2, cs, cs, ALU.mult)
        tt(s2, sn, sn, ALU.mult)
        tt(sc_, sn, cs, ALU.mult)
        cs = sm(); sn = sm()
        tt(cs, c2, s2, ALU.subtract)
        ts(sn, sc_, 2.0, ALU.mult)
    ab_re = sm("ab_re"); ab_im = sm("ab_im")
    tt(ab_re, mag, cs, ALU.mult)
    tt(ab_im, mag, sn, ALU.mult)
    den = sm(); t_a = sm(); t_b = sm()
    tt(t_a, lr, lr, ALU.mult)
    tt(t_b, li, li, ALU.mult)
    tt(den, t_a, t_b, ALU.add)
    rden = sm()
    kb.op("dve", lambda e: e.reciprocal(out=rden[:], in_=den[:]), r=[den], w=[rden])
    nr = sm()
    ts(nr, ab_re, -1.0, ALU.add)
    f_re = sm("f_re"); f_im = sm("f_im")
    u1 = sm(); u2 = sm(); u3 = sm()
    tt(u1, nr, lr, ALU.mult)
    tt(u2, ab_im, li, ALU.mult)
    tt(u3, u1, u2, ALU.add)
    tt(f_re, u3, rden, ALU.mult)
    v1 = sm(); v2 = sm(); v3 = sm()
    tt(v1, ab_im, lr, ALU.mult)
    tt(v2, nr, li, ALU.mult)
    tt(v3, v1, v2, ALU.subtract)
    tt(f_im, v3, rden, ALU.mult)
    nsn_unused = None

    Er = kb.sb([128, NTD, TC + 1], F32, "Er")
    Ei = kb.sb([128, NTD, TC + 1], F32, "Ei")
    kb.op("dve", lambda e: e.memset(Er[:, :, 0:1], 1.0), w=[Er])
    kb.op("dve", lambda e: e.memset(Ei[:, :, 0:1], 0.0), w=[Ei])
    pr, pi_ = cs, sn
    n = 1
    while n <= TC:
        m = min(n, TC + 1 - n)
        npi = sm()
        ts(npi, pi_, -1.0, ALU.mult)
        ta = kb.sb([128, NTD, m], F32, f"eta{n}")
        tb = kb.sb([128, NTD, m], F32, f"etb{n}")
        for td in range(NTD):
            kb.op("dve", lambda e, td=td: e.tensor_scalar(out=ta[:, td, :], in0=Er[:, td, 0:m], scalar1=pr[:, td:td + 1], scalar2=None, op0=ALU.mult),
                  r=[Er, pr], w=[ta])
            kb.op("dve", lambda e, td=td: e.scalar_tensor_tensor(out=Er[:, td, n:n + m], in0=Ei[:, td, 0:m], scalar=npi[:, td:td + 1], in1=ta[:, td, :],
                                                                 op0=ALU.mult, op1=ALU.add), r=[Ei, npi, ta], w=[Er])
            kb.op("dve", lambda e, td=td: e.tensor_scalar(out=tb[:, td, :], in0=Er[:, td, 0:m], scalar1=pi_[:, td:td + 1], scalar2=None, op0=ALU.mult),
                  r=[Er, pi_], w=[tb])
            kb.op("dve", lambda e, td=td: e.scalar_tensor_tensor(out=Ei[:, td, n:n + m], in0=Ei[:, td, 0:m], scalar=pr[:, td:td + 1], in1=tb[:, td, :],
                                                                 op0=ALU.mult, op1=ALU.add), r=[Ei, pr, tb], w=[Ei])
        c2 = sm(); s2 = sm(); sc_ = sm()
        tt(c2, pr, pr, ALU.mult)
        tt(s2, pi_, pi_, ALU.mult)
        tt(sc_, pr, pi_, ALU.mult)
        pr = sm(); pi_ = sm()
        tt(pr, c2, s2, ALU.subtract)
        ts(pi_, sc_, 2.0, ALU.mult)
        n *= 2
    Rr = kb.sb([128, NTD, TC], F32, "Rr")
    Ri = kb.sb([128, NTD, TC], F32, "Ri")
    rmul = kb.sb([128, NTD * TC], F32, "rmul")
    rmul3 = rmul[:].rearrange("p (n t) -> p n t", t=TC)
    onesT = kb.sb([128, TC], F32, "onesT")
    kb.op("dve", lambda e: e.memset(onesT[:], 1.0), w=[onesT])
    nf_re = sm()
    ts(nf_re, f_re, -1.0, ALU.mult)
    tr = kb.sb([128, NTD, TC], F32, "trtmp")
    for td in range(NTD):
        kb.op("dve", lambda e, td=td: e.tensor_scalar(out=tr[:, td, :], in0=Er[:, td, 0:TC], scalar1=f_re[:, td:td + 1], scalar2=None, op0=ALU.mult),
              r=[Er, f_re], w=[tr])
        kb.op("dve", lambda e, td=td: e.scalar_tensor_tensor(out=Rr[:, td, :], in0=Ei[:, td, 0:TC], scalar=f_im[:, td:td + 1], in1=tr[:, td, :],
                                                             op0=ALU.mult, op1=ALU.add), r=[Ei, f_im, tr], w=[Rr])
        kb.op("dve", lambda e, td=td: e.tensor_scalar(out=tr[:, td, :], in0=Er[:, td, 0:TC], scalar1=f_im[:, td:td + 1], scalar2=None, op0=ALU.mult),
              r=[Er, f_im], w=[tr])
        kb.op("dve", lambda e, td=td: e.scalar_tensor_tensor(out=Ri[:, td, :], in0=Ei[:, td, 0:TC], scalar=nf_re[:, td:td + 1], in1=tr[:, td, :],
                                                             op0=ALU.mult, op1=ALU.add), r=[Ei, nf_re, tr], w=[Ri])
        kb.op("dve", lambda e, td=td: e.tensor_scalar(out=rmul3[:, td, :], in0=onesT[:], scalar1=mag[:, td:td + 1], scalar2=None, op0=ALU.mult),
              r=[onesT, mag], w=[rmul])
    kb.op("dve", lambda e: e.memset(rmul3[:, :, 0:1], 0.0), w=[rmul])

    Q4 = 4 * TC
    pX = [kb.ps([128, Q4], F32, f"pX{d}") for d in range(2)]
    pY = [kb.ps([128, 512], F32, f"pY{d}") for d in range(2)]
    W = {}
    for nm in ("xre", "xim", "t1", "t2", "t3", "t4", "gr", "gi"):
        W[nm] = [kb.sb([128, Q4], F32, f"w_{nm}{i}") for i in range(2)]
    for nm in ("b1", "b2", "b3", "b4"):
        W[nm] = [kb.sb([128, Q4], BF16, f"w_{nm}{i}") for i in range(2)]
    yo = [[kb.sb([128, TC], F32, f"yo{d}_{i}") for i in range(2)] for d in range(2)]
    ufc = [kb.sb([128, TC], F32, f"ufc{i}") for i in range(2)]
    carry = [[kb.sb([128, 8], F32, f"carry{d}_{i}") for i in range(2)] for d in range(2)]
    for d in range(2):
        kb.op("dve", lambda e, d=d: e.memset(carry[d][0][:], 0.0), w=[carry[d][0]])
    ctmp = [kb.sb([128, 16], F32, f"ctmp{d}") for d in range(2)]

    def v3(t):
        return t[:].rearrange("p (q t) -> p q t", t=TC)

    def body(d, c):
        ops = []
        A = ops.append
        qs = slice(d * 4, d * 4 + 4)
        c0 = c * TC
        py = pY[d]
        px = pX[d]
        w = {k: v[d] for k, v in W.items()}
        if d == 0:
            A(lambda: kb.dma("sp", ufc[c % 2][:], uT.ap()[0, :, c0:c0 + TC], w=[ufc[c % 2]]))

        def mm_in(p):
            for q in range(4):
                kb.op("pe", lambda e, q=q: e.matmul(px[:, q * TC:(q + 1) * TC], lhsT=Bb[:, d, p, q, :], rhs=ubf[d][:, c0:c0 + TC], start=True, stop=True),
                      r=[Bb, ubf[d]], w=[px])
        A(lambda: mm_in(0))
        A(lambda: kb.op("act", lambda e: e.copy(out=w["xre"][:], in_=px[:]), r=[px], w=[w["xre"]]))
        A(lambda: mm_in(1))
        A(lambda: kb.op("act", lambda e: e.copy(out=w["xim"][:], in_=px[:]), r=[px], w=[w["xim"]]))
        A(lambda: kb.op("dve", lambda e: e.tensor_tensor(out=v3(w["t1"]), in0=v3(w["xre"]), in1=Rr[:, qs, :], op=ALU.mult), r=[w["xre"], Rr], w=[w["t1"]]))
        A(lambda: kb.op("dve", lambda e: e.tensor_tensor(out=v3(w["t2"]), in0=v3(w["xim"]), in1=Ri[:, qs, :], op=ALU.mult), r=[w["xim"], Ri], w=[w["t2"]]))
        A(lambda: kb.op("dve", lambda e: e.tensor_tensor(out=v3(w["t3"]), in0=v3(w["xre"]), in1=Ri[:, qs, :], op=ALU.mult), r=[w["xre"], Ri], w=[w["t3"]]))
        A(lambda: kb.op("dve", lambda e: e.tensor_tensor(out=v3(w["t4"]), in0=v3(w["xim"]), in1=Rr[:, qs, :], op=ALU.mult), r=[w["xim"], Rr], w=[w["t4"]]))
        cin = carry[d][c % 2]
        cout = carry[d][(c + 1) % 2]
        ct = ctmp[d]
        A(lambda: kb.op("dve", lambda e: e.tensor_tensor(out=ct[:, 0:4], in0=cin[:, 0:4], in1=mag[:, qs], op=ALU.mult), r=[cin, mag], w=[ct]))
        A(lambda: kb.op("dve", lambda e: e.tensor_tensor(out=ct[:, 4:8], in0=cin[:, 4:8], in1=mag[:, qs], op=ALU.mult), r=[cin, mag], w=[ct]))
        A(lambda: kb.op("dve", lambda e: e.tensor_tensor(out=w["t1"][:], in0=w["t1"][:], in1=w["t2"][:], op=ALU.subtract), r=[w["t1"], w["t2"]], w=[w["t1"]]))
        A(lambda: kb.op("dve", lambda e: e.tensor_tensor(out=w["t3"][:], in0=w["t3"][:], in1=w["t4"][:], op=ALU.add), r=[w["t3"], w["t4"]], w=[w["t3"]]))
        A(lambda: kb.op("dve", lambda e: e.tensor_tensor(out=v3(w["t1"])[:, :, 0], in0=v3(w["t1"])[:, :, 0], in1=ct[:, 0:4], op=ALU.add), r=[w["t1"], ct], w=[w["t1"]]))
        A(lambda: kb.op("dve", lambda e: e.tensor_tensor(out=v3(w["t3"])[:, :, 0], in0=v3(w["t3"])[:, :, 0], in1=ct[:, 4:8], op=ALU.add), r=[w["t3"], ct], w=[w["t3"]]))
        A(lambda: kb.op("dve", lambda e: e.tensor_tensor_scan(out=w["gr"][:], data0=rmul[:, d * Q4:(d + 1) * Q4], data1=w["t1"][:], initial=0.0,
                                                              op0=ALU.mult, op1=ALU.add), r=[rmul, w["t1"]], w=[w["gr"]]))
        A(lambda: kb.op("dve", lambda e: e.tensor_tensor_scan(out=w["gi"][:], data0=rmul[:, d * Q4:(d + 1) * Q4], data1=w["t3"][:], initial=0.0,
                                                              op0=ALU.mult, op1=ALU.add), r=[rmul, w["t3"]], w=[w["gi"]]))
        grl = v3(w["gr"])[:, :, TC - 1]
        gil = v3(w["gi"])[:, :, TC - 1]
        ETr = Er[:, qs, TC]
        ETi = Ei[:, qs, TC]
        A(lambda: kb.op("dve", lambda e: e.tensor_tensor(out=ct[:, 12:16], in0=grl, in1=ETi, op=ALU.mult), r=[w["gr"], Ei], w=[ct]))
        A(lambda: kb.op("dve", lambda e: e.tensor_tensor(out=cout[:, 0:4], in0=grl, in1=ETr, op=ALU.mult), r=[w["gr"], Er], w=[cout]))
        A(lambda: kb.op("dve", lambda e: e.tensor_tensor(out=ct[:, 8:12], in0=gil, in1=ETi, op=ALU.mult), r=[w["gi"], Ei], w=[ct]))
        A(lambda: kb.op("dve", lambda e: e.tensor_tensor(out=cout[:, 4:8], in0=gil, in1=ETr, op=ALU.mult), r=[w["gi"], Er], w=[cout]))
        A(lambda: kb.op("dve", lambda e: e.tensor_tensor(out=cout[:, 0:4], in0=cout[:, 0:4], in1=ct[:, 8:12], op=ALU.subtract), r=[cout, ct], w=[cout]))
        A(lambda: kb.op("dve", lambda e: e.tensor_tensor(out=cout[:, 4:8], in0=cout[:, 4:8], in1=ct[:, 12:16], op=ALU.add), r=[cout, ct], w=[cout]))
        A(lambda: kb.op("dve", lambda e: e.tensor_tensor(out=v3(w["b1"]), in0=v3(w["gr"]), in1=Er[:, qs, 0:TC], op=ALU.mult), r=[w["gr"], Er], w=[w["b1"]]))
        A(lambda: kb.op("dve", lambda e: e.tensor_tensor(out=v3(w["b2"]), in0=v3(w["gi"]), in1=Ei[:, qs, 0:TC], op=ALU.mult), r=[w["gi"], Ei], w=[w["b2"]]))
        A(lambda: kb.op("dve", lambda e: e.tensor_tensor(out=v3(w["b3"]), in0=v3(w["gr"]), in1=Ei[:, qs, 0:TC], op=ALU.mult), r=[w["gr"], Ei], w=[w["b3"]]))
        A(lambda: kb.op("dve", lambda e: e.tensor_tensor(out=v3(w["b4"]), in0=v3(w["gi"]), in1=Er[:, qs, 0:TC], op=ALU.mult), r=[w["gi"], Er], w=[w["b4"]]))

        def mm_out():
            n = 0
            for q in range(4):
                for (lh, src) in ((Cb[:, d, 0, q, :], "b1"), (Cn[:, d, q, :], "b2"), (Cb[:, d, 1, q, :], "b3"), (Cb[:, d, 1, q, :], "b4")):
                    kb.op("pe", lambda e, lh=lh, src=src, q=q, n=n: e.matmul(py[:, :TC], lhsT=lh, rhs=w[src][:, q * TC:(q + 1) * TC], start=(n == 0), stop=(n == 15)),
                          r=[Cb, Cn, w[src]], w=[py])
                    n += 1
        A(mm_out)
        o = yo[d][c % 2]
        if d == 0:
            A(lambda: kb.op("dve", lambda e: e.scalar_tensor_tensor(out=o[:], in0=ufc[c % 2][:], scalar=dv[:, 0:1], in1=py[:, :TC], op0=ALU.mult, op1=ALU.add),
                            r=[ufc[c % 2], dv, py], w=[o]))
        else:
            A(lambda: kb.op("act", lambda e: e.copy(out=o[:], in_=py[:, :TC]), r=[py], w=[o]))
        A(lambda: kb.dma("sp", yT.ap()[d, :, c0:c0 + TC], o[:], r=[o], w=[yT]))
        return ops

    for c in range(NCH):
        l0 = body(0, c)
        l1 = body(1, c)
        for i in range(max(len(l0), len(l1))):
            if i < len(l0):
                l0[i]()
            if i < len(l1):
                l1[i]()
    kb.finish()
    return kb


LT = 8448
NC64 = LT // 64


def build_k2b():
    kb = KB()
    qk = kb.dram("qk", [2, 128, LT], F32, kind="ExternalInput")
    hw1 = kb.dram("hw1", [16, LT], F32, kind="ExternalInput")
    w2b = kb.dram("w2b", [16, 128], F32, kind="ExternalInput")
    nb = kb.dram("nb", [128, 1], F32, kind="ExternalInput")
    vtok = kb.dram("vtok", [64, NC64, 256], F32, kind="ExternalInput")
    cst = kb.dram("cst", [128, 512 + 64 + 128], F32, kind="ExternalInput")
    oT = kb.dram("oT", [2, 128, LT], F32, kind="ExternalOutput")

    vb = kb.sb([64, NC64, 256], BF16, "vb")
    for c0 in range(0, NC64, 8):
        c1 = min(NC64, c0 + 8)
        kb.dma("pool", vb[:, c0:c1, :], vtok.ap()[:, c0:c1, :], w=[vb])
    cs_ = kb.sb([128, 704], F32, "cs_")
    kb.dma("sp", cs_[:], cst.ap(), w=[cs_])
    identb = kb.sb([128, 128], BF16, "identb")
    kb.op("dve", lambda e: e.tensor_copy(out=identb[:], in_=cs_[:, 576:704]), r=[cs_], w=[identb])
    w2s = kb.sb([16, 128], F32, "w2s")
    kb.dma("sp", w2s[:], w2b.ap(), w=[w2s])
    bs_ = kb.sb([128, 1], F32, "bs_")
    kb.dma("sp", bs_[:], nb.ap(), w=[bs_])
    nbs = kb.sb([128, 1], F32, "nbs")
    kb.op("dve", lambda e: e.tensor_scalar(out=nbs[:], in0=bs_[:], scalar1=-1.0, scalar2=None, op0=ALU.mult), r=[bs_], w=[nbs])
    qe = kb.sb([128, LT], BF16, "qe")
    ke = kb.sb([128, LT], BF16, "ke")
    kl = kb.sb([128, LT], BF16, "kl")
    ebl = kb.sb([128, NC64], F32, "ebl")

    BL = 512
    nblk = (LT + BL - 1) // BL
    A = {}
    for nm in ("q", "k", "e1", "la", "bc", "eb", "enb", "kef"):
        A[nm] = [kb.sb([128, BL], F32, f"a_{nm}{i}") for i in range(2)]
    A["h"] = [kb.sb([16, BL], F32, f"a_h{i}") for i in range(2)]
    pso = [kb.ps([128, 512], F32, f"pso{i}") for i in range(2)]
    pg = pso
    scale = 128 ** -0.5
    for bi in range(nblk):
        c0 = bi * BL
        cn = min(BL, LT - c0)
        b = bi % 2
        a = {k: v[b] for k, v in A.items()}
        kb.dma("sp", a["q"][:, :cn], qk.ap()[0, :, c0:c0 + cn], w=[a["q"]])
        kb.dma("act", a["k"][:, :cn], qk.ap()[1, :, c0:c0 + cn], w=[a["k"]])
        kb.dma("sp", a["h"][:, :cn], hw1.ap()[:, c0:c0 + cn], w=[a["h"]])
        kb.op("pe", lambda e: e.matmul(pg[b][:, :cn], lhsT=w2s[:], rhs=a["h"][:, :cn], start=True, stop=True), r=[w2s, a["h"]], w=[pg[b]])
        kb.op("act", lambda e: e.activation(out=a["e1"][:, :cn], in_=pg[b][:, :cn], func=AF.Exp, bias=nbs[:, 0:1], scale=-1.0),
              r=[pg[b], nbs], w=[a["e1"]])
        kb.op("act", lambda e: e.activation(out=a["la"][:, :cn], in_=a["e1"][:, :cn], func=AF.Ln, bias=1.0), r=[a["e1"]], w=[a["la"]])
        kb.op("dve", lambda e: e.tensor_scalar(out=a["la"][:, :cn], in0=a["la"][:, :cn], scalar1=-1.0 / 16.0, scalar2=None, op0=ALU.mult),
              r=[a["la"]], w=[a["la"]])
        kb.op("dve", lambda e: e.tensor_tensor_scan(out=a["bc"][:, :cn], data0=cs_[:, 0:cn], data1=a["la"][:, :cn], initial=0.0,
                                                    op0=ALU.mult, op1=ALU.add), r=[cs_, a["la"]], w=[a["bc"]])
        kb.op("act", lambda e: e.activation(out=a["eb"][:, :cn], in_=a["bc"][:, :cn], func=AF.Exp), r=[a["bc"]], w=[a["eb"]])
        kb.op("act", lambda e: e.activation(out=a["enb"][:, :cn], in_=a["bc"][:, :cn], func=AF.Exp, scale=-1.0), r=[a["bc"]], w=[a["enb"]])
        kb.op("dve", lambda e: e.scalar_tensor_tensor(out=qe[:, c0:c0 + cn], in0=a["q"][:, :cn], scalar=scale, in1=a["eb"][:, :cn],
                                                      op0=ALU.mult, op1=ALU.mult), r=[a["q"], a["eb"]], w=[qe])
        kb.op("dve", lambda e: e.tensor_tensor(out=a["kef"][:, :cn], in0=a["k"][:, :cn], in1=a["enb"][:, :cn], op=ALU.mult),
              r=[a["k"], a["enb"]], w=[a["kef"]])
        kb.op("act", lambda e: e.copy(out=ke[:, c0:c0 + cn], in_=a["kef"][:, :cn]), r=[a["kef"]], w=[ke])
        nch = cn // 64
        ch0 = c0 // 64
        kb.op("dve", lambda e: e.tensor_copy(out=ebl[:, ch0:ch0 + nch],
                                             in_=a["eb"][:, :cn].rearrange("p (c j) -> p c j", j=64)[:, :, 63]), r=[a["eb"]], w=[ebl])
        for cc in range(nch):
            kb.op("dve", lambda e, cc=cc: e.tensor_scalar(out=kl[:, c0 + cc * 64:c0 + (cc + 1) * 64], in0=a["kef"][:, cc * 64:(cc + 1) * 64],
                                                          scalar1=ebl[:, ch0 + cc:ch0 + cc + 1], scalar2=None, op0=ALU.mult),
                  r=[a["kef"], ebl], w=[kl])

    S = [kb.sb([128, 256], F32, f"S{i}") for i in range(2)]
    Sb = [kb.sb([128, 256], BF16, f"Sb{i}") for i in range(2)]
    kb.op("dve", lambda e: e.memset(S[0][:], 0.0), w=[S[0]])
    kb.op("dve", lambda e: e.memset(Sb[0][:], 0.0), w=[Sb[0]])
    psc = [kb.ps([64, 512], F32, f"psc{i}") for i in range(2)]
    ptr = [kb.ps([64, 128], BF16, f"ptr{i}") for i in range(2)]
    pst = [kb.ps([128, 512], F32, f"pst{i}") for i in range(1)]
    pm = [kb.sb([64, 64], BF16, f"pm{i}") for i in range(2)]
    klT = [kb.sb([64, 128], BF16, f"klT{i}") for i in range(2)]
    GB = 8
    ost = [kb.sb([128, 2, GB * 64], F32, f"ost{i}") for i in range(2)]
    for c in range(NC64):
        b = c % 2
        cols = slice(c * 64, (c + 1) * 64)
        So, Sn = S[c % 2], S[(c + 1) % 2]
        Sbo, Sbn = Sb[c % 2], Sb[(c + 1) % 2]
        kb.op("pe", lambda e: e.matmul(psc[b][:, 0:64], lhsT=ke[:, cols], rhs=qe[:, cols], start=True, stop=True), r=[ke, qe], w=[psc[b]])
        kb.op("dve", lambda e: e.tensor_tensor(out=pm[b][:], in0=psc[b][:, 0:64], in1=cs_[0:64, 512:576], op=ALU.mult), r=[psc[b], cs_], w=[pm[b]])
        kb.op("pe", lambda e: e.transpose(ptr[b][:], kl[:, cols], identb[:]), r=[kl, identb], w=[ptr[b]])
        kb.op("act", lambda e: e.copy(out=klT[b][:], in_=ptr[b][:]), r=[ptr[b]], w=[klT[b]])
        for vt in range(2):
            kb.op("pe", lambda e, vt=vt: e.matmul(pso[b][:, vt * 64:(vt + 1) * 64], lhsT=vb[:, c, vt * 128:(vt + 1) * 128], rhs=pm[b][:], start=True, stop=False),
                  r=[vb, pm[b]], w=[pso[b]])
            kb.op("pe", lambda e, vt=vt: e.matmul(pso[b][:, vt * 64:(vt + 1) * 64], lhsT=Sbo[:, vt * 128:(vt + 1) * 128], rhs=qe[:, cols], start=False, stop=True),
                  r=[Sbo, qe], w=[pso[b]])
        o = ost[(c // GB) % 2]
        oc = (c % GB) * 64
        kb.op("act", lambda e: e.copy(out=o[:, :, oc:oc + 64], in_=pso[b][:, 0:128].rearrange("p (v i) -> p v i", v=2)), r=[pso[b]], w=[o])
        kb.op("pe", lambda e: e.matmul(pst[0][:, 0:256], lhsT=klT[b][:], rhs=vb[:, c, :], start=True, stop=True), r=[klT[b], vb], w=[pst[0]])
        kb.op("dve", lambda e: e.scalar_tensor_tensor(out=Sn[:], in0=So[:], scalar=ebl[:, c:c + 1], in1=pst[0][:, 0:256], op0=ALU.mult, op1=ALU.add),
              r=[So, ebl, pst[0]], w=[Sn])
        kb.op("act", lambda e: e.copy(out=Sbn[:], in_=Sn[:]), r=[Sn], w=[Sbn])
        if c % GB == GB - 1 or c == NC64 - 1:
            g0 = (c // GB) * GB * 64
            gn = (c % GB + 1) * 64
            for vt in range(2):
                kb.dma("sp", oT.ap()[vt, :, g0:g0 + gn], o[:, vt, 0:gn], r=[o], w=[oT])
    kb.finish()
    return kb


L = 8192
NBQ = 64


def build_k2c():
    kb = KB()
    qT = kb.dram("qT", [2, 128, L], F32, kind="ExternalInput")
    kT = kb.dram("kT", [2, 128, L], F32, kind="ExternalInput")
    cs = kb.dram("cs", [2, 128, L], F32, kind="ExternalInput")
    cx = kb.dram("cx", [2, 128, 256], F32, kind="ExternalInput")
    vtok = kb.dram("vtok", [128, 66, 64], F32, kind="ExternalInput")
    masks = kb.dram("masks", [2, 128, 128], F32, kind="ExternalInput")
    sink = kb.dram("sink", [64, 2], F32, kind="ExternalInput")
    yT = kb.dram("yT", [128, L], F32, kind="ExternalOutput")
    ycT = kb.dram("ycT", [128, 256], F32, kind="ExternalOutput")

    qr = kb.sb([128, L + 256], BF16, "qr")
    kr = kb.sb([128, L + 256], BF16, "kr")
    vb = kb.sb([128, 66, 64], BF16, "vb")
    kb.dma("pool", vb[:], vtok.ap(), w=[vb])
    mk = kb.sb([128, 2, 128], BF16, "mk")
    kb.dma("pool", mk[:], masks.ap().rearrange("m j i -> j m i"), w=[mk])
    onesb = kb.sb([128, 64], BF16, "onesb")
    kb.op("dve", lambda e: e.memset(onesb[:], 1.0), w=[onesb])
    sk = kb.sb([64, 2], F32, "sk")
    kb.dma("sp", sk[:], sink.ap(), w=[sk])
    es = kb.sb([64, 2], F32, "es")
    kb.op("act", lambda e: e.activation(out=es[:], in_=sk[:], func=AF.Exp), r=[sk], w=[es])
    kb.dma("pool", qr[:, L:L + 256], cx.ap()[0], w=[qr])
    kb.dma("pool", kr[:, L:L + 256], cx.ap()[1], w=[kr])
    RB = 1024
    bufs = {}
    for nm in ("a", "ap", "c", "s", "t1", "t2"):
        bufs[nm] = [kb.sb([128, RB], F32, f"r_{nm}{i}") for i in range(2)]
    it = 0
    for src, dst in ((qT, qr), (kT, kr)):
        for c0 in range(0, L, RB):
            b = it % 2
            it += 1
            a, ap_, c_, s_, t1, t2 = (bufs[nm][b] for nm in ("a", "ap", "c", "s", "t1", "t2"))
            kb.dma("sp", a[:], src.ap()[0, :, c0:c0 + RB], w=[a])
            kb.dma("act", ap_[:], src.ap()[1, :, c0:c0 + RB], w=[ap_])
            kb.dma("sp", c_[:], cs.ap()[0, :, c0:c0 + RB], w=[c_])
            kb.dma("act", s_[:], cs.ap()[1, :, c0:c0 + RB], w=[s_])
            kb.op("dve", lambda e: e.tensor_tensor(out=t1[:], in0=a[:], in1=c_[:], op=ALU.mult), r=[a, c_], w=[t1])
            kb.op("dve", lambda e: e.tensor_tensor(out=t2[:], in0=ap_[:], in1=s_[:], op=ALU.mult), r=[ap_, s_], w=[t2])
            kb.op("dve", lambda e: e.tensor_tensor(out=dst[:, c0:c0 + RB], in0=t1[:], in1=t2[:], op=ALU.add), r=[t1, t2], w=[dst])

    ps1 = [kb.ps([128, 512], F32, f"ps1_{i}") for i in range(2)]
    ps2 = [kb.ps([128, 512], F32, f"ps2_{i}") for i in range(2)]
    po = [kb.ps([64, 512], F32, f"po{i}") for i in range(2)]
    pb1 = [kb.sb([128, 512], BF16, f"pb1_{i}") for i in range(2)]
    pb2 = [kb.sb([128, 128], BF16, f"pb2_{i}") for i in range(2)]
    den = [kb.sb([64, 128], F32, f"den{i}") for i in range(2)]
    rden = [kb.sb([64, 128], F32, f"rden{i}") for i in range(2)]
    GB = 8
    ob = [kb.sb([64, 2, GB * 128], F32, f"ob{i}") for i in range(2)]
    it = 0
    nblocks = NBQ + 2
    for n in range(nblocks):
        isctx = n >= NBQ
        o = ob[(n // GB) % 2]
        for hh in range(2):
            b = it % 2
            it += 1
            hs = slice(hh * 64, hh * 64 + 64)
            if not isctx:
                qcols = slice(n * 128, (n + 1) * 128)
                tiles = []
                if n > 0:
                    tiles.append((0, n - 1, 0))
                tiles.append((1, n, None))
                if n < NBQ - 1:
                    tiles.append((2, n + 1, 1))
                tiles.append((3, 64, None))
                tiles.append((4, 65, None))
            else:
                qcols = slice(L + (n - NBQ) * 128, L + (n - NBQ + 1) * 128)
                tiles = [(3, 64, None), (4, 65, None)]
            p1, p2 = ps1[b], ps2[b]
            for slot, kt, m in tiles:
                dstp = p1[:, slot * 128:(slot + 1) * 128] if slot < 4 else p2[:, 0:128]
                pt = p1 if slot < 4 else p2
                kb.op("pe", lambda e: e.matmul(dstp, lhsT=kr[hs, kt * 128:(kt + 1) * 128], rhs=qr[hs, qcols], start=True, stop=True),
                      r=[kr, qr], w=[pt])
            slots = [t[0] for t in tiles if t[0] < 4]
            lo, hi = min(slots) * 128, (max(slots) + 1) * 128
            kb.op("act", lambda e: e.activation(out=pb1[b][:, lo:hi], in_=p1[:, lo:hi], func=AF.Exp, scale=0.125), r=[p1], w=[pb1[b]])
            kb.op("act", lambda e: e.activation(out=pb2[b][:], in_=p2[:, 0:128], func=AF.Exp, scale=0.125), r=[p2], w=[pb2[b]])
            for slot, kt, m in tiles:
                if m is not None:
                    kb.op("dve", lambda e: e.tensor_tensor(out=pb1[b][:, slot * 128:(slot + 1) * 128], in0=pb1[b][:, slot * 128:(slot + 1) * 128],
                                                           in1=mk[:, m, :], op=ALU.mult), r=[pb1[b], mk], w=[pb1[b]])
            pp = po[b]
            for ti, (slot, kt, m) in enumerate(tiles):
                src = pb1[b][:, slot * 128:(slot + 1) * 128] if slot < 4 else pb2[b][:]
                st = pb1[b] if slot < 4 else pb2[b]
                kb.op("pe", lambda e: e.matmul(pp[:, 0:128], lhsT=vb[:, kt, :], rhs=src, start=(ti == 0), stop=(ti == len(tiles) - 1)),
                      r=[vb, st], w=[pp])
            for ti, (slot, kt, m) in enumerate(tiles):
                src = pb1[b][:, slot * 128:(slot + 1) * 128] if slot < 4 else pb2[b][:]
                st = pb1[b] if slot < 4 else pb2[b]
                kb.op("pe", lambda e: e.matmul(pp[:, 128:256], lhsT=onesb[:], rhs=src, start=(ti == 0), stop=(ti == len(tiles) - 1)),
                      r=[onesb, st], w=[pp])
            kb.op("dve", lambda e: e.tensor_scalar(out=den[b][:], in0=pp[:, 128:256], scalar1=es[:, hh:hh + 1], scalar2=None, op0=ALU.add),
                  r=[pp, es], w=[den[b]])
            kb.op("dve", lambda e: e.reciprocal(out=rden[b][:], in_=den[b][:]), r=[den[b]], w=[rden[b]])
            oc = (n % GB) * 128
            kb.op("dve", lambda e: e.tensor_tensor(out=o[:, hh, oc:oc + 128], in0=pp[:, 0:128], in1=rden[b][:], op=ALU.mult),
                  r=[pp, rden[b]], w=[o])
        if n < NBQ and n % GB == GB - 1:
            g0 = (n // GB) * GB * 128
            for hh in range(2):
                kb.dma("sp", yT.ap()[hh * 64:(hh + 1) * 64, g0:g0 + GB * 128], o[:, hh, :], r=[o], w=[yT])
        if n == nblocks - 1:
            for hh in range(2):
                kb.dma("sp", ycT.ap()[hh * 64:(hh + 1) * 64, :], o[:, hh, 0:256], r=[o], w=[ycT])
    kb.finish()
    return kb


def build_k3a():
    kb = KB()
    ys = kb.dram("ys", [2, 1024, TOK], F32, kind="ExternalInput")
    og = kb.dram("og", [2, 1024, TOK], F32, kind="ExternalInput")
    ya = kb.dram("ya", [1024, TOK], F32, kind="ExternalInput")
    zr = kb.dram("zr", [1024, TOK], F32, kind="ExternalInput")
    zg = kb.dram("zg", [6144, TOK], F32, kind="ExternalInput")
    xT = kb.dram("xT", [D, TOK], F32, kind="ExternalInput")
    wglu = kb.dram("wglu", [1024, 1024], F32, kind="ExternalInput")
    wbr = kb.dram("wbr", [3, 1024, D], F32, kind="ExternalInput")
    wout = kb.dram("wout", [D, D], F32, kind="ExternalInput")
    vecs = kb.dram("vecs", [3, 128, KT], F32, kind="ExternalInput")
    xo = kb.dram("xo", [D, TOK], F32, kind="ExternalOutput")

    vt_ = []
    for i in range(3):
        t = kb.sb([128, KT], F32, f"vec{i}")
        kb.dma("sp", t[:], vecs.ap()[i], w=[t])
        vt_.append(t)
    gn, m2l, m2c = vt_
    ones = kb.sb([128, 128], F32, "ones")
    kb.op("dve", lambda e: e.memset(ones[:], 1.0), w=[ones])
    PS = [kb.ps([128, 512], F32, f"ps{i}") for i in range(6)]
    psi = [0]

    def nps():
        psi[0] += 1
        return PS[psi[0] % 6]

    W = {}

    def wk(nm, n=2):
        if nm not in W:
            W[nm] = [[kb.sb([128, 512], F32, f"w_{nm}{i}") for i in range(n)], 0]
        W[nm][1] += 1
        return W[nm][0][W[nm][1] % len(W[nm][0])]

    ys5 = kb.sb([128, 8, TOK], BF16, "ys5")
    ygla = kb.sb([128, 8, TOK], BF16, "ygla")
    yatt = kb.sb([128, 8, TOK], BF16, "yatt")
    for kt in range(8):
        kb.dma("pool", yatt[:, kt, :], ya.ap()[kt * 128:(kt + 1) * 128, :], w=[yatt])

    gf = kb.sb([128, 8, TOK], F32, "gf")
    gb = kb.sb([128, 8, TOK], BF16, "gb")
    for kt in range(8):
        for (c0, cn) in BLKS:
            a = wk("a"); b = wk("b"); y = wk("y"); t = wk("t"); s = wk("s")
            kb.dma("sp", a[:, :cn], ys.ap()[0, kt * 128:(kt + 1) * 128, c0:c0 + cn], w=[a])
            kb.dma("act", b[:, :cn], ys.ap()[1, kt * 128:(kt + 1) * 128, c0:c0 + cn], w=[b])
            kb.op("dve", lambda e: e.tensor_tensor(out=y[:, :cn], in0=a[:, :cn], in1=b[:, :cn], op=ALU.add), r=[a, b], w=[y])
            kb.op("dve", lambda e: e.tensor_tensor(out=t[:, :cn], in0=y[:, :cn], in1=y[:, :cn], op=ALU.mult), r=[y], w=[t])
            kb.op("dve", lambda e: e.tensor_scalar(out=t[:, :cn], in0=t[:, :cn], scalar1=0.044715, scalar2=1.0, op0=ALU.mult, op1=ALU.add), r=[t], w=[t])
            kb.op("dve", lambda e: e.tensor_tensor(out=t[:, :cn], in0=t[:, :cn], in1=y[:, :cn], op=ALU.mult), r=[t, y], w=[t])
            kb.op("act", lambda e: e.activation(out=s[:, :cn], in_=t[:, :cn], func=AF.Sigmoid, scale=1.5957691216057308), r=[t], w=[s])
            kb.op("dve", lambda e: e.tensor_tensor(out=gf[:, kt, c0:c0 + cn], in0=y[:, :cn], in1=s[:, :cn], op=ALU.mult), r=[y, s], w=[gf])
            kb.op("dve", lambda e: e.tensor_copy(out=gb[:, kt, c0:c0 + cn], in_=gf[:, kt, c0:c0 + cn]), r=[gf], w=[gb])
    wgs = [kb.sb([128, 8, 128], BF16, f"wg{i}") for i in range(2)]
    wgv = wglu.ap().rearrange("(kt p) n -> p kt n", p=128)
    for nt in range(8):
        wg = wgs[nt % 2]
        kb.dma("pool", wg[:], wgv[:, :, nt * 128:(nt + 1) * 128], w=[wg])
        for (c0, cn) in BLKS:
            ps = nps()
            for kt in range(8):
                kb.op("pe", lambda e, kt=kt: e.matmul(ps[:, :cn], lhsT=wg[:, kt, :], rhs=gb[:, kt, c0:c0 + cn], start=(kt == 0), stop=(kt == 7)),
                      r=[wg, gb], w=[ps])
            s = wk("s")
            kb.op("act", lambda e: e.activation(out=s[:, :cn], in_=ps[:, :cn], func=AF.Sigmoid), r=[ps], w=[s])
            kb.op("dve", lambda e: e.tensor_tensor(out=ys5[:, nt, c0:c0 + cn], in0=gf[:, nt, c0:c0 + cn], in1=s[:, :cn], op=ALU.mult), r=[gf, s], w=[ys5])

    of = gf
    for hd in range(4):
        for (c0, cn) in BLKS:
            ps = nps()
            for v in range(2):
                kt = hd * 2 + v
                a = wk("a"); b = wk("b"); sq = wk("y")
                kb.dma("sp", a[:, :cn], og.ap()[0, kt * 128:(kt + 1) * 128, c0:c0 + cn], w=[a])
                kb.dma("act", b[:, :cn], og.ap()[1, kt * 128:(kt + 1) * 128, c0:c0 + cn], w=[b])
                kb.op("dve", lambda e: e.tensor_tensor(out=of[:, kt, c0:c0 + cn], in0=a[:, :cn], in1=b[:, :cn], op=ALU.add), r=[a, b], w=[of])
                kb.op("act", lambda e: e.activation(out=sq[:, :cn], in_=of[:, kt, c0:c0 + cn], func=AF.Square), r=[of], w=[sq])
                kb.op("pe", lambda e: e.matmul(ps[:, :cn], lhsT=ones[:], rhs=sq[:, :cn], start=(v == 0), stop=(v == 1)), r=[ones, sq], w=[ps])
            sd = wk("t"); rs = wk("s", 3)
            kb.op("act", lambda e: e.activation(out=sd[:, :cn], in_=ps[:, :cn], func=AF.Sqrt, bias=EPS, scale=1.0 / 256), r=[ps], w=[sd])
            kb.op("dve", lambda e: e.reciprocal(out=rs[:, :cn], in_=sd[:, :cn]), r=[sd], w=[rs])
            for v in range(2):
                kt = hd * 2 + v
                r_ = wk("a"); sg = wk("b"); sr = wk("y"); yy = wk("t")
                kb.dma("sp", r_[:, :cn], zr.ap()[kt * 128:(kt + 1) * 128, c0:c0 + cn], w=[r_])
                kb.op("act", lambda e: e.activation(out=sg[:, :cn], in_=r_[:, :cn], func=AF.Sigmoid), r=[r_], w=[sg])
                kb.op("dve", lambda e: e.tensor_tensor(out=sr[:, :cn], in0=r_[:, :cn], in1=sg[:, :cn], op=ALU.mult), r=[r_, sg], w=[sr])
                kb.op("dve", lambda e: e.tensor_tensor(out=yy[:, :cn], in0=of[:, kt, c0:c0 + cn], in1=rs[:, :cn], op=ALU.mult), r=[of, rs], w=[yy])
                kb.op("dve", lambda e: e.scalar_tensor_tensor(out=ygla[:, kt, c0:c0 + cn], in0=yy[:, :cn], scalar=gn[:, kt:kt + 1], in1=sr[:, :cn],
                                                              op0=ALU.mult, op1=ALU.mult), r=[yy, gn, sr], w=[ygla])

    mb = kb.sb([128, KT, TOK], BF16, "mb")
    wbs = [[kb.sb([128, 8, 128], BF16, f"wb{b}_{i}") for i in range(2)] for b in range(3)]
    ysrc = [ys5, ygla, yatt]
    for nt in range(KT):
        wts = []
        for b in range(3):
            wt = wbs[b][nt % 2]
            kb.dma("pool", wt[:], wbr.ap()[b].rearrange("(kt p) n -> p kt n", p=128)[:, :, nt * 128:(nt + 1) * 128], w=[wt])
            wts.append(wt)
        for (c0, cn) in BLKS:
            pss = []
            for b in range(3):
                ps = nps()
                for kt in range(8):
                    kb.op("pe", lambda e, kt=kt, b=b: e.matmul(ps[:, :cn], lhsT=wts[b][:, kt, :], rhs=ysrc[b][:, kt, c0:c0 + cn], start=(kt == 0), stop=(kt == 7)),
                          r=[wts[b], ysrc[b]], w=[ps])
                pss.append(ps)
            acc = wk("acc")
            for b in range(3):
                gt = wk("a"); sg = wk("b"); tm = wk("y")
                kb.dma("sp" if b != 1 else "act", gt[:, :cn], zg.ap()[b * 2048 + nt * 128:b * 2048 + (nt + 1) * 128, c0:c0 + cn], w=[gt])
                kb.op("act", lambda e: e.activation(out=sg[:, :cn], in_=gt[:, :cn], func=AF.Sigmoid), r=[gt], w=[sg])
                if b == 0:
                    kb.op("dve", lambda e: e.tensor_tensor(out=acc[:, :cn], in0=pss[b][:, :cn], in1=sg[:, :cn], op=ALU.mult), r=[pss[b], sg], w=[acc])
                else:
                    kb.op("dve", lambda e: e.tensor_tensor(out=tm[:, :cn], in0=pss[b][:, :cn], in1=sg[:, :cn], op=ALU.mult), r=[pss[b], sg], w=[tm])
                    if b == 1:
                        kb.op("dve", lambda e: e.tensor_tensor(out=acc[:, :cn], in0=acc[:, :cn], in1=tm[:, :cn], op=ALU.add), r=[acc, tm], w=[acc])
                    else:
                        kb.op("dve", lambda e: e.tensor_tensor(out=mb[:, nt, c0:c0 + cn], in0=acc[:, :cn], in1=tm[:, :cn], op=ALU.add), r=[acc, tm], w=[mb])

    wos = [kb.sb([128, KT, 128], BF16, f"wo{i}") for i in range(2)]
    xbs = [kb.sb([128, TOK], F32, f"xb{i}") for i in range(2)]
    wov = wout.ap().rearrange("(kt p) n -> p kt n", p=128)
    for nt in range(KT):
        wo = wos[nt % 2]
        xb = xbs[nt % 2]
        kb.dma("pool", wo[:], wov[:, :, nt * 128:(nt + 1) * 128], w=[wo])
        kb.dma("sp", xb[:], xT.ap()[nt * 128:(nt + 1) * 128, :], w=[xb])
        for bi, (c0, cn) in enumerate(BLKS):
            ps = nps()
            for kt in range(KT):
                kb.op("pe", lambda e, kt=kt: e.matmul(ps[:, :cn], lhsT=wo[:, kt, :], rhs=mb[:, kt, c0:c0 + cn], start=(kt == 0), stop=(kt == KT - 1)),
                      r=[wo, mb], w=[ps])
            mm = m2c if bi == 2 else m2l
            kb.op("dve", lambda e: e.scalar_tensor_tensor(out=xb[:, c0:c0 + cn], in0=ps[:, :cn], scalar=mm[:, nt:nt + 1], in1=xb[:, c0:c0 + cn],
                                                          op0=ALU.mult, op1=ALU.add), r=[ps, mm, xb], w=[xb])
        kb.dma("act", xo.ap()[nt * 128:(nt + 1) * 128, :], xb[:], r=[xb], w=[xo])
    kb.finish()
    return kb


def build_k3b():
    kb = KB()
    xT = kb.dram("xT", [D, TOK], F32, kind="ExternalInput")
    vecs = kb.dram("vecs", [5, 128, KT], F32, kind="ExternalInput")
    rt = kb.dram("rt", [D, 16], F32, kind="ExternalInput")
    h2 = kb.dram("h2", [D, TOK], F32, kind="ExternalOutput")
    aff = kb.dram("aff", [16, TOK], F32, kind="ExternalOutput")
    xs = kb.sb([128, KT, TOK], F32, "xs")
    xv = xT.ap().rearrange("(kt p) t -> p kt t", p=128)
    for kt in range(KT):
        kb.dma("sp" if kt % 2 == 0 else "act", xs[:, kt, :], xv[:, kt, :], w=[xs])
    vt = []
    for i in range(5):
        t = kb.sb([128, KT], F32, f"vec{i}")
        kb.dma("sp", t[:], vecs.ap()[i], w=[t])
        vt.append(t)
    ones = kb.sb([128, 128], F32, "ones")
    kb.op("dve", lambda e: e.memset(ones[:], 1.0), w=[ones])
    hT = kb.sb([128, KT, TOK], F32, "hT")
    emit_norm_mod(kb, xs, hT, vt[0], vt[1], vt[2], vt[3], vt[4], ones)
    h2v = h2.ap().rearrange("(kt p) t -> p kt t", p=128)
    for kt in range(KT):
        kb.dma("sp" if kt % 2 == 0 else "act", h2v[:, kt, :], hT[:, kt, :], r=[hT], w=[h2])
    rs = kb.sb([128, KT, 16], F32, "rs")
    kb.dma("sp", rs[:], rt.ap().rearrange("(kt p) e -> p kt e", p=128), w=[rs])
    pl = [kb.ps([16, 512], F32, f"pl{i}") for i in range(2)]
    pz = [kb.ps([16, 512], F32, f"pz{i}") for i in range(2)]
    ex = kb.sb([16, TOK], F32, "ex")
    rz = kb.sb([16, TOK], F32, "rz")
    af = kb.sb([16, TOK], F32, "af")
    for bi, (c0, cn) in enumerate(BLKS):
        p = pl[bi % 2]
        for kt in range(KT):
            kb.op("pe", lambda e, kt=kt: e.matmul(p[:, :cn], lhsT=rs[:, kt, :], rhs=hT[:, kt, c0:c0 + cn], start=(kt == 0), stop=(kt == KT - 1)),
                  r=[rs, hT], w=[p])
        kb.op("act", lambda e: e.activation(out=ex[:, c0:c0 + cn], in_=p[:, :cn], func=AF.Exp), r=[p], w=[ex])
        z = pz[bi % 2]
        kb.op("pe", lambda e: e.matmul(z[:, :cn], lhsT=ones[0:16, 0:16], rhs=ex[:, c0:c0 + cn], start=True, stop=True), r=[ones, ex], w=[z])
        kb.op("dve", lambda e: e.reciprocal(out=rz[:, c0:c0 + cn], in_=z[:, :cn]), r=[z], w=[rz])
        kb.op("dve", lambda e: e.tensor_tensor(out=af[:, c0:c0 + cn], in0=ex[:, c0:c0 + cn], in1=rz[:, c0:c0 + cn], op=ALU.mult), r=[ex, rz], w=[af])
    kb.dma("sp", aff.ap(), af[:], r=[af], w=[aff])
    kb.finish()
    return kb


D = 2048
NIT = 34
BIG = 1.0e6
SLOTS = 1056
SBLK = [(0, 512), (512, 512), (1024, 32)]


def make_route(kb, A, ntt, cap, nst, tri, ones, iota_s, tokid, tagp, Rb, G, rcol):
    def sm(shape, dt=F32, nm=""):
        return kb.sb(shape, dt, tagp + nm)
    lo = sm([128, 1], nm="lo"); hi = sm([128, 1], nm="hi"); mid = sm([128, 1], nm="mid")
    junk = sm([128, ntt], nm="junk"); cnt = sm([128, 1], nm="cnt"); ge = sm([128, 1], nm="ge")
    d1 = sm([128, 1], nm="d1"); d2 = sm([128, 1], nm="d2"); sm_ = sm([128, 1], nm="sm")
    pc = Rb[:, rcol:rcol + 1]
    bis = []
    B = bis.append
    B(lambda: kb.op("dve", lambda e: e.memset(lo[:], 0.0), w=[lo]))
    B(lambda: kb.op("dve", lambda e: e.memset(hi[:], 1.0), w=[hi]))
    B(lambda: kb.op("dve", lambda e: e.memset(mid[:], 0.5), w=[mid]))
    for it in range(NIT):
        B(lambda: kb.op("dve", lambda e: e.tensor_scalar(out=junk[:], in0=A[:], scalar1=mid[:, 0:1], scalar2=0.0, op0=ALU.is_gt, op1=ALU.add, accum_out=cnt[:, 0:1]),
                        r=[A, mid], w=[junk, cnt]))
        B(lambda: kb.op("pe", lambda e: e.matmul(pc, lhsT=ones[:], rhs=cnt[:, 0:1], start=True, stop=True), r=[ones, cnt], w=[Rb]))
        B(lambda: kb.op("dve", lambda e: e.tensor_scalar(out=ge[:], in0=pc, scalar1=float(cap) - 0.5, scalar2=None, op0=ALU.is_gt), r=[Rb], w=[ge]))
        B(lambda: kb.op("dve", lambda e: e.tensor_tensor(out=d1[:], in0=mid[:], in1=lo[:], op=ALU.subtract), r=[mid, lo], w=[d1]))
        B(lambda: kb.op("dve", lambda e: e.tensor_tensor(out=d2[:], in0=hi[:], in1=mid[:], op=ALU.subtract), r=[hi, mid], w=[d2]))
        B(lambda: kb.op("dve", lambda e: e.scalar_tensor_tensor(out=lo[:], in0=d1[:], scalar=ge[:, 0:1], in1=lo[:], op0=ALU.mult, op1=ALU.add), r=[d1, ge, lo], w=[lo]))
        B(lambda: kb.op("dve", lambda e: e.scalar_tensor_tensor(out=hi[:], in0=d2[:], scalar=ge[:, 0:1], in1=mid[:], op0=ALU.mult, op1=ALU.add), r=[d2, ge, mid], w=[hi]))
        B(lambda: kb.op("dve", lambda e: e.tensor_tensor(out=sm_[:], in0=lo[:], in1=hi[:], op=ALU.add), r=[lo, hi], w=[sm_]))
        B(lambda: kb.op("dve", lambda e: e.tensor_scalar(out=mid[:], in0=sm_[:], scalar1=0.5, scalar2=None, op0=ALU.mult), r=[sm_], w=[mid]))
    rest = []
    R = rest.append
    mask = sm([128, ntt], nm="mask")
    R(lambda: kb.op("dve", lambda e: e.tensor_scalar(out=mask[:], in0=A[:], scalar1=lo[:, 0:1], scalar2=None, op0=ALU.is_gt), r=[A, lo], w=[mask]))
    ppre = Rb[:, 16:16 + ntt]
    ptot = Rb[:, 128:128 + ntt]
    R(lambda: kb.op("pe", lambda e: e.matmul(ppre, lhsT=tri[:], rhs=mask[:], start=True, stop=True), r=[tri, mask], w=[Rb]))
    R(lambda: kb.op("pe", lambda e: e.matmul(ptot, lhsT=ones[:], rhs=mask[:], start=True, stop=True), r=[ones, mask], w=[Rb]))
    tot = sm([128, ntt], nm="tot"); cum = sm([128, ntt], nm="cum"); pos = sm([128, ntt], nm="pos")
    onesr = sm([128, ntt], nm="onesr")
    R(lambda: kb.op("dve", lambda e: e.memset(onesr[:], 1.0), w=[onesr]))
    R(lambda: kb.op("dve", lambda e: e.tensor_copy(out=tot[:], in_=ptot), r=[Rb], w=[tot]))
    R(lambda: kb.op("dve", lambda e: e.tensor_tensor_scan(out=cum[:], data0=onesr[:], data1=tot[:], initial=0.0, op0=ALU.mult, op1=ALU.add), r=[onesr, tot], w=[cum]))
    R(lambda: kb.op("dve", lambda e: e.tensor_tensor(out=cum[:], in0=cum[:], in1=tot[:], op=ALU.subtract), r=[cum, tot], w=[cum]))
    R(lambda: kb.op("dve", lambda e: e.tensor_tensor(out=pos[:], in0=ppre, in1=cum[:], op=ALU.add), r=[Rb, cum], w=[pos]))
    pen = sm([128, ntt], nm="pen"); posm = sm([128, ntt], nm="posm")
    R(lambda: kb.op("dve", lambda e: e.tensor_scalar(out=pen[:], in0=mask[:], scalar1=-BIG, scalar2=BIG, op0=ALU.mult, op1=ALU.add), r=[mask], w=[pen]))
    R(lambda: kb.op("dve", lambda e: e.tensor_tensor(out=posm[:], in0=pos[:], in1=pen[:], op=ALU.add), r=[pos, pen], w=[posm]))
    posc_ = sm([128, ntt], nm="poscl")
    R(lambda: kb.op("dve", lambda e: e.tensor_scalar(out=posc_[:], in0=posm[:], scalar1=float(cap), scalar2=None, op0=ALU.min), r=[posm], w=[posc_]))
    posi = sm([128, ntt], I32, nm="posi")
    R(lambda: kb.op("dve", lambda e: e.tensor_copy(out=posi[:], in_=posc_[:]), r=[posc_], w=[posi]))
    ns = nst * 128
    pgr = [G[i] for i in range((ns + 511) // 512)]
    ohs = [sm([128, ns], nm=f"oh{i}") for i in range(2)]
    arep = [sm([128, 128], nm=f"arep{i}") for i in range(2)]
    for tt in range(ntt):
        oh = ohs[tt % 2]; ar = arep[tt % 2]
        R(lambda tt=tt, oh=oh: kb.op("dve", lambda e: e.tensor_scalar(out=oh[:], in0=iota_s[:, 0:ns], scalar1=posm[:, tt:tt + 1], scalar2=None, op0=ALU.is_equal),
                                     r=[iota_s, posm], w=[oh]))
        R(lambda tt=tt, ar=ar: kb.op("act", lambda e: e.activation(out=ar[:], in_=ones[:], func=AF.Copy, scale=A[:, tt:tt + 1]), r=[ones, A], w=[ar]))

        def mm(tt=tt, oh=oh, ar=ar):
            for st in range(nst):
                kb.op("pe", lambda e, st=st: e.matmul(Rb[:, 256 + st:256 + st + 1], lhsT=oh[:, st * 128:(st + 1) * 128], rhs=tokid[:, tt:tt + 1],
                                                      start=(tt == 0 and st == 0), stop=(tt == ntt - 1 and st == nst - 1), skip_group_check=True), r=[oh, tokid], w=[Rb])
            for gi, pg in enumerate(pgr):
                n0 = gi * 512
                nn = min(512, ns - n0)
                kb.op("pe", lambda e, pg=pg, n0=n0, nn=nn: e.matmul(pg[:, 0:nn], lhsT=ar[:], rhs=oh[:, n0:n0 + nn], start=(tt == 0), stop=(tt == ntt - 1)),
                      r=[ar, oh], w=[pg])
        R(mm)
    idxf = sm([128, nst], nm="idxf")
    idxi = sm([128, nst], I32, nm="idxi")
    R(lambda: kb.op("dve", lambda e: e.tensor_copy(out=idxf[:], in_=Rb[:, 256:256 + nst]), r=[Rb], w=[idxf]))
    R(lambda: kb.op("dve", lambda e: e.tensor_copy(out=idxi[:], in_=idxf[:]), r=[idxf], w=[idxi]))
    grow = sm([128, ns], nm="grow")
    for gi, pg in enumerate(pgr):
        n0 = gi * 512
        nn = min(512, ns - n0)
        R(lambda pg=pg, n0=n0, nn=nn: kb.op("act", lambda e: e.copy(out=grow[:, n0:n0 + nn], in_=pg[:, 0:nn]), r=[pg], w=[grow]))
    return bis, rest, {"posi": posi, "idxi": idxi, "grow": grow}


def interleave(lists, weights=None):
    n = [len(l) for l in lists]
    pos = [0] * len(lists)
    total = max(n) if n else 0
    for step in range(total):
        for i, l in enumerate(lists):
            tgt = ((step + 1) * n[i] + total - 1) // total
            while pos[i] < min(tgt, n[i]):
                l[pos[i]]()
                pos[i] += 1


def build_k4(with_ffn=True):
    kb = KB()
    nc = kb.nc
    affl = kb.dram("affl", [2, 128, 64], F32, kind="ExternalInput")
    affc = kb.dram("affc", [2, 128, 2], F32, kind="ExternalInput")
    h2 = kb.dram("h2", [8192, D], F32, kind="ExternalInput")
    h2c = kb.dram("h2c", [256, D], F32, kind="ExternalInput")
    wg = kb.dram("wg", [2, D, D], F32, kind="ExternalInput")
    wu = kb.dram("wu", [2, D, D], F32, kind="ExternalInput")
    wd = kb.dram("wd", [2, D, D], F32, kind="ExternalInput")
    cst = kb.dram("cst", [128, 128 + 1024 + 64 + 128], F32, kind="ExternalInput")
    yeT = kb.dram("yeT", [2, D, SLOTS], F32, kind="ExternalOutput")
    posl = kb.dram("posl", [2, 128, 64], I32, kind="ExternalOutput")
    posc = kb.dram("posc", [2, 128, 2], I32, kind="ExternalOutput")
    dbg = kb.dram("dbg", [2, 128, 9], I32, kind="ExternalOutput")

    cs_ = kb.sb([128, 1344], F32, "cs_")
    kb.dma("sp", cs_[:], cst.ap(), w=[cs_])
    tri = kb.sb([128, 128], F32, "tri"); iota_s = kb.sb([128, 1024], F32, "iota_s"); tokid = kb.sb([128, 64], F32, "tokid")
    identb = kb.sb([128, 128], BF16, "identb"); ones = kb.sb([128, 128], F32, "ones")
    kb.op("dve", lambda e: e.tensor_copy(out=tri[:], in_=cs_[:, 0:128]), r=[cs_], w=[tri])
    kb.op("dve", lambda e: e.tensor_copy(out=iota_s[:], in_=cs_[:, 128:1152]), r=[cs_], w=[iota_s])
    kb.op("dve", lambda e: e.tensor_copy(out=tokid[:], in_=cs_[:, 1152:1216]), r=[cs_], w=[tokid])
    kb.op("dve", lambda e: e.tensor_copy(out=identb[:], in_=cs_[:, 1216:1344]), r=[cs_], w=[identb])
    kb.op("dve", lambda e: e.memset(ones[:], 1.0), w=[ones])

    Rb = [kb.ps([128, 512], F32, f"Rb{i}") for i in range(1)]
    G = [kb.ps([128, 512], F32, f"G{i}") for i in range(2)]
    F = [kb.ps([128, 512], F32, f"F{i}") for i in range(4)]
    ptr = kb.ps([128, 1024], BF16, "ptr")
    xsT = kb.sb([128, 16, SLOTS], BF16, "xsT")
    actT = kb.sb([128, 16, SLOTS], BF16, "actT")
    xg = [kb.sb([128, D], F32, f"xg{i}") for i in range(2)]
    xgb = [kb.sb([128, D], BF16, f"xgb{i}") for i in range(2)]
    wgs = [kb.sb([128, 16, 128], BF16, f"wgs{i}") for i in range(2)]
    wus = [kb.sb([128, 16, 128], BF16, f"wus{i}") for i in range(2)]
    sg = [kb.sb([128, 512], F32, f"sg{i}") for i in range(2)]
    tq = [kb.sb([128, 512], F32, f"tq{i}") for i in range(2)]
    ob = [kb.sb([128, SLOTS], F32, f"ob{i}") for i in range(2)]

    routes = {}
    for e2 in range(2):
        Al = kb.sb([128, 64], F32, f"Al{e2}")
        Ac = kb.sb([128, 2], F32, f"Ac{e2}")
        kb.dma("sp", Al[:], affl.ap()[e2], w=[Al])
        kb.dma("sp", Ac[:], affc.ap()[e2], w=[Ac])
        routes[(e2, 0)] = make_route(kb, Al, 64, 1024, 8, tri, ones, iota_s, tokid, f"rl{e2}_", Rb[0], G, e2 * 2)
        routes[(e2, 1)] = make_route(kb, Ac, 2, 32, 1, tri, ones, iota_s, tokid, f"rc{e2}_", Rb[0], G, e2 * 2 + 1)

    def outputs(e2):
        ol, oc = routes[(e2, 0)][2], routes[(e2, 1)][2]
        kb.dma("sp", posl.ap()[e2], ol["posi"][:], r=[ol["posi"]], w=[posl])
        kb.dma("sp", posc.ap()[e2], oc["posi"][:], r=[oc["posi"]], w=[posc])
        kb.dma("sp", dbg.ap()[e2, :, 0:8], ol["idxi"][:], r=[ol["idxi"]], w=[dbg], allow_slow_non_contiguous=True)
        kb.dma("sp", dbg.ap()[e2, :, 8:9], oc["idxi"][:], r=[oc["idxi"]], w=[dbg], allow_slow_non_contiguous=True)

    def ffn_ops(e2):
        ops = []
        Aop = ops.append
        idx_l, idx_c = routes[(e2, 0)][2]["idxi"], routes[(e2, 1)][2]["idxi"]
        grow_l, grow_c = routes[(e2, 0)][2]["grow"], routes[(e2, 1)][2]["grow"]
        for st in range(9):
            b = st % 2
            rows = 128 if st < 8 else 32

            def gather(st=st, b=b, rows=rows):
                idx_t = idx_l if st < 8 else idx_c
                idx_ap = idx_l[:, st:st + 1] if st < 8 else idx_c[0:32, 0:1]
                src = h2.ap() if st < 8 else h2c.ap()
                need = kb._need([idx_t], [xg[b]])
                kb._wait("pool", need)
                ins = nc.gpsimd.indirect_dma_start(out=xg[b][0:rows, :], out_offset=None, in_=src,
                                                   in_offset=bass.IndirectOffsetOnAxis(ap=idx_ap, axis=0))
                t = xg[b]
                if t.dsem is None:
                    t.dsem = kb.st.enter_context(nc.semaphore("d_" + t.name))
                t.dcount += 16
                ins.then_inc(t.dsem, 16)
                key = "d_" + t.name
                idx_t.r[key] = (t.dsem, t.dcount)
                t.w[key] = (t.dsem, t.dcount)
            Aop(gather)
            Aop(lambda b=b, rows=rows: kb.op("act", lambda e: e.copy(out=xgb[b][0:rows, :], in_=xg[b][0:rows, :]), r=[xg[b]], w=[xgb[b]]))
            for half in range(2):
                def tr(half=half, b=b, rows=rows, st=st):
                    for k8 in range(8):
                        kt = half * 8 + k8
                        kb.op("pe", lambda e, kt=kt, k8=k8: e.transpose(ptr[:, k8 * 128:k8 * 128 + rows], xgb[b][0:rows, kt * 128:(kt + 1) * 128], identb[0:rows, 0:rows]),
                              r=[xgb[b], identb], w=[ptr])
                    kb.op("dve", lambda e: e.tensor_copy(out=xsT[:, half * 8:(half + 1) * 8, st * 128:st * 128 + rows],
                                                         in_=ptr[:].rearrange("p (k c) -> p k c", k=8)[:, :, 0:rows]), r=[ptr], w=[xsT])
                Aop(tr)
        PA = [F[0], F[1]]
        PU = [F[2], F[3]]
        wgv = wg.ap()[e2].rearrange("(kt p) n -> p kt n", p=128)
        wuv = wu.ap()[e2].rearrange("(kt p) n -> p kt n", p=128)
        wdv = wd.ap()[e2].rearrange("(kt p) n -> p kt n", p=128)
        n = 0
        for ft in range(16):
            wgt = wgs[ft % 2]; wut = wus[ft % 2]
            Aop(lambda ft=ft, wgt=wgt, wut=wut: (kb.dma("pool", wgt[:], wgv[:, :, ft * 128:(ft + 1) * 128], w=[wgt]),
                                                  kb.dma("pool", wut[:], wuv[:, :, ft * 128:(ft + 1) * 128], w=[wut])))
            for (c0, cn) in SBLK:
                pA = PA[n % 2]; pU = PU[n % 2]; s_ = sg[n % 2]; t_ = tq[n % 2]
                n += 1

                def blk(ft=ft, wgt=wgt, wut=wut, pA=pA, pU=pU, s_=s_, t_=t_, c0=c0, cn=cn):
                    for kt in range(16):
                        kb.op("pe", lambda e, kt=kt: e.matmul(pA[:, :cn], lhsT=wgt[:, kt, :], rhs=xsT[:, kt, c0:c0 + cn], start=(kt == 0), stop=(kt == 15)),
                              r=[wgt, xsT], w=[pA])
                    for kt in range(16):
                        kb.op("pe", lambda e, kt=kt: e.matmul(pU[:, :cn], lhsT=wut[:, kt, :], rhs=xsT[:, kt, c0:c0 + cn], start=(kt == 0), stop=(kt == 15)),
                              r=[wut, xsT], w=[pU])
                    kb.op("act", lambda e: e.activation(out=s_[:, :cn], in_=pA[:, :cn], func=AF.Sigmoid), r=[pA], w=[s_])
                    kb.op("dve", lambda e: e.tensor_tensor(out=t_[:, :cn], in0=pA[:, :cn], in1=s_[:, :cn], op=ALU.mult), r=[pA, s_], w=[t_])
                    kb.op("dve", lambda e: e.tensor_tensor(out=actT[:, ft, c0:c0 + cn], in0=pU[:, :cn], in1=t_[:, :cn], op=ALU.mult), r=[pU, t_], w=[actT])
                Aop(blk)
        for dt_ in range(16):
            wdt = wgs[dt_ % 2]
            o = ob[dt_ % 2]
            Aop(lambda dt_=dt_, wdt=wdt: kb.dma("pool", wdt[:], wdv[:, :, dt_ * 128:(dt_ + 1) * 128], w=[wdt]))
            for (c0, cn) in SBLK:
                pA = PA[n % 2]
                n += 1

                def dblk(dt_=dt_, wdt=wdt, o=o, pA=pA, c0=c0, cn=cn):
                    for kt in range(16):
                        kb.op("pe", lambda e, kt=kt: e.matmul(pA[:, :cn], lhsT=wdt[:, kt, :], rhs=actT[:, kt, c0:c0 + cn], start=(kt == 0), stop=(kt == 15)),
                              r=[wdt, actT], w=[pA])
                    if c0 < 1024:
                        kb.op("dve", lambda e: e.tensor_tensor(out=o[:, c0:c0 + cn], in0=pA[:, :cn], in1=grow_l[:, c0:c0 + cn], op=ALU.mult), r=[pA, grow_l], w=[o])
                    else:
                        kb.op("dve", lambda e: e.tensor_tensor(out=o[:, c0:c0 + cn], in0=pA[:, :cn], in1=grow_c[:, 0:cn], op=ALU.mult), r=[pA, grow_c], w=[o])
                Aop(dblk)
            Aop(lambda dt_=dt_, o=o: kb.dma("sp", yeT.ap()[e2, dt_ * 128:(dt_ + 1) * 128, :], o[:], r=[o], w=[yeT]))
        return ops

    interleave([routes[k][0] for k in ((0, 0), (0, 1), (1, 0), (1, 1))])
    for th in routes[(0, 0)][1]:
        th()
    for th in routes[(0, 1)][1]:
        th()
    outputs(0)
    if with_ffn:
        interleave([routes[(1, 0)][1] + routes[(1, 1)][1], ffn_ops(0)])
    else:
        for th in routes[(1, 0)][1] + routes[(1, 1)][1]:
            th()
    outputs(1)
    if with_ffn:
        for th in ffn_ops(1):
            th()
    kb.finish()
    return kb


D = 2048


def build_k5(final):
    kb = KB()
    nc = kb.nc
    x = kb.dram("x", [1056, D], F32, kind="ExternalInput")
    yl = [kb.dram(f"yl{e}", [1025, D], F32, kind="ExternalInput") for e in range(16)]
    yc = [kb.dram(f"yc{e}", [33, D], F32, kind="ExternalInput") for e in range(16)]
    pos = kb.dram("pos", [128, 9, 16], I32, kind="ExternalInput")
    rep = kb.dram("rep", [3, 128, D], F32, kind="ExternalInput")
    xo = kb.dram("xo", [1056, D], F32, kind="ExternalOutput")
    ps_ = kb.sb([128, 9, 16], I32, "pos_sb")
    kb.dma("sp", ps_[:], pos.ap(), w=[ps_])
    reps = []
    for i in range(3):
        t = kb.sb([128, D], F32, f"rep{i}")
        kb.dma("sp" if i % 2 == 0 else "act", t[:], rep.ap()[i], w=[t])
        reps.append(t)
    G = [kb.sb([128, D], F32, f"g{i}") for i in range(10)]
    ACC = [kb.sb([128, D], F32, f"acc{i}") for i in range(2)]
    XT = [kb.sb([128, D], F32, f"xt{i}") for i in range(2)]
    ss = [kb.sb([128, 1], F32, f"ss{i}") for i in range(2)]
    sd = [kb.sb([128, 1], F32, f"sd{i}") for i in range(2)]
    junk = kb.sb([128, D], F32, "junk")
    gi = 0
    for tl in range(9):
        rows = 128 if tl < 8 else 32
        r0 = tl * 128
        acc = ACC[tl % 2]; xt = XT[tl % 2]
        kb.dma("sp", xt[0:rows, :], x.ap()[r0:r0 + rows, :], w=[xt])
        for e in range(16):
            g = G[gi % 10] if e > 0 else acc
            gi += 1
            src = yl[e] if tl < 8 else yc[e]
            need = kb._need([ps_], [g])
            kb._wait("pool", need)
            ins = nc.gpsimd.indirect_dma_start(out=g[0:rows, :], out_offset=None, in_=src.ap(),
                                               in_offset=bass.IndirectOffsetOnAxis(ap=ps_[0:rows, tl, e:e + 1], axis=0))
            if g.dsem is None:
                g.dsem = kb.st.enter_context(nc.semaphore("d_" + g.name))
            g.dcount += 16
            ins.then_inc(g.dsem, 16)
            key = "d_" + g.name
            ps_.r[key] = (g.dsem, g.dcount)
            g.w[key] = (g.dsem, g.dcount)
            if e > 0:
                eng = "dve"
                kb.op(eng, lambda en: en.tensor_tensor(out=acc[0:rows, :], in0=acc[0:rows, :], in1=g[0:rows, :], op=ALU.add), r=[acc, g], w=[acc])
        m5 = reps[0] if tl < 8 else reps[1]
        kb.op("dve", lambda en: en.tensor_tensor(out=acc[0:rows, :], in0=acc[0:rows, :], in1=m5[0:rows, :], op=ALU.mult), r=[acc, m5], w=[acc])
        kb.op("dve", lambda en: en.tensor_tensor(out=xt[0:rows, :], in0=xt[0:rows, :], in1=acc[0:rows, :], op=ALU.add), r=[xt, acc], w=[xt])
        if final and tl < 8:
            s_ = ss[tl % 2]; d_ = sd[tl % 2]
            kb.op("act", lambda en: en.activation(out=junk[0:rows, :], in_=xt[0:rows, :], func=AF.Square), r=[xt], w=[junk])
            kb.op("dve", lambda en: en.tensor_scalar(out=junk[0:rows, :], in0=junk[0:rows, :], scalar1=1.0, scalar2=0.0, op0=ALU.mult, op1=ALU.add,
                                                     accum_out=s_[0:rows, 0:1]), r=[junk], w=[junk, s_])
            kb.op("act", lambda en: en.activation(out=d_[0:rows, :], in_=s_[0:rows, :], func=AF.Sqrt, bias=1e-6, scale=1.0 / D), r=[s_], w=[d_])
            kb.op("dve", lambda en: en.reciprocal(out=s_[0:rows, :], in_=d_[0:rows, :]), r=[d_], w=[s_])
            kb.op("dve", lambda en: en.scalar_tensor_tensor(out=xt[0:rows, :], in0=xt[0:rows, :], scalar=s_[0:rows, 0:1], in1=reps[2][0:rows, :],
                                                            op0=ALU.mult, op1=ALU.mult), r=[xt, s_, reps[2]], w=[xt])
        kb.dma("act", xo.ap()[r0:r0 + rows, :], xt[0:rows, :], r=[xt], w=[xo])
    kb.finish()
    return kb

import numpy as np
QA0 = 4096
def rope_tables():
    L = 8192
    row = np.repeat(np.arange(L // 64), 64).astype(np.float32)
    col = np.tile(np.arange(64), L // 64).astype(np.float32)
    inv = (np.float32(10000.0) ** (-np.arange(16, dtype=np.float32) / np.float32(16))).astype(np.float32)
    ang = np.concatenate([row[:, None] * inv, col[:, None] * inv], -1).astype(np.float32)
    c = np.cos(ang).astype(np.float32); s = np.sin(ang).astype(np.float32)
    cosT = np.concatenate([c, c], 1).T
    sinT = np.concatenate([-s, s], 1).T
    cs = np.stack([np.concatenate([cosT, cosT], 0), np.concatenate([sinT, sinT], 0)])
    return np.ascontiguousarray(cs.astype(np.float32))
def attn_masks():
    j = np.arange(128)[:, None]; i = np.arange(128)[None, :]
    return np.ascontiguousarray(np.stack([(j >= i), (j <= i)]).astype(np.float32))
def perm64(w, nh):
    sh = w.shape[:-1]
    w = w.reshape(sh + (nh, 2, 32))
    return w[..., ::-1, :].reshape(sh + (nh * 64,))
def attn_inputs(ci, q_all, k_all, v_all, qc_all, kc_all, vc_all, sink_l, cs, masks):
    kvh = ci // 2
    q = q_all[:, ci * 128:(ci + 1) * 128]
    qp = perm64(q, 2)
    k = k_all[:, kvh * 64:(kvh + 1) * 64]; kp = perm64(k, 1)
    kd = np.concatenate([k, k], 1); kpd = np.concatenate([kp, kp], 1)
    qT = np.ascontiguousarray(np.stack([q.T, qp.T])); kT = np.ascontiguousarray(np.stack([kd.T, kpd.T]))
    kc = kc_all[:, kvh * 64:(kvh + 1) * 64]
    cx = np.ascontiguousarray(np.stack([qc_all[:, ci * 128:(ci + 1) * 128].T, np.concatenate([kc, kc], 1).T]))
    v = np.concatenate([v_all[:, kvh * 64:(kvh + 1) * 64], vc_all[:, kvh * 64:(kvh + 1) * 64]], 0)
    vtok = np.ascontiguousarray(v.reshape(66, 128, 64).transpose(1, 0, 2))
    sk = np.ascontiguousarray(np.broadcast_to(sink_l[2 * ci:2 * ci + 2][None, :], (64, 2)).astype(np.float32))
    return {"qT": qT, "kT": kT, "cs": cs, "cx": cx, "vtok": vtok, "masks": masks, "sink": sk}
GQ0 = 1024; GK0 = 1536; GV0 = 2048; GR0 = 3072
def dir_order(a, dd):
    if dd == 0: return a
    return np.concatenate([a[:256][::-1], a[256:][::-1]], 0)
def gla_consts():
    c = np.zeros((128, 704), np.float32)
    m = np.ones(512, np.float32); m[::64] = 0
    c[:, :512] = m
    j = np.arange(64)[:, None]; i = np.arange(64)[None, :]
    c[:64, 512:576] = (j <= i)
    c[:, 576:704] = np.eye(128)
    return c
def gla_inputs(ci, l, qg, kg, vg, hw1_all, d, consts):
    hd = ci // 2; dd = ci % 2
    q = dir_order(qg[:, hd * 128:(hd + 1) * 128], dd); k = dir_order(kg[:, hd * 128:(hd + 1) * 128], dd)
    v = dir_order(vg[:, hd * 256:(hd + 1) * 256], dd)
    hw = dir_order(hw1_all[:, dd * 16:(dd + 1) * 16], dd)
    return {"qk": np.ascontiguousarray(np.stack([q.T, k.T]).astype(np.float32)), "hw1": np.ascontiguousarray(hw.T.astype(np.float32)),
            "w2b": np.ascontiguousarray(d["gla_w2"][l, dd][:, hd * 128:(hd + 1) * 128]),
            "nb": np.ascontiguousarray(d["gla_b"][l, dd, hd * 128:(hd + 1) * 128].reshape(128, 1)),
            "vtok": np.ascontiguousarray(v.reshape(132, 64, 256).transpose(1, 0, 2).astype(np.float32)), "cst": consts}
def moe_consts():
    c = np.zeros((128, 1344), np.float32)
    pj = np.arange(128)
    c[:, 0:128] = (pj[:, None] < pj[None, :])
    c[:, 128:1152] = np.arange(1024)[None, :]
    c[:, 1152:1216] = (np.arange(64)[None, :] * 128 + pj[:, None])
    c[:, 1216:1344] = np.eye(128)
    return c
def moe_inputs(ci, l, aff_lat, aff_ctx, h2_tok, d, consts):
    es = [2 * ci, 2 * ci + 1]
    affl = np.stack([aff_lat[:, e].reshape(64, 128).T for e in es]).astype(np.float32)
    affc = np.stack([aff_ctx[:, e].reshape(2, 128).T for e in es]).astype(np.float32)
    return {"affl": np.ascontiguousarray(affl), "affc": np.ascontiguousarray(affc), "h2": np.ascontiguousarray(h2_tok[:8192]), "h2c": np.ascontiguousarray(h2_tok[8192:]),
            "wg": np.ascontiguousarray(d["moe_w_gate"][l, es[0]:es[0] + 2]), "wu": np.ascontiguousarray(d["moe_w_up"][l, es[0]:es[0] + 2]),
            "wd": np.ascontiguousarray(d["moe_w_down"][l, es[0]:es[0] + 2]), "cst": consts}

QA0_ = 4096
GQ0_, GK0_, GV0_, GR0_ = 1024, 1536, 2048, 3072


def _pvec(v):
    return np.ascontiguousarray(np.asarray(v, np.float32).reshape(-1, 128).T)


def _build_wext(w_in, w1):
    qa = w_in[:, QA0_:QA0_ + 1024]
    ka = w_in[:, QA0_ + 1024:QA0_ + 1280]
    W = np.zeros((2048, NW), np.float32)
    W[:, :11776] = w_in
    W[:, 11776:12800] = perm64(qa, 16)
    W[:, 12800:13056] = perm64(ka, 4)
    W[:, 13056:13072] = w1[0]
    W[:, 13072:13088] = w1[1]
    return W


def _rev_cols(a):
    return np.concatenate([a[:, :256][:, ::-1], a[:, 256:][:, ::-1]], 1)


def _s5_inputs(l, gi, uall_T, p):
    uT = np.ascontiguousarray(np.stack([uall_T, _rev_cols(uall_T)]).astype(np.float32))
    Bblk = np.zeros((2, 2, 4, 128, 128), np.float32)
    Cblk = np.zeros((2, 2, 4, 128, 128), np.float32)
    lam = np.zeros((3, 128, 8), np.float32)
    for dd in range(2):
        for q in range(4):
            for g2 in range(2):
                gl = 2 * q + g2
                g = 8 * gi + gl
                for pi, (bn, cn) in enumerate((("s5_b_re", "s5_c_re"), ("s5_b_im", "s5_c_im"))):
                    Bblk[dd, pi, q, gl * 16:(gl + 1) * 16, g2 * 64:(g2 + 1) * 64] = p[bn][l, dd, g].T
                    Cblk[dd, pi, q, g2 * 64:(g2 + 1) * 64, gl * 16:(gl + 1) * 16] = p[cn][l, dd, g].T
                lam[0, g2 * 64:(g2 + 1) * 64, dd * 4 + q] = p["s5_lam_re"][l, dd, g]
                lam[1, g2 * 64:(g2 + 1) * 64, dd * 4 + q] = p["s5_lam_im"][l, dd, g]
                lam[2, g2 * 64:(g2 + 1) * 64, dd * 4 + q] = p["s5_log_dt"][l, dd, g]
    dvec = np.ascontiguousarray(p["s5_d"][l, gi * 128:(gi + 1) * 128].reshape(128, 1))
    return {"uT": uT, "Bblk": Bblk, "Cblk": Cblk, "lam": lam, "dvec": dvec}


def _run(kb, ims):
    res = run_bass_kernel_spmd(kb.nc, ims, core_ids=list(range(NCORES)))
    return res.results


def kernel(x, c, ctx, c_ctx, ada_w, ada_b, norm1_g, norm2_g, w_in, s5_lam_re, s5_lam_im, s5_log_dt,
           s5_b_re, s5_b_im, s5_c_re, s5_c_im, s5_d, s5_w_glu, gla_w1, gla_w2, gla_b, gla_norm_g,
           attn_sink, w_branch_s5, w_branch_gla, w_branch_attn, w_out, moe_router, moe_w_gate,
           moe_w_up, moe_w_down, final_g):
    p = {k: np.asarray(v, np.float32) for k, v in dict(
        s5_lam_re=s5_lam_re, s5_lam_im=s5_lam_im, s5_log_dt=s5_log_dt, s5_b_re=s5_b_re, s5_b_im=s5_b_im,
        s5_c_re=s5_c_re, s5_c_im=s5_c_im, s5_d=s5_d, gla_w2=gla_w2, gla_b=gla_b, moe_w_gate=moe_w_gate,
        moe_w_up=moe_w_up, moe_w_down=moe_w_down).items()}
    f32 = np.float32
    x_lat = np.asarray(x, f32)[0].copy()
    xc = np.asarray(ctx, f32)[0].copy()
    ada_w = np.asarray(ada_w, f32); ada_b = np.asarray(ada_b, f32)
    cT = np.stack([np.asarray(c, f32)[0], np.asarray(c_ctx, f32)], axis=1)
    cT = np.ascontiguousarray(cT.reshape(16, 128, 2).transpose(1, 0, 2))
    ims = []
    for i in range(NCORES):
        aw = np.ascontiguousarray(ada_w[:, :, i * 1536:(i + 1) * 1536])
        ab = np.ascontiguousarray(ada_b[:, i * 1536:(i + 1) * 1536].reshape(2, 12, 128).transpose(0, 2, 1))
        ims.append({"cT": cT, "adaw": aw, "adab": ab})
    r = _run(build_k0(), ims)
    mod = np.zeros((2, 12288, 2), f32)
    for i in range(NCORES):
        mod[:, i * 1536:(i + 1) * 1536, :] = r[i]["modT"].transpose(0, 2, 1, 3).reshape(2, 1536, 2)
    cs_tab = rope_tables(); amask = attn_masks(); gconst = gla_consts(); mconst = moe_consts()

    def shard_cols(A, ci):
        return np.ascontiguousarray(np.concatenate([A[:, 256 + ci * 1024:256 + (ci + 1) * 1024], A[:, ci * 32:(ci + 1) * 32]], 1))

    for l in range(2):
        m = mod[l].reshape(6, 2048, 2)
        W = _build_wext(np.asarray(w_in[l], f32), np.asarray(gla_w1[l], f32))
        vecs = np.stack([_pvec(norm1_g[l]), _pvec(m[1, :, 0]), _pvec(m[0, :, 0]), _pvec(m[1, :, 1]), _pvec(m[0, :, 1])])
        xTs = [np.ascontiguousarray(np.concatenate([x_lat[i * 1024:(i + 1) * 1024], xc[i * 32:(i + 1) * 32]], 0).T) for i in range(NCORES)]
        r = _run(build_k1(), [{"xT": xTs[i], "W": W, "vecs": vecs} for i in range(NCORES)])
        ZT = np.empty((NW, 8448), f32)
        for i in range(NCORES):
            z = r[i]["zT"]
            ZT[:, 256 + i * 1024:256 + (i + 1) * 1024] = z[:, :1024]
            ZT[:, i * 32:(i + 1) * 32] = z[:, 1024:]
        del r, W
        r = _run(build_k2a(), [_s5_inputs(l, gi, ZT[gi * 128:(gi + 1) * 128], p) for gi in range(NCORES)])
        YS = np.empty((2, 1024, 8448), f32)
        for gi in range(NCORES):
            YS[0, gi * 128:(gi + 1) * 128] = r[gi]["yT"][0]
            YS[1, gi * 128:(gi + 1) * 128] = _rev_cols(r[gi]["yT"][1])
        ims = []
        for ci in range(NCORES):
            hd, dd = ci // 2, ci % 2
            od = (lambda a: a) if dd == 0 else _rev_cols
            q = od(ZT[GQ0_ + hd * 128:GQ0_ + (hd + 1) * 128]); k = od(ZT[GK0_ + hd * 128:GK0_ + (hd + 1) * 128])
            v = od(ZT[GV0_ + hd * 256:GV0_ + (hd + 1) * 256])
            hw = od(ZT[13056 + dd * 16:13056 + (dd + 1) * 16])
            ims.append({"qk": np.ascontiguousarray(np.stack([q, k])), "hw1": np.ascontiguousarray(hw),
                        "w2b": np.ascontiguousarray(p["gla_w2"][l, dd][:, hd * 128:(hd + 1) * 128]),
                        "nb": np.ascontiguousarray(p["gla_b"][l, dd, hd * 128:(hd + 1) * 128].reshape(128, 1)),
                        "vtok": np.ascontiguousarray(v.T.reshape(132, 64, 256).transpose(1, 0, 2)), "cst": gconst})
        r = _run(build_k2b(), ims)
        OG = np.empty((2, 1024, 8448), f32)
        for ci in range(NCORES):
            hd, dd = ci // 2, ci % 2
            o = r[ci]["oT"].reshape(256, 8448)
            OG[dd, hd * 256:(hd + 1) * 256] = o if dd == 0 else _rev_cols(o)
        ims = []
        for ci in range(NCORES):
            kvh = ci // 2
            lat = slice(256, 8448); cx_ = slice(0, 256)
            q = ZT[QA0_ + ci * 128:QA0_ + (ci + 1) * 128]; qp = ZT[11776 + ci * 128:11776 + (ci + 1) * 128]
            k = ZT[QA0_ + 1024 + kvh * 64:QA0_ + 1024 + (kvh + 1) * 64]; kp = ZT[12800 + kvh * 64:12800 + (kvh + 1) * 64]
            v = ZT[QA0_ + 1280 + kvh * 64:QA0_ + 1280 + (kvh + 1) * 64]
            qT = np.ascontiguousarray(np.stack([q[:, lat], qp[:, lat]]))
            kT = np.ascontiguousarray(np.stack([np.concatenate([k[:, lat], k[:, lat]], 0), np.concatenate([kp[:, lat], kp[:, lat]], 0)]))
            cxx = np.ascontiguousarray(np.stack([q[:, cx_], np.concatenate([k[:, cx_], k[:, cx_]], 0)]))
            vt = np.concatenate([v[:, lat], v[:, cx_]], 1).T
            vtok = np.ascontiguousarray(vt.reshape(66, 128, 64).transpose(1, 0, 2))
            sk = np.ascontiguousarray(np.broadcast_to(np.asarray(attn_sink[l], f32)[2 * ci:2 * ci + 2][None, :], (64, 2)))
            ims.append({"qT": qT, "kT": kT, "cs": cs_tab, "cx": cxx, "vtok": vtok, "masks": amask, "sink": sk})
        r = _run(build_k2c(), ims)
        YA = np.empty((1024, 8448), f32)
        for ci in range(NCORES):
            YA[ci * 128:(ci + 1) * 128, 256:] = r[ci]["yT"]
            YA[ci * 128:(ci + 1) * 128, :256] = r[ci]["ycT"]
        gng = np.zeros(2048, f32); gng[:1024] = np.asarray(gla_norm_g[l], f32).reshape(-1)
        vecs = np.stack([_pvec(gng), _pvec(m[2, :, 0]), _pvec(m[2, :, 1])])
        wbr = np.ascontiguousarray(np.stack([np.asarray(w_branch_s5[l], f32), np.asarray(w_branch_gla[l], f32), np.asarray(w_branch_attn[l], f32)]))
        wgl = np.ascontiguousarray(np.asarray(s5_w_glu[l], f32)); wo_ = np.ascontiguousarray(np.asarray(w_out[l], f32))
        ims = []
        for ci in range(NCORES):
            ims.append({"ys": np.stack([shard_cols(YS[0], ci), shard_cols(YS[1], ci)]), "og": np.stack([shard_cols(OG[0], ci), shard_cols(OG[1], ci)]),
                        "ya": shard_cols(YA, ci), "zr": shard_cols(ZT[GR0_:GR0_ + 1024], ci), "zg": shard_cols(ZT[5632:11776], ci),
                        "xT": xTs[ci], "wglu": wgl, "wbr": wbr, "wout": wo_, "vecs": vecs})
        r = _run(build_k3a(), ims)
        xmidT = [r[ci]["xo"] for ci in range(NCORES)]
        del ims, YS, OG, YA, ZT
        vecs = np.stack([_pvec(norm2_g[l]), _pvec(m[4, :, 0]), _pvec(m[3, :, 0]), _pvec(m[4, :, 1]), _pvec(m[3, :, 1])])
        rt = np.ascontiguousarray(np.asarray(moe_router[l], f32))
        r = _run(build_k3b(), [{"xT": xmidT[ci], "vecs": vecs, "rt": rt} for ci in range(NCORES)])
        h2l = np.empty((8192, 2048), f32); h2c = np.empty((256, 2048), f32)
        affl = np.empty((8192, 16), f32); affc = np.empty((256, 16), f32)
        for ci in range(NCORES):
            h = r[ci]["h2"]; a = r[ci]["aff"]
            h2l[ci * 1024:(ci + 1) * 1024] = h[:, :1024].T; h2c[ci * 32:(ci + 1) * 32] = h[:, 1024:].T
            affl[ci * 1024:(ci + 1) * 1024] = a[:, :1024].T; affc[ci * 32:(ci + 1) * 32] = a[:, 1024:].T
        ims = []
        for ci in range(NCORES):
            es = [2 * ci, 2 * ci + 1]
            ims.append({"affl": np.ascontiguousarray(np.stack([affl[:, e].reshape(64, 128).T for e in es])),
                        "affc": np.ascontiguousarray(np.stack([affc[:, e].reshape(2, 128).T for e in es])),
                        "h2": h2l, "h2c": h2c,
                        "wg": np.ascontiguousarray(p["moe_w_gate"][l, es[0]:es[0] + 2]), "wu": np.ascontiguousarray(p["moe_w_up"][l, es[0]:es[0] + 2]),
                        "wd": np.ascontiguousarray(p["moe_w_down"][l, es[0]:es[0] + 2]), "cst": mconst})
        r = _run(build_k4(), ims)
        del ims
        yl = []; yc = []
        posl = np.empty((16, 128, 64), np.int32); posc = np.empty((16, 128, 2), np.int32)
        zrow = np.zeros((1, 2048), f32)
        for ci in range(NCORES):
            for e2 in range(2):
                e = 2 * ci + e2
                yt = r[ci]["yeT"][e2]
                yl.append(np.ascontiguousarray(np.concatenate([yt[:, :1024].T, zrow], 0)))
                yc.append(np.ascontiguousarray(np.concatenate([yt[:, 1024:1056].T, zrow], 0)))
                posl[e] = r[ci]["posl"][e2]; posc[e] = r[ci]["posc"][e2]
        rep = np.ascontiguousarray(np.stack([np.broadcast_to(m[5, :, 0], (128, 2048)), np.broadcast_to(m[5, :, 1], (128, 2048)),
                                             np.broadcast_to(np.asarray(final_g, f32), (128, 2048))]).astype(f32))
        ims = []
        for cj in range(NCORES):
            pos = np.full((128, 9, 16), 32, np.int32)
            pos[:, 0:8, :] = posl[:, :, 8 * cj:8 * cj + 8].transpose(1, 2, 0)
            pos[0:32, 8, :] = posc[:, (cj % 4) * 32:(cj % 4) * 32 + 32, cj // 4].T
            im = {"x": np.ascontiguousarray(xmidT[cj].T), "pos": np.ascontiguousarray(pos), "rep": rep}
            for e in range(16):
                im[f"yl{e}"] = yl[e]; im[f"yc{e}"] = yc[e]
            ims.append(im)
        r = _run(build_k5(l == 1), ims)
        del ims
        for cj in range(NCORES):
            xo = r[cj]["xo"]
            x_lat[cj * 1024:(cj + 1) * 1024] = xo[:1024]
            xc[cj * 32:(cj + 1) * 32] = xo[1024:]
    return x_lat.reshape(1, 8192, 2048).astype(np.float32)
```
